# Optimizing a Trainium2 kernel written in Bass

```python
import jax, jax.numpy as jnp
from jax import lax
import numpy as np

D_MODEL = 2048
BATCH = 1
SEQ = 8192
DEPTH = 2
DEC_BATCH = 16
DEC_SEQ = 2048
PAST_LEN = 128

GRID_W = 64
N_MIXERS = 2
NA_HEADS = 16
NA_HEAD_DIM = 128
NA_WIN_H_MAX = 8
NA_WIN_W = 16
NA_QBLK_W = 16
NA_KBLK_W = 32
MLA_HEADS = 16
MLA_Q_LORA = 512
MLA_KV_LORA = 512
MLA_NOPE = 128
MLA_ROPE = 64
MLA_V = 128
MLA_QBLK = 128
ROPE_THETA = 10000.0
D_FF = 5632
N_EXPERTS = 8
TOP_K = 2
D_FF_EXPERT = 5632
EPS = 1e-6
NEG_INF = -1e30
N_EVEN = (DEPTH + 1) // 2
N_ODD = DEPTH // 2

kernel_name = "hybrid_natten_mla_encoder"


def rms_norm(x, g):
    xf = x.astype(jnp.float32)
    y = xf * lax.rsqrt(jnp.mean(xf * xf, axis=-1, keepdims=True) + EPS)
    return (y * g.astype(jnp.float32)).astype(x.dtype)


def swiglu(h, w_gate, w_up, w_down):
    a = jnp.einsum('btd,df->btf', h, w_gate)
    b = jnp.einsum('btd,df->btf', h, w_up)
    return jnp.einsum('btf,fd->btd', jax.nn.silu(a) * b, w_down)


def neighborhood_attention(h, w_qkv, q_gain, k_gain, rpb, w_o):
    B, T, _ = h.shape
    rows = T // GRID_W
    kh = min(NA_WIN_H_MAX, rows)
    qkv = jnp.einsum('btd,de->bte', h, w_qkv).reshape(B, rows, GRID_W, 3, NA_HEADS, NA_HEAD_DIM)
    q = rms_norm(qkv[:, :, :, 0], q_gain)
    k = rms_norm(qkv[:, :, :, 1], k_gain)
    v = qkv[:, :, :, 2]
    n_cb = GRID_W // NA_QBLK_W
    q_cols = jnp.arange(GRID_W).reshape(n_cb, NA_QBLK_W)
    band_start = jnp.clip(jnp.arange(n_cb) * NA_QBLK_W - NA_WIN_W // 2, 0, GRID_W - NA_KBLK_W)
    k_cols = band_start[:, None] + jnp.arange(NA_KBLK_W)
    win_start = jnp.clip(q_cols - NA_WIN_W // 2, 0, GRID_W - NA_WIN_W)
    kc = k_cols[:, None, :]
    col_valid = (kc >= win_start[..., None]) & (kc < win_start[..., None] + NA_WIN_W)
    dc_idx = jnp.clip(kc - q_cols[..., None] + NA_WIN_W - 1, 0, 2 * NA_WIN_W - 2)
    rpb_c = rpb[:, :, dc_idx].astype(jnp.float32)
    row_start = jnp.clip(jnp.arange(rows) - kh // 2, 0, rows - kh)
    scale = NA_HEAD_DIM ** -0.5

    def row_block(args):
        q_r, r, rs = args
        k_b = lax.dynamic_slice_in_dim(k, rs, kh, axis=1)[:, :, k_cols]
        v_b = lax.dynamic_slice_in_dim(v, rs, kh, axis=1)[:, :, k_cols]
        q_b = q_r.reshape(B, n_cb, NA_QBLK_W, NA_HEADS, NA_HEAD_DIM)
        s = jnp.einsum('bjqhd,bkjwhd->bhjqkw', q_b, k_b).astype(jnp.float32) * scale
        dr_idx = rs + jnp.arange(kh) - r + NA_WIN_H_MAX - 1
        bias = jnp.take(rpb_c, dr_idx, axis=1).transpose(0, 2, 3, 1, 4)
        s = jnp.where(col_valid[:, :, None, :], s + bias[None], NEG_INF)
        p = jax.nn.softmax(s, axis=(-2, -1))
        o = jnp.einsum('bhjqkw,bkjwhd->bjqhd', p.astype(v.dtype), v_b)
        return o.reshape(B, GRID_W, NA_HEADS * NA_HEAD_DIM)

    out = lax.map(row_block, (jnp.moveaxis(q, 1, 0), jnp.arange(rows), row_start))
    out = jnp.moveaxis(out, 0, 1).reshape(B, T, NA_HEADS * NA_HEAD_DIM)
    return jnp.einsum('bte,ed->btd', out, w_o)


def axial_rope_tables(T):
    t = jnp.arange(T)
    row = (t // GRID_W).astype(jnp.float32)
    col = (t % GRID_W).astype(jnp.float32)
    n_pairs = MLA_ROPE // 4
    inv = ROPE_THETA ** (-jnp.arange(n_pairs, dtype=jnp.float32) / n_pairs)
    ang = jnp.concatenate([row[:, None] * inv, col[:, None] * inv], axis=-1)
    return jnp.cos(ang), jnp.sin(ang)


def apply_rope(x, cos, sin):
    half = x.shape[-1] // 2
    x1 = x[..., :half].astype(jnp.float32)
    x2 = x[..., half:].astype(jnp.float32)
    return jnp.concatenate([x1 * cos - x2 * sin, x1 * sin + x2 * cos], axis=-1).astype(x.dtype)


def mla(h, w_dqkv, q_lora_gain, kv_lora_gain, w_uq, w_ukv, qn_gain, qr_gain, kn_gain, kr_gain, w_o):
    B, T, _ = h.shape
    lat = jnp.einsum('btd,de->bte', h, w_dqkv)
    c_q = rms_norm(lat[..., :MLA_Q_LORA], q_lora_gain)
    c_kv = rms_norm(lat[..., MLA_Q_LORA:MLA_Q_LORA + MLA_KV_LORA], kv_lora_gain)
    k_rope_raw = lat[..., MLA_Q_LORA + MLA_KV_LORA:]
    cos, sin = axial_rope_tables(T)
    q = jnp.einsum('btc,ce->bte', c_q, w_uq).reshape(B, T, MLA_HEADS, MLA_NOPE + MLA_ROPE)
    q_nope = rms_norm(q[..., :MLA_NOPE], qn_gain)
    q_rope = apply_rope(rms_norm(q[..., MLA_NOPE:], qr_gain), cos[:, None, :], sin[:, None, :])
    kv = jnp.einsum('btc,ce->bte', c_kv, w_ukv).reshape(B, T, MLA_HEADS, MLA_NOPE + MLA_V)
    k_nope = rms_norm(kv[..., :MLA_NOPE], kn_gain)
    v = kv[..., MLA_NOPE:]
    k_rope = apply_rope(rms_norm(k_rope_raw, kr_gain), cos, sin)
    scale = (MLA_NOPE + MLA_ROPE) ** -0.5
    nb = T // MLA_QBLK
    qn_b = jnp.moveaxis(q_nope.reshape(B, nb, MLA_QBLK, MLA_HEADS, MLA_NOPE), 1, 0)
    qr_b = jnp.moveaxis(q_rope.reshape(B, nb, MLA_QBLK, MLA_HEADS, MLA_ROPE), 1, 0)

    def q_block(args):
        qn, qr = args
        s = (jnp.einsum('bqhd,bkhd->bhqk', qn, k_nope)
             + jnp.einsum('bqhr,bkr->bhqk', qr, k_rope)).astype(jnp.float32) * scale
        p = jax.nn.softmax(s, axis=-1)
        return jnp.einsum('bhqk,bkhd->bqhd', p.astype(v.dtype), v)

    o = lax.map(q_block, (qn_b, qr_b))
    o = jnp.moveaxis(o, 0, 1).reshape(B, T, MLA_HEADS * MLA_V)
    return jnp.einsum('bte,ed->btd', o, w_o)


def moe_swiglu(h, w_router, w_gate, w_up, w_down):
    logits = jnp.einsum('btd,de->bte', h, w_router).astype(jnp.float32)
    top_val, top_idx = lax.top_k(logits, TOP_K)
    gates = jax.nn.softmax(top_val, axis=-1)
    combine = jnp.sum(jax.nn.one_hot(top_idx, N_EXPERTS, dtype=jnp.float32) * gates[..., None], axis=-2)
    combine = combine.astype(h.dtype)
    out = jnp.zeros_like(h)
    for e in range(N_EXPERTS):
        out = out + combine[..., e:e + 1] * swiglu(h, w_gate[e], w_up[e], w_down[e])
    return out


def trunk(x, mix_norm, ffn_norm,
          na_w_qkv, na_q_gain, na_k_gain, na_rpb, na_w_o,
          mla_w_dqkv, mla_q_lora_gain, mla_kv_lora_gain, mla_w_uq, mla_w_ukv,
          mla_qn_gain, mla_qr_gain, mla_kn_gain, mla_kr_gain, mla_w_o,
          ffn_w_gate, ffn_w_up, ffn_w_down,
          moe_w_router, moe_w_gate, moe_w_up, moe_w_down):
    for i in range(DEPTH):
        j = i // N_MIXERS
        h = rms_norm(x, mix_norm[i])
        if i % N_MIXERS == 0:
            x = x + neighborhood_attention(h, na_w_qkv[j], na_q_gain[j], na_k_gain[j], na_rpb[j], na_w_o[j])
        else:
            x = x + mla(h, mla_w_dqkv[j], mla_q_lora_gain[j], mla_kv_lora_gain[j], mla_w_uq[j], mla_w_ukv[j],
                        mla_qn_gain[j], mla_qr_gain[j], mla_kn_gain[j], mla_kr_gain[j], mla_w_o[j])
        h = rms_norm(x, ffn_norm[i])
        if i % 2 == 0:
            x = x + swiglu(h, ffn_w_gate[j], ffn_w_up[j], ffn_w_down[j])
        else:
            x = x + moe_swiglu(h, moe_w_router[j], moe_w_gate[j], moe_w_up[j], moe_w_down[j])
    return x


def setup_inputs(seed: int = 0) -> dict:
    key = jax.random.key(seed)
    ks = jax.random.split(key, 32)
    f32 = jnp.float32

    def w(k, shape, fan_in):
        return jax.random.normal(k, shape, f32) * (fan_in ** -0.5)

    def gain(k, shape):
        return 1.0 + 0.02 * jax.random.normal(k, shape, f32)

    na_dim = NA_HEADS * NA_HEAD_DIM
    return {
        "x_prompt": jax.random.normal(ks[0], (BATCH, SEQ, D_MODEL), f32),
        "x_sample": jax.random.normal(ks[1], (DEC_BATCH, DEC_SEQ, D_MODEL), f32),
        "mix_norm": gain(ks[2], (DEPTH, D_MODEL)),
        "ffn_norm": gain(ks[3], (DEPTH, D_MODEL)),
        "na_w_qkv": w(ks[4], (N_EVEN, D_MODEL, 3 * na_dim), D_MODEL),
        "na_q_gain": gain(ks[5], (N_EVEN, NA_HEAD_DIM)),
        "na_k_gain": gain(ks[6], (N_EVEN, NA_HEAD_DIM)),
        "na_rpb": 0.1 * jax.random.normal(ks[7], (N_EVEN, NA_HEADS, 2 * NA_WIN_H_MAX - 1, 2 * NA_WIN_W - 1), f32),
        "na_w_o": w(ks[8], (N_EVEN, na_dim, D_MODEL), na_dim),
        "mla_w_dqkv": w(ks[9], (N_ODD, D_MODEL, MLA_Q_LORA + MLA_KV_LORA + MLA_ROPE), D_MODEL),
        "mla_q_lora_gain": gain(ks[10], (N_ODD, MLA_Q_LORA)),
        "mla_kv_lora_gain": gain(ks[11], (N_ODD, MLA_KV_LORA)),
        "mla_w_uq": w(ks[12], (N_ODD, MLA_Q_LORA, MLA_HEADS * (MLA_NOPE + MLA_ROPE)), MLA_Q_LORA),
        "mla_w_ukv": w(ks[13], (N_ODD, MLA_KV_LORA, MLA_HEADS * (MLA_NOPE + MLA_V)), MLA_KV_LORA),
        "mla_qn_gain": gain(ks[14], (N_ODD, MLA_NOPE)),
        "mla_qr_gain": gain(ks[15], (N_ODD, MLA_ROPE)),
        "mla_kn_gain": gain(ks[16], (N_ODD, MLA_NOPE)),
        "mla_kr_gain": gain(ks[17], (N_ODD, MLA_ROPE)),
        "mla_w_o": w(ks[18], (N_ODD, MLA_HEADS * MLA_V, D_MODEL), MLA_HEADS * MLA_V),
        "ffn_w_gate": w(ks[19], (N_EVEN, D_MODEL, D_FF), D_MODEL),
        "ffn_w_up": w(ks[20], (N_EVEN, D_MODEL, D_FF), D_MODEL),
        "ffn_w_down": w(ks[21], (N_EVEN, D_FF, D_MODEL), D_FF),
        "moe_w_router": w(ks[22], (N_ODD, D_MODEL, N_EXPERTS), D_MODEL),
        "moe_w_gate": w(ks[23], (N_ODD, N_EXPERTS, D_MODEL, D_FF_EXPERT), D_MODEL),
        "moe_w_up": w(ks[24], (N_ODD, N_EXPERTS, D_MODEL, D_FF_EXPERT), D_MODEL),
        "moe_w_down": w(ks[25], (N_ODD, N_EXPERTS, D_FF_EXPERT, D_MODEL), D_FF_EXPERT),
    }


def reference(x_prompt, x_sample, mix_norm, ffn_norm,
              na_w_qkv, na_q_gain, na_k_gain, na_rpb, na_w_o,
              mla_w_dqkv, mla_q_lora_gain, mla_kv_lora_gain, mla_w_uq, mla_w_ukv,
              mla_qn_gain, mla_qr_gain, mla_kn_gain, mla_kr_gain, mla_w_o,
              ffn_w_gate, ffn_w_up, ffn_w_down,
              moe_w_router, moe_w_gate, moe_w_up, moe_w_down):
    params = (mix_norm, ffn_norm,
              na_w_qkv, na_q_gain, na_k_gain, na_rpb, na_w_o,
              mla_w_dqkv, mla_q_lora_gain, mla_kv_lora_gain, mla_w_uq, mla_w_ukv,
              mla_qn_gain, mla_qr_gain, mla_kn_gain, mla_kr_gain, mla_w_o,
              ffn_w_gate, ffn_w_up, ffn_w_down,
              moe_w_router, moe_w_gate, moe_w_up, moe_w_down)
    y_prompt = trunk(x_prompt, *params)
    y_sample = trunk(x_sample, *params)
    return (y_prompt, y_sample)
```

```python
import contextlib
import numpy as np
import concourse.bass as bass
import concourse.mybir as mybir
from concourse.bass_utils import run_bass_kernel_spmd

F32 = mybir.dt.float32
BF16 = mybir.dt.bfloat16
I32 = mybir.dt.int32
AF = mybir.ActivationFunctionType
ALU = mybir.AluOpType
AX = mybir.AxisListType
ENGS = ("sync", "gpsimd", "scalar", "vector", "tensor")
EPS = 1e-6
N_CORES = 8

CFG_FULL = dict(D=2048, H=16, FF=5632, E=8, RS=32, RP=128, NS=2)


class Sched:
    def __init__(self, nc, stack, n_dma_sems=96):
        self.nc = nc
        self.ops = {e: [] for e in ENGS}
        self.esem = {e: stack.enter_context(nc.semaphore("es_" + e)) for e in ENGS}
        self.ecnt = {e: 0 for e in ENGS}
        self.seen = {e: {} for e in ENGS}
        self.free_sems = {"sync": [[stack.enter_context(nc.semaphore("dh%d" % i)), 0] for i in range(28)],
                          "gpsimd": [[stack.enter_context(nc.semaphore("dg%d" % i)), 0] for i in range(n_dma_sems - 28)]}
        self.dsem = {}
        self.lastw = {}
        self.reads = {}

    def _dma_sem(self, key, eng):
        key = (eng, key)
        if key not in self.dsem:
            self.dsem[key] = self.free_sems[eng].pop()
        return self.dsem[key]

    @staticmethod
    def _isdram(k):
        return isinstance(k, tuple) and len(k) > 0 and k[0] == "dram"

    def _deps(self, reads, writes):
        deps = {}

        def add(d):
            for sid, ev in d.items():
                if sid not in deps or deps[sid][1] < ev[1]:
                    deps[sid] = ev
        for r in reads:
            add(self.lastw.get(r, {}))
        for w in writes:
            if self._isdram(w):
                continue
            add(self.lastw.get(w, {}))
            add(self.reads.get(w, {}))
        return deps

    def _commit(self, reads, writes, ev):
        sid = id(ev[0])
        for r in reads:
            self.reads.setdefault(r, {})[sid] = ev
        for w in writes:
            if self._isdram(w):
                self.lastw.setdefault(w, {})[sid] = ev
            else:
                self.lastw[w] = {sid: ev}
                self.reads[w] = {}

    def _waits(self, eng, deps, skip_own):
        waits = []
        seen = self.seen[eng]
        own = id(self.esem[eng])
        for sid, (sem, val) in deps.items():
            if skip_own and sid == own:
                continue
            if seen.get(sid, 0) >= val:
                continue
            seen[sid] = val
            waits.append((sem, val))
        return waits

    def op(self, eng, fn, reads=(), writes=()):
        waits = self._waits(eng, self._deps(reads, writes), eng == "tensor")
        self.ecnt[eng] += 1
        ev = (self.esem[eng], self.ecnt[eng])

        def emit(e, fn=fn, waits=waits, sem=ev[0]):
            for s, v in waits:
                e.wait_ge(s, v)
            fn(e).then_inc(sem, 1)
        self.ops[eng].append(emit)
        self._commit(reads, writes, ev)

    def dma(self, eng, fn, semkey, reads=(), writes=()):
        waits = self._waits(eng, self._deps(reads, writes), False)
        ds = self._dma_sem(semkey, eng)
        ds[1] += 16
        ev = (ds[0], ds[1])

        def emit(e, fn=fn, waits=waits, sem=ev[0]):
            for s, v in waits:
                e.wait_ge(s, v)
            fn(e).then_inc(sem, 16)
        self.ops[eng].append(emit)
        self._commit(reads, writes, ev)

    def barrier(self):
        finals = [(ds[0], ds[1]) for ds in self.dsem.values() if ds[1] > 0]
        finals += [(self.esem[e], self.ecnt[e]) for e in ENGS if self.ecnt[e] > 0]
        for eng in ENGS:
            seen = self.seen[eng]
            w = []
            for s, v in finals:
                if seen.get(id(s), 0) >= v:
                    continue
                if id(s) == id(self.esem[eng]) and eng == "tensor":
                    continue
                seen[id(s)] = v
                w.append((s, v))

            def emit(e, w=w):
                for s, v in w:
                    e.wait_ge(s, v)
            self.ops[eng].append(emit)
        for (eng, _k), pair in self.dsem.items():
            self.free_sems[eng].append(pair)
        self.dsem = {}

    def run(self):
        ops = self.ops
        with self.nc.Block() as block:
            @block.sync
            def _(e):
                for f in ops["sync"]:
                    f(e)

            @block.gpsimd
            def _(e):
                for f in ops["gpsimd"]:
                    f(e)

            @block.scalar
            def _(e):
                for f in ops["scalar"]:
                    f(e)

            @block.vector
            def _(e):
                for f in ops["vector"]:
                    f(e)

            @block.tensor
            def _(e):
                for f in ops["tensor"]:
                    f(e)
        self.ops = {e: [] for e in ENGS}


_UCNT = [0]


def _u(name):
    _UCNT[0] += 1
    return "%s_%d" % (name, _UCNT[0])


def _chunk_div(n, cap):
    for c in range(min(n, cap), 0, -1):
        if n % c == 0:
            return c
    return 1


def build_program(cfg):
    D, H, FF, E = cfg["D"], cfg["H"], cfg["FF"], cfg["E"]
    RS, RP, NS = cfg["RS"], cfg["RP"], cfg["NS"]
    KD = D // 128
    NFC = FF // 128
    HD = H * 128
    KH = HD // 128
    assert D % 512 == 0 and FF % 512 == 0 and HD == D
    TS = RS // 8
    TP = RP // 8
    NOWN = RP // 64
    NT0 = NS * TS + TP
    NT1 = NT0 + NOWN
    NL1 = NS * TS + NOWN
    seqs0 = [(s * TS, TS, RS) for s in range(NS)] + [(NS * TS, TP, RP)]
    l1_tiles = list(range(NS * TS)) + [NT0 + i for i in range(NOWN)]
    QL = KVL = 512
    LAT = QL + KVL + 64

    nc = bass.Bass("TRN2", target_bir_lowering=False)

    def din(name, shape, dt=F32):
        return nc.dram_tensor(name, list(shape), dt, kind="ExternalInput").ap()

    def dscr(name, shape, dt):
        kind = "ExternalOutput" if name in cfg.get("dbg", ()) else "Internal"
        return nc.dram_tensor(name, list(shape), dt, kind=kind).ap()

    x_all = din("x_all", [NT0 * 512, D])
    own_idx = din("own_idx", [128, NOWN * 4], I32)
    rope_in = din("rope", [NT1 * 512, 64])
    ident_in = din("ident", [128, 128])
    g_mix = din("g_mix", [128, 2, KD])
    g_ffn = din("g_ffn", [128, 2, KD])
    g_naq = din("g_naq", [128, 1])
    g_nak = din("g_nak", [128, 1])
    rpbg = din("rpbg", [128, H, 14, 64])
    namask = din("namask", [128, 64])
    g_ql = din("g_ql", [128, 4])
    g_kvl = din("g_kvl", [128, 4])
    g_qn = din("g_qn", [128, 1])
    g_kn = din("g_kn", [128, 1])
    g_qr = din("g_qr", [128, 64])
    g_kr = din("g_kr", [128, 64])
    w_router = din("w_router", [128, KD, E])
    Wf = {
        "qkv": din("na_w_qkv", [D, 3 * HD]), "nao": din("na_w_o", [HD, D]),
        "fg": din("ffn_w_gate", [D, FF]), "fu": din("ffn_w_up", [D, FF]), "fd": din("ffn_w_down", [FF, D]),
        "dqkv": din("mla_w_dqkv", [D, LAT]), "uq": din("mla_w_uq", [QL, H * 192]),
        "ukv": din("mla_w_ukv", [KVL, H * 256]), "mo": din("mla_w_o", [HD, D]),
    }
    for e_ in range(E):
        Wf["mg%d" % e_] = din("moe_w_gate%d" % e_, [D, FF])
        Wf["mu%d" % e_] = din("moe_w_up%d" % e_, [D, FF])
        Wf["md%d" % e_] = din("moe_w_down%d" % e_, [FF, D])
    Wb = {k: dscr("wb_" + k, v.shape, BF16) for k, v in Wf.items()}

    ys = nc.dram_tensor("ys", [NS * TS * 512, D], F32, kind="ExternalOutput").ap()
    yp = nc.dram_tensor("yp", [NOWN * 512, D], F32, kind="ExternalOutput").ap()

    qk0 = dscr("qk0", [NT0, 2 * H, 128, 512], BF16)
    v0 = dscr("v0", [NT0 * 512, HD], BF16)
    oT0 = dscr("oT0", [NT0, H, 128, 512], BF16)
    x1 = dscr("x1", [NT1 * 512, D], F32)
    q1n = dscr("q1n", [NT1, H, 128, 512], BF16)
    q1r = dscr("q1r", [NT1, H, 64, 512], BF16)
    k1n = dscr("k1n", [NT1, H, 128, 512], BF16)
    k1r = dscr("k1r", [NT1, 64, 512], BF16)
    v1 = dscr("v1", [NT1 * 512, HD], BF16)
    oT1 = dscr("oT1", [NL1, H, 128, 512], BF16)

    with contextlib.ExitStack() as gst:
        S = Sched(nc, gst)
        PS = [gst.enter_context(nc.psum_tensor("ps%d" % i, [128, 512], F32)) for i in range(8)]
        ident_f = gst.enter_context(nc.sbuf_tensor(_u("ident_f"), [128, 128], F32))
        ident_b = gst.enter_context(nc.sbuf_tensor(_u("ident_b"), [128, 128], BF16))
        ones_b = gst.enter_context(nc.sbuf_tensor(_u("ones_b"), [128, 128], BF16))
        gmix = gst.enter_context(nc.sbuf_tensor(_u("gmix"), [128, 2, KD], F32))
        gffn = gst.enter_context(nc.sbuf_tensor(_u("gffn"), [128, 2, KD], F32))
        gsm = gst.enter_context(nc.sbuf_tensor(_u("gsm"), [128, 12], F32))
        gqr = gst.enter_context(nc.sbuf_tensor(_u("gqr"), [128, 64], F32))
        gkr = gst.enter_context(nc.sbuf_tensor(_u("gkr"), [128, 64], F32))
        WS = []
        ws_i = [0]

        def alloc_ws(stk, n):
            WS[:] = [stk.enter_context(nc.sbuf_tensor(_u("ws%d" % i), [128, 8192], BF16)) for i in range(n)]

        def ld(dst, src, key, eng="sync", reads=()):
            S.dma(eng, lambda e: e.dma_start(out=dst, in_=src), key, reads=reads, writes=[key])

        ld(ident_f[:], ident_in[:, :], "ident_f")
        ld(gmix[:], g_mix[:, :, :], "gmix")
        ld(gffn[:], g_ffn[:, :, :], "gffn")
        ld(gsm[:, 0:1], g_naq[:, :], "gsm")
        ld(gsm[:, 1:2], g_nak[:, :], "gsm")
        ld(gsm[:, 2:3], g_qn[:, :], "gsm")
        ld(gsm[:, 3:4], g_kn[:, :], "gsm")
        ld(gsm[:, 4:8], g_ql[:, :], "gsm")
        ld(gsm[:, 8:12], g_kvl[:, :], "gsm")
        ld(gqr[:], g_qr[:, :], "gqr")
        ld(gkr[:], g_kr[:, :], "gkr")
        S.op("vector", lambda e: e.tensor_copy(out=ident_b[:], in_=ident_f[:]), reads=["ident_f"], writes=["ident_b"])
        S.op("vector", lambda e: e.memset(ones_b[:], 1.0), writes=["ones_b"])

        def conv(name):
            src, dst = Wf[name], Wb[name]
            rows, cols = src.shape
            step = max(128, (4 * 1024 * 1024 // cols) // 128 * 128)
            for r0 in range(0, rows, step):
                r1 = min(rows, r0 + step)
                S.dma("gpsimd", lambda e, r0=r0, r1=r1: e.dma_start(out=dst[r0:r1, :], in_=src[r0:r1, :]),
                      ("cv", name), writes=[("dram", "wb_" + name)])

        for nm in ("qkv", "nao", "fg", "fu", "fd", "dqkv", "uq", "ukv", "mo"):
            conv(nm)
        moe_conv_todo = []
        for e_ in range(E):
            moe_conv_todo += ["mg%d" % e_, "mu%d" % e_, "md%d" % e_]

        def conv_some(n):
            for _ in range(n):
                if moe_conv_todo:
                    conv(moe_conv_todo.pop(0))

        def wload(name, k0, nk, c0, ncols):
            s = ws_i[0] % len(WS)
            ws_i[0] += 1
            src = Wb[name][k0 * 128:(k0 + nk) * 128, c0:c0 + ncols].rearrange("(k p) f -> p k f", p=128)
            view = WS[s][:, 0:nk * ncols].rearrange("p (k c) -> p k c", c=ncols)
            S.dma("sync", lambda e: e.dma_start(out=view, in_=src), ("w", s),
                  reads=[("dram", "wb_" + name)], writes=[("w", s)])
            return ("w", s), view

        def psb(i):
            return PS[i][:, :].bitcast(BF16)

        def norm_tile(xt, xkey, xn, junk, st, gain, hT, hkey, tp_banks, hT32=None):
            if callable(xt):
                xsrc = xt
            else:
                xsrc = lambda b: (xt[:, b, :], xkey)
            for b in range(4):
                xa, xk = xsrc(b)
                S.op("vector", lambda e, xa=xa, b=b: e.scalar_tensor_tensor(
                    out=junk[:, :], in0=xa, scalar=1.0, in1=xa, op0=ALU.mult, op1=ALU.mult, accum_out=st[:, b:b + 1]),
                    reads=[xk], writes=["junk", ("st", b)])
                S.op("scalar", lambda e, b=b: e.activation(out=st[:, 4 + b:5 + b], in_=st[:, b:b + 1], func=AF.Sqrt,
                                                           scale=1.0 / D, bias=EPS),
                     reads=[("st", b)], writes=["st2"])
                S.op("vector", lambda e, b=b: e.reciprocal(out=st[:, 4 + b:5 + b], in_=st[:, 4 + b:5 + b]),
                     reads=["st2"], writes=["st2"])
                S.op("scalar", lambda e, b=b, xa=xa: e.activation(out=xn[:, b, :], in_=xa, func=AF.Copy,
                                                                  scale=st[:, 4 + b:5 + b]),
                     reads=[xk, "st2"], writes=[("xn", b)])
            for k in range(KD):
                bank = tp_banks[(k // 2) % len(tp_banks)]
                half = k % 2
                tpv = psb(bank)[:, half * 512:(half + 1) * 512]
                for b in range(4):
                    S.op("tensor", lambda e, b=b, k=k, tpv=tpv: e.transpose(
                        out=tpv[:, b * 128:(b + 1) * 128], in_=xn[:, b, k * 128:(k + 1) * 128], identity=ident_b[:]),
                        reads=[("xn", b), "ident_b"], writes=[("ps", bank)])
                if k % 2 == 0:
                    S.op("vector", lambda e, k=k, tpv=tpv: e.tensor_scalar(
                        out=hT[:, k, :], in0=tpv, scalar1=gain[:, k:k + 1], scalar2=None, op0=ALU.mult),
                        reads=[("ps", bank), "gmix", "gffn"], writes=[(hkey, k)])
                else:
                    S.op("scalar", lambda e, k=k, tpv=tpv: e.activation(
                        out=hT[:, k, :], in_=tpv, func=AF.Copy, scale=gain[:, k:k + 1]),
                        reads=[("ps", bank), "gmix", "gffn"], writes=[(hkey, k)])

        if cfg.get("stop", 99) >= 1:
         with contextlib.ExitStack() as st_:
            alloc_ws(st_, 3)
            xt = st_.enter_context(nc.sbuf_tensor(_u("xt"), [128, 4, D], F32))
            xn = st_.enter_context(nc.sbuf_tensor(_u("xn"), [128, 4, D], BF16))
            junk = st_.enter_context(nc.sbuf_tensor(_u("junk"), [128, D], F32))
            stt = st_.enter_context(nc.sbuf_tensor(_u("stt"), [128, 8], F32))
            hT = st_.enter_context(nc.sbuf_tensor(_u("hT"), [128, KD, 512], BF16))
            sq = [st_.enter_context(nc.sbuf_tensor(_u("sq%d" % i), [128, 512], BF16)) for i in range(2)]
            rs_ = [st_.enter_context(nc.sbuf_tensor(_u("rs%d" % i), [128, 512], F32)) for i in range(2)]
            qo = [st_.enter_context(nc.sbuf_tensor(_u("qo%d" % i), [128, 512], BF16)) for i in range(3)]
            vo = [st_.enter_context(nc.sbuf_tensor(_u("vo%d" % i), [128, 512], BF16)) for i in range(3)]
            cnt = 0
            vcnt = 0
            for t in range(NT0):
                ld(xt[:], x_all[t * 512:(t + 1) * 512, :].rearrange("(b p) d -> p b d", p=128), "xt")
                norm_tile(xt, "xt", xn, junk, stt, gmix[:, 0, :], hT, "hT", [6, 7])
                for ch in range(2 * H // 4):
                    wkey, wv = wload("qkv", 0, KD, ch * 512, 512)
                    for j in range(4):
                        hj = ch * 4 + j
                        pb = cnt % 4
                        sb = 4 + cnt % 2
                        i2 = cnt % 2
                        i3 = cnt % 3
                        cnt += 1
                        for k in range(KD):
                            S.op("tensor", lambda e, k=k, j=j, pb=pb, wv=wv: e.matmul(
                                PS[pb][:, :], lhsT=wv[:, k, j * 128:(j + 1) * 128], rhs=hT[:, k, :],
                                start=(k == 0), stop=(k == KD - 1)),
                                reads=[wkey, ("hT", k)], writes=[("ps", pb)])
                        S.op("scalar", lambda e, pb=pb, i2=i2: e.activation(out=sq[i2][:], in_=PS[pb][:, :], func=AF.Square),
                             reads=[("ps", pb)], writes=[("sq", i2)])
                        S.op("tensor", lambda e, sb=sb, i2=i2: e.matmul(PS[sb][:, :], lhsT=ones_b[:], rhs=sq[i2][:],
                                                                         start=True, stop=True),
                             reads=[("sq", i2), "ones_b"], writes=[("ps", sb)])
                        S.op("scalar", lambda e, sb=sb, i2=i2: e.activation(
                            out=rs_[i2][:], in_=PS[sb][:, :], func=AF.Sqrt, scale=1.0 / 128, bias=EPS),
                            reads=[("ps", sb)], writes=[("rs", i2)])
                        S.op("vector", lambda e, i2=i2: e.reciprocal(out=rs_[i2][:], in_=rs_[i2][:]),
                             reads=[("rs", i2)], writes=[("rs", i2)])
                        gcol = 0 if hj < H else 1
                        S.op("vector", lambda e, pb=pb, i2=i2, i3=i3, gcol=gcol: e.scalar_tensor_tensor(
                            out=qo[i3][:], in0=PS[pb][:, :], scalar=gsm[:, gcol:gcol + 1], in1=rs_[i2][:],
                            op0=ALU.mult, op1=ALU.mult),
                            reads=[("ps", pb), ("rs", i2), "gsm"], writes=[("qo", i3)])
                        S.dma("gpsimd", lambda e, i3=i3, t=t, hj=hj: e.dma_start(out=qk0[t, hj, :, :], in_=qo[i3][:]),
                              ("st_qo", i3), reads=[("qo", i3)], writes=[("dram", "qk0")])
                for cg in range(HD // 512):
                    wkey, wv = wload("qkv", 0, KD, 2 * HD + cg * 512, 512)
                    for b in range(4):
                        pb = cnt % 4
                        cnt += 1
                        i3 = vcnt % 3
                        vcnt += 1
                        for k in range(KD):
                            S.op("tensor", lambda e, k=k, b=b, pb=pb, wv=wv: e.matmul(
                                PS[pb][:, :], lhsT=hT[:, k, b * 128:(b + 1) * 128], rhs=wv[:, k, :],
                                start=(k == 0), stop=(k == KD - 1)),
                                reads=[wkey, ("hT", k)], writes=[("ps", pb)])
                        S.op("scalar", lambda e, pb=pb, i3=i3: e.copy(out=vo[i3][:], in_=PS[pb][:, :]),
                             reads=[("ps", pb)], writes=[("vo", i3)])
                        r0 = t * 512 + b * 128
                        S.dma("gpsimd", lambda e, i3=i3, r0=r0, cg=cg: e.dma_start(
                            out=v0[r0:r0 + 128, cg * 512:(cg + 1) * 512], in_=vo[i3][:]),
                            ("st_vo", i3), reads=[("vo", i3)], writes=[("dram", "v0")])
            S.barrier()
            S.run()

        if cfg.get("stop", 99) >= 2:
         with contextlib.ExitStack() as st_:
            TT = st_.enter_context(nc.sbuf_tensor(_u("TT"), [128, H, 14, 64], BF16))
            msk = st_.enter_context(nc.sbuf_tensor(_u("msk"), [128, 64], F32))
            rp = [st_.enter_context(nc.sbuf_tensor(_u("rp%d" % i), [128, 14, 64], F32)) for i in range(2)]
            WRmax = 16
            kw = st_.enter_context(nc.sbuf_tensor(_u("kw"), [128, H, WRmax * 64], BF16))
            vE = st_.enter_context(nc.sbuf_tensor(_u("vE"), [128, WRmax // 2, HD], BF16))
            vO = st_.enter_context(nc.sbuf_tensor(_u("vO"), [128, WRmax // 2 - 1, HD], BF16))
            qT = st_.enter_context(nc.sbuf_tensor(_u("qT"), [128, H, 512], BF16))
            oS = st_.enter_context(nc.sbuf_tensor(_u("oS"), [128, H, 512], BF16))
            Eb = [st_.enter_context(nc.sbuf_tensor(_u("Eb%d" % i), [128, 2, 4, 64], BF16)) for i in range(2)]
            Pb = [st_.enter_context(nc.sbuf_tensor(_u("Pb%d" % i), [128, 2, 4, 64], BF16)) for i in range(2)]
            rc = [st_.enter_context(nc.sbuf_tensor(_u("rc%d" % i), [128, 128], F32)) for i in range(2)]
            ld(msk[:], namask[:, :], "msk")
            for h in range(H):
                i2 = h % 2
                ld(rp[i2][:], rpbg[:, h, :, :], ("rp", i2))
                S.op("scalar", lambda e, i2=i2: e.activation(out=rp[i2][:], in_=rp[i2][:], func=AF.Exp),
                     reads=[("rp", i2)], writes=[("rp", i2)])
                S.op("vector", lambda e, i2=i2, h=h: e.tensor_tensor(
                    out=TT[:, h, :, :], in0=rp[i2][:], in1=msk[:, :].unsqueeze(1).to_broadcast([128, 14, 64]), op=ALU.mult),
                    reads=[("rp", i2), "msk"], writes=["TT"])
            scale = 128 ** -0.5
            gc = 0
            for (tile0, ntl, rows) in seqs0:
                WR = min(16, rows)
                for tl in range(ntl):
                    t = tile0 + tl
                    r0 = tl * 8
                    w0 = min(max(r0 - 4, 0), rows - WR)
                    rr = w0
                    while rr < w0 + WR:
                        st_tile = rr // 8
                        re = min(w0 + WR, (st_tile + 1) * 8)
                        n = (re - rr) * 64
                        off = (rr - st_tile * 8) * 64
                        dst = (rr - w0) * 64
                        S.dma("sync", lambda e, st_tile=st_tile, off=off, n=n, dst=dst, tile0=tile0: e.dma_start(
                            out=kw[:, :, dst:dst + n],
                            in_=qk0[tile0 + st_tile, H:2 * H, :, off:off + n].rearrange("h d t -> d h t")),
                            "kw", reads=[("dram", "qk0")], writes=["kw"])
                        rr = re
                    tok0 = tile0 * 512 + w0 * 64
                    S.dma("sync", lambda e, tok0=tok0, WR=WR: e.dma_start(
                        out=vE[:, 0:WR // 2, :], in_=v0[tok0:tok0 + WR * 64, :].rearrange("(b p) e -> p b e", p=128)),
                        "vE", reads=[("dram", "v0")], writes=["vE"])
                    if WR > 8:
                        S.dma("sync", lambda e, tok0=tok0, WR=WR: e.dma_start(
                            out=vO[:, 0:WR // 2 - 1, :],
                            in_=v0[tok0 + 64:tok0 + 64 + (WR - 2) * 64, :].rearrange("(b p) e -> p b e", p=128)),
                            "vO", reads=[("dram", "v0")], writes=["vO"])
                    S.dma("sync", lambda e, t=t: e.dma_start(
                        out=qT[:, :, :], in_=qk0[t, 0:H, :, :].rearrange("h d t -> d h t")),
                        "qT", reads=[("dram", "qk0")], writes=["qT"])
                    for rl in range(8):
                        r = r0 + rl
                        rs = min(max(r - 4, 0), rows - 8)
                        o = r - rs
                        rel = rs - w0
                        m0 = 7 - o
                        for hp in range(H // 2):
                            sbk = gc % 4
                            obk = 4 + gc % 2
                            dbk = 6 + gc % 2
                            i2 = gc % 2
                            gc += 1
                            for hh in range(2):
                                h = hp * 2 + hh
                                for p in range(4):
                                    kt0 = (rel + 2 * p) * 64
                                    S.op("tensor", lambda e, h=h, hh=hh, p=p, kt0=kt0, sbk=sbk, rl=rl: e.matmul(
                                        PS[sbk][:, (hh * 4 + p) * 64:(hh * 4 + p + 1) * 64],
                                        lhsT=kw[:, h, kt0:kt0 + 128], rhs=qT[:, h, rl * 64:(rl + 1) * 64],
                                        start=True, stop=True),
                                        reads=["kw", "qT"], writes=[("ps", sbk)])
                            S.op("scalar", lambda e, sbk=sbk, i2=i2: e.activation(
                                out=Eb[i2][:].rearrange("p a b c -> p (a b c)"), in_=PS[sbk][:, :], func=AF.Exp, scale=scale),
                                reads=[("ps", sbk)], writes=[("Eb", i2)])
                            S.op("vector", lambda e, i2=i2, hp=hp, m0=m0: e.tensor_tensor(
                                out=Pb[i2][:], in0=Eb[i2][:], in1=TT[:, 2 * hp:2 * hp + 2, m0:m0 + 7:2, :], op=ALU.mult),
                                reads=[("Eb", i2), "TT"], writes=[("Pb", i2)])
                            for hh in range(2):
                                h = hp * 2 + hh
                                for p in range(4):
                                    if rel % 2 == 0:
                                        vv = vE[:, rel // 2 + p, h * 128:(h + 1) * 128]
                                        vk = "vE"
                                    else:
                                        vv = vO[:, (rel - 1) // 2 + p, h * 128:(h + 1) * 128]
                                        vk = "vO"
                                    S.op("tensor", lambda e, hh=hh, p=p, vv=vv, obk=obk, i2=i2: e.matmul(
                                        PS[obk][:, hh * 64:(hh + 1) * 64], lhsT=vv, rhs=Pb[i2][:, hh, p, :],
                                        start=(p == 0), stop=(p == 3)),
                                        reads=[vk, ("Pb", i2)], writes=[("ps", obk)])
                            for p in range(4):
                                S.op("tensor", lambda e, p=p, dbk=dbk, i2=i2: e.matmul(
                                    PS[dbk][:, 0:128].rearrange("q (a b) -> q a b", a=2), lhsT=ones_b[:],
                                    rhs=Pb[i2][:, :, p, :], start=(p == 0), stop=(p == 3)),
                                    reads=[("Pb", i2), "ones_b"], writes=[("ps", dbk)])
                            S.op("vector", lambda e, dbk=dbk, i2=i2: e.reciprocal(out=rc[i2][:], in_=PS[dbk][:, 0:128]),
                                 reads=[("ps", dbk)], writes=[("rc", i2)])
                            S.op("vector", lambda e, obk=obk, i2=i2, hp=hp, rl=rl: e.tensor_tensor(
                                out=oS[:, 2 * hp:2 * hp + 2, rl * 64:(rl + 1) * 64],
                                in0=PS[obk][:, 0:128].rearrange("q (a b) -> q a b", a=2),
                                in1=rc[i2][:].rearrange("q (a b) -> q a b", a=2), op=ALU.mult),
                                reads=[("ps", obk), ("rc", i2)], writes=["oS"])
                    S.dma("gpsimd", lambda e, t=t: e.dma_start(
                        out=oT0[t, :, :, :].rearrange("h d t -> d h t"), in_=oS[:, :, :]),
                        "st_oS", reads=["oS"], writes=[("dram", "oT0")])
                    conv_some(1)
            S.barrier()
            S.run()

        GC = 4
        DC = _chunk_div(NFC, 16)

        def swiglu_tile(hT, hkey, act, sg, names, epilogue, cnt0):
            cnt = cnt0
            ng, nu, nd = names
            for c in range(NFC // GC):
                wkg, wg = wload(ng, 0, KD, c * GC * 128, GC * 128)
                gb = []
                for j in range(GC):
                    pb = cnt % 4
                    cnt += 1
                    gb.append(pb)
                    for k in range(KD):
                        S.op("tensor", lambda e, k=k, j=j, pb=pb, wg=wg: e.matmul(
                            PS[pb][:, :], lhsT=wg[:, k, j * 128:(j + 1) * 128], rhs=hT[:, k, :],
                            start=(k == 0), stop=(k == KD - 1)),
                            reads=[wkg, (hkey, k)], writes=[("ps", pb)])
                    S.op("scalar", lambda e, j=j, pb=pb: e.activation(out=sg[j][:], in_=PS[pb][:, :], func=AF.Silu),
                         reads=[("ps", pb)], writes=[("sg", j)])
                wku, wu = wload(nu, 0, KD, c * GC * 128, GC * 128)
                for j in range(GC):
                    pb = 4 + cnt % 4
                    cnt += 1
                    fc = c * GC + j
                    for k in range(KD):
                        S.op("tensor", lambda e, k=k, j=j, pb=pb, wu=wu: e.matmul(
                            PS[pb][:, :], lhsT=wu[:, k, j * 128:(j + 1) * 128], rhs=hT[:, k, :],
                            start=(k == 0), stop=(k == KD - 1)),
                            reads=[wku, (hkey, k)], writes=[("ps", pb)])
                    S.op("vector", lambda e, j=j, pb=pb, fc=fc: e.tensor_tensor(
                        out=act[:, fc, :], in0=PS[pb][:, :], in1=sg[j][:], op=ALU.mult),
                        reads=[("ps", pb), ("sg", j)], writes=[("act", fc)])
            for cg in range(D // 512):
                base = (cg % 2) * 4
                for dc in range(NFC // DC):
                    wkd, wd = wload(nd, dc * DC, DC, cg * 512, 512)
                    for f in range(DC):
                        fc = dc * DC + f
                        for b in range(4):
                            S.op("tensor", lambda e, f=f, fc=fc, b=b, base=base, wd=wd: e.matmul(
                                PS[base + b][:, :], lhsT=act[:, fc, b * 128:(b + 1) * 128], rhs=wd[:, f, :],
                                start=(fc == 0), stop=(fc == NFC - 1)),
                                reads=[wkd, ("act", fc)], writes=[("ps", base + b)])
                for b in range(4):
                    epilogue(cg, b, base + b)
            return cnt

        def wo_tile(oT, okey, wname, xt, xkey):
            for cg in range(D // 512):
                base = (cg % 2) * 4
                wk, wv = wload(wname, 0, KH, cg * 512, 512)
                for b in range(4):
                    for k in range(KH):
                        S.op("tensor", lambda e, k=k, b=b, base=base, wv=wv: e.matmul(
                            PS[base + b][:, :], lhsT=oT[:, k, b * 128:(b + 1) * 128], rhs=wv[:, k, :],
                            start=(k == 0), stop=(k == KH - 1)),
                            reads=[wk, okey], writes=[("ps", base + b)])
                    S.op("vector", lambda e, b=b, base=base, cg=cg: e.tensor_tensor(
                        out=xt[:, b, cg * 512:(cg + 1) * 512], in0=PS[base + b][:, :],
                        in1=xt[:, b, cg * 512:(cg + 1) * 512], op=ALU.add),
                        reads=[("ps", base + b), xkey], writes=[xkey])

        if cfg.get("stop", 99) >= 3:
         with contextlib.ExitStack() as st_:
            alloc_ws(st_, 3)
            xt = st_.enter_context(nc.sbuf_tensor(_u("xt"), [128, 4, D], F32))
            xn = st_.enter_context(nc.sbuf_tensor(_u("xn"), [128, 4, D], BF16))
            junk = st_.enter_context(nc.sbuf_tensor(_u("junk"), [128, D], F32))
            stt = st_.enter_context(nc.sbuf_tensor(_u("stt"), [128, 8], F32))
            hT = st_.enter_context(nc.sbuf_tensor(_u("hT"), [128, KD, 512], BF16))
            oT = st_.enter_context(nc.sbuf_tensor(_u("oT"), [128, H, 512], BF16))
            act = st_.enter_context(nc.sbuf_tensor(_u("act"), [128, NFC, 512], BF16))
            sg = [st_.enter_context(nc.sbuf_tensor(_u("sg%d" % i), [128, 512], BF16)) for i in range(GC)]
            cnt = 0
            for t in range(NT0):
                ld(xt[:], x_all[t * 512:(t + 1) * 512, :].rearrange("(b p) d -> p b d", p=128), "xt")
                ld(oT[:], oT0[t, :, :, :].rearrange("h d t -> d h t"), "oT", reads=[("dram", "oT0")])
                wo_tile(oT, "oT", "nao", xt, "xt")
                norm_tile(xt, "xt", xn, junk, stt, gffn[:, 0, :], hT, "hT", [6, 7])

                def epi(cg, b, bank):
                    S.op("vector", lambda e: e.tensor_tensor(
                        out=xt[:, b, cg * 512:(cg + 1) * 512], in0=PS[bank][:, :],
                        in1=xt[:, b, cg * 512:(cg + 1) * 512], op=ALU.add),
                        reads=[("ps", bank), "xt"], writes=["xt"])
                cnt = swiglu_tile(hT, "hT", act, sg, ("fg", "fu", "fd"), epi, cnt)
                S.dma("gpsimd", lambda e, t=t: e.dma_start(
                    out=x1[t * 512:(t + 1) * 512, :].rearrange("(b p) d -> p b d", p=128), in_=xt[:]),
                    "st_xt", reads=["xt"], writes=[("dram", "x1")])
                conv_some(1)
            conv_some(1000)
            oi = st_.enter_context(nc.sbuf_tensor(_u("oi"), [128, NOWN * 4], I32))
            ld(oi[:], own_idx[:, :], "oi")
            for j in range(NOWN * 4):
                S.dma("gpsimd", lambda e, j=j: e.indirect_dma_start(
                    out=xt[:, j % 4, :], out_offset=None, in_=x1[0:NT0 * 512, :],
                    in_offset=bass.IndirectOffsetOnAxis(ap=oi[:, j:j + 1], axis=0)),
                    "xt", reads=[("dram", "x1"), "oi"], writes=["xt"])
                if j % 4 == 3:
                    tt_ = NT0 + j // 4
                    S.dma("gpsimd", lambda e, tt_=tt_: e.dma_start(
                        out=x1[tt_ * 512:(tt_ + 1) * 512, :].rearrange("(b p) d -> p b d", p=128), in_=xt[:]),
                        "st_xt", reads=["xt"], writes=[("dram", "x1")])
            S.barrier()
            S.run()

        if cfg.get("stop", 99) >= 4:
         with contextlib.ExitStack() as st_:
            alloc_ws(st_, 3)
            xb = [st_.enter_context(nc.sbuf_tensor(_u("xb%d" % i), [128, D], F32)) for i in range(2)]
            xn = st_.enter_context(nc.sbuf_tensor(_u("xn"), [128, 4, D], BF16))
            junk = st_.enter_context(nc.sbuf_tensor(_u("junk"), [128, D], F32))
            stt = st_.enter_context(nc.sbuf_tensor(_u("stt"), [128, 8], F32))
            hT = st_.enter_context(nc.sbuf_tensor(_u("hT"), [128, KD, 512], BF16))
            cT = st_.enter_context(nc.sbuf_tensor(_u("cT"), [128, 8, 512], BF16))
            cn = st_.enter_context(nc.sbuf_tensor(_u("cn"), [128, 1024], BF16))
            s2 = st_.enter_context(nc.sbuf_tensor(_u("s2"), [128, 8], F32))
            latf = st_.enter_context(nc.sbuf_tensor(_u("latf"), [128, 1088], F32))
            kvf = [st_.enter_context(nc.sbuf_tensor(_u("kvf%d" % i), [128, 512], F32)) for i in range(2)]
            rpt = st_.enter_context(nc.sbuf_tensor(_u("rpt"), [128, 4, 64], F32))
            kr = st_.enter_context(nc.sbuf_tensor(_u("kr"), [128, 64], F32))
            kr2 = st_.enter_context(nc.sbuf_tensor(_u("kr2"), [128, 64], F32))
            krb = st_.enter_context(nc.sbuf_tensor(_u("krb"), [128, 64], BF16))
            krT = st_.enter_context(nc.sbuf_tensor(_u("krT"), [64, 512], BF16))
            qf = st_.enter_context(nc.sbuf_tensor(_u("qf"), [128, H, 192], F32))
            qsq = st_.enter_context(nc.sbuf_tensor(_u("qsq"), [128, H, 192], F32))
            qs = st_.enter_context(nc.sbuf_tensor(_u("qs"), [128, 4 * H], F32))
            qb = st_.enter_context(nc.sbuf_tensor(_u("qb"), [128, H, 192], BF16))
            qr1 = st_.enter_context(nc.sbuf_tensor(_u("qr1"), [128, H, 64], F32))
            qr2 = st_.enter_context(nc.sbuf_tensor(_u("qr2"), [128, H, 64], F32))
            tq = st_.enter_context(nc.sbuf_tensor(_u("tq"), [128, H, 32], F32))
            kf = st_.enter_context(nc.sbuf_tensor(_u("kf"), [128, H, 128], F32))
            kb = st_.enter_context(nc.sbuf_tensor(_u("kb"), [128, H, 128], BF16))
            vb = st_.enter_context(nc.sbuf_tensor(_u("vb"), [128, H, 128], BF16))
            qnT = [st_.enter_context(nc.sbuf_tensor(_u("qnT%d" % i), [128, H, 128], BF16)) for i in range(2)]
            qrT = st_.enter_context(nc.sbuf_tensor(_u("qrT"), [64, H, 128], BF16))
            knT = [st_.enter_context(nc.sbuf_tensor(_u("knT%d" % i), [128, H, 128], BF16)) for i in range(2)]
            P4S = cfg.get("p4s", 9)
            NQG = (H * 192) // 384
            NKG = (H * 256) // 512
            for t in range(NT1):
                def xsrc4(b, t=t):
                    r0 = t * 512 + b * 128
                    ld(xb[b % 2][:, :], x1[r0:r0 + 128, :], ("xb", b % 2), reads=[("dram", "x1")])
                    return xb[b % 2][:, :], ("xb", b % 2)
                ld(rpt[:], rope_in[t * 512:(t + 1) * 512, :].rearrange("(b p) c -> p b c", p=128), "rpt")
                norm_tile(xsrc4, None, xn, junk, stt, gmix[:, 1, :], hT, "hT", [6, 7])
                wk0, w0v = wload("dqkv", 0, KD, 0, 512)
                wk1, w1v = wload("dqkv", 0, KD, 512, 512)
                wk2, w2v = wload("dqkv", 0, KD, 1024, 64)
                for b in range(4):
                    for (bank, wk, wv, ncol) in ((0, wk0, w0v, 512), (1, wk1, w1v, 512), (2, wk2, w2v, 64)):
                        for k in range(KD):
                            S.op("tensor", lambda e, k=k, b=b, bank=bank, wv=wv, ncol=ncol: e.matmul(
                                PS[bank][:, 0:ncol], lhsT=hT[:, k, b * 128:(b + 1) * 128], rhs=wv[:, k, :],
                                start=(k == 0), stop=(k == KD - 1)),
                                reads=[wk, ("hT", k)], writes=[("ps", bank)])
                    for li, ncol in ((0, 512), (1, 512), (2, 64)):
                        S.op("scalar", lambda e, li=li, ncol=ncol: e.copy(out=latf[:, li * 512:li * 512 + ncol], in_=PS[li][:, 0:ncol]),
                             reads=[("ps", li)], writes=[("latf", li)])
                    for li, ncol in ((0, 512), (1, 512), (2, 64)):
                        S.op("vector", lambda e, li=li, ncol=ncol: e.tensor_tensor(
                            out=junk[:, 0:ncol], in0=latf[:, li * 512:li * 512 + ncol], in1=latf[:, li * 512:li * 512 + ncol],
                            op=ALU.mult), reads=[("latf", li)], writes=["junk"])
                        S.op("vector", lambda e, li=li, ncol=ncol: e.tensor_reduce(
                            out=s2[:, li:li + 1], in_=junk[:, 0:ncol], axis=AX.X, op=ALU.add),
                            reads=["junk"], writes=[("s2", li)])
                    S.op("scalar", lambda e: e.activation(out=s2[:, 4:6], in_=s2[:, 0:2], func=AF.Sqrt, scale=1.0 / 512, bias=EPS),
                         reads=[("s2", 0), ("s2", 1)], writes=["s2b"])
                    S.op("scalar", lambda e: e.activation(out=s2[:, 6:7], in_=s2[:, 2:3], func=AF.Sqrt, scale=1.0 / 64, bias=EPS),
                         reads=[("s2", 2), "s2b"], writes=["s2b"])
                    S.op("vector", lambda e: e.reciprocal(out=s2[:, 4:7], in_=s2[:, 4:7]), reads=["s2b"], writes=["s2b"])
                    for li in range(2):
                        S.op("scalar", lambda e, li=li: e.activation(
                            out=cn[:, li * 512:(li + 1) * 512], in_=latf[:, li * 512:(li + 1) * 512], func=AF.Copy,
                            scale=s2[:, 4 + li:5 + li]),
                            reads=[("latf", li), "s2b"], writes=[("cn", li)])
                    S.op("vector", lambda e: e.scalar_tensor_tensor(
                        out=kr[:], in0=latf[:, 1024:1088], scalar=s2[:, 6:7], in1=gkr[:], op0=ALU.mult, op1=ALU.mult),
                        reads=[("latf", 2), "s2b", "gkr"], writes=["kr"])
                    S.op("vector", lambda e, b=b: e.tensor_tensor(out=kr2[:, 0:32], in0=kr[:, 0:32], in1=rpt[:, b, 0:32], op=ALU.mult),
                         reads=["kr", "rpt"], writes=["kr2a"])
                    S.op("vector", lambda e, b=b: e.tensor_tensor(out=kr2[:, 32:64], in0=kr[:, 32:64], in1=rpt[:, b, 32:64], op=ALU.mult),
                         reads=["kr", "rpt"], writes=["kr2b"])
                    S.op("vector", lambda e: e.tensor_tensor(out=krb[:, 0:32], in0=kr2[:, 0:32], in1=kr2[:, 32:64], op=ALU.subtract),
                         reads=["kr2a", "kr2b"], writes=["krb0"])
                    S.op("vector", lambda e, b=b: e.tensor_tensor(out=kr2[:, 0:32], in0=kr[:, 0:32], in1=rpt[:, b, 32:64], op=ALU.mult),
                         reads=["kr", "rpt", "krb0"], writes=["kr2a"])
                    S.op("vector", lambda e, b=b: e.tensor_tensor(out=kr2[:, 32:64], in0=kr[:, 32:64], in1=rpt[:, b, 0:32], op=ALU.mult),
                         reads=["kr", "rpt", "krb0"], writes=["kr2b"])
                    S.op("vector", lambda e: e.tensor_tensor(out=krb[:, 32:64], in0=kr2[:, 0:32], in1=kr2[:, 32:64], op=ALU.add),
                         reads=["kr2a", "kr2b"], writes=["krb1"])
                    S.op("tensor", lambda e, b=b: e.transpose(out=psb(3)[0:64, b * 128:(b + 1) * 128], in_=krb[:, :],
                                                             identity=ident_b[:]),
                         reads=["krb0", "krb1", "ident_b"], writes=[("ps", 3)])
                    for li in range(2):
                        for k in range(4):
                            S.op("tensor", lambda e, li=li, k=k: e.transpose(
                                out=psb(4 + li)[:, k * 128:(k + 1) * 128], in_=cn[:, li * 512 + k * 128:li * 512 + (k + 1) * 128],
                                identity=ident_b[:]),
                                reads=[("cn", li), "ident_b"], writes=[("ps", 4 + li)])
                        gofs = 4 + 4 * li
                        S.op("vector", lambda e, li=li, gofs=gofs, b=b: e.tensor_tensor(
                            out=cT[:, 4 * li:4 * li + 4, b * 128:(b + 1) * 128],
                            in0=psb(4 + li)[:, 0:512].rearrange("p (k t) -> p k t", k=4),
                            in1=gsm[:, gofs:gofs + 4].unsqueeze(2).to_broadcast([128, 4, 128]), op=ALU.mult),
                            reads=[("ps", 4 + li), "gsm"], writes=[("cT", li, b)])
                S.op("scalar", lambda e: e.copy(out=krT[:, :], in_=psb(3)[0:64, 0:512]), reads=[("ps", 3)], writes=["krT"])
                S.dma("gpsimd", lambda e, t=t: e.dma_start(out=k1r[t, :, :], in_=krT[:, :]), "st_krT",
                      reads=["krT"], writes=[("dram", "k1r")])
                for b in range(4 if P4S >= 2 else 0):
                    for g in range(NQG):
                        wk, wv = wload("uq", 0, 4, g * 384, 384)
                        bank = g % 3
                        for k in range(4):
                            S.op("tensor", lambda e, k=k, b=b, bank=bank, wv=wv: e.matmul(
                                PS[bank][:, 0:384], lhsT=cT[:, k, b * 128:(b + 1) * 128], rhs=wv[:, k, :],
                                start=(k == 0), stop=(k == 3)),
                                reads=[wk, ("cT", 0, b)], writes=[("ps", bank)])
                        S.op("scalar", lambda e, g=g, bank=bank: e.copy(
                            out=qf[:, 2 * g:2 * g + 2, :].rearrange("p a c -> p (a c)"), in_=PS[bank][:, 0:384]),
                            reads=[("ps", bank)], writes=[("qf", g)])
                    if P4S < 2.2:
                        continue
                    qfk = [("qf", g) for g in range(NQG)]
                    S.op("vector", lambda e: e.tensor_tensor(out=qsq[:], in0=qf[:], in1=qf[:], op=ALU.mult),
                         reads=qfk, writes=["qsq"])
                    S.op("vector", lambda e: e.tensor_reduce(out=qs[:, 0:H], in_=qsq[:, :, 0:128], axis=AX.X, op=ALU.add),
                         reads=["qsq"], writes=["qs0"])
                    S.op("vector", lambda e: e.tensor_reduce(out=qs[:, H:2 * H], in_=qsq[:, :, 128:192], axis=AX.X, op=ALU.add),
                         reads=["qsq"], writes=["qs1"])
                    S.op("scalar", lambda e: e.activation(out=qs[:, 2 * H:3 * H], in_=qs[:, 0:H], func=AF.Sqrt, scale=1.0 / 128, bias=EPS),
                         reads=["qs0"], writes=["qs2"])
                    S.op("scalar", lambda e: e.activation(out=qs[:, 3 * H:4 * H], in_=qs[:, H:2 * H], func=AF.Sqrt, scale=1.0 / 64, bias=EPS),
                         reads=["qs1", "qs2"], writes=["qs2"])
                    S.op("vector", lambda e: e.reciprocal(out=qs[:, 2 * H:4 * H], in_=qs[:, 2 * H:4 * H]), reads=["qs2"], writes=["qs2"])
                    if P4S < 2.4:
                        continue
                    S.op("vector", lambda e: e.tensor_tensor(
                        out=qb[:, :, 0:128], in0=qf[:, :, 0:128],
                        in1=qs[:, 2 * H:3 * H].unsqueeze(2).to_broadcast([128, H, 128]), op=ALU.mult),
                        reads=qfk + ["qs2"], writes=["qbn"])
                    S.op("vector", lambda e: e.tensor_tensor(
                        out=qr1[:], in0=qf[:, :, 128:192],
                        in1=qs[:, 3 * H:4 * H].unsqueeze(2).to_broadcast([128, H, 64]), op=ALU.mult),
                        reads=qfk + ["qs2"], writes=["qr1"])
                    S.op("vector", lambda e: e.tensor_tensor(
                        out=qr1[:], in0=qr1[:], in1=gqr[:, :].unsqueeze(1).to_broadcast([128, H, 64]), op=ALU.mult),
                        reads=["qr1", "gqr"], writes=["qr1"])
                    cosb = rpt[:, b, 0:32].unsqueeze(1).to_broadcast([128, H, 32])
                    sinb = rpt[:, b, 32:64].unsqueeze(1).to_broadcast([128, H, 32])
                    S.op("vector", lambda e, cosb=cosb: e.tensor_tensor(out=qr2[:, :, 0:32], in0=qr1[:, :, 0:32], in1=cosb, op=ALU.mult),
                         reads=["qr1", "rpt"], writes=["qr2a"])
                    S.op("vector", lambda e, sinb=sinb: e.tensor_tensor(out=tq[:], in0=qr1[:, :, 32:64], in1=sinb, op=ALU.mult),
                         reads=["qr1", "rpt"], writes=["tq"])
                    S.op("vector", lambda e: e.tensor_tensor(out=qb[:, :, 128:160], in0=qr2[:, :, 0:32], in1=tq[:], op=ALU.subtract),
                         reads=["qr2a", "tq"], writes=["qbr0"])
                    S.op("vector", lambda e, sinb=sinb: e.tensor_tensor(out=qr2[:, :, 32:64], in0=qr1[:, :, 0:32], in1=sinb, op=ALU.mult),
                         reads=["qr1", "rpt"], writes=["qr2b"])
                    S.op("vector", lambda e, cosb=cosb: e.tensor_tensor(out=tq[:], in0=qr1[:, :, 32:64], in1=cosb, op=ALU.mult),
                         reads=["qr1", "rpt", "qbr0"], writes=["tq"])
                    S.op("vector", lambda e: e.tensor_tensor(out=qb[:, :, 160:192], in0=qr2[:, :, 32:64], in1=tq[:], op=ALU.add),
                         reads=["qr2b", "tq"], writes=["qbr1"])
                    if P4S < 2.6:
                        continue
                    for h4 in range(H // 4):
                        bank = 4 + h4 % 2
                        for hh in range(4):
                            h = h4 * 4 + hh
                            S.op("tensor", lambda e, h=h, hh=hh, bank=bank: e.transpose(
                                out=psb(bank)[:, hh * 128:(hh + 1) * 128], in_=qb[:, h, 0:128], identity=ident_b[:]),
                                reads=["qbn", "ident_b"], writes=[("ps", bank)])
                            if P4S >= 2.8: S.op("tensor", lambda e, h=h, hh=hh, bank=bank: e.transpose(
                                out=psb(bank)[0:64, 512 + hh * 128:512 + (hh + 1) * 128], in_=qb[:, h, 128:192],
                                identity=ident_b[:]),
                                reads=["qbr0", "qbr1", "ident_b"], writes=[("ps", bank)])
                        S.op("scalar", lambda e, h4=h4, bank=bank, b=b: e.activation(
                            out=qnT[b % 2][:, h4 * 4:h4 * 4 + 4, :],
                            in_=psb(bank)[:, 0:512].rearrange("p (a t) -> p a t", a=4), func=AF.Copy, scale=gsm[:, 2:3]),
                            reads=[("ps", bank), "gsm"], writes=[("qnT", b % 2)])
                        if P4S >= 2.9: S.op("scalar", lambda e, h4=h4, bank=bank, b=b: e.copy(
                            out=qrT[:, h4 * 4:h4 * 4 + 4, :],
                            in_=psb(bank)[0:64, 512:1024].rearrange("p (a t) -> p a t", a=4)),
                            reads=[("ps", bank)], writes=["qrT"])
                    if P4S >= 4:
                        S.dma("gpsimd", lambda e, t=t, b=b: e.dma_start(
                            out=q1n[t, :, :, b * 128:(b + 1) * 128].rearrange("h d t -> d h t"), in_=qnT[b % 2][:]),
                            ("st_qnT", b % 2), reads=[("qnT", b % 2)], writes=[("dram", "q1n")])
                        S.dma("gpsimd", lambda e, t=t, b=b: e.dma_start(
                            out=q1r[t, :, :, b * 128:(b + 1) * 128].rearrange("h d t -> d h t"), in_=qrT[:]),
                            "st_qrT", reads=["qrT"], writes=[("dram", "q1r")])
                    if P4S < 3:
                        continue
                    for g in range(NKG):
                        wk, wv = wload("ukv", 0, 4, g * 512, 512)
                        bank = g % 3
                        for k in range(4):
                            S.op("tensor", lambda e, k=k, b=b, bank=bank, wv=wv: e.matmul(
                                PS[bank][:, :], lhsT=cT[:, 4 + k, b * 128:(b + 1) * 128], rhs=wv[:, k, :],
                                start=(k == 0), stop=(k == 3)),
                                reads=[wk, ("cT", 1, b)], writes=[("ps", bank)])
                        pv = lambda bank=bank: PS[bank][:, :].rearrange("p (a c) -> p a c", a=2)
                        g2 = g % 2
                        S.op("scalar", lambda e, g2=g2, bank=bank: e.copy(out=kvf[g2][:], in_=PS[bank][:, :]),
                             reads=[("ps", bank)], writes=[("kvf", g2)])
                        kvv = kvf[g2][:].rearrange("p (a c) -> p a c", a=2)
                        S.op("vector", lambda e, g=g, kvv=kvv: e.tensor_copy(out=kf[:, 2 * g:2 * g + 2, :], in_=kvv[:, :, 0:128]),
                             reads=[("kvf", g2)], writes=[("kf", g)])
                        S.op("vector", lambda e, g=g, kvv=kvv: e.tensor_copy(out=vb[:, 2 * g:2 * g + 2, :], in_=kvv[:, :, 128:256]),
                             reads=[("kvf", g2)], writes=[("vb", g)])
                    kfk = [("kf", g) for g in range(NKG)]
                    S.op("vector", lambda e: e.tensor_tensor(out=qsq[:, :, 0:128], in0=kf[:], in1=kf[:], op=ALU.mult),
                         reads=kfk, writes=["qsq"])
                    S.op("vector", lambda e: e.tensor_reduce(out=qs[:, 0:H], in_=qsq[:, :, 0:128], axis=AX.X, op=ALU.add),
                         reads=["qsq"], writes=["qs0"])
                    S.op("scalar", lambda e: e.activation(out=qs[:, 2 * H:3 * H], in_=qs[:, 0:H], func=AF.Sqrt, scale=1.0 / 128, bias=EPS),
                         reads=["qs0"], writes=["qs2"])
                    S.op("vector", lambda e: e.reciprocal(out=qs[:, 2 * H:3 * H], in_=qs[:, 2 * H:3 * H]), reads=["qs2"], writes=["qs2"])
                    S.op("vector", lambda e: e.tensor_tensor(
                        out=kb[:], in0=kf[:], in1=qs[:, 2 * H:3 * H].unsqueeze(2).to_broadcast([128, H, 128]), op=ALU.mult),
                        reads=kfk + ["qs2"], writes=["kb"])
                    for h4 in range(H // 4):
                        bank = 6 + h4 % 2
                        for hh in range(4):
                            h = h4 * 4 + hh
                            S.op("tensor", lambda e, h=h, hh=hh, bank=bank: e.transpose(
                                out=psb(bank)[:, hh * 128:(hh + 1) * 128], in_=kb[:, h, :], identity=ident_b[:]),
                                reads=["kb", "ident_b"], writes=[("ps", bank)])
                        S.op("scalar", lambda e, h4=h4, bank=bank, b=b: e.activation(
                            out=knT[b % 2][:, h4 * 4:h4 * 4 + 4, :],
                            in_=psb(bank)[:, 0:512].rearrange("p (a t) -> p a t", a=4), func=AF.Copy, scale=gsm[:, 3:4]),
                            reads=[("ps", bank), "gsm"], writes=[("knT", b % 2)])
                    if P4S >= 4:
                        S.dma("gpsimd", lambda e, t=t, b=b: e.dma_start(
                            out=k1n[t, :, :, b * 128:(b + 1) * 128].rearrange("h d t -> d h t"), in_=knT[b % 2][:]),
                            ("st_knT", b % 2), reads=[("knT", b % 2)], writes=[("dram", "k1n")])
                    r0 = t * 512 + b * 128
                    S.dma("gpsimd", lambda e, r0=r0: e.dma_start(
                        out=v1[r0:r0 + 128, :].rearrange("p (h c) -> p h c", h=H), in_=vb[:]),
                        "st_vb", reads=[("vb", g) for g in range(NKG)], writes=[("dram", "v1")])
            S.barrier()
            S.run()

        if cfg.get("stop", 99) >= 5:
         with contextlib.ExitStack() as st_:
            TK = max(RS * 64, RP * 64)
            krS = st_.enter_context(nc.sbuf_tensor(_u("krS"), [64, TK], BF16))
            knS = [st_.enter_context(nc.sbuf_tensor(_u("knS%d" % i), [128, TK], BF16)) for i in range(2)]
            vS = [st_.enter_context(nc.sbuf_tensor(_u("vS%d" % i), [128, TK // 128, 128], BF16)) for i in range(2)]
            qnS = [st_.enter_context(nc.sbuf_tensor(_u("qnS%d" % i), [128, 512], BF16)) for i in range(2)]
            qrS = [st_.enter_context(nc.sbuf_tensor(_u("qrS%d" % i), [64, 512], BF16)) for i in range(2)]
            pT = [st_.enter_context(nc.sbuf_tensor(_u("pT%d" % i), [128, 512], BF16)) for i in range(3)]
            rcp = [st_.enter_context(nc.sbuf_tensor(_u("rcp%d" % i), [128, 512], F32)) for i in range(2)]
            oo = [st_.enter_context(nc.sbuf_tensor(_u("oo%d" % i), [128, 512], BF16)) for i in range(2)]
            scale1 = 192 ** -0.5
            seqs1 = []
            for s in range(NS):
                seqs1.append(([s * TS + i for i in range(TS)], [s * TS + i for i in range(TS)],
                              [s * TS + i for i in range(TS)]))
            seqs1.append(([NT0 + i for i in range(NOWN)], [NS * TS + i for i in range(TP)],
                          [NS * TS + i for i in range(NOWN)]))
            hc = 0
            qc = 0
            cc = 0
            for (qtiles, kvtiles, otiles) in seqs1:
                T = len(kvtiles) * 512
                NC = T // 128
                for i, kt in enumerate(kvtiles):
                    S.dma("sync", lambda e, i=i, kt=kt: e.dma_start(out=krS[:, i * 512:(i + 1) * 512], in_=k1r[kt, :, :]),
                          "krS", reads=[("dram", "k1r")], writes=["krS"])
                for h in range(H):
                    hs = hc % 2
                    hc += 1
                    for i, kt in enumerate(kvtiles):
                        S.dma("sync", lambda e, i=i, kt=kt, h=h, hs=hs: e.dma_start(
                            out=knS[hs][:, i * 512:(i + 1) * 512], in_=k1n[kt, h, :, :]),
                            ("knS", hs), reads=[("dram", "k1n")], writes=[("knS", hs)])
                    tok0 = kvtiles[0] * 512
                    S.dma("sync", lambda e, h=h, hs=hs, tok0=tok0, T=T, NC=NC: e.dma_start(
                        out=vS[hs][:, 0:NC, :],
                        in_=v1[tok0:tok0 + T, h * 128:(h + 1) * 128].rearrange("(b p) c -> p b c", p=128)),
                        ("vS", hs), reads=[("dram", "v1")], writes=[("vS", hs)])
                    for qi, qt in enumerate(qtiles):
                        q2 = qc % 2
                        qc += 1
                        S.dma("sync", lambda e, qt=qt, h=h, q2=q2: e.dma_start(out=qnS[q2][:], in_=q1n[qt, h, :, :]),
                              ("qnS", q2), reads=[("dram", "q1n")], writes=[("qnS", q2)])
                        S.dma("sync", lambda e, qt=qt, h=h, q2=q2: e.dma_start(out=qrS[q2][:], in_=q1r[qt, h, :, :]),
                              ("qrS", q2), reads=[("dram", "q1r")], writes=[("qrS", q2)])
                        obk = 4 + q2
                        dbk = 6 + q2

                        def pv_step(c, p3, obk=obk, dbk=dbk, hs=hs, NC=NC):
                            S.op("tensor", lambda e: e.matmul(PS[obk][:, :], lhsT=vS[hs][:, c, :], rhs=pT[p3][:],
                                                              start=(c == 0), stop=(c == NC - 1)),
                                 reads=[("vS", hs), ("pT", p3)], writes=[("ps", obk)])
                            S.op("tensor", lambda e: e.matmul(PS[dbk][:, :], lhsT=ones_b[:], rhs=pT[p3][:],
                                                              start=(c == 0), stop=(c == NC - 1)),
                                 reads=["ones_b", ("pT", p3)], writes=[("ps", dbk)])
                        prev = None
                        for c in range(NC):
                            sbk = cc % 4
                            p3 = cc % 3
                            cc += 1
                            S.op("tensor", lambda e, c=c, sbk=sbk, hs=hs, q2=q2: e.matmul(
                                PS[sbk][:, :], lhsT=knS[hs][:, c * 128:(c + 1) * 128], rhs=qnS[q2][:], start=True, stop=False),
                                reads=[("knS", hs), ("qnS", q2)], writes=[("ps", sbk)])
                            S.op("tensor", lambda e, c=c, sbk=sbk, q2=q2: e.matmul(
                                PS[sbk][:, :], lhsT=krS[:, c * 128:(c + 1) * 128], rhs=qrS[q2][:], start=False, stop=True),
                                reads=["krS", ("qrS", q2)], writes=[("ps", sbk)])
                            S.op("scalar", lambda e, sbk=sbk, p3=p3: e.activation(out=pT[p3][:], in_=PS[sbk][:, :],
                                                                                  func=AF.Exp, scale=scale1),
                                 reads=[("ps", sbk)], writes=[("pT", p3)])
                            if prev is not None:
                                pv_step(*prev)
                            prev = (c, p3)
                        pv_step(*prev)
                        S.op("vector", lambda e, q2=q2, dbk=dbk: e.reciprocal(out=rcp[q2][:], in_=PS[dbk][:, :]),
                             reads=[("ps", dbk)], writes=[("rcp", q2)])
                        S.op("vector", lambda e, q2=q2, obk=obk: e.tensor_tensor(out=oo[q2][:], in0=PS[obk][:, :],
                                                                                 in1=rcp[q2][:], op=ALU.mult),
                             reads=[("ps", obk), ("rcp", q2)], writes=[("oo", q2)])
                        ot = otiles[qi]
                        S.dma("gpsimd", lambda e, ot=ot, h=h, q2=q2: e.dma_start(out=oT1[ot, h, :, :], in_=oo[q2][:]),
                              ("st_oo", q2), reads=[("oo", q2)], writes=[("dram", "oT1")])
            S.barrier()
            S.run()

        if cfg.get("stop", 99) >= 6:
         with contextlib.ExitStack() as st_:
            alloc_ws(st_, 3)
            xt = st_.enter_context(nc.sbuf_tensor(_u("xt"), [128, 4, D], F32))
            xn = st_.enter_context(nc.sbuf_tensor(_u("xn"), [128, 4, D], BF16))
            junk = st_.enter_context(nc.sbuf_tensor(_u("junk"), [128, D], F32))
            stt = st_.enter_context(nc.sbuf_tensor(_u("stt"), [128, 8], F32))
            hT = st_.enter_context(nc.sbuf_tensor(_u("hT"), [128, KD, 512], BF16))
            oT = st_.enter_context(nc.sbuf_tensor(_u("oT"), [128, H, 512], BF16))
            act = st_.enter_context(nc.sbuf_tensor(_u("act"), [128, NFC, 512], BF16))
            sg = [st_.enter_context(nc.sbuf_tensor(_u("sg%d" % i), [128, 512], BF16)) for i in range(GC)]
            wr = st_.enter_context(nc.sbuf_tensor(_u("wr"), [128, KD, E], F32))
            h32 = st_.enter_context(nc.sbuf_tensor(_u("h32"), [128, KD, 128], F32))
            lg = st_.enter_context(nc.sbuf_tensor(_u("lg"), [128, 4, E], F32))
            cmb = st_.enter_context(nc.sbuf_tensor(_u("cmb"), [128, 4, E], F32))
            m1 = st_.enter_context(nc.sbuf_tensor(_u("m1"), [128, 8], F32))
            mk1 = st_.enter_context(nc.sbuf_tensor(_u("mk1"), [128, E], F32))
            mk2 = st_.enter_context(nc.sbuf_tensor(_u("mk2"), [128, E], F32))
            lg2 = st_.enter_context(nc.sbuf_tensor(_u("lg2"), [128, E], F32))
            ld(wr[:], w_router[:, :, :], "wr")
            cnt = 0
            for ti, t in enumerate(l1_tiles):
                ld(xt[:], x1[t * 512:(t + 1) * 512, :].rearrange("(b p) d -> p b d", p=128), "xt", reads=[("dram", "x1")])
                ld(oT[:], oT1[ti, :, :, :].rearrange("h d t -> d h t"), "oT", reads=[("dram", "oT1")])
                wo_tile(oT, "oT", "mo", xt, "xt")
                norm_tile(xt, "xt", xn, junk, stt, gffn[:, 1, :], hT, "hT", [6, 7])
                for b in range(4):
                    S.op("scalar", lambda e, b=b: e.activation(out=junk[:], in_=xt[:, b, :], func=AF.Copy,
                                                               scale=stt[:, 4 + b:5 + b]),
                         reads=["xt", "st2"], writes=["junk"])
                    for k4 in range(KD // 4):
                        bank = k4 % 2
                        for kk in range(4):
                            k = k4 * 4 + kk
                            S.op("tensor", lambda e, k=k, kk=kk, bank=bank: e.transpose(
                                out=PS[bank][:, kk * 128:(kk + 1) * 128], in_=junk[:, k * 128:(k + 1) * 128],
                                identity=ident_f[:]),
                                reads=["junk", "ident_f"], writes=[("ps", bank)])
                        S.op("vector", lambda e, k4=k4, bank=bank: e.tensor_tensor(
                            out=h32[:, k4 * 4:k4 * 4 + 4, :], in0=PS[bank][:, :].rearrange("p (a t) -> p a t", a=4),
                            in1=gffn[:, 1, k4 * 4:k4 * 4 + 4].unsqueeze(2).to_broadcast([128, 4, 128]), op=ALU.mult),
                            reads=[("ps", bank), "gffn"], writes=[("h32", k4)])
                    for k in range(KD):
                        S.op("tensor", lambda e, k=k: e.matmul(PS[2][:, 0:E], lhsT=h32[:, k, :], rhs=wr[:, k, :],
                                                               start=(k == 0), stop=(k == KD - 1)),
                             reads=[("h32", k // 4), "wr"], writes=[("ps", 2)])
                    S.op("vector", lambda e, b=b: e.tensor_scalar(out=lg[:, b, :], in0=PS[2][:, 0:E], scalar1=1.0, scalar2=None, op0=ALU.mult),
                         reads=[("ps", 2)], writes=[("lg", b)])
                    S.op("vector", lambda e, b=b: e.tensor_reduce(out=m1[:, 0:1], in_=lg[:, b, :], axis=AX.X, op=ALU.max),
                         reads=[("lg", b)], writes=["m1a"])
                    S.op("vector", lambda e, b=b: e.tensor_scalar(out=mk1[:], in0=lg[:, b, :], scalar1=m1[:, 0:1], scalar2=None,
                                                                  op0=ALU.is_equal), reads=[("lg", b), "m1a"], writes=["mk1"])
                    S.op("vector", lambda e, b=b: e.scalar_tensor_tensor(
                        out=lg2[:], in0=mk1[:], scalar=-1e30, in1=lg[:, b, :], op0=ALU.mult, op1=ALU.add),
                        reads=["mk1", ("lg", b)], writes=["lg2"])
                    S.op("vector", lambda e: e.tensor_reduce(out=m1[:, 1:2], in_=lg2[:], axis=AX.X, op=ALU.max),
                         reads=["lg2"], writes=["m1b"])
                    S.op("vector", lambda e: e.tensor_scalar(out=mk2[:], in0=lg2[:], scalar1=m1[:, 1:2], scalar2=None,
                                                             op0=ALU.is_equal), reads=["lg2", "m1b"], writes=["mk2"])
                    S.op("vector", lambda e: e.tensor_tensor(out=m1[:, 2:3], in0=m1[:, 1:2], in1=m1[:, 0:1], op=ALU.subtract),
                         reads=["m1a", "m1b"], writes=["m1c"])
                    S.op("scalar", lambda e: e.activation(out=m1[:, 3:4], in_=m1[:, 2:3], func=AF.Exp),
                         reads=["m1c"], writes=["m1d"])
                    S.op("vector", lambda e: e.tensor_scalar(out=m1[:, 4:5], in0=m1[:, 3:4], scalar1=1.0, scalar2=None,
                                                             op0=ALU.add), reads=["m1d"], writes=["m1e"])
                    S.op("vector", lambda e: e.reciprocal(out=m1[:, 5:6], in_=m1[:, 4:5]), reads=["m1e"], writes=["m1f"])
                    S.op("vector", lambda e: e.tensor_tensor(out=m1[:, 6:7], in0=m1[:, 3:4], in1=m1[:, 5:6], op=ALU.mult),
                         reads=["m1d", "m1f"], writes=["m1g"])
                    S.op("vector", lambda e: e.tensor_scalar(out=mk1[:], in0=mk1[:], scalar1=m1[:, 5:6], scalar2=None,
                                                             op0=ALU.mult), reads=["mk1", "m1f", "lg2"], writes=["mk1"])
                    S.op("vector", lambda e, b=b: e.scalar_tensor_tensor(
                        out=cmb[:, b, :], in0=mk2[:], scalar=m1[:, 6:7], in1=mk1[:], op0=ALU.mult, op1=ALU.add),
                        reads=["mk2", "m1g", "mk1"], writes=[("cmb", b)])
                for e_ in range(E):
                    def epi(cg, b, bank, e_=e_):
                        S.op("vector", lambda e: e.scalar_tensor_tensor(
                            out=xt[:, b, cg * 512:(cg + 1) * 512], in0=PS[bank][:, :], scalar=cmb[:, b, e_:e_ + 1],
                            in1=xt[:, b, cg * 512:(cg + 1) * 512], op0=ALU.mult, op1=ALU.add),
                            reads=[("ps", bank), "xt", ("cmb", b)], writes=["xt"])
                    cnt = swiglu_tile(hT, "hT", act, sg, ("mg%d" % e_, "mu%d" % e_, "md%d" % e_), epi, cnt)
                if ti < NS * TS:
                    dst = ys[ti * 512:(ti + 1) * 512, :]
                else:
                    dst = yp[(ti - NS * TS) * 512:(ti - NS * TS + 1) * 512, :]
                S.dma("gpsimd", lambda e, dst=dst: e.dma_start(out=dst.rearrange("(b p) d -> p b d", p=128), in_=xt[:]),
                      "st_xt", reads=["xt"], writes=[("dram", "y")])
            S.barrier()
            S.run()
    return nc


def _rope_table(pos_tokens, grid_w=64, theta=10000.0):
    t = np.asarray(pos_tokens)
    row = (t // grid_w).astype(np.float32)
    col = (t % grid_w).astype(np.float32)
    n_pairs = 16
    inv = (np.float32(theta) ** (-np.arange(n_pairs, dtype=np.float32) / np.float32(n_pairs))).astype(np.float32)
    ang = np.concatenate([row[:, None] * inv, col[:, None] * inv], axis=-1).astype(np.float32)
    return np.concatenate([np.cos(ang), np.sin(ang)], axis=-1).astype(np.float32)


def make_core_inputs(cfg, inp, core):
    D, H, FF, E = cfg["D"], cfg["H"], cfg["FF"], cfg["E"]
    RS, RP, NS = cfg["RS"], cfg["RP"], cfg["NS"]
    KD = D // 128
    f = lambda a: np.ascontiguousarray(np.asarray(a, dtype=np.float32))
    xs = inp["x_sample"]
    xp = inp["x_prompt"]
    x_all = np.concatenate([f(xs[core * NS + s]) for s in range(NS)] + [f(xp[0])], axis=0)
    n_own_rows = RP // 8 * 64
    NOWN = RP // 64
    base = NS * RS * 64
    own = base + core * n_own_rows + np.arange(n_own_rows)
    own_idx = np.ascontiguousarray(own.reshape(NOWN * 4, 128).T.astype(np.int32))
    pos = np.concatenate([np.arange(RS * 64)] * NS + [np.arange(RP * 64)] + [core * n_own_rows + np.arange(n_own_rows)])
    rope = _rope_table(pos)
    pk = lambda g: np.ascontiguousarray(f(g).reshape(-1, 128).T)
    g_mix = np.ascontiguousarray(np.stack([pk(inp["mix_norm"][l]) for l in range(2)], axis=1))
    g_ffn = np.ascontiguousarray(np.stack([pk(inp["ffn_norm"][l]) for l in range(2)], axis=1))
    rpb = f(inp["na_rpb"][0])
    kc = np.arange(64)[:, None]
    qc = np.arange(64)[None, :]
    dc = np.clip(kc - qc + 15, 0, 30)
    ws = np.clip(qc - 8, 0, 48)
    valid = ((kc >= ws) & (kc < ws + 16)).astype(np.float32)
    rpbg = np.zeros((128, H, 14, 64), np.float32)
    for a in range(2):
        for m in range(14):
            rpbg[a * 64:(a + 1) * 64, :, m, :] = np.transpose(rpb[:, m + a][:, dc], (1, 0, 2))
    namask = np.concatenate([valid, valid], axis=0)
    d = {
        "x_all": x_all, "own_idx": own_idx, "rope": rope, "ident": np.eye(128, dtype=np.float32),
        "g_mix": g_mix, "g_ffn": g_ffn,
        "g_naq": pk(inp["na_q_gain"][0]), "g_nak": pk(inp["na_k_gain"][0]),
        "rpbg": rpbg, "namask": namask,
        "g_ql": pk(inp["mla_q_lora_gain"][0]), "g_kvl": pk(inp["mla_kv_lora_gain"][0]),
        "g_qn": pk(inp["mla_qn_gain"][0]), "g_kn": pk(inp["mla_kn_gain"][0]),
        "g_qr": np.ascontiguousarray(np.broadcast_to(f(inp["mla_qr_gain"][0])[None, :], (128, 64))),
        "g_kr": np.ascontiguousarray(np.broadcast_to(f(inp["mla_kr_gain"][0])[None, :], (128, 64))),
        "w_router": np.ascontiguousarray(f(inp["moe_w_router"][0]).reshape(KD, 128, E).transpose(1, 0, 2)),
        "na_w_qkv": f(inp["na_w_qkv"][0]), "na_w_o": f(inp["na_w_o"][0]),
        "ffn_w_gate": f(inp["ffn_w_gate"][0]), "ffn_w_up": f(inp["ffn_w_up"][0]), "ffn_w_down": f(inp["ffn_w_down"][0]),
        "mla_w_dqkv": f(inp["mla_w_dqkv"][0]), "mla_w_uq": f(inp["mla_w_uq"][0]), "mla_w_ukv": f(inp["mla_w_ukv"][0]),
        "mla_w_o": f(inp["mla_w_o"][0]),
    }
    for e_ in range(E):
        d["moe_w_gate%d" % e_] = f(inp["moe_w_gate"][0, e_])
        d["moe_w_up%d" % e_] = f(inp["moe_w_up"][0, e_])
        d["moe_w_down%d" % e_] = f(inp["moe_w_down"][0, e_])
    return d


def run_cfg(cfg, inp, trace=False):
    nc = build_program(cfg)
    shared = None
    in_maps = []
    for c in range(N_CORES):
        d = make_core_inputs(cfg, inp, c)
        if shared is None:
            shared = d
        else:
            for k in d:
                if k not in ("x_all", "own_idx", "rope"):
                    d[k] = shared[k]
        in_maps.append(d)
    res = run_bass_kernel_spmd(nc, in_maps, core_ids=list(range(N_CORES)), trace=trace)
    RS, RP, NS, D = cfg["RS"], cfg["RP"], cfg["NS"], cfg["D"]
    ys = np.stack([np.asarray(res.results[c]["ys"]).reshape(NS, RS * 64, D) for c in range(N_CORES)], axis=0)
    y_sample = ys.reshape(N_CORES * NS, RS * 64, D).astype(np.float32)
    y_prompt = np.concatenate([np.asarray(res.results[c]["yp"]) for c in range(N_CORES)], axis=0)[None].astype(np.float32)
    return (y_prompt, y_sample), res


def kernel(**inputs):
    out, _ = run_cfg(CFG_FULL, inputs)
    return out
```

```python
import contextlib
import numpy as np
import concourse.bass as bass
import concourse.mybir as mybir
from concourse.bass_utils import run_bass_kernel_spmd

F32 = mybir.dt.float32
BF16 = mybir.dt.bfloat16
I32 = mybir.dt.int32
AF = mybir.ActivationFunctionType
ALU = mybir.AluOpType
AX = mybir.AxisListType
ENGS = ("sync", "gpsimd", "scalar", "vector", "tensor")
EPS = 1e-6
N_CORES = 8

CFG_FULL = dict(D=2048, H=16, FF=5632, E=8, RS=32, RP=128, NS=2)


class Sched:
    def __init__(self, nc, stack, n_dma_sems=96):
        self.nc = nc
        self.ops = {e: [] for e in ENGS}
        self.esem = {e: stack.enter_context(nc.semaphore("es_" + e)) for e in ENGS}
        self.ecnt = {e: 0 for e in ENGS}
        self.seen = {e: {} for e in ENGS}
        self.free_sems = {"sync": [[stack.enter_context(nc.semaphore("dh%d" % i)), 0] for i in range(28)],
                          "gpsimd": [[stack.enter_context(nc.semaphore("dg%d" % i)), 0] for i in range(n_dma_sems - 28)]}
        self.dsem = {}
        self.lastw = {}
        self.reads = {}

    def _dma_sem(self, key, eng):
        key = (eng, key)
        if key not in self.dsem:
            self.dsem[key] = self.free_sems[eng].pop()
        return self.dsem[key]

    @staticmethod
    def _isdram(k):
        return isinstance(k, tuple) and len(k) > 0 and k[0] == "dram"

    def _deps(self, reads, writes):
        deps = {}

        def add(d):
            for sid, ev in d.items():
                if sid not in deps or deps[sid][1] < ev[1]:
                    deps[sid] = ev
        for r in reads:
            add(self.lastw.get(r, {}))
        for w in writes:
            if self._isdram(w):
                continue
            add(self.lastw.get(w, {}))
            add(self.reads.get(w, {}))
        return deps

    def _commit(self, reads, writes, ev):
        sid = id(ev[0])
        for r in reads:
            self.reads.setdefault(r, {})[sid] = ev
        for w in writes:
            if self._isdram(w):
                self.lastw.setdefault(w, {})[sid] = ev
            else:
                self.lastw[w] = {sid: ev}
                self.reads[w] = {}

    def _waits(self, eng, deps, skip_own):
        waits = []
        seen = self.seen[eng]
        own = id(self.esem[eng])
        for sid, (sem, val) in deps.items():
            if skip_own and sid == own:
                continue
            if seen.get(sid, 0) >= val:
                continue
            seen[sid] = val
            waits.append((sem, val))
        return waits

    def op(self, eng, fn, reads=(), writes=()):
        waits = self._waits(eng, self._deps(reads, writes), eng == "tensor")
        self.ecnt[eng] += 1
        ev = (self.esem[eng], self.ecnt[eng])

        def emit(e, fn=fn, waits=waits, sem=ev[0]):
            for s, v in waits:
                e.wait_ge(s, v)
            fn(e).then_inc(sem, 1)
        self.ops[eng].append(emit)
        self._commit(reads, writes, ev)

    def dma(self, eng, fn, semkey, reads=(), writes=()):
        waits = self._waits(eng, self._deps(reads, writes), False)
        ds = self._dma_sem(semkey, eng)
        ds[1] += 16
        ev = (ds[0], ds[1])

        def emit(e, fn=fn, waits=waits, sem=ev[0]):
            for s, v in waits:
                e.wait_ge(s, v)
            fn(e).then_inc(sem, 16)
        self.ops[eng].append(emit)
        self._commit(reads, writes, ev)

    def barrier(self):
        finals = [(ds[0], ds[1]) for ds in self.dsem.values() if ds[1] > 0]
        finals += [(self.esem[e], self.ecnt[e]) for e in ENGS if self.ecnt[e] > 0]
        for eng in ENGS:
            seen = self.seen[eng]
            w = []
            for s, v in finals:
                if seen.get(id(s), 0) >= v:
                    continue
                if id(s) == id(self.esem[eng]) and eng == "tensor":
                    continue
                seen[id(s)] = v
                w.append((s, v))

            def emit(e, w=w):
                for s, v in w:
                    e.wait_ge(s, v)
            self.ops[eng].append(emit)
        for (eng, _k), pair in self.dsem.items():
            self.free_sems[eng].append(pair)
        self.dsem = {}

    def run(self):
        ops = self.ops
        with self.nc.Block() as block:
            @block.sync
            def _(e):
                for f in ops["sync"]:
                    f(e)

            @block.gpsimd
            def _(e):
                for f in ops["gpsimd"]:
                    f(e)

            @block.scalar
            def _(e):
                for f in ops["scalar"]:
                    f(e)

            @block.vector
            def _(e):
                for f in ops["vector"]:
                    f(e)

            @block.tensor
            def _(e):
                for f in ops["tensor"]:
                    f(e)
        self.ops = {e: [] for e in ENGS}


_UCNT = [0]


def _u(name):
    _UCNT[0] += 1
    return "%s_%d" % (name, _UCNT[0])


def _chunk_div(n, cap):
    for c in range(min(n, cap), 0, -1):
        if n % c == 0:
            return c
    return 1


def build_program(cfg):
    D, H, FF, E = cfg["D"], cfg["H"], cfg["FF"], cfg["E"]
    RS, RP, NS = cfg["RS"], cfg["RP"], cfg["NS"]
    KD = D // 128
    NFC = FF // 128
    HD = H * 128
    KH = HD // 128
    assert D % 512 == 0 and FF % 512 == 0 and HD == D
    TS = RS // 8
    TP = RP // 8
    NOWN = RP // 64
    NT0 = NS * TS + TP
    NT1 = NT0 + NOWN
    NL1 = NS * TS + NOWN
    seqs0 = [(s * TS, TS, RS) for s in range(NS)] + [(NS * TS, TP, RP)]
    l1_tiles = list(range(NS * TS)) + [NT0 + i for i in range(NOWN)]
    QL = KVL = 512
    LAT = QL + KVL + 64

    nc = bass.Bass("TRN2", target_bir_lowering=False)

    def din(name, shape, dt=F32):
        return nc.dram_tensor(name, list(shape), dt, kind="ExternalInput").ap()

    def dscr(name, shape, dt):
        kind = "ExternalOutput" if name in cfg.get("dbg", ()) else "Internal"
        return nc.dram_tensor(name, list(shape), dt, kind=kind).ap()

    x_all = din("x_all", [NT0 * 512, D])
    own_idx = din("own_idx", [128, NOWN * 4], I32)
    rope_in = din("rope", [NT1 * 512, 64])
    ident_in = din("ident", [128, 128])
    g_mix = din("g_mix", [128, 2, KD])
    g_ffn = din("g_ffn", [128, 2, KD])
    g_naq = din("g_naq", [128, 1])
    g_nak = din("g_nak", [128, 1])
    rpbg = din("rpbg", [128, H, 14, 64])
    namask = din("namask", [128, 64])
    g_ql = din("g_ql", [128, 4])
    g_kvl = din("g_kvl", [128, 4])
    g_qn = din("g_qn", [128, 1])
    g_kn = din("g_kn", [128, 1])
    g_qr = din("g_qr", [128, 64])
    g_kr = din("g_kr", [128, 64])
    w_router = din("w_router", [128, KD, E])
    tri_in = din("tri", [128, 128])
    cst_in = din("cst", [128, 64])
    Wf = {
        "qkv": din("na_w_qkv", [D, 3 * HD]), "nao": din("na_w_o", [HD, D]),
        "fg": din("ffn_w_gate", [D, FF]), "fu": din("ffn_w_up", [D, FF]), "fd": din("ffn_w_down", [FF, D]),
        "dqkv": din("mla_w_dqkv", [D, LAT]), "uq": din("mla_w_uq", [QL, H * 192]),
        "ukv": din("mla_w_ukv", [KVL, H * 256]), "mo": din("mla_w_o", [HD, D]),
    }
    for e_ in range(E):
        Wf["mg%d" % e_] = din("moe_w_gate%d" % e_, [D, FF])
        Wf["mu%d" % e_] = din("moe_w_up%d" % e_, [D, FF])
        Wf["md%d" % e_] = din("moe_w_down%d" % e_, [FF, D])
    Wb = {k: dscr("wb_" + k, v.shape, BF16) for k, v in Wf.items() if not k.startswith("m") or k == "mo"}

    ys = nc.dram_tensor("ys", [NS * TS * 512, D], F32, kind="ExternalOutput").ap()
    yp = nc.dram_tensor("yp", [NOWN * 512, D], F32, kind="ExternalOutput").ap()

    qk0 = dscr("qk0", [NT0, 2 * H, 128, 512], BF16)
    v0 = dscr("v0", [NT0 * 512, HD], BF16)
    oT0 = dscr("oT0", [NT0, H, 128, 512], BF16)
    x1 = dscr("x1", [NT1 * 512, D], F32)
    q1n = dscr("q1n", [NT1, H, 128, 512], BF16)
    q1r = dscr("q1r", [NT1, H, 64, 512], BF16)
    k1n = dscr("k1n", [NT1, H, 128, 512], BF16)
    k1r = dscr("k1r", [NT1, 64, 512], BF16)
    v1 = dscr("v1", [NT1 * 512, HD], BF16)
    oT1 = dscr("oT1", [NL1, H, 128, 512], BF16)

    with contextlib.ExitStack() as gst:
        S = Sched(nc, gst)
        PS = [gst.enter_context(nc.psum_tensor("ps%d" % i, [128, 512], F32)) for i in range(8)]
        ident_f = gst.enter_context(nc.sbuf_tensor(_u("ident_f"), [128, 128], F32))
        ident_b = gst.enter_context(nc.sbuf_tensor(_u("ident_b"), [128, 128], BF16))
        ones_b = gst.enter_context(nc.sbuf_tensor(_u("ones_b"), [128, 128], BF16))
        gmix = gst.enter_context(nc.sbuf_tensor(_u("gmix"), [128, 2, KD], F32))
        gffn = gst.enter_context(nc.sbuf_tensor(_u("gffn"), [128, 2, KD], F32))
        gsm = gst.enter_context(nc.sbuf_tensor(_u("gsm"), [128, 12], F32))
        gqr = gst.enter_context(nc.sbuf_tensor(_u("gqr"), [128, 64], F32))
        gkr = gst.enter_context(nc.sbuf_tensor(_u("gkr"), [128, 64], F32))
        WS = []
        ws_i = [0]

        def alloc_ws(stk, n):
            WS[:] = [stk.enter_context(nc.sbuf_tensor(_u("ws%d" % i), [128, 8192], BF16)) for i in range(n)]

        def ld(dst, src, key, eng="sync", reads=()):
            S.dma(eng, lambda e: e.dma_start(out=dst, in_=src), key, reads=reads, writes=[key])

        ld(ident_f[:], ident_in[:, :], "ident_f")
        ld(gmix[:], g_mix[:, :, :], "gmix")
        ld(gffn[:], g_ffn[:, :, :], "gffn")
        ld(gsm[:, 0:1], g_naq[:, :], "gsm")
        ld(gsm[:, 1:2], g_nak[:, :], "gsm")
        ld(gsm[:, 2:3], g_qn[:, :], "gsm")
        ld(gsm[:, 3:4], g_kn[:, :], "gsm")
        ld(gsm[:, 4:8], g_ql[:, :], "gsm")
        ld(gsm[:, 8:12], g_kvl[:, :], "gsm")
        ld(gqr[:], g_qr[:, :], "gqr")
        ld(gkr[:], g_kr[:, :], "gkr")
        S.op("vector", lambda e: e.tensor_copy(out=ident_b[:], in_=ident_f[:]), reads=["ident_f"], writes=["ident_b"])
        S.op("vector", lambda e: e.memset(ones_b[:], 1.0), writes=["ones_b"])

        def conv(name):
            src, dst = Wf[name], Wb[name]
            rows, cols = src.shape
            step = max(128, (4 * 1024 * 1024 // cols) // 128 * 128)
            for r0 in range(0, rows, step):
                r1 = min(rows, r0 + step)
                S.dma("gpsimd", lambda e, r0=r0, r1=r1: e.dma_start(out=dst[r0:r1, :], in_=src[r0:r1, :]),
                      ("cv", name), writes=[("dram", "wb_" + name)])

        for nm in ("qkv", "nao", "fg", "fu", "fd", "dqkv", "uq", "ukv", "mo"):
            conv(nm)
        NCH = FF // 512
        NCG = D // 512
        NDC = NFC // _chunk_div(NFC, 16)
        DCm = _chunk_div(NFC, 16)
        war_g = dscr("war_g", [E * NCH * 128, KD * 512], BF16)
        war_u = dscr("war_u", [E * NCH * 128, KD * 512], BF16)
        war_d = dscr("war_d", [E * NCG * NDC * 128, DCm * 512], BF16)
        moe_conv_todo = []

        def _mk_gu(dst_t, src, e_, c, nm):
            r0 = (e_ * NCH + c) * 128
            return lambda e: e.dma_start(
                out=dst_t[r0:r0 + 128, :].rearrange("p (k f) -> p k f", f=512),
                in_=src[:, c * 512:(c + 1) * 512].rearrange("(k p) f -> p k f", p=128))

        def _mk_d(src, e_, cg, dc):
            r0 = ((e_ * NCG + cg) * NDC + dc) * 128
            return lambda e: e.dma_start(
                out=war_d[r0:r0 + 128, :].rearrange("p (f c) -> p f c", c=512),
                in_=src[dc * DCm * 128:(dc + 1) * DCm * 128, cg * 512:(cg + 1) * 512].rearrange("(f p) c -> p f c", p=128))

        for e_ in range(E):
            for c in range(NCH):
                moe_conv_todo.append((_mk_gu(war_g, Wf["mg%d" % e_], e_, c, "g"), "war_g"))
                moe_conv_todo.append((_mk_gu(war_u, Wf["mu%d" % e_], e_, c, "u"), "war_u"))
            for cg in range(NCG):
                for dc in range(NDC):
                    moe_conv_todo.append((_mk_d(Wf["md%d" % e_], e_, cg, dc), "war_d"))
        n_moe_conv = len(moe_conv_todo)
        cv_rr = [0]

        def conv_some(n):
            for _ in range(n):
                if moe_conv_todo:
                    fn, nm = moe_conv_todo.pop(0)
                    cv_rr[0] += 1
                    S.dma("gpsimd", fn, ("cvm", cv_rr[0] % 8), writes=[("dram", nm)])

        def wload(name, k0, nk, c0, ncols):
            s = ws_i[0] % len(WS)
            ws_i[0] += 1
            src = Wb[name][k0 * 128:(k0 + nk) * 128, c0:c0 + ncols].rearrange("(k p) f -> p k f", p=128)
            view = WS[s][:, 0:nk * ncols].rearrange("p (k c) -> p k c", c=ncols)
            S.dma("sync", lambda e: e.dma_start(out=view, in_=src), ("w", s),
                  reads=[("dram", "wb_" + name)], writes=[("w", s)])
            return ("w", s), view

        def psb(i):
            return PS[i][:, :].bitcast(BF16)

        def norm_tile(xt, xkey, xn, junk, st, gain, hT, hkey, tp_banks, hT32=None):
            if callable(xt):
                xsrc = xt
            else:
                xsrc = lambda b: (xt[:, b, :], xkey)
            for b in range(4):
                xa, xk = xsrc(b)
                S.op("vector", lambda e, xa=xa, b=b: e.scalar_tensor_tensor(
                    out=junk[:, :], in0=xa, scalar=1.0, in1=xa, op0=ALU.mult, op1=ALU.mult, accum_out=st[:, b:b + 1]),
                    reads=[xk], writes=["junk", ("st", b)])
                S.op("scalar", lambda e, b=b: e.activation(out=st[:, 4 + b:5 + b], in_=st[:, b:b + 1], func=AF.Sqrt,
                                                           scale=1.0 / D, bias=EPS),
                     reads=[("st", b)], writes=["st2"])
                S.op("vector", lambda e, b=b: e.reciprocal(out=st[:, 4 + b:5 + b], in_=st[:, 4 + b:5 + b]),
                     reads=["st2"], writes=["st2"])
                S.op("scalar", lambda e, b=b, xa=xa: e.activation(out=xn[:, b, :], in_=xa, func=AF.Copy,
                                                                  scale=st[:, 4 + b:5 + b]),
                     reads=[xk, "st2"], writes=[("xn", b)])
            if hT is None:
                return
            transpose_tile(xn, gain, hT, hkey, tp_banks)

        def transpose_tile(xn, gain, hT, hkey, tp_banks):
            for k in range(KD):
                bank = tp_banks[(k // 2) % len(tp_banks)]
                half = k % 2
                tpv = psb(bank)[:, half * 512:(half + 1) * 512]
                for b in range(4):
                    S.op("tensor", lambda e, b=b, k=k, tpv=tpv: e.transpose(
                        out=tpv[:, b * 128:(b + 1) * 128], in_=xn[:, b, k * 128:(k + 1) * 128], identity=ident_b[:]),
                        reads=[("xn", b), "ident_b"], writes=[("ps", bank)])
                if k % 2 == 0:
                    S.op("vector", lambda e, k=k, tpv=tpv: e.tensor_scalar(
                        out=hT[:, k, :], in0=tpv, scalar1=gain[:, k:k + 1], scalar2=None, op0=ALU.mult),
                        reads=[("ps", bank), "gmix", "gffn"], writes=[(hkey, k)])
                else:
                    S.op("scalar", lambda e, k=k, tpv=tpv: e.activation(
                        out=hT[:, k, :], in_=tpv, func=AF.Copy, scale=gain[:, k:k + 1]),
                        reads=[("ps", bank), "gmix", "gffn"], writes=[(hkey, k)])

        if cfg.get("stop", 99) >= 1:
         with contextlib.ExitStack() as st_:
            alloc_ws(st_, 3)
            xt = st_.enter_context(nc.sbuf_tensor(_u("xt"), [128, 4, D], F32))
            xn = st_.enter_context(nc.sbuf_tensor(_u("xn"), [128, 4, D], BF16))
            junk = st_.enter_context(nc.sbuf_tensor(_u("junk"), [128, D], F32))
            stt = st_.enter_context(nc.sbuf_tensor(_u("stt"), [128, 8], F32))
            hT = st_.enter_context(nc.sbuf_tensor(_u("hT"), [128, KD, 512], BF16))
            sq = [st_.enter_context(nc.sbuf_tensor(_u("sq%d" % i), [128, 512], BF16)) for i in range(2)]
            rs_ = [st_.enter_context(nc.sbuf_tensor(_u("rs%d" % i), [128, 512], F32)) for i in range(2)]
            qo = [st_.enter_context(nc.sbuf_tensor(_u("qo%d" % i), [128, 512], BF16)) for i in range(3)]
            vo = [st_.enter_context(nc.sbuf_tensor(_u("vo%d" % i), [128, 512], BF16)) for i in range(3)]
            cnt = 0
            vcnt = 0
            for t in range(NT0):
                ld(xt[:], x_all[t * 512:(t + 1) * 512, :].rearrange("(b p) d -> p b d", p=128), "xt")
                norm_tile(xt, "xt", xn, junk, stt, gmix[:, 0, :], hT, "hT", [6, 7])
                for ch in range(2 * H // 4):
                    wkey, wv = wload("qkv", 0, KD, ch * 512, 512)
                    for j in range(4):
                        hj = ch * 4 + j
                        pb = cnt % 4
                        sb = 4 + cnt % 2
                        i2 = cnt % 2
                        i3 = cnt % 3
                        cnt += 1
                        for k in range(KD):
                            S.op("tensor", lambda e, k=k, j=j, pb=pb, wv=wv: e.matmul(
                                PS[pb][:, :], lhsT=wv[:, k, j * 128:(j + 1) * 128], rhs=hT[:, k, :],
                                start=(k == 0), stop=(k == KD - 1)),
                                reads=[wkey, ("hT", k)], writes=[("ps", pb)])
                        S.op("scalar", lambda e, pb=pb, i2=i2: e.activation(out=sq[i2][:], in_=PS[pb][:, :], func=AF.Square),
                             reads=[("ps", pb)], writes=[("sq", i2)])
                        S.op("tensor", lambda e, sb=sb, i2=i2: e.matmul(PS[sb][:, :], lhsT=ones_b[:], rhs=sq[i2][:],
                                                                         start=True, stop=True),
                             reads=[("sq", i2), "ones_b"], writes=[("ps", sb)])
                        S.op("scalar", lambda e, sb=sb, i2=i2: e.activation(
                            out=rs_[i2][:], in_=PS[sb][:, :], func=AF.Sqrt, scale=1.0 / 128, bias=EPS),
                            reads=[("ps", sb)], writes=[("rs", i2)])
                        S.op("vector", lambda e, i2=i2: e.reciprocal(out=rs_[i2][:], in_=rs_[i2][:]),
                             reads=[("rs", i2)], writes=[("rs", i2)])
                        gcol = 0 if hj < H else 1
                        S.op("vector", lambda e, pb=pb, i2=i2, i3=i3, gcol=gcol: e.scalar_tensor_tensor(
                            out=qo[i3][:], in0=PS[pb][:, :], scalar=gsm[:, gcol:gcol + 1], in1=rs_[i2][:],
                            op0=ALU.mult, op1=ALU.mult),
                            reads=[("ps", pb), ("rs", i2), "gsm"], writes=[("qo", i3)])
                        S.dma("gpsimd", lambda e, i3=i3, t=t, hj=hj: e.dma_start(out=qk0[t, hj, :, :], in_=qo[i3][:]),
                              ("st_qo", i3), reads=[("qo", i3)], writes=[("dram", "qk0")])
                for cg in range(HD // 512):
                    wkey, wv = wload("qkv", 0, KD, 2 * HD + cg * 512, 512)
                    for b in range(4):
                        pb = cnt % 4
                        cnt += 1
                        i3 = vcnt % 3
                        vcnt += 1
                        for k in range(KD):
                            S.op("tensor", lambda e, k=k, b=b, pb=pb, wv=wv: e.matmul(
                                PS[pb][:, :], lhsT=hT[:, k, b * 128:(b + 1) * 128], rhs=wv[:, k, :],
                                start=(k == 0), stop=(k == KD - 1)),
                                reads=[wkey, ("hT", k)], writes=[("ps", pb)])
                        S.op("scalar", lambda e, pb=pb, i3=i3: e.copy(out=vo[i3][:], in_=PS[pb][:, :]),
                             reads=[("ps", pb)], writes=[("vo", i3)])
                        r0 = t * 512 + b * 128
                        S.dma("gpsimd", lambda e, i3=i3, r0=r0, cg=cg: e.dma_start(
                            out=v0[r0:r0 + 128, cg * 512:(cg + 1) * 512], in_=vo[i3][:]),
                            ("st_vo", i3), reads=[("vo", i3)], writes=[("dram", "v0")])
            S.barrier()
            S.run()

        if cfg.get("stop", 99) >= 2:
         with contextlib.ExitStack() as st_:
            TT = st_.enter_context(nc.sbuf_tensor(_u("TT"), [128, H, 14, 64], BF16))
            msk = st_.enter_context(nc.sbuf_tensor(_u("msk"), [128, 64], F32))
            rp = [st_.enter_context(nc.sbuf_tensor(_u("rp%d" % i), [128, 14, 64], F32)) for i in range(2)]
            WRmax = 16
            kw = st_.enter_context(nc.sbuf_tensor(_u("kw"), [128, H, WRmax * 64], BF16))
            vE = st_.enter_context(nc.sbuf_tensor(_u("vE"), [128, WRmax // 2, HD], BF16))
            vO = st_.enter_context(nc.sbuf_tensor(_u("vO"), [128, WRmax // 2 - 1, HD], BF16))
            qT = st_.enter_context(nc.sbuf_tensor(_u("qT"), [128, H, 512], BF16))
            oS = st_.enter_context(nc.sbuf_tensor(_u("oS"), [128, H, 512], BF16))
            Eb = [st_.enter_context(nc.sbuf_tensor(_u("Eb%d" % i), [128, 2, 4, 64], BF16)) for i in range(2)]
            Pb = [st_.enter_context(nc.sbuf_tensor(_u("Pb%d" % i), [128, 2, 4, 64], BF16)) for i in range(2)]
            rc = [st_.enter_context(nc.sbuf_tensor(_u("rc%d" % i), [128, 128], F32)) for i in range(2)]
            ld(msk[:], namask[:, :], "msk")
            for h in range(H):
                i2 = h % 2
                ld(rp[i2][:], rpbg[:, h, :, :], ("rp", i2))
                S.op("scalar", lambda e, i2=i2: e.activation(out=rp[i2][:], in_=rp[i2][:], func=AF.Exp),
                     reads=[("rp", i2)], writes=[("rp", i2)])
                S.op("vector", lambda e, i2=i2, h=h: e.tensor_tensor(
                    out=TT[:, h, :, :], in0=rp[i2][:], in1=msk[:, :].unsqueeze(1).to_broadcast([128, 14, 64]), op=ALU.mult),
                    reads=[("rp", i2), "msk"], writes=["TT"])
            scale = 128 ** -0.5
            gc = 0
            for (tile0, ntl, rows) in seqs0:
                WR = min(16, rows)
                for tl in range(ntl):
                    t = tile0 + tl
                    r0 = tl * 8
                    w0 = min(max(r0 - 4, 0), rows - WR)
                    rr = w0
                    while rr < w0 + WR:
                        st_tile = rr // 8
                        re = min(w0 + WR, (st_tile + 1) * 8)
                        n = (re - rr) * 64
                        off = (rr - st_tile * 8) * 64
                        dst = (rr - w0) * 64
                        S.dma("sync", lambda e, st_tile=st_tile, off=off, n=n, dst=dst, tile0=tile0: e.dma_start(
                            out=kw[:, :, dst:dst + n],
                            in_=qk0[tile0 + st_tile, H:2 * H, :, off:off + n].rearrange("h d t -> d h t")),
                            "kw", reads=[("dram", "qk0")], writes=["kw"])
                        rr = re
                    tok0 = tile0 * 512 + w0 * 64
                    S.dma("sync", lambda e, tok0=tok0, WR=WR: e.dma_start(
                        out=vE[:, 0:WR // 2, :], in_=v0[tok0:tok0 + WR * 64, :].rearrange("(b p) e -> p b e", p=128)),
                        "vE", reads=[("dram", "v0")], writes=["vE"])
                    if WR > 8:
                        S.dma("sync", lambda e, tok0=tok0, WR=WR: e.dma_start(
                            out=vO[:, 0:WR // 2 - 1, :],
                            in_=v0[tok0 + 64:tok0 + 64 + (WR - 2) * 64, :].rearrange("(b p) e -> p b e", p=128)),
                            "vO", reads=[("dram", "v0")], writes=["vO"])
                    S.dma("sync", lambda e, t=t: e.dma_start(
                        out=qT[:, :, :], in_=qk0[t, 0:H, :, :].rearrange("h d t -> d h t")),
                        "qT", reads=[("dram", "qk0")], writes=["qT"])
                    for rl in range(8):
                        r = r0 + rl
                        rs = min(max(r - 4, 0), rows - 8)
                        o = r - rs
                        rel = rs - w0
                        m0 = 7 - o
                        for hp in range(H // 2):
                            sbk = gc % 4
                            obk = 4 + gc % 2
                            dbk = 6 + gc % 2
                            i2 = gc % 2
                            gc += 1
                            for hh in range(2):
                                h = hp * 2 + hh
                                for p in range(4):
                                    kt0 = (rel + 2 * p) * 64
                                    S.op("tensor", lambda e, h=h, hh=hh, p=p, kt0=kt0, sbk=sbk, rl=rl: e.matmul(
                                        PS[sbk][:, (hh * 4 + p) * 64:(hh * 4 + p + 1) * 64],
                                        lhsT=kw[:, h, kt0:kt0 + 128], rhs=qT[:, h, rl * 64:(rl + 1) * 64],
                                        start=True, stop=True),
                                        reads=["kw", "qT"], writes=[("ps", sbk)])
                            S.op("scalar", lambda e, sbk=sbk, i2=i2: e.activation(
                                out=Eb[i2][:].rearrange("p a b c -> p (a b c)"), in_=PS[sbk][:, :], func=AF.Exp, scale=scale),
                                reads=[("ps", sbk)], writes=[("Eb", i2)])
                            S.op("vector", lambda e, i2=i2, hp=hp, m0=m0: e.tensor_tensor(
                                out=Pb[i2][:], in0=Eb[i2][:], in1=TT[:, 2 * hp:2 * hp + 2, m0:m0 + 7:2, :], op=ALU.mult),
                                reads=[("Eb", i2), "TT"], writes=[("Pb", i2)])
                            for hh in range(2):
                                h = hp * 2 + hh
                                for p in range(4):
                                    if rel % 2 == 0:
                                        vv = vE[:, rel // 2 + p, h * 128:(h + 1) * 128]
                                        vk = "vE"
                                    else:
                                        vv = vO[:, (rel - 1) // 2 + p, h * 128:(h + 1) * 128]
                                        vk = "vO"
                                    S.op("tensor", lambda e, hh=hh, p=p, vv=vv, obk=obk, i2=i2: e.matmul(
                                        PS[obk][:, hh * 64:(hh + 1) * 64], lhsT=vv, rhs=Pb[i2][:, hh, p, :],
                                        start=(p == 0), stop=(p == 3)),
                                        reads=[vk, ("Pb", i2)], writes=[("ps", obk)])
                            for p in range(4):
                                S.op("tensor", lambda e, p=p, dbk=dbk, i2=i2: e.matmul(
                                    PS[dbk][:, 0:128].rearrange("q (a b) -> q a b", a=2), lhsT=ones_b[:],
                                    rhs=Pb[i2][:, :, p, :], start=(p == 0), stop=(p == 3)),
                                    reads=[("Pb", i2), "ones_b"], writes=[("ps", dbk)])
                            S.op("vector", lambda e, dbk=dbk, i2=i2: e.reciprocal(out=rc[i2][:], in_=PS[dbk][:, 0:128]),
                                 reads=[("ps", dbk)], writes=[("rc", i2)])
                            S.op("vector", lambda e, obk=obk, i2=i2, hp=hp, rl=rl: e.tensor_tensor(
                                out=oS[:, 2 * hp:2 * hp + 2, rl * 64:(rl + 1) * 64],
                                in0=PS[obk][:, 0:128].rearrange("q (a b) -> q a b", a=2),
                                in1=rc[i2][:].rearrange("q (a b) -> q a b", a=2), op=ALU.mult),
                                reads=[("ps", obk), ("rc", i2)], writes=["oS"])
                    S.dma("gpsimd", lambda e, t=t: e.dma_start(
                        out=oT0[t, :, :, :].rearrange("h d t -> d h t"), in_=oS[:, :, :]),
                        "st_oS", reads=["oS"], writes=[("dram", "oT0")])
                    conv_some((n_moe_conv + 2 * NT0 - 1) // (2 * NT0))
            S.barrier()
            S.run()

        GC = 4
        DC = _chunk_div(NFC, 16)

        def swiglu_tile(hT, hkey, act, sg, names, epilogue, cnt0):
            cnt = cnt0
            if callable(names):
                loader = names
            else:
                ng, nu, nd = names

                def loader(kind, *a):
                    if kind == "g":
                        return wload(ng, 0, KD, a[0] * GC * 128, GC * 128)
                    if kind == "u":
                        return wload(nu, 0, KD, a[0] * GC * 128, GC * 128)
                    return wload(nd, a[1] * DC, DC, a[0] * 512, 512)
            for c in range(NFC // GC):
                wkg, wg = loader("g", c)
                gb = []
                for j in range(GC):
                    pb = cnt % 4
                    cnt += 1
                    gb.append(pb)
                    for k in range(KD):
                        S.op("tensor", lambda e, k=k, j=j, pb=pb, wg=wg: e.matmul(
                            PS[pb][:, :], lhsT=wg[:, k, j * 128:(j + 1) * 128], rhs=hT[:, k, :],
                            start=(k == 0), stop=(k == KD - 1)),
                            reads=[wkg, (hkey, k)], writes=[("ps", pb)])
                    S.op("scalar", lambda e, j=j, pb=pb: e.activation(out=sg[j][:], in_=PS[pb][:, :], func=AF.Silu),
                         reads=[("ps", pb)], writes=[("sg", j)])
                wku, wu = loader("u", c)
                for j in range(GC):
                    pb = 4 + cnt % 4
                    cnt += 1
                    fc = c * GC + j
                    for k in range(KD):
                        S.op("tensor", lambda e, k=k, j=j, pb=pb, wu=wu: e.matmul(
                            PS[pb][:, :], lhsT=wu[:, k, j * 128:(j + 1) * 128], rhs=hT[:, k, :],
                            start=(k == 0), stop=(k == KD - 1)),
                            reads=[wku, (hkey, k)], writes=[("ps", pb)])
                    S.op("vector", lambda e, j=j, pb=pb, fc=fc: e.tensor_tensor(
                        out=act[:, fc, :], in0=PS[pb][:, :], in1=sg[j][:], op=ALU.mult),
                        reads=[("ps", pb), ("sg", j)], writes=[("act", fc)])
            for cg in range(D // 512):
                base = (cg % 2) * 4
                for dc in range(NFC // DC):
                    wkd, wd = loader("d", cg, dc)
                    for f in range(DC):
                        fc = dc * DC + f
                        for b in range(4):
                            S.op("tensor", lambda e, f=f, fc=fc, b=b, base=base, wd=wd: e.matmul(
                                PS[base + b][:, :], lhsT=act[:, fc, b * 128:(b + 1) * 128], rhs=wd[:, f, :],
                                start=(fc == 0), stop=(fc == NFC - 1)),
                                reads=[wkd, ("act", fc)], writes=[("ps", base + b)])
                for b in range(4):
                    epilogue(cg, b, base + b)
            return cnt

        def wo_tile(oT, okey, wname, xt, xkey):
            for cg in range(D // 512):
                base = (cg % 2) * 4
                wk, wv = wload(wname, 0, KH, cg * 512, 512)
                for b in range(4):
                    for k in range(KH):
                        S.op("tensor", lambda e, k=k, b=b, base=base, wv=wv: e.matmul(
                            PS[base + b][:, :], lhsT=oT[:, k, b * 128:(b + 1) * 128], rhs=wv[:, k, :],
                            start=(k == 0), stop=(k == KH - 1)),
                            reads=[wk, okey], writes=[("ps", base + b)])
                    S.op("vector", lambda e, b=b, base=base, cg=cg: e.tensor_tensor(
                        out=xt[:, b, cg * 512:(cg + 1) * 512], in0=PS[base + b][:, :],
                        in1=xt[:, b, cg * 512:(cg + 1) * 512], op=ALU.add),
                        reads=[("ps", base + b), xkey], writes=[xkey])

        if cfg.get("stop", 99) >= 3:
         with contextlib.ExitStack() as st_:
            alloc_ws(st_, 3)
            xt = st_.enter_context(nc.sbuf_tensor(_u("xt"), [128, 4, D], F32))
            xn = st_.enter_context(nc.sbuf_tensor(_u("xn"), [128, 4, D], BF16))
            junk = st_.enter_context(nc.sbuf_tensor(_u("junk"), [128, D], F32))
            stt = st_.enter_context(nc.sbuf_tensor(_u("stt"), [128, 8], F32))
            hT = st_.enter_context(nc.sbuf_tensor(_u("hT"), [128, KD, 512], BF16))
            oT = st_.enter_context(nc.sbuf_tensor(_u("oT"), [128, H, 512], BF16))
            act = st_.enter_context(nc.sbuf_tensor(_u("act"), [128, NFC, 512], BF16))
            sg = [st_.enter_context(nc.sbuf_tensor(_u("sg%d" % i), [128, 512], BF16)) for i in range(GC)]
            cnt = 0
            for t in range(NT0):
                ld(xt[:], x_all[t * 512:(t + 1) * 512, :].rearrange("(b p) d -> p b d", p=128), "xt")
                ld(oT[:], oT0[t, :, :, :].rearrange("h d t -> d h t"), "oT", reads=[("dram", "oT0")])
                wo_tile(oT, "oT", "nao", xt, "xt")
                norm_tile(xt, "xt", xn, junk, stt, gffn[:, 0, :], hT, "hT", [6, 7])

                def epi(cg, b, bank):
                    S.op("vector", lambda e: e.tensor_tensor(
                        out=xt[:, b, cg * 512:(cg + 1) * 512], in0=PS[bank][:, :],
                        in1=xt[:, b, cg * 512:(cg + 1) * 512], op=ALU.add),
                        reads=[("ps", bank), "xt"], writes=["xt"])
                cnt = swiglu_tile(hT, "hT", act, sg, ("fg", "fu", "fd"), epi, cnt)
                S.dma("gpsimd", lambda e, t=t: e.dma_start(
                    out=x1[t * 512:(t + 1) * 512, :].rearrange("(b p) d -> p b d", p=128), in_=xt[:]),
                    "st_xt", reads=["xt"], writes=[("dram", "x1")])
                conv_some((n_moe_conv + 2 * NT0 - 1) // (2 * NT0))
            conv_some(100000)
            oi = st_.enter_context(nc.sbuf_tensor(_u("oi"), [128, NOWN * 4], I32))
            ld(oi[:], own_idx[:, :], "oi")
            for j in range(NOWN * 4):
                S.dma("gpsimd", lambda e, j=j: e.indirect_dma_start(
                    out=xt[:, j % 4, :], out_offset=None, in_=x1[0:NT0 * 512, :],
                    in_offset=bass.IndirectOffsetOnAxis(ap=oi[:, j:j + 1], axis=0)),
                    "xt", reads=[("dram", "x1"), "oi"], writes=["xt"])
                if j % 4 == 3:
                    tt_ = NT0 + j // 4
                    S.dma("gpsimd", lambda e, tt_=tt_: e.dma_start(
                        out=x1[tt_ * 512:(tt_ + 1) * 512, :].rearrange("(b p) d -> p b d", p=128), in_=xt[:]),
                        "st_xt", reads=["xt"], writes=[("dram", "x1")])
            S.barrier()
            S.run()

        if cfg.get("stop", 99) >= 4:
         with contextlib.ExitStack() as st_:
            alloc_ws(st_, 3)
            xb = [st_.enter_context(nc.sbuf_tensor(_u("xb%d" % i), [128, D], F32)) for i in range(2)]
            xn = st_.enter_context(nc.sbuf_tensor(_u("xn"), [128, 4, D], BF16))
            junk = st_.enter_context(nc.sbuf_tensor(_u("junk"), [128, D], F32))
            stt = st_.enter_context(nc.sbuf_tensor(_u("stt"), [128, 8], F32))
            hT = st_.enter_context(nc.sbuf_tensor(_u("hT"), [128, KD, 512], BF16))
            cT = st_.enter_context(nc.sbuf_tensor(_u("cT"), [128, 8, 512], BF16))
            cn = st_.enter_context(nc.sbuf_tensor(_u("cn"), [128, 1024], BF16))
            s2 = st_.enter_context(nc.sbuf_tensor(_u("s2"), [128, 8], F32))
            latf = st_.enter_context(nc.sbuf_tensor(_u("latf"), [128, 1088], F32))
            kvf = [st_.enter_context(nc.sbuf_tensor(_u("kvf%d" % i), [128, 512], F32)) for i in range(2)]
            rpt = st_.enter_context(nc.sbuf_tensor(_u("rpt"), [128, 4, 64], F32))
            kr = st_.enter_context(nc.sbuf_tensor(_u("kr"), [128, 64], F32))
            kr2 = st_.enter_context(nc.sbuf_tensor(_u("kr2"), [128, 64], F32))
            krb = st_.enter_context(nc.sbuf_tensor(_u("krb"), [128, 64], BF16))
            krT = st_.enter_context(nc.sbuf_tensor(_u("krT"), [64, 512], BF16))
            qf = st_.enter_context(nc.sbuf_tensor(_u("qf"), [128, H, 192], F32))
            qsq = st_.enter_context(nc.sbuf_tensor(_u("qsq"), [128, H, 192], F32))
            qs = st_.enter_context(nc.sbuf_tensor(_u("qs"), [128, 4 * H], F32))
            qb = st_.enter_context(nc.sbuf_tensor(_u("qb"), [128, H, 192], BF16))
            qr1 = st_.enter_context(nc.sbuf_tensor(_u("qr1"), [128, H, 64], F32))
            qr2 = st_.enter_context(nc.sbuf_tensor(_u("qr2"), [128, H, 64], F32))
            tq = st_.enter_context(nc.sbuf_tensor(_u("tq"), [128, H, 32], F32))
            kf = st_.enter_context(nc.sbuf_tensor(_u("kf"), [128, H, 128], F32))
            kb = st_.enter_context(nc.sbuf_tensor(_u("kb"), [128, H, 128], BF16))
            vb = st_.enter_context(nc.sbuf_tensor(_u("vb"), [128, H, 128], BF16))
            qnT = [st_.enter_context(nc.sbuf_tensor(_u("qnT%d" % i), [128, H, 128], BF16)) for i in range(2)]
            qrT = st_.enter_context(nc.sbuf_tensor(_u("qrT"), [64, H, 128], BF16))
            knT = [st_.enter_context(nc.sbuf_tensor(_u("knT%d" % i), [128, H, 128], BF16)) for i in range(2)]
            P4S = cfg.get("p4s", 9)
            NQG = (H * 192) // 384
            NKG = (H * 256) // 512
            for t in range(NT1):
                def xsrc4(b, t=t):
                    r0 = t * 512 + b * 128
                    ld(xb[b % 2][:, :], x1[r0:r0 + 128, :], ("xb", b % 2), reads=[("dram", "x1")])
                    return xb[b % 2][:, :], ("xb", b % 2)
                ld(rpt[:], rope_in[t * 512:(t + 1) * 512, :].rearrange("(b p) c -> p b c", p=128), "rpt")
                norm_tile(xsrc4, None, xn, junk, stt, gmix[:, 1, :], hT, "hT", [6, 7])
                wk0, w0v = wload("dqkv", 0, KD, 0, 512)
                wk1, w1v = wload("dqkv", 0, KD, 512, 512)
                wk2, w2v = wload("dqkv", 0, KD, 1024, 64)
                for b in range(4):
                    for (bank, wk, wv, ncol) in ((0, wk0, w0v, 512), (1, wk1, w1v, 512), (2, wk2, w2v, 64)):
                        for k in range(KD):
                            S.op("tensor", lambda e, k=k, b=b, bank=bank, wv=wv, ncol=ncol: e.matmul(
                                PS[bank][:, 0:ncol], lhsT=hT[:, k, b * 128:(b + 1) * 128], rhs=wv[:, k, :],
                                start=(k == 0), stop=(k == KD - 1)),
                                reads=[wk, ("hT", k)], writes=[("ps", bank)])
                    for li, ncol in ((0, 512), (1, 512), (2, 64)):
                        S.op("scalar", lambda e, li=li, ncol=ncol: e.copy(out=latf[:, li * 512:li * 512 + ncol], in_=PS[li][:, 0:ncol]),
                             reads=[("ps", li)], writes=[("latf", li)])
                    for li, ncol in ((0, 512), (1, 512), (2, 64)):
                        S.op("vector", lambda e, li=li, ncol=ncol: e.tensor_tensor(
                            out=junk[:, 0:ncol], in0=latf[:, li * 512:li * 512 + ncol], in1=latf[:, li * 512:li * 512 + ncol],
                            op=ALU.mult), reads=[("latf", li)], writes=["junk"])
                        S.op("vector", lambda e, li=li, ncol=ncol: e.tensor_reduce(
                            out=s2[:, li:li + 1], in_=junk[:, 0:ncol], axis=AX.X, op=ALU.add),
                            reads=["junk"], writes=[("s2", li)])
                    S.op("scalar", lambda e: e.activation(out=s2[:, 4:6], in_=s2[:, 0:2], func=AF.Sqrt, scale=1.0 / 512, bias=EPS),
                         reads=[("s2", 0), ("s2", 1)], writes=["s2b"])
                    S.op("scalar", lambda e: e.activation(out=s2[:, 6:7], in_=s2[:, 2:3], func=AF.Sqrt, scale=1.0 / 64, bias=EPS),
                         reads=[("s2", 2), "s2b"], writes=["s2b"])
                    S.op("vector", lambda e: e.reciprocal(out=s2[:, 4:7], in_=s2[:, 4:7]), reads=["s2b"], writes=["s2b"])
                    for li in range(2):
                        S.op("scalar", lambda e, li=li: e.activation(
                            out=cn[:, li * 512:(li + 1) * 512], in_=latf[:, li * 512:(li + 1) * 512], func=AF.Copy,
                            scale=s2[:, 4 + li:5 + li]),
                            reads=[("latf", li), "s2b"], writes=[("cn", li)])
                    S.op("vector", lambda e: e.scalar_tensor_tensor(
                        out=kr[:], in0=latf[:, 1024:1088], scalar=s2[:, 6:7], in1=gkr[:], op0=ALU.mult, op1=ALU.mult),
                        reads=[("latf", 2), "s2b", "gkr"], writes=["kr"])
                    S.op("vector", lambda e, b=b: e.tensor_tensor(out=kr2[:, 0:32], in0=kr[:, 0:32], in1=rpt[:, b, 0:32], op=ALU.mult),
                         reads=["kr", "rpt"], writes=["kr2a"])
                    S.op("vector", lambda e, b=b: e.tensor_tensor(out=kr2[:, 32:64], in0=kr[:, 32:64], in1=rpt[:, b, 32:64], op=ALU.mult),
                         reads=["kr", "rpt"], writes=["kr2b"])
                    S.op("vector", lambda e: e.tensor_tensor(out=krb[:, 0:32], in0=kr2[:, 0:32], in1=kr2[:, 32:64], op=ALU.subtract),
                         reads=["kr2a", "kr2b"], writes=["krb0"])
                    S.op("vector", lambda e, b=b: e.tensor_tensor(out=kr2[:, 0:32], in0=kr[:, 0:32], in1=rpt[:, b, 32:64], op=ALU.mult),
                         reads=["kr", "rpt", "krb0"], writes=["kr2a"])
                    S.op("vector", lambda e, b=b: e.tensor_tensor(out=kr2[:, 32:64], in0=kr[:, 32:64], in1=rpt[:, b, 0:32], op=ALU.mult),
                         reads=["kr", "rpt", "krb0"], writes=["kr2b"])
                    S.op("vector", lambda e: e.tensor_tensor(out=krb[:, 32:64], in0=kr2[:, 0:32], in1=kr2[:, 32:64], op=ALU.add),
                         reads=["kr2a", "kr2b"], writes=["krb1"])
                    S.op("tensor", lambda e, b=b: e.transpose(out=psb(3)[0:64, b * 128:(b + 1) * 128], in_=krb[:, :],
                                                             identity=ident_b[:]),
                         reads=["krb0", "krb1", "ident_b"], writes=[("ps", 3)])
                    for li in range(2):
                        for k in range(4):
                            S.op("tensor", lambda e, li=li, k=k: e.transpose(
                                out=psb(4 + li)[:, k * 128:(k + 1) * 128], in_=cn[:, li * 512 + k * 128:li * 512 + (k + 1) * 128],
                                identity=ident_b[:]),
                                reads=[("cn", li), "ident_b"], writes=[("ps", 4 + li)])
                        gofs = 4 + 4 * li
                        S.op("vector", lambda e, li=li, gofs=gofs, b=b: e.tensor_tensor(
                            out=cT[:, 4 * li:4 * li + 4, b * 128:(b + 1) * 128],
                            in0=psb(4 + li)[:, 0:512].rearrange("p (k t) -> p k t", k=4),
                            in1=gsm[:, gofs:gofs + 4].unsqueeze(2).to_broadcast([128, 4, 128]), op=ALU.mult),
                            reads=[("ps", 4 + li), "gsm"], writes=[("cT", li, b)])
                S.op("scalar", lambda e: e.copy(out=krT[:, :], in_=psb(3)[0:64, 0:512]), reads=[("ps", 3)], writes=["krT"])
                S.dma("gpsimd", lambda e, t=t: e.dma_start(out=k1r[t, :, :], in_=krT[:, :]), "st_krT",
                      reads=["krT"], writes=[("dram", "k1r")])
                for b in range(4 if P4S >= 2 else 0):
                    for g in range(NQG):
                        wk, wv = wload("uq", 0, 4, g * 384, 384)
                        bank = g % 3
                        for k in range(4):
                            S.op("tensor", lambda e, k=k, b=b, bank=bank, wv=wv: e.matmul(
                                PS[bank][:, 0:384], lhsT=cT[:, k, b * 128:(b + 1) * 128], rhs=wv[:, k, :],
                                start=(k == 0), stop=(k == 3)),
                                reads=[wk, ("cT", 0, b)], writes=[("ps", bank)])
                        S.op("scalar", lambda e, g=g, bank=bank: e.copy(
                            out=qf[:, 2 * g:2 * g + 2, :].rearrange("p a c -> p (a c)"), in_=PS[bank][:, 0:384]),
                            reads=[("ps", bank)], writes=[("qf", g)])
                    if P4S < 2.2:
                        continue
                    qfk = [("qf", g) for g in range(NQG)]
                    S.op("vector", lambda e: e.tensor_tensor(out=qsq[:], in0=qf[:], in1=qf[:], op=ALU.mult),
                         reads=qfk, writes=["qsq"])
                    S.op("vector", lambda e: e.tensor_reduce(out=qs[:, 0:H], in_=qsq[:, :, 0:128], axis=AX.X, op=ALU.add),
                         reads=["qsq"], writes=["qs0"])
                    S.op("vector", lambda e: e.tensor_reduce(out=qs[:, H:2 * H], in_=qsq[:, :, 128:192], axis=AX.X, op=ALU.add),
                         reads=["qsq"], writes=["qs1"])
                    S.op("scalar", lambda e: e.activation(out=qs[:, 2 * H:3 * H], in_=qs[:, 0:H], func=AF.Sqrt, scale=1.0 / 128, bias=EPS),
                         reads=["qs0"], writes=["qs2"])
                    S.op("scalar", lambda e: e.activation(out=qs[:, 3 * H:4 * H], in_=qs[:, H:2 * H], func=AF.Sqrt, scale=1.0 / 64, bias=EPS),
                         reads=["qs1", "qs2"], writes=["qs2"])
                    S.op("vector", lambda e: e.reciprocal(out=qs[:, 2 * H:4 * H], in_=qs[:, 2 * H:4 * H]), reads=["qs2"], writes=["qs2"])
                    if P4S < 2.4:
                        continue
                    S.op("vector", lambda e: e.tensor_tensor(
                        out=qb[:, :, 0:128], in0=qf[:, :, 0:128],
                        in1=qs[:, 2 * H:3 * H].unsqueeze(2).to_broadcast([128, H, 128]), op=ALU.mult),
                        reads=qfk + ["qs2"], writes=["qbn"])
                    S.op("vector", lambda e: e.tensor_tensor(
                        out=qr1[:], in0=qf[:, :, 128:192],
                        in1=qs[:, 3 * H:4 * H].unsqueeze(2).to_broadcast([128, H, 64]), op=ALU.mult),
                        reads=qfk + ["qs2"], writes=["qr1"])
                    S.op("vector", lambda e: e.tensor_tensor(
                        out=qr1[:], in0=qr1[:], in1=gqr[:, :].unsqueeze(1).to_broadcast([128, H, 64]), op=ALU.mult),
                        reads=["qr1", "gqr"], writes=["qr1"])
                    cosb = rpt[:, b, 0:32].unsqueeze(1).to_broadcast([128, H, 32])
                    sinb = rpt[:, b, 32:64].unsqueeze(1).to_broadcast([128, H, 32])
                    S.op("vector", lambda e, cosb=cosb: e.tensor_tensor(out=qr2[:, :, 0:32], in0=qr1[:, :, 0:32], in1=cosb, op=ALU.mult),
                         reads=["qr1", "rpt"], writes=["qr2a"])
                    S.op("vector", lambda e, sinb=sinb: e.tensor_tensor(out=tq[:], in0=qr1[:, :, 32:64], in1=sinb, op=ALU.mult),
                         reads=["qr1", "rpt"], writes=["tq"])
                    S.op("vector", lambda e: e.tensor_tensor(out=qb[:, :, 128:160], in0=qr2[:, :, 0:32], in1=tq[:], op=ALU.subtract),
                         reads=["qr2a", "tq"], writes=["qbr0"])
                    S.op("vector", lambda e, sinb=sinb: e.tensor_tensor(out=qr2[:, :, 32:64], in0=qr1[:, :, 0:32], in1=sinb, op=ALU.mult),
                         reads=["qr1", "rpt"], writes=["qr2b"])
                    S.op("vector", lambda e, cosb=cosb: e.tensor_tensor(out=tq[:], in0=qr1[:, :, 32:64], in1=cosb, op=ALU.mult),
                         reads=["qr1", "rpt", "qbr0"], writes=["tq"])
                    S.op("vector", lambda e: e.tensor_tensor(out=qb[:, :, 160:192], in0=qr2[:, :, 32:64], in1=tq[:], op=ALU.add),
                         reads=["qr2b", "tq"], writes=["qbr1"])
                    if P4S < 2.6:
                        continue
                    for h4 in range(H // 4):
                        bank = 4 + h4 % 2
                        for hh in range(4):
                            h = h4 * 4 + hh
                            S.op("tensor", lambda e, h=h, hh=hh, bank=bank: e.transpose(
                                out=psb(bank)[:, hh * 128:(hh + 1) * 128], in_=qb[:, h, 0:128], identity=ident_b[:]),
                                reads=["qbn", "ident_b"], writes=[("ps", bank)])
                            if P4S >= 2.8: S.op("tensor", lambda e, h=h, hh=hh, bank=bank: e.transpose(
                                out=psb(bank)[0:64, 512 + hh * 128:512 + (hh + 1) * 128], in_=qb[:, h, 128:192],
                                identity=ident_b[:]),
                                reads=["qbr0", "qbr1", "ident_b"], writes=[("ps", bank)])
                        S.op("scalar", lambda e, h4=h4, bank=bank, b=b: e.activation(
                            out=qnT[b % 2][:, h4 * 4:h4 * 4 + 4, :],
                            in_=psb(bank)[:, 0:512].rearrange("p (a t) -> p a t", a=4), func=AF.Copy, scale=gsm[:, 2:3]),
                            reads=[("ps", bank), "gsm"], writes=[("qnT", b % 2)])
                        if P4S >= 2.9: S.op("scalar", lambda e, h4=h4, bank=bank, b=b: e.copy(
                            out=qrT[:, h4 * 4:h4 * 4 + 4, :],
                            in_=psb(bank)[0:64, 512:1024].rearrange("p (a t) -> p a t", a=4)),
                            reads=[("ps", bank)], writes=["qrT"])
                    if P4S >= 4:
                        S.dma("gpsimd", lambda e, t=t, b=b: e.dma_start(
                            out=q1n[t, :, :, b * 128:(b + 1) * 128].rearrange("h d t -> d h t"), in_=qnT[b % 2][:]),
                            ("st_qnT", b % 2), reads=[("qnT", b % 2)], writes=[("dram", "q1n")])
                        S.dma("gpsimd", lambda e, t=t, b=b: e.dma_start(
                            out=q1r[t, :, :, b * 128:(b + 1) * 128].rearrange("h d t -> d h t"), in_=qrT[:]),
                            "st_qrT", reads=["qrT"], writes=[("dram", "q1r")])
                    if P4S < 3:
                        continue
                    for g in range(NKG):
                        wk, wv = wload("ukv", 0, 4, g * 512, 512)
                        bank = g % 3
                        for k in range(4):
                            S.op("tensor", lambda e, k=k, b=b, bank=bank, wv=wv: e.matmul(
                                PS[bank][:, :], lhsT=cT[:, 4 + k, b * 128:(b + 1) * 128], rhs=wv[:, k, :],
                                start=(k == 0), stop=(k == 3)),
                                reads=[wk, ("cT", 1, b)], writes=[("ps", bank)])
                        pv = lambda bank=bank: PS[bank][:, :].rearrange("p (a c) -> p a c", a=2)
                        g2 = g % 2
                        S.op("scalar", lambda e, g2=g2, bank=bank: e.copy(out=kvf[g2][:], in_=PS[bank][:, :]),
                             reads=[("ps", bank)], writes=[("kvf", g2)])
                        kvv = kvf[g2][:].rearrange("p (a c) -> p a c", a=2)
                        S.op("vector", lambda e, g=g, kvv=kvv: e.tensor_copy(out=kf[:, 2 * g:2 * g + 2, :], in_=kvv[:, :, 0:128]),
                             reads=[("kvf", g2)], writes=[("kf", g)])
                        S.op("vector", lambda e, g=g, kvv=kvv: e.tensor_copy(out=vb[:, 2 * g:2 * g + 2, :], in_=kvv[:, :, 128:256]),
                             reads=[("kvf", g2)], writes=[("vb", g)])
                    kfk = [("kf", g) for g in range(NKG)]
                    S.op("vector", lambda e: e.tensor_tensor(out=qsq[:, :, 0:128], in0=kf[:], in1=kf[:], op=ALU.mult),
                         reads=kfk, writes=["qsq"])
                    S.op("vector", lambda e: e.tensor_reduce(out=qs[:, 0:H], in_=qsq[:, :, 0:128], axis=AX.X, op=ALU.add),
                         reads=["qsq"], writes=["qs0"])
                    S.op("scalar", lambda e: e.activation(out=qs[:, 2 * H:3 * H], in_=qs[:, 0:H], func=AF.Sqrt, scale=1.0 / 128, bias=EPS),
                         reads=["qs0"], writes=["qs2"])
                    S.op("vector", lambda e: e.reciprocal(out=qs[:, 2 * H:3 * H], in_=qs[:, 2 * H:3 * H]), reads=["qs2"], writes=["qs2"])
                    S.op("vector", lambda e: e.tensor_tensor(
                        out=kb[:], in0=kf[:], in1=qs[:, 2 * H:3 * H].unsqueeze(2).to_broadcast([128, H, 128]), op=ALU.mult),
                        reads=kfk + ["qs2"], writes=["kb"])
                    for h4 in range(H // 4):
                        bank = 6 + h4 % 2
                        for hh in range(4):
                            h = h4 * 4 + hh
                            S.op("tensor", lambda e, h=h, hh=hh, bank=bank: e.transpose(
                                out=psb(bank)[:, hh * 128:(hh + 1) * 128], in_=kb[:, h, :], identity=ident_b[:]),
                                reads=["kb", "ident_b"], writes=[("ps", bank)])
                        S.op("scalar", lambda e, h4=h4, bank=bank, b=b: e.activation(
                            out=knT[b % 2][:, h4 * 4:h4 * 4 + 4, :],
                            in_=psb(bank)[:, 0:512].rearrange("p (a t) -> p a t", a=4), func=AF.Copy, scale=gsm[:, 3:4]),
                            reads=[("ps", bank), "gsm"], writes=[("knT", b % 2)])
                    if P4S >= 4:
                        S.dma("gpsimd", lambda e, t=t, b=b: e.dma_start(
                            out=k1n[t, :, :, b * 128:(b + 1) * 128].rearrange("h d t -> d h t"), in_=knT[b % 2][:]),
                            ("st_knT", b % 2), reads=[("knT", b % 2)], writes=[("dram", "k1n")])
                    r0 = t * 512 + b * 128
                    S.dma("gpsimd", lambda e, r0=r0: e.dma_start(
                        out=v1[r0:r0 + 128, :].rearrange("p (h c) -> p h c", h=H), in_=vb[:]),
                        "st_vb", reads=[("vb", g) for g in range(NKG)], writes=[("dram", "v1")])
            S.barrier()
            S.run()

        if cfg.get("stop", 99) >= 5:
         with contextlib.ExitStack() as st_:
            TK = max(RS * 64, RP * 64)
            krS = st_.enter_context(nc.sbuf_tensor(_u("krS"), [64, TK], BF16))
            knS = [st_.enter_context(nc.sbuf_tensor(_u("knS%d" % i), [128, TK], BF16)) for i in range(2)]
            vS = [st_.enter_context(nc.sbuf_tensor(_u("vS%d" % i), [128, TK // 128, 128], BF16)) for i in range(2)]
            qnS = [st_.enter_context(nc.sbuf_tensor(_u("qnS%d" % i), [128, 512], BF16)) for i in range(2)]
            qrS = [st_.enter_context(nc.sbuf_tensor(_u("qrS%d" % i), [64, 512], BF16)) for i in range(2)]
            pT = [st_.enter_context(nc.sbuf_tensor(_u("pT%d" % i), [128, 512], BF16)) for i in range(3)]
            rcp = [st_.enter_context(nc.sbuf_tensor(_u("rcp%d" % i), [128, 512], F32)) for i in range(2)]
            oo = [st_.enter_context(nc.sbuf_tensor(_u("oo%d" % i), [128, 512], BF16)) for i in range(2)]
            scale1 = 192 ** -0.5
            seqs1 = []
            for s in range(NS):
                seqs1.append(([s * TS + i for i in range(TS)], [s * TS + i for i in range(TS)],
                              [s * TS + i for i in range(TS)]))
            seqs1.append(([NT0 + i for i in range(NOWN)], [NS * TS + i for i in range(TP)],
                          [NS * TS + i for i in range(NOWN)]))
            hc = 0
            qc = 0
            cc = 0
            for (qtiles, kvtiles, otiles) in seqs1:
                T = len(kvtiles) * 512
                NC = T // 128
                for i, kt in enumerate(kvtiles):
                    S.dma("sync", lambda e, i=i, kt=kt: e.dma_start(out=krS[:, i * 512:(i + 1) * 512], in_=k1r[kt, :, :]),
                          "krS", reads=[("dram", "k1r")], writes=["krS"])
                for h in range(H):
                    hs = hc % 2
                    hc += 1
                    for i, kt in enumerate(kvtiles):
                        S.dma("sync", lambda e, i=i, kt=kt, h=h, hs=hs: e.dma_start(
                            out=knS[hs][:, i * 512:(i + 1) * 512], in_=k1n[kt, h, :, :]),
                            ("knS", hs), reads=[("dram", "k1n")], writes=[("knS", hs)])
                    tok0 = kvtiles[0] * 512
                    S.dma("sync", lambda e, h=h, hs=hs, tok0=tok0, T=T, NC=NC: e.dma_start(
                        out=vS[hs][:, 0:NC, :],
                        in_=v1[tok0:tok0 + T, h * 128:(h + 1) * 128].rearrange("(b p) c -> p b c", p=128)),
                        ("vS", hs), reads=[("dram", "v1")], writes=[("vS", hs)])
                    for qi, qt in enumerate(qtiles):
                        q2 = qc % 2
                        qc += 1
                        S.dma("sync", lambda e, qt=qt, h=h, q2=q2: e.dma_start(out=qnS[q2][:], in_=q1n[qt, h, :, :]),
                              ("qnS", q2), reads=[("dram", "q1n")], writes=[("qnS", q2)])
                        S.dma("sync", lambda e, qt=qt, h=h, q2=q2: e.dma_start(out=qrS[q2][:], in_=q1r[qt, h, :, :]),
                              ("qrS", q2), reads=[("dram", "q1r")], writes=[("qrS", q2)])
                        obk = 4 + q2
                        dbk = 6 + q2

                        def pv_step(c, p3, obk=obk, dbk=dbk, hs=hs, NC=NC):
                            S.op("tensor", lambda e: e.matmul(PS[obk][:, :], lhsT=vS[hs][:, c, :], rhs=pT[p3][:],
                                                              start=(c == 0), stop=(c == NC - 1)),
                                 reads=[("vS", hs), ("pT", p3)], writes=[("ps", obk)])
                            S.op("tensor", lambda e: e.matmul(PS[dbk][:, :], lhsT=ones_b[:], rhs=pT[p3][:],
                                                              start=(c == 0), stop=(c == NC - 1)),
                                 reads=["ones_b", ("pT", p3)], writes=[("ps", dbk)])
                        prev = None
                        for c in range(NC):
                            sbk = cc % 4
                            p3 = cc % 3
                            cc += 1
                            S.op("tensor", lambda e, c=c, sbk=sbk, hs=hs, q2=q2: e.matmul(
                                PS[sbk][:, :], lhsT=knS[hs][:, c * 128:(c + 1) * 128], rhs=qnS[q2][:], start=True, stop=False),
                                reads=[("knS", hs), ("qnS", q2)], writes=[("ps", sbk)])
                            S.op("tensor", lambda e, c=c, sbk=sbk, q2=q2: e.matmul(
                                PS[sbk][:, :], lhsT=krS[:, c * 128:(c + 1) * 128], rhs=qrS[q2][:], start=False, stop=True),
                                reads=["krS", ("qrS", q2)], writes=[("ps", sbk)])
                            S.op("scalar", lambda e, sbk=sbk, p3=p3: e.activation(out=pT[p3][:], in_=PS[sbk][:, :],
                                                                                  func=AF.Exp, scale=scale1),
                                 reads=[("ps", sbk)], writes=[("pT", p3)])
                            if prev is not None:
                                pv_step(*prev)
                            prev = (c, p3)
                        pv_step(*prev)
                        S.op("vector", lambda e, q2=q2, dbk=dbk: e.reciprocal(out=rcp[q2][:], in_=PS[dbk][:, :]),
                             reads=[("ps", dbk)], writes=[("rcp", q2)])
                        S.op("vector", lambda e, q2=q2, obk=obk: e.tensor_tensor(out=oo[q2][:], in0=PS[obk][:, :],
                                                                                 in1=rcp[q2][:], op=ALU.mult),
                             reads=[("ps", obk), ("rcp", q2)], writes=[("oo", q2)])
                        ot = otiles[qi]
                        S.dma("gpsimd", lambda e, ot=ot, h=h, q2=q2: e.dma_start(out=oT1[ot, h, :, :], in_=oo[q2][:]),
                              ("st_oo", q2), reads=[("oo", q2)], writes=[("dram", "oT1")])
            S.barrier()
            S.run()

        NB = 4 * NL1
        NST = 2 * NL1 + E - 1
        x2 = dscr("x2", [NL1 * 512, D], F32)
        hn = dscr("hn", [NL1 * 512, D], BF16)
        hs = dscr("hs", [NST * 512, D], BF16)
        ysl = dscr("ysl", [NST * 512, D], F32)
        MK = gst.enter_context(nc.sbuf_tensor(_u("MK"), [128, 2, NB, E], F32))
        GG = gst.enter_context(nc.sbuf_tensor(_u("GG"), [128, 2, NB], F32))
        SLI = gst.enter_context(nc.sbuf_tensor(_u("SLI"), [128, 2, NB], I32))
        IXG = gst.enter_context(nc.sbuf_tensor(_u("IXG"), [128, NST, NCH], I32))
        IXD = gst.enter_context(nc.sbuf_tensor(_u("IXD"), [128, NST, NCG * NDC], I32))
        if cfg.get("stop", 99) >= 6:
         with contextlib.ExitStack() as st_:
            alloc_ws(st_, 3)
            xt = st_.enter_context(nc.sbuf_tensor(_u("xt"), [128, 4, D], F32))
            xn = st_.enter_context(nc.sbuf_tensor(_u("xn"), [128, 4, D], BF16))
            junk = st_.enter_context(nc.sbuf_tensor(_u("junk"), [128, D], F32))
            stt = st_.enter_context(nc.sbuf_tensor(_u("stt"), [128, 8], F32))
            oT = st_.enter_context(nc.sbuf_tensor(_u("oT"), [128, H, 512], BF16))
            wr = st_.enter_context(nc.sbuf_tensor(_u("wr"), [128, KD, E], F32))
            h32 = st_.enter_context(nc.sbuf_tensor(_u("h32"), [128, KD, 128], F32))
            lg = st_.enter_context(nc.sbuf_tensor(_u("lg"), [128, 4, E], F32))
            m1 = st_.enter_context(nc.sbuf_tensor(_u("m1"), [128, 8], F32))
            lg2 = st_.enter_context(nc.sbuf_tensor(_u("lg2"), [128, E], F32))
            ld(wr[:], w_router[:, :, :], "wr")
            for ti, t in enumerate(l1_tiles):
                ld(xt[:], x1[t * 512:(t + 1) * 512, :].rearrange("(b p) d -> p b d", p=128), "xt", reads=[("dram", "x1")])
                ld(oT[:], oT1[ti, :, :, :].rearrange("h d t -> d h t"), "oT", reads=[("dram", "oT1")])
                wo_tile(oT, "oT", "mo", xt, "xt")
                S.dma("gpsimd", lambda e, ti=ti: e.dma_start(
                    out=x2[ti * 512:(ti + 1) * 512, :].rearrange("(b p) d -> p b d", p=128), in_=xt[:]),
                    "st_xt", reads=["xt"], writes=[("dram", "x2")])
                norm_tile(xt, "xt", xn, junk, stt, gffn[:, 1, :], None, "hT", [6, 7])
                S.dma("gpsimd", lambda e, ti=ti: e.dma_start(
                    out=hn[ti * 512:(ti + 1) * 512, :].rearrange("(b p) d -> p b d", p=128), in_=xn[:]),
                    "st_xn", reads=[("xn", b) for b in range(4)], writes=[("dram", "hn")])
                for b in range(4):
                    gb = ti * 4 + b
                    S.op("scalar", lambda e, b=b: e.activation(out=junk[:], in_=xt[:, b, :], func=AF.Copy,
                                                               scale=stt[:, 4 + b:5 + b]),
                         reads=["xt", "st2"], writes=["junk"])
                    for k4 in range(KD // 4):
                        bank = k4 % 2
                        for kk in range(4):
                            k = k4 * 4 + kk
                            S.op("tensor", lambda e, k=k, kk=kk, bank=bank: e.transpose(
                                out=PS[bank][:, kk * 128:(kk + 1) * 128], in_=junk[:, k * 128:(k + 1) * 128],
                                identity=ident_f[:]),
                                reads=["junk", "ident_f"], writes=[("ps", bank)])
                        S.op("vector", lambda e, k4=k4, bank=bank: e.tensor_tensor(
                            out=h32[:, k4 * 4:k4 * 4 + 4, :], in0=PS[bank][:, :].rearrange("p (a t) -> p a t", a=4),
                            in1=gffn[:, 1, k4 * 4:k4 * 4 + 4].unsqueeze(2).to_broadcast([128, 4, 128]), op=ALU.mult),
                            reads=[("ps", bank), "gffn"], writes=[("h32", k4)])
                    for k in range(KD):
                        S.op("tensor", lambda e, k=k: e.matmul(PS[2][:, 0:E], lhsT=h32[:, k, :], rhs=wr[:, k, :],
                                                               start=(k == 0), stop=(k == KD - 1)),
                             reads=[("h32", k // 4), "wr"], writes=[("ps", 2)])
                    S.op("vector", lambda e, b=b: e.tensor_scalar(out=lg[:, b, :], in0=PS[2][:, 0:E], scalar1=1.0, scalar2=None,
                                                                  op0=ALU.mult), reads=[("ps", 2)], writes=[("lg", b)])
                    S.op("vector", lambda e, b=b: e.tensor_reduce(out=m1[:, 0:1], in_=lg[:, b, :], axis=AX.X, op=ALU.max),
                         reads=[("lg", b)], writes=["m1a"])
                    S.op("vector", lambda e, b=b, gb=gb: e.tensor_scalar(out=MK[:, 0, gb, :], in0=lg[:, b, :], scalar1=m1[:, 0:1],
                                                                         scalar2=None, op0=ALU.is_equal),
                         reads=[("lg", b), "m1a"], writes=["MK"])
                    S.op("vector", lambda e, b=b, gb=gb: e.scalar_tensor_tensor(
                        out=lg2[:], in0=MK[:, 0, gb, :], scalar=-1e30, in1=lg[:, b, :], op0=ALU.mult, op1=ALU.add),
                        reads=["MK", ("lg", b)], writes=["lg2"])
                    S.op("vector", lambda e: e.tensor_reduce(out=m1[:, 1:2], in_=lg2[:], axis=AX.X, op=ALU.max),
                         reads=["lg2"], writes=["m1b"])
                    S.op("vector", lambda e, gb=gb: e.tensor_scalar(out=MK[:, 1, gb, :], in0=lg2[:], scalar1=m1[:, 1:2], scalar2=None,
                                                                    op0=ALU.is_equal), reads=["lg2", "m1b"], writes=["MK"])
                    S.op("vector", lambda e: e.tensor_tensor(out=m1[:, 2:3], in0=m1[:, 1:2], in1=m1[:, 0:1], op=ALU.subtract),
                         reads=["m1a", "m1b"], writes=["m1c"])
                    S.op("scalar", lambda e: e.activation(out=m1[:, 3:4], in_=m1[:, 2:3], func=AF.Exp),
                         reads=["m1c"], writes=["m1d"])
                    S.op("vector", lambda e: e.tensor_scalar(out=m1[:, 4:5], in0=m1[:, 3:4], scalar1=1.0, scalar2=None,
                                                             op0=ALU.add), reads=["m1d"], writes=["m1e"])
                    S.op("vector", lambda e, gb=gb: e.reciprocal(out=GG[:, 0, gb:gb + 1], in_=m1[:, 4:5]),
                         reads=["m1e"], writes=["GG"])
                    S.op("vector", lambda e, gb=gb: e.tensor_tensor(out=GG[:, 1, gb:gb + 1], in0=m1[:, 3:4], in1=GG[:, 0, gb:gb + 1],
                                                                    op=ALU.mult), reads=["m1d", "GG"], writes=["GG"])
            S.barrier()
            S.run()

        if cfg.get("stop", 99) >= 6:
         with contextlib.ExitStack() as st_:
            tri_f = st_.enter_context(nc.sbuf_tensor(_u("tri_f"), [128, 128], F32))
            tri_b = st_.enter_context(nc.sbuf_tensor(_u("tri_b"), [128, 128], BF16))
            cst = st_.enter_context(nc.sbuf_tensor(_u("cst"), [128, 64], F32))
            Mb = st_.enter_context(nc.sbuf_tensor(_u("Mb"), [128, NB, E], BF16))
            wi = st_.enter_context(nc.sbuf_tensor(_u("wi"), [128, NB, E], F32))
            tot = st_.enter_context(nc.sbuf_tensor(_u("tot"), [128, NB, E], F32))
            off = st_.enter_context(nc.sbuf_tensor(_u("off"), [128, NB, E], F32))
            sm = st_.enter_context(nc.sbuf_tensor(_u("sm"), [128, 8, E], F32))
            cmpA = st_.enter_context(nc.sbuf_tensor(_u("cmpA"), [128, E, NL1], F32))
            cmpB = st_.enter_context(nc.sbuf_tensor(_u("cmpB"), [128, NST, E], F32))
            prod = st_.enter_context(nc.sbuf_tensor(_u("prod"), [128, NB, E], F32))
            slf = st_.enter_context(nc.sbuf_tensor(_u("slf"), [128, 2, NB], F32))
            ej = st_.enter_context(nc.sbuf_tensor(_u("ej"), [128, NST], F32))
            ixf = st_.enter_context(nc.sbuf_tensor(_u("ixf"), [128, NST, max(NCH, NCG * NDC)], F32))
            ld(tri_f[:], tri_in[:, :], "tri_f")
            ld(cst[:], cst_in[:, :], "cst")
            S.op("vector", lambda e: e.tensor_copy(out=tri_b[:], in_=tri_f[:]), reads=["tri_f"], writes=["tri_b"])
            S.op("vector", lambda e: e.tensor_tensor(out=prod[:], in0=MK[:, 0, :, :], in1=MK[:, 1, :, :], op=ALU.add),
                 reads=["MK"], writes=["prod"])
            S.op("vector", lambda e: e.tensor_copy(out=Mb[:], in_=prod[:]), reads=["prod"], writes=["Mb"])
            mbf = Mb[:].rearrange("p a b -> p (a b)")
            S.op("tensor", lambda e: e.matmul(PS[0][:, 0:NB * E], lhsT=tri_b[:], rhs=mbf, start=True, stop=True),
                 reads=["tri_b", "Mb"], writes=[("ps", 0)])
            S.op("tensor", lambda e: e.matmul(PS[1][:, 0:NB * E], lhsT=ones_b[:], rhs=mbf, start=True, stop=True),
                 reads=["ones_b", "Mb"], writes=[("ps", 1)])
            S.op("scalar", lambda e: e.copy(out=wi[:].rearrange("p a b -> p (a b)"), in_=PS[0][:, 0:NB * E]),
                 reads=[("ps", 0)], writes=["wi"])
            S.op("scalar", lambda e: e.copy(out=tot[:].rearrange("p a b -> p (a b)"), in_=PS[1][:, 0:NB * E]),
                 reads=[("ps", 1)], writes=["tot"])
            S.op("vector", lambda e: e.memset(off[:, 0, :], 0.0), writes=["off"])
            for blk in range(1, NB):
                S.op("vector", lambda e, blk=blk: e.tensor_tensor(out=off[:, blk, :], in0=off[:, blk - 1, :], in1=tot[:, blk - 1, :],
                                                                  op=ALU.add), reads=["off", "tot"], writes=["off"])
            S.op("vector", lambda e: e.tensor_tensor(out=sm[:, 0, :], in0=off[:, NB - 1, :], in1=tot[:, NB - 1, :], op=ALU.add),
                 reads=["off", "tot"], writes=["sm0"])
            S.op("vector", lambda e: e.tensor_tensor(
                out=cmpA[:], in0=sm[:, 0, :].unsqueeze(2).to_broadcast([128, E, NL1]),
                in1=cst[:, 1:1 + NL1].unsqueeze(1).to_broadcast([128, E, NL1]), op=ALU.is_gt),
                reads=["sm0", "cst"], writes=["cmpA"])
            S.op("vector", lambda e: e.tensor_reduce(out=sm[:, 1, :], in_=cmpA[:], axis=AX.X, op=ALU.add),
                 reads=["cmpA"], writes=["sm1"])
            S.op("vector", lambda e: e.memset(sm[:, 2, 0:1], 0.0), reads=["sm1"], writes=["sm2"])
            for e_ in range(1, E):
                S.op("vector", lambda e, e_=e_: e.tensor_tensor(out=sm[:, 2, e_:e_ + 1], in0=sm[:, 2, e_ - 1:e_],
                                                                in1=sm[:, 1, e_ - 1:e_], op=ALU.add),
                     reads=["sm1", "sm2"], writes=["sm2"])
            S.op("vector", lambda e: e.tensor_tensor(out=sm[:, 3, :], in0=sm[:, 2, :], in1=sm[:, 1, :], op=ALU.add),
                 reads=["sm1", "sm2"], writes=["sm3"])
            S.op("vector", lambda e: e.tensor_scalar(out=sm[:, 4, :], in0=sm[:, 2, :], scalar1=512.0, scalar2=None, op0=ALU.mult),
                 reads=["sm2"], writes=["sm4"])
            S.op("vector", lambda e: e.tensor_tensor(out=wi[:], in0=wi[:], in1=off[:], op=ALU.add),
                 reads=["wi", "off"], writes=["wi"])
            S.op("vector", lambda e: e.tensor_tensor(out=wi[:], in0=wi[:], in1=sm[:, 4, :].unsqueeze(1).to_broadcast([128, NB, E]),
                                                     op=ALU.add), reads=["wi", "sm4"], writes=["wi"])
            for kk in range(2):
                S.op("vector", lambda e, kk=kk: e.tensor_tensor(out=prod[:], in0=MK[:, kk, :, :], in1=wi[:], op=ALU.mult),
                     reads=["MK", "wi"], writes=["prod"])
                S.op("vector", lambda e, kk=kk: e.tensor_reduce(out=slf[:, kk, :], in_=prod[:], axis=AX.X, op=ALU.add),
                     reads=["prod"], writes=["slf"])
            S.op("vector", lambda e: e.tensor_copy(out=SLI[:], in_=slf[:]), reads=["slf"], writes=["SLI"])
            S.op("vector", lambda e: e.tensor_tensor(
                out=cmpB[:], in0=sm[:, 3, :].unsqueeze(1).to_broadcast([128, NST, E]),
                in1=cst[:, 16:16 + NST].unsqueeze(2).to_broadcast([128, NST, E]), op=ALU.is_le),
                reads=["sm3", "cst"], writes=["cmpB"])
            S.op("vector", lambda e: e.tensor_reduce(out=ej[:], in_=cmpB[:], axis=AX.X, op=ALU.add),
                 reads=["cmpB"], writes=["ej"])
            S.op("vector", lambda e: e.tensor_scalar(out=ej[:], in0=ej[:], scalar1=float(E - 1), scalar2=None, op0=ALU.min),
                 reads=["ej"], writes=["ej"])
            for (IX, nper) in ((IXG, NCH), (IXD, NCG * NDC)):
                S.op("vector", lambda e, nper=nper: e.tensor_scalar(
                    out=ixf[:, :, 0:nper], in0=ej[:].unsqueeze(2).to_broadcast([128, NST, nper]),
                    scalar1=float(nper * 128), scalar2=cst[:, 0:1], op0=ALU.mult, op1=ALU.add),
                    reads=["ej", "cst"], writes=["ixf"])
                S.op("vector", lambda e, nper=nper: e.tensor_tensor(
                    out=ixf[:, :, 0:nper], in0=ixf[:, :, 0:nper],
                    in1=cst[:, 48:48 + nper].unsqueeze(1).to_broadcast([128, NST, nper]), op=ALU.add),
                    reads=["ixf", "cst"], writes=["ixf"])
                S.op("vector", lambda e, IX=IX, nper=nper: e.tensor_copy(out=IX[:], in_=ixf[:, :, 0:nper]),
                     reads=["ixf"], writes=["IX%d" % nper])
            S.barrier()
            S.run()

        if cfg.get("stop", 99) >= 6:
         with contextlib.ExitStack() as st_:
            zt = st_.enter_context(nc.sbuf_tensor(_u("zt"), [128, 4, D], BF16))
            hb = [st_.enter_context(nc.sbuf_tensor(_u("hb%d" % i), [128, D], BF16)) for i in range(2)]
            S.op("vector", lambda e: e.memset(zt[:], 0.0), writes=["zt"])
            for j in range(NST):
                S.dma("sync", lambda e, j=j: e.dma_start(
                    out=hs[j * 512:(j + 1) * 512, :].rearrange("(b p) d -> p b d", p=128), in_=zt[:]),
                    "st_zt", reads=["zt"], writes=[("dram", "hs0")])
            for blk in range(NB):
                i2 = blk % 2
                ld(hb[i2][:], hn[blk * 128:(blk + 1) * 128, :], ("hb", i2), reads=[("dram", "hn")])
                for kk in range(2):
                    S.dma("gpsimd", lambda e, i2=i2, kk=kk, blk=blk: e.indirect_dma_start(
                        out=hs[:, :], out_offset=bass.IndirectOffsetOnAxis(ap=SLI[:, kk, blk:blk + 1], axis=0),
                        in_=hb[i2][:, :], in_offset=None),
                        ("sc", i2), reads=[("hb", i2), "SLI", ("dram", "hs0")], writes=[("dram", "hs")])
            S.barrier()
            S.run()

        if cfg.get("stop", 99) >= 6:
         with contextlib.ExitStack() as st_:
            alloc_ws(st_, 3)
            xn = st_.enter_context(nc.sbuf_tensor(_u("xn"), [128, 4, D], BF16))
            hT = st_.enter_context(nc.sbuf_tensor(_u("hT"), [128, KD, 512], BF16))
            act = st_.enter_context(nc.sbuf_tensor(_u("act"), [128, NFC, 512], BF16))
            sg = [st_.enter_context(nc.sbuf_tensor(_u("sg%d" % i), [128, 512], BF16)) for i in range(GC)]
            yo = st_.enter_context(nc.sbuf_tensor(_u("yo"), [128, 4, D], F32))
            cnt = 0
            for j in range(NST):
                S.dma("sync", lambda e, j=j: e.dma_start(
                    out=xn[:], in_=hs[j * 512:(j + 1) * 512, :].rearrange("(b p) d -> p b d", p=128)),
                    "xn", reads=[("dram", "hs"), ("dram", "hs0")], writes=[("xn", b) for b in range(4)])
                transpose_tile(xn, gffn[:, 1, :], hT, "hT", [6, 7])

                def loader(kind, *a, j=j):
                    s_ = ws_i[0] % len(WS)
                    ws_i[0] += 1
                    if kind in ("g", "u"):
                        src_t, nmw = (war_g, "war_g") if kind == "g" else (war_u, "war_u")
                        ix = IXG[:, j, a[0]:a[0] + 1]
                        n = KD * 512
                        ikey = "IX%d" % NCH
                    else:
                        i_ = a[0] * NDC + a[1]
                        src_t, nmw, ix = war_d, "war_d", IXD[:, j, i_:i_ + 1]
                        n = DCm * 512
                        ikey = "IX%d" % (NCG * NDC)
                    view = WS[s_][:, 0:n].rearrange("p (k c) -> p k c", c=512)
                    S.dma("gpsimd", lambda e, s_=s_, n=n, src_t=src_t, ix=ix: e.indirect_dma_start(
                        out=WS[s_][:, 0:n], out_offset=None, in_=src_t[:, :],
                        in_offset=bass.IndirectOffsetOnAxis(ap=ix, axis=0)),
                        ("w", s_), reads=[("dram", nmw), ikey], writes=[("w", s_)])
                    return ("w", s_), view

                def epi(cg, b, bank):
                    if (cg + b) % 2 == 0:
                        S.op("scalar", lambda e: e.copy(out=yo[:, b, cg * 512:(cg + 1) * 512], in_=PS[bank][:, :]),
                             reads=[("ps", bank)], writes=[("yo", b, cg)])
                    else:
                        S.op("vector", lambda e: e.tensor_scalar(out=yo[:, b, cg * 512:(cg + 1) * 512], in0=PS[bank][:, :],
                                                                 scalar1=1.0, scalar2=None, op0=ALU.mult),
                             reads=[("ps", bank)], writes=[("yo", b, cg)])
                cnt = swiglu_tile(hT, "hT", act, sg, loader, epi, cnt)
                S.dma("sync", lambda e, j=j: e.dma_start(
                    out=ysl[j * 512:(j + 1) * 512, :].rearrange("(b p) d -> p b d", p=128), in_=yo[:]),
                    "st_yo", reads=[("yo", b, cg) for b in range(4) for cg in range(NCG)], writes=[("dram", "ysl")])
            S.barrier()
            S.run()

        if cfg.get("stop", 99) >= 6:
         with contextlib.ExitStack() as st_:
            xb2 = [st_.enter_context(nc.sbuf_tensor(_u("xb2%d" % i), [128, D], F32)) for i in range(2)]
            ya = [st_.enter_context(nc.sbuf_tensor(_u("ya%d" % i), [128, D], F32)) for i in range(2)]
            yb = [st_.enter_context(nc.sbuf_tensor(_u("yb%d" % i), [128, D], F32)) for i in range(2)]
            for blk in range(NB):
                i2 = blk % 2
                ld(xb2[i2][:], x2[blk * 128:(blk + 1) * 128, :], ("xb2", i2), reads=[("dram", "x2")])
                for kk, yy, nm in ((0, ya, "ya"), (1, yb, "yb")):
                    S.dma("gpsimd", lambda e, i2=i2, kk=kk, blk=blk, yy=yy: e.indirect_dma_start(
                        out=yy[i2][:, :], out_offset=None, in_=ysl[:, :],
                        in_offset=bass.IndirectOffsetOnAxis(ap=SLI[:, kk, blk:blk + 1], axis=0)),
                        (nm, i2), reads=[("dram", "ysl"), "SLI"], writes=[(nm, i2)])
                S.op("vector", lambda e, i2=i2, blk=blk: e.scalar_tensor_tensor(
                    out=xb2[i2][:], in0=ya[i2][:], scalar=GG[:, 0, blk:blk + 1], in1=xb2[i2][:], op0=ALU.mult, op1=ALU.add),
                    reads=[("ya", i2), "GG", ("xb2", i2)], writes=[("xb2", i2)])
                S.op("vector", lambda e, i2=i2, blk=blk: e.scalar_tensor_tensor(
                    out=xb2[i2][:], in0=yb[i2][:], scalar=GG[:, 1, blk:blk + 1], in1=xb2[i2][:], op0=ALU.mult, op1=ALU.add),
                    reads=[("yb", i2), "GG", ("xb2", i2)], writes=[("xb2", i2)])
                ti, b = blk // 4, blk % 4
                if ti < NS * TS:
                    dst = ys[ti * 512 + b * 128:ti * 512 + (b + 1) * 128, :]
                else:
                    r0 = (ti - NS * TS) * 512 + b * 128
                    dst = yp[r0:r0 + 128, :]
                S.dma("sync", lambda e, dst=dst, i2=i2: e.dma_start(out=dst, in_=xb2[i2][:]),
                      ("st_xb2", i2), reads=[("xb2", i2)], writes=[("dram", "y")])
            S.barrier()
            S.run()
    return nc


def _rope_table(pos_tokens, grid_w=64, theta=10000.0):
    t = np.asarray(pos_tokens)
    row = (t // grid_w).astype(np.float32)
    col = (t % grid_w).astype(np.float32)
    n_pairs = 16
    inv = (np.float32(theta) ** (-np.arange(n_pairs, dtype=np.float32) / np.float32(n_pairs))).astype(np.float32)
    ang = np.concatenate([row[:, None] * inv, col[:, None] * inv], axis=-1).astype(np.float32)
    return np.concatenate([np.cos(ang), np.sin(ang)], axis=-1).astype(np.float32)


def _cst_table():
    c = np.zeros((128, 64), np.float32)
    c[:, 0] = np.arange(128)
    c[:, 1:16] = 512.0 * np.arange(15)[None, :]
    c[:, 16:48] = np.arange(32)[None, :]
    c[:, 48:64] = 128.0 * np.arange(16)[None, :]
    return c


def make_core_inputs(cfg, inp, core):
    D, H, FF, E = cfg["D"], cfg["H"], cfg["FF"], cfg["E"]
    RS, RP, NS = cfg["RS"], cfg["RP"], cfg["NS"]
    KD = D // 128
    f = lambda a: np.ascontiguousarray(np.asarray(a, dtype=np.float32))
    xs = inp["x_sample"]
    xp = inp["x_prompt"]
    x_all = np.concatenate([f(xs[core * NS + s]) for s in range(NS)] + [f(xp[0])], axis=0)
    n_own_rows = RP // 8 * 64
    NOWN = RP // 64
    base = NS * RS * 64
    own = base + core * n_own_rows + np.arange(n_own_rows)
    own_idx = np.ascontiguousarray(own.reshape(NOWN * 4, 128).T.astype(np.int32))
    pos = np.concatenate([np.arange(RS * 64)] * NS + [np.arange(RP * 64)] + [core * n_own_rows + np.arange(n_own_rows)])
    rope = _rope_table(pos)
    pk = lambda g: np.ascontiguousarray(f(g).reshape(-1, 128).T)
    g_mix = np.ascontiguousarray(np.stack([pk(inp["mix_norm"][l]) for l in range(2)], axis=1))
    g_ffn = np.ascontiguousarray(np.stack([pk(inp["ffn_norm"][l]) for l in range(2)], axis=1))
    rpb = f(inp["na_rpb"][0])
    kc = np.arange(64)[:, None]
    qc = np.arange(64)[None, :]
    dc = np.clip(kc - qc + 15, 0, 30)
    ws = np.clip(qc - 8, 0, 48)
    valid = ((kc >= ws) & (kc < ws + 16)).astype(np.float32)
    rpbg = np.zeros((128, H, 14, 64), np.float32)
    for a in range(2):
        for m in range(14):
            rpbg[a * 64:(a + 1) * 64, :, m, :] = np.transpose(rpb[:, m + a][:, dc], (1, 0, 2))
    namask = np.concatenate([valid, valid], axis=0)
    d = {
        "x_all": x_all, "own_idx": own_idx, "rope": rope, "ident": np.eye(128, dtype=np.float32),
        "g_mix": g_mix, "g_ffn": g_ffn,
        "g_naq": pk(inp["na_q_gain"][0]), "g_nak": pk(inp["na_k_gain"][0]),
        "rpbg": rpbg, "namask": namask,
        "g_ql": pk(inp["mla_q_lora_gain"][0]), "g_kvl": pk(inp["mla_kv_lora_gain"][0]),
        "g_qn": pk(inp["mla_qn_gain"][0]), "g_kn": pk(inp["mla_kn_gain"][0]),
        "g_qr": np.ascontiguousarray(np.broadcast_to(f(inp["mla_qr_gain"][0])[None, :], (128, 64))),
        "g_kr": np.ascontiguousarray(np.broadcast_to(f(inp["mla_kr_gain"][0])[None, :], (128, 64))),
        "w_router": np.ascontiguousarray(f(inp["moe_w_router"][0]).reshape(KD, 128, E).transpose(1, 0, 2)),
        "tri": np.triu(np.ones((128, 128), np.float32), 1), "cst": _cst_table(),
        "na_w_qkv": f(inp["na_w_qkv"][0]), "na_w_o": f(inp["na_w_o"][0]),
        "ffn_w_gate": f(inp["ffn_w_gate"][0]), "ffn_w_up": f(inp["ffn_w_up"][0]), "ffn_w_down": f(inp["ffn_w_down"][0]),
        "mla_w_dqkv": f(inp["mla_w_dqkv"][0]), "mla_w_uq": f(inp["mla_w_uq"][0]), "mla_w_ukv": f(inp["mla_w_ukv"][0]),
        "mla_w_o": f(inp["mla_w_o"][0]),
    }
    for e_ in range(E):
        d["moe_w_gate%d" % e_] = f(inp["moe_w_gate"][0, e_])
        d["moe_w_up%d" % e_] = f(inp["moe_w_up"][0, e_])
        d["moe_w_down%d" % e_] = f(inp["moe_w_down"][0, e_])
    return d


def run_cfg(cfg, inp, trace=False):
    nc = build_program(cfg)
    shared = None
    in_maps = []
    for c in range(N_CORES):
        d = make_core_inputs(cfg, inp, c)
        if shared is None:
            shared = d
        else:
            for k in d:
                if k not in ("x_all", "own_idx", "rope"):
                    d[k] = shared[k]
        in_maps.append(d)
    res = run_bass_kernel_spmd(nc, in_maps, core_ids=list(range(N_CORES)), trace=trace)
    RS, RP, NS, D = cfg["RS"], cfg["RP"], cfg["NS"], cfg["D"]
    ys = np.stack([np.asarray(res.results[c]["ys"]).reshape(NS, RS * 64, D) for c in range(N_CORES)], axis=0)
    y_sample = ys.reshape(N_CORES * NS, RS * 64, D).astype(np.float32)
    y_prompt = np.concatenate([np.asarray(res.results[c]["yp"]) for c in range(N_CORES)], axis=0)[None].astype(np.float32)
    return (y_prompt, y_sample), res


def kernel(**inputs):
    out, _ = run_cfg(CFG_FULL, inputs)
    return out
```

```python
import contextlib
import numpy as np
import concourse.bass as bass
import concourse.mybir as mybir
from concourse.bass_utils import run_bass_kernel_spmd

F32 = mybir.dt.float32
BF16 = mybir.dt.bfloat16
I32 = mybir.dt.int32
AF = mybir.ActivationFunctionType
ALU = mybir.AluOpType
AX = mybir.AxisListType
ENGS = ("sync", "gpsimd", "scalar", "vector", "tensor")
EPS = 1e-6
N_CORES = 8

CFG_FULL = dict(D=2048, H=16, FF=5632, E=8, RS=32, RP=128, NS=2)


class Sched:
    def __init__(self, nc, stack, n_dma_sems=96):
        self.nc = nc
        self.ops = {e: [] for e in ENGS}
        self.esem = {e: stack.enter_context(nc.semaphore("es_" + e)) for e in ENGS}
        self.ecnt = {e: 0 for e in ENGS}
        self.seen = {e: {} for e in ENGS}
        self.free_sems = {"sync": [[stack.enter_context(nc.semaphore("dh%d" % i)), 0] for i in range(28)],
                          "gpsimd": [[stack.enter_context(nc.semaphore("dg%d" % i)), 0] for i in range(n_dma_sems - 28)]}
        self.dsem = {}
        self.lastw = {}
        self.reads = {}

    def _dma_sem(self, key, eng):
        key = (eng, key)
        if key not in self.dsem:
            self.dsem[key] = self.free_sems[eng].pop()
        return self.dsem[key]

    @staticmethod
    def _isdram(k):
        return isinstance(k, tuple) and len(k) > 0 and k[0] == "dram"

    def _deps(self, reads, writes):
        deps = {}

        def add(d):
            for sid, ev in d.items():
                if sid not in deps or deps[sid][1] < ev[1]:
                    deps[sid] = ev
        for r in reads:
            add(self.lastw.get(r, {}))
        for w in writes:
            if self._isdram(w):
                continue
            add(self.lastw.get(w, {}))
            add(self.reads.get(w, {}))
        return deps

    def _commit(self, reads, writes, ev):
        sid = id(ev[0])
        for r in reads:
            self.reads.setdefault(r, {})[sid] = ev
        for w in writes:
            if self._isdram(w):
                self.lastw.setdefault(w, {})[sid] = ev
            else:
                self.lastw[w] = {sid: ev}
                self.reads[w] = {}

    def _waits(self, eng, deps, skip_own):
        waits = []
        seen = self.seen[eng]
        own = id(self.esem[eng])
        for sid, (sem, val) in deps.items():
            if skip_own and sid == own:
                continue
            if seen.get(sid, 0) >= val:
                continue
            seen[sid] = val
            waits.append((sem, val))
        return waits

    def op(self, eng, fn, reads=(), writes=()):
        waits = self._waits(eng, self._deps(reads, writes), eng == "tensor")
        self.ecnt[eng] += 1
        ev = (self.esem[eng], self.ecnt[eng])

        def emit(e, fn=fn, waits=waits, sem=ev[0]):
            for s, v in waits:
                e.wait_ge(s, v)
            fn(e).then_inc(sem, 1)
        self.ops[eng].append(emit)
        self._commit(reads, writes, ev)

    def dma(self, eng, fn, semkey, reads=(), writes=()):
        waits = self._waits(eng, self._deps(reads, writes), False)
        ds = self._dma_sem(semkey, eng)
        ds[1] += 16
        ev = (ds[0], ds[1])

        def emit(e, fn=fn, waits=waits, sem=ev[0]):
            for s, v in waits:
                e.wait_ge(s, v)
            fn(e).then_inc(sem, 16)
        self.ops[eng].append(emit)
        self._commit(reads, writes, ev)

    def barrier(self):
        finals = [(ds[0], ds[1]) for ds in self.dsem.values() if ds[1] > 0]
        finals += [(self.esem[e], self.ecnt[e]) for e in ENGS if self.ecnt[e] > 0]
        for eng in ENGS:
            seen = self.seen[eng]
            w = []
            for s, v in finals:
                if seen.get(id(s), 0) >= v:
                    continue
                if id(s) == id(self.esem[eng]) and eng == "tensor":
                    continue
                seen[id(s)] = v
                w.append((s, v))

            def emit(e, w=w):
                for s, v in w:
                    e.wait_ge(s, v)
            self.ops[eng].append(emit)
        for (eng, _k), pair in self.dsem.items():
            self.free_sems[eng].append(pair)
        self.dsem = {}

    def run(self):
        ops = self.ops
        with self.nc.Block() as block:
            @block.sync
            def _(e):
                for f in ops["sync"]:
                    f(e)

            @block.gpsimd
            def _(e):
                for f in ops["gpsimd"]:
                    f(e)

            @block.scalar
            def _(e):
                for f in ops["scalar"]:
                    f(e)

            @block.vector
            def _(e):
                for f in ops["vector"]:
                    f(e)

            @block.tensor
            def _(e):
                for f in ops["tensor"]:
                    f(e)
        self.ops = {e: [] for e in ENGS}


_UCNT = [0]


def _u(name):
    _UCNT[0] += 1
    return "%s_%d" % (name, _UCNT[0])


def _chunk_div(n, cap):
    for c in range(min(n, cap), 0, -1):
        if n % c == 0:
            return c
    return 1


def build_program(cfg):
    D, H, FF, E = cfg["D"], cfg["H"], cfg["FF"], cfg["E"]
    RS, RP, NS = cfg["RS"], cfg["RP"], cfg["NS"]
    KD = D // 128
    NFC = FF // 128
    HD = H * 128
    KH = HD // 128
    assert D % 512 == 0 and FF % 512 == 0 and HD == D
    TS = RS // 8
    TP = RP // 8
    NOWN = RP // 64
    NT0 = NS * TS + TP
    NT1 = NT0 + NOWN
    NL1 = NS * TS + NOWN
    seqs0 = [(s * TS, TS, RS) for s in range(NS)] + [(NS * TS, TP, RP)]
    l1_tiles = list(range(NS * TS)) + [NT0 + i for i in range(NOWN)]
    QL = KVL = 512
    LAT = QL + KVL + 64

    nc = bass.Bass("TRN2", target_bir_lowering=False)

    def din(name, shape, dt=F32):
        return nc.dram_tensor(name, list(shape), dt, kind="ExternalInput").ap()

    def dscr(name, shape, dt):
        kind = "ExternalOutput" if name in cfg.get("dbg", ()) else "Internal"
        return nc.dram_tensor(name, list(shape), dt, kind=kind).ap()

    x_all = din("x_all", [NT0 * 512, D])
    own_idx = din("own_idx", [128, NOWN * 4], I32)
    rope_in = din("rope", [NT1 * 512, 64])
    ident_in = din("ident", [128, 128])
    g_mix = din("g_mix", [128, 2, KD])
    g_ffn = din("g_ffn", [128, 2, KD])
    g_naq = din("g_naq", [128, 1])
    g_nak = din("g_nak", [128, 1])
    rpbg = din("rpbg", [128, H, 14, 64])
    namask = din("namask", [128, 64])
    g_ql = din("g_ql", [128, 4])
    g_kvl = din("g_kvl", [128, 4])
    g_qn = din("g_qn", [128, 1])
    g_kn = din("g_kn", [128, 1])
    g_qr = din("g_qr", [128, 64])
    g_kr = din("g_kr", [128, 64])
    w_router = din("w_router", [128, KD, E])
    tri_in = din("tri", [128, 128])
    cst_in = din("cst", [128, 64])
    Wf = {
        "qkv": din("na_w_qkv", [D, 3 * HD]), "nao": din("na_w_o", [HD, D]),
        "fg": din("ffn_w_gate", [D, FF]), "fu": din("ffn_w_up", [D, FF]), "fd": din("ffn_w_down", [FF, D]),
        "dqkv": din("mla_w_dqkv", [D, LAT]), "uq": din("mla_w_uq", [QL, H * 192]),
        "ukv": din("mla_w_ukv", [KVL, H * 256]), "mo": din("mla_w_o", [HD, D]),
    }
    for e_ in range(E):
        Wf["mg%d" % e_] = din("moe_w_gate%d" % e_, [D, FF])
        Wf["mu%d" % e_] = din("moe_w_up%d" % e_, [D, FF])
        Wf["md%d" % e_] = din("moe_w_down%d" % e_, [FF, D])
    Wb = {k: dscr("wb_" + k, v.shape, BF16) for k, v in Wf.items() if not k.startswith("m") or k == "mo"}

    ys = nc.dram_tensor("ys", [NS * TS * 512, D], F32, kind="ExternalOutput").ap()
    yp = nc.dram_tensor("yp", [NOWN * 512, D], F32, kind="ExternalOutput").ap()

    qk0 = dscr("qk0", [NT0, 2 * H, 128, 512], BF16)
    v0 = dscr("v0", [NT0 * 512, HD], BF16)
    oT0 = dscr("oT0", [NT0, H, 128, 512], BF16)
    x1 = dscr("x1", [NT1 * 512, D], F32)
    q1n = dscr("q1n", [NT1, H, 128, 512], BF16)
    q1r = dscr("q1r", [NT1, H, 64, 512], BF16)
    k1n = dscr("k1n", [NT1, H, 128, 512], BF16)
    k1r = dscr("k1r", [NT1, 64, 512], BF16)
    v1 = dscr("v1", [NT1 * 512, HD], BF16)
    oT1 = dscr("oT1", [NL1, H, 128, 512], BF16)

    with contextlib.ExitStack() as gst:
        S = Sched(nc, gst)
        PS = [gst.enter_context(nc.psum_tensor("ps%d" % i, [128, 512], F32)) for i in range(8)]
        ident_f = gst.enter_context(nc.sbuf_tensor(_u("ident_f"), [128, 128], F32))
        ident_b = gst.enter_context(nc.sbuf_tensor(_u("ident_b"), [128, 128], BF16))
        ones_b = gst.enter_context(nc.sbuf_tensor(_u("ones_b"), [128, 128], BF16))
        gmix = gst.enter_context(nc.sbuf_tensor(_u("gmix"), [128, 2, KD], F32))
        gffn = gst.enter_context(nc.sbuf_tensor(_u("gffn"), [128, 2, KD], F32))
        gsm = gst.enter_context(nc.sbuf_tensor(_u("gsm"), [128, 12], F32))
        gqr = gst.enter_context(nc.sbuf_tensor(_u("gqr"), [128, 64], F32))
        gkr = gst.enter_context(nc.sbuf_tensor(_u("gkr"), [128, 64], F32))
        WS = []
        ws_i = [0]

        def alloc_ws(stk, n):
            WS[:] = [stk.enter_context(nc.sbuf_tensor(_u("ws%d" % i), [128, 8192], BF16)) for i in range(n)]

        def ld(dst, src, key, eng="sync", reads=()):
            S.dma(eng, lambda e: e.dma_start(out=dst, in_=src), key, reads=reads, writes=[key])

        ld(ident_f[:], ident_in[:, :], "ident_f")
        ld(gmix[:], g_mix[:, :, :], "gmix")
        ld(gffn[:], g_ffn[:, :, :], "gffn")
        ld(gsm[:, 0:1], g_naq[:, :], "gsm")
        ld(gsm[:, 1:2], g_nak[:, :], "gsm")
        ld(gsm[:, 2:3], g_qn[:, :], "gsm")
        ld(gsm[:, 3:4], g_kn[:, :], "gsm")
        ld(gsm[:, 4:8], g_ql[:, :], "gsm")
        ld(gsm[:, 8:12], g_kvl[:, :], "gsm")
        ld(gqr[:], g_qr[:, :], "gqr")
        ld(gkr[:], g_kr[:, :], "gkr")
        S.op("vector", lambda e: e.tensor_copy(out=ident_b[:], in_=ident_f[:]), reads=["ident_f"], writes=["ident_b"])
        S.op("vector", lambda e: e.memset(ones_b[:], 1.0), writes=["ones_b"])

        def conv(name):
            src, dst = Wf[name], Wb[name]
            rows, cols = src.shape
            step = max(128, (4 * 1024 * 1024 // cols) // 128 * 128)
            for r0 in range(0, rows, step):
                r1 = min(rows, r0 + step)
                S.dma("gpsimd", lambda e, r0=r0, r1=r1: e.dma_start(out=dst[r0:r1, :], in_=src[r0:r1, :]),
                      ("cv", name), writes=[("dram", "wb_" + name)])

        for nm in ("qkv", "nao", "fg", "fu", "fd", "dqkv", "uq", "ukv", "mo"):
            conv(nm)
        NCH = FF // 512
        NCG = D // 512
        NDC = NFC // _chunk_div(NFC, 16)
        DCm = _chunk_div(NFC, 16)
        war_g = dscr("war_g", [E * NCH * 128, KD * 512], BF16)
        war_u = dscr("war_u", [E * NCH * 128, KD * 512], BF16)
        war_d = dscr("war_d", [E * NCG * NDC * 128, DCm * 512], BF16)
        moe_conv_todo = []

        def _mk_gu(dst_t, src, e_, c, nm):
            r0 = (e_ * NCH + c) * 128
            return lambda e: e.dma_start(
                out=dst_t[r0:r0 + 128, :].rearrange("p (k f) -> p k f", f=512),
                in_=src[:, c * 512:(c + 1) * 512].rearrange("(k p) f -> p k f", p=128))

        def _mk_d(src, e_, cg, dc):
            r0 = ((e_ * NCG + cg) * NDC + dc) * 128
            return lambda e: e.dma_start(
                out=war_d[r0:r0 + 128, :].rearrange("p (f c) -> p f c", c=512),
                in_=src[dc * DCm * 128:(dc + 1) * DCm * 128, cg * 512:(cg + 1) * 512].rearrange("(f p) c -> p f c", p=128))

        for e_ in range(E):
            for c in range(NCH):
                moe_conv_todo.append((_mk_gu(war_g, Wf["mg%d" % e_], e_, c, "g"), "war_g"))
                moe_conv_todo.append((_mk_gu(war_u, Wf["mu%d" % e_], e_, c, "u"), "war_u"))
            for cg in range(NCG):
                for dc in range(NDC):
                    moe_conv_todo.append((_mk_d(Wf["md%d" % e_], e_, cg, dc), "war_d"))
        n_moe_conv = len(moe_conv_todo)
        cv_rr = [0]

        def conv_some(n):
            for _ in range(n):
                if moe_conv_todo:
                    fn, nm = moe_conv_todo.pop(0)
                    cv_rr[0] += 1
                    S.dma("gpsimd", fn, ("cvm", cv_rr[0] % 8), writes=[("dram", nm)])

        def wload(name, k0, nk, c0, ncols):
            s = ws_i[0] % len(WS)
            ws_i[0] += 1
            src = Wb[name][k0 * 128:(k0 + nk) * 128, c0:c0 + ncols].rearrange("(k p) f -> p k f", p=128)
            view = WS[s][:, 0:nk * ncols].rearrange("p (k c) -> p k c", c=ncols)
            S.dma("sync", lambda e: e.dma_start(out=view, in_=src), ("w", s),
                  reads=[("dram", "wb_" + name)], writes=[("w", s)])
            return ("w", s), view

        def psb(i):
            return PS[i][:, :].bitcast(BF16)

        def norm_tile(xt, xkey, xn, junk, st, gain, hT, hkey, tp_banks, hT32=None):
            if callable(xt):
                xsrc = xt
            else:
                xsrc = lambda b: (xt[:, b, :], xkey)
            for b in range(4):
                xa, xk = xsrc(b)
                S.op("vector", lambda e, xa=xa, b=b: e.scalar_tensor_tensor(
                    out=junk[:, :], in0=xa, scalar=1.0, in1=xa, op0=ALU.mult, op1=ALU.mult, accum_out=st[:, b:b + 1]),
                    reads=[xk], writes=["junk", ("st", b)])
                S.op("scalar", lambda e, b=b: e.activation(out=st[:, 4 + b:5 + b], in_=st[:, b:b + 1], func=AF.Sqrt,
                                                           scale=1.0 / D, bias=EPS),
                     reads=[("st", b)], writes=["st2"])
                S.op("vector", lambda e, b=b: e.reciprocal(out=st[:, 4 + b:5 + b], in_=st[:, 4 + b:5 + b]),
                     reads=["st2"], writes=["st2"])
                S.op("scalar", lambda e, b=b, xa=xa: e.activation(out=xn[:, b, :], in_=xa, func=AF.Copy,
                                                                  scale=st[:, 4 + b:5 + b]),
                     reads=[xk, "st2"], writes=[("xn", b)])
            if hT is None:
                return
            transpose_tile(xn, gain, hT, hkey, tp_banks)

        def transpose_tile(xn, gain, hT, hkey, tp_banks):
            for k in range(KD):
                bank = tp_banks[(k // 2) % len(tp_banks)]
                half = k % 2
                tpv = psb(bank)[:, half * 512:(half + 1) * 512]
                for b in range(4):
                    S.op("tensor", lambda e, b=b, k=k, tpv=tpv: e.transpose(
                        out=tpv[:, b * 128:(b + 1) * 128], in_=xn[:, b, k * 128:(k + 1) * 128], identity=ident_b[:]),
                        reads=[("xn", b), "ident_b"], writes=[("ps", bank)])
                if k % 2 == 0:
                    S.op("vector", lambda e, k=k, tpv=tpv: e.tensor_scalar(
                        out=hT[:, k, :], in0=tpv, scalar1=gain[:, k:k + 1], scalar2=None, op0=ALU.mult),
                        reads=[("ps", bank), "gmix", "gffn"], writes=[(hkey, k)])
                else:
                    S.op("scalar", lambda e, k=k, tpv=tpv: e.activation(
                        out=hT[:, k, :], in_=tpv, func=AF.Copy, scale=gain[:, k:k + 1]),
                        reads=[("ps", bank), "gmix", "gffn"], writes=[(hkey, k)])

        if cfg.get("stop", 99) >= 1:
         with contextlib.ExitStack() as st_:
            alloc_ws(st_, 3)
            xt = st_.enter_context(nc.sbuf_tensor(_u("xt"), [128, 4, D], F32))
            xn = st_.enter_context(nc.sbuf_tensor(_u("xn"), [128, 4, D], BF16))
            junk = st_.enter_context(nc.sbuf_tensor(_u("junk"), [128, D], F32))
            stt = st_.enter_context(nc.sbuf_tensor(_u("stt"), [128, 8], F32))
            hT = st_.enter_context(nc.sbuf_tensor(_u("hT"), [128, KD, 512], BF16))
            sq = [st_.enter_context(nc.sbuf_tensor(_u("sq%d" % i), [128, 512], BF16)) for i in range(2)]
            rs_ = [st_.enter_context(nc.sbuf_tensor(_u("rs%d" % i), [128, 512], F32)) for i in range(2)]
            qo = [st_.enter_context(nc.sbuf_tensor(_u("qo%d" % i), [128, 512], BF16)) for i in range(3)]
            vo = [st_.enter_context(nc.sbuf_tensor(_u("vo%d" % i), [128, 512], BF16)) for i in range(3)]
            cnt = 0
            vcnt = 0
            for t in range(NT0):
                ld(xt[:], x_all[t * 512:(t + 1) * 512, :].rearrange("(b p) d -> p b d", p=128), "xt")
                norm_tile(xt, "xt", xn, junk, stt, gmix[:, 0, :], hT, "hT", [6, 7])
                for ch in range(2 * H // 4):
                    wkey, wv = wload("qkv", 0, KD, ch * 512, 512)
                    for j in range(4):
                        hj = ch * 4 + j
                        pb = cnt % 4
                        sb = 4 + cnt % 2
                        i2 = cnt % 2
                        i3 = cnt % 3
                        cnt += 1
                        for k in range(KD):
                            S.op("tensor", lambda e, k=k, j=j, pb=pb, wv=wv: e.matmul(
                                PS[pb][:, :], lhsT=wv[:, k, j * 128:(j + 1) * 128], rhs=hT[:, k, :],
                                start=(k == 0), stop=(k == KD - 1)),
                                reads=[wkey, ("hT", k)], writes=[("ps", pb)])
                        S.op("scalar", lambda e, pb=pb, i2=i2: e.activation(out=sq[i2][:], in_=PS[pb][:, :], func=AF.Square),
                             reads=[("ps", pb)], writes=[("sq", i2)])
                        S.op("tensor", lambda e, sb=sb, i2=i2: e.matmul(PS[sb][:, :], lhsT=ones_b[:], rhs=sq[i2][:],
                                                                         start=True, stop=True),
                             reads=[("sq", i2), "ones_b"], writes=[("ps", sb)])
                        S.op("scalar", lambda e, sb=sb, i2=i2: e.activation(
                            out=rs_[i2][:], in_=PS[sb][:, :], func=AF.Sqrt, scale=1.0 / 128, bias=EPS),
                            reads=[("ps", sb)], writes=[("rs", i2)])
                        S.op("vector", lambda e, i2=i2: e.reciprocal(out=rs_[i2][:], in_=rs_[i2][:]),
                             reads=[("rs", i2)], writes=[("rs", i2)])
                        gcol = 0 if hj < H else 1
                        S.op("vector", lambda e, pb=pb, i2=i2, i3=i3, gcol=gcol: e.scalar_tensor_tensor(
                            out=qo[i3][:], in0=PS[pb][:, :], scalar=gsm[:, gcol:gcol + 1], in1=rs_[i2][:],
                            op0=ALU.mult, op1=ALU.mult),
                            reads=[("ps", pb), ("rs", i2), "gsm"], writes=[("qo", i3)])
                        S.dma("gpsimd", lambda e, i3=i3, t=t, hj=hj: e.dma_start(out=qk0[t, hj, :, :], in_=qo[i3][:]),
                              ("st_qo", i3), reads=[("qo", i3)], writes=[("dram", "qk0")])
                for cg in range(HD // 512):
                    wkey, wv = wload("qkv", 0, KD, 2 * HD + cg * 512, 512)
                    for b in range(4):
                        pb = cnt % 4
                        cnt += 1
                        i3 = vcnt % 3
                        vcnt += 1
                        for k in range(KD):
                            S.op("tensor", lambda e, k=k, b=b, pb=pb, wv=wv: e.matmul(
                                PS[pb][:, :], lhsT=hT[:, k, b * 128:(b + 1) * 128], rhs=wv[:, k, :],
                                start=(k == 0), stop=(k == KD - 1)),
                                reads=[wkey, ("hT", k)], writes=[("ps", pb)])
                        S.op("scalar", lambda e, pb=pb, i3=i3: e.copy(out=vo[i3][:], in_=PS[pb][:, :]),
                             reads=[("ps", pb)], writes=[("vo", i3)])
                        r0 = t * 512 + b * 128
                        S.dma("gpsimd", lambda e, i3=i3, r0=r0, cg=cg: e.dma_start(
                            out=v0[r0:r0 + 128, cg * 512:(cg + 1) * 512], in_=vo[i3][:]),
                            ("st_vo", i3), reads=[("vo", i3)], writes=[("dram", "v0")])
            S.barrier()
            S.run()

        if cfg.get("stop", 99) >= 2:
         with contextlib.ExitStack() as st_:
            TT = st_.enter_context(nc.sbuf_tensor(_u("TT"), [128, H, 14, 64], BF16))
            msk = st_.enter_context(nc.sbuf_tensor(_u("msk"), [128, 64], F32))
            rp = [st_.enter_context(nc.sbuf_tensor(_u("rp%d" % i), [128, 14, 64], F32)) for i in range(2)]
            WRmax = 16
            kw = st_.enter_context(nc.sbuf_tensor(_u("kw"), [128, H, WRmax * 64], BF16))
            vE = st_.enter_context(nc.sbuf_tensor(_u("vE"), [128, WRmax // 2, HD], BF16))
            vO = st_.enter_context(nc.sbuf_tensor(_u("vO"), [128, WRmax // 2 - 1, HD], BF16))
            qT = st_.enter_context(nc.sbuf_tensor(_u("qT"), [128, H, 512], BF16))
            oS = st_.enter_context(nc.sbuf_tensor(_u("oS"), [128, H, 512], BF16))
            Eb = [st_.enter_context(nc.sbuf_tensor(_u("Eb%d" % i), [128, 2, 4, 64], BF16)) for i in range(2)]
            Pb = [st_.enter_context(nc.sbuf_tensor(_u("Pb%d" % i), [128, 2, 4, 64], BF16)) for i in range(2)]
            rc = [st_.enter_context(nc.sbuf_tensor(_u("rc%d" % i), [128, 128], F32)) for i in range(2)]
            ld(msk[:], namask[:, :], "msk")
            for h in range(H):
                i2 = h % 2
                ld(rp[i2][:], rpbg[:, h, :, :], ("rp", i2))
                S.op("scalar", lambda e, i2=i2: e.activation(out=rp[i2][:], in_=rp[i2][:], func=AF.Exp),
                     reads=[("rp", i2)], writes=[("rp", i2)])
                S.op("vector", lambda e, i2=i2, h=h: e.tensor_tensor(
                    out=TT[:, h, :, :], in0=rp[i2][:], in1=msk[:, :].unsqueeze(1).to_broadcast([128, 14, 64]), op=ALU.mult),
                    reads=[("rp", i2), "msk"], writes=["TT"])
            scale = 128 ** -0.5
            gc = 0
            pend = [None]
            for (tile0, ntl, rows) in seqs0:
                WR = min(16, rows)
                for tl in range(ntl):
                    t = tile0 + tl
                    r0 = tl * 8
                    w0 = min(max(r0 - 4, 0), rows - WR)
                    rr = w0
                    while rr < w0 + WR:
                        st_tile = rr // 8
                        re = min(w0 + WR, (st_tile + 1) * 8)
                        n = (re - rr) * 64
                        off = (rr - st_tile * 8) * 64
                        dst = (rr - w0) * 64
                        S.dma("sync", lambda e, st_tile=st_tile, off=off, n=n, dst=dst, tile0=tile0: e.dma_start(
                            out=kw[:, :, dst:dst + n],
                            in_=qk0[tile0 + st_tile, H:2 * H, :, off:off + n].rearrange("h d t -> d h t")),
                            "kw", reads=[("dram", "qk0")], writes=["kw"])
                        rr = re
                    tok0 = tile0 * 512 + w0 * 64
                    S.dma("sync", lambda e, tok0=tok0, WR=WR: e.dma_start(
                        out=vE[:, 0:WR // 2, :], in_=v0[tok0:tok0 + WR * 64, :].rearrange("(b p) e -> p b e", p=128)),
                        "vE", reads=[("dram", "v0")], writes=["vE"])
                    if WR > 8:
                        S.dma("sync", lambda e, tok0=tok0, WR=WR: e.dma_start(
                            out=vO[:, 0:WR // 2 - 1, :],
                            in_=v0[tok0 + 64:tok0 + 64 + (WR - 2) * 64, :].rearrange("(b p) e -> p b e", p=128)),
                            "vO", reads=[("dram", "v0")], writes=["vO"])
                    S.dma("sync", lambda e, t=t: e.dma_start(
                        out=qT[:, :, :], in_=qk0[t, 0:H, :, :].rearrange("h d t -> d h t")),
                        "qT", reads=[("dram", "qk0")], writes=["qT"])
                    for rl in range(8):
                        r = r0 + rl
                        rs = min(max(r - 4, 0), rows - 8)
                        o = r - rs
                        rel = rs - w0
                        m0 = 7 - o
                        for hp in range(H // 2):
                            sbk = gc % 4
                            obk = 4 + gc % 2
                            dbk = 6 + gc % 2
                            i2 = gc % 2
                            gc += 1
                            for hh in range(2):
                                h = hp * 2 + hh
                                for p in range(4):
                                    kt0 = (rel + 2 * p) * 64
                                    S.op("tensor", lambda e, h=h, hh=hh, p=p, kt0=kt0, sbk=sbk, rl=rl: e.matmul(
                                        PS[sbk][:, (hh * 4 + p) * 64:(hh * 4 + p + 1) * 64],
                                        lhsT=kw[:, h, kt0:kt0 + 128], rhs=qT[:, h, rl * 64:(rl + 1) * 64],
                                        start=True, stop=True),
                                        reads=["kw", "qT"], writes=[("ps", sbk)])
                            S.op("scalar", lambda e, sbk=sbk, i2=i2: e.activation(
                                out=Eb[i2][:].rearrange("p a b c -> p (a b c)"), in_=PS[sbk][:, :], func=AF.Exp, scale=scale),
                                reads=[("ps", sbk)], writes=[("Eb", i2)])
                            S.op("vector", lambda e, i2=i2, hp=hp, m0=m0: e.tensor_tensor(
                                out=Pb[i2][:], in0=Eb[i2][:], in1=TT[:, 2 * hp:2 * hp + 2, m0:m0 + 7:2, :], op=ALU.mult),
                                reads=[("Eb", i2), "TT"], writes=[("Pb", i2)])
                            def tail(hp=hp, rl=rl, rel=rel, i2=i2, obk=obk, dbk=dbk):
                                for hh in range(2):
                                    h = hp * 2 + hh
                                    for p in range(4):
                                        if rel % 2 == 0:
                                            vv = vE[:, rel // 2 + p, h * 128:(h + 1) * 128]
                                            vk = "vE"
                                        else:
                                            vv = vO[:, (rel - 1) // 2 + p, h * 128:(h + 1) * 128]
                                            vk = "vO"
                                        S.op("tensor", lambda e, hh=hh, p=p, vv=vv, obk=obk, i2=i2: e.matmul(
                                            PS[obk][:, hh * 64:(hh + 1) * 64], lhsT=vv, rhs=Pb[i2][:, hh, p, :],
                                            start=(p == 0), stop=(p == 3)),
                                            reads=[vk, ("Pb", i2)], writes=[("ps", obk)])
                                for p in range(4):
                                    S.op("tensor", lambda e, p=p, dbk=dbk, i2=i2: e.matmul(
                                        PS[dbk][:, 0:128].rearrange("q (a b) -> q a b", a=2), lhsT=ones_b[:],
                                        rhs=Pb[i2][:, :, p, :], start=(p == 0), stop=(p == 3)),
                                        reads=[("Pb", i2), "ones_b"], writes=[("ps", dbk)])
                                S.op("vector", lambda e, dbk=dbk, i2=i2: e.reciprocal(out=rc[i2][:], in_=PS[dbk][:, 0:128]),
                                     reads=[("ps", dbk)], writes=[("rc", i2)])
                                S.op("vector", lambda e, obk=obk, i2=i2, hp=hp, rl=rl: e.tensor_tensor(
                                    out=oS[:, 2 * hp:2 * hp + 2, rl * 64:(rl + 1) * 64],
                                    in0=PS[obk][:, 0:128].rearrange("q (a b) -> q a b", a=2),
                                    in1=rc[i2][:].rearrange("q (a b) -> q a b", a=2), op=ALU.mult),
                                    reads=[("ps", obk), ("rc", i2)], writes=["oS"])
                            if pend[0] is not None:
                                pend[0]()
                            pend[0] = tail
                    if pend[0] is not None:
                        pend[0]()
                        pend[0] = None
                    S.dma("gpsimd", lambda e, t=t: e.dma_start(
                        out=oT0[t, :, :, :].rearrange("h d t -> d h t"), in_=oS[:, :, :]),
                        "st_oS", reads=["oS"], writes=[("dram", "oT0")])
                    conv_some((n_moe_conv + 2 * NT0 - 1) // (2 * NT0))
            S.barrier()
            S.run()

        GC = 4
        DC = _chunk_div(NFC, 16)

        def swiglu_tile(hT, hkey, act, sg, names, epilogue, cnt0):
            cnt = cnt0
            if callable(names):
                loader = names
            else:
                ng, nu, nd = names

                def loader(kind, *a):
                    if kind == "g":
                        return wload(ng, 0, KD, a[0] * GC * 128, GC * 128)
                    if kind == "u":
                        return wload(nu, 0, KD, a[0] * GC * 128, GC * 128)
                    return wload(nd, a[1] * DC, DC, a[0] * 512, 512)
            for c in range(NFC // GC):
                wkg, wg = loader("g", c)
                gb = []
                for j in range(GC):
                    pb = cnt % 4
                    cnt += 1
                    gb.append(pb)
                    for k in range(KD):
                        S.op("tensor", lambda e, k=k, j=j, pb=pb, wg=wg: e.matmul(
                            PS[pb][:, :], lhsT=wg[:, k, j * 128:(j + 1) * 128], rhs=hT[:, k, :],
                            start=(k == 0), stop=(k == KD - 1)),
                            reads=[wkg, (hkey, k)], writes=[("ps", pb)])
                    S.op("scalar", lambda e, j=j, pb=pb: e.activation(out=sg[j][:], in_=PS[pb][:, :], func=AF.Silu),
                         reads=[("ps", pb)], writes=[("sg", j)])
                wku, wu = loader("u", c)
                for j in range(GC):
                    pb = 4 + cnt % 4
                    cnt += 1
                    fc = c * GC + j
                    for k in range(KD):
                        S.op("tensor", lambda e, k=k, j=j, pb=pb, wu=wu: e.matmul(
                            PS[pb][:, :], lhsT=wu[:, k, j * 128:(j + 1) * 128], rhs=hT[:, k, :],
                            start=(k == 0), stop=(k == KD - 1)),
                            reads=[wku, (hkey, k)], writes=[("ps", pb)])
                    S.op("vector", lambda e, j=j, pb=pb, fc=fc: e.tensor_tensor(
                        out=act[:, fc, :], in0=PS[pb][:, :], in1=sg[j][:], op=ALU.mult),
                        reads=[("ps", pb), ("sg", j)], writes=[("act", fc)])
            for cg in range(D // 512):
                base = (cg % 2) * 4
                for dc in range(NFC // DC):
                    wkd, wd = loader("d", cg, dc)
                    for f in range(DC):
                        fc = dc * DC + f
                        for b in range(4):
                            S.op("tensor", lambda e, f=f, fc=fc, b=b, base=base, wd=wd: e.matmul(
                                PS[base + b][:, :], lhsT=act[:, fc, b * 128:(b + 1) * 128], rhs=wd[:, f, :],
                                start=(fc == 0), stop=(fc == NFC - 1)),
                                reads=[wkd, ("act", fc)], writes=[("ps", base + b)])
                for b in range(4):
                    epilogue(cg, b, base + b)
            return cnt

        def wo_tile(oT, okey, wname, xt, xkey):
            for cg in range(D // 512):
                base = (cg % 2) * 4
                wk, wv = wload(wname, 0, KH, cg * 512, 512)
                for b in range(4):
                    for k in range(KH):
                        S.op("tensor", lambda e, k=k, b=b, base=base, wv=wv: e.matmul(
                            PS[base + b][:, :], lhsT=oT[:, k, b * 128:(b + 1) * 128], rhs=wv[:, k, :],
                            start=(k == 0), stop=(k == KH - 1)),
                            reads=[wk, okey], writes=[("ps", base + b)])
                    S.op("vector", lambda e, b=b, base=base, cg=cg: e.tensor_tensor(
                        out=xt[:, b, cg * 512:(cg + 1) * 512], in0=PS[base + b][:, :],
                        in1=xt[:, b, cg * 512:(cg + 1) * 512], op=ALU.add),
                        reads=[("ps", base + b), xkey], writes=[xkey])

        if cfg.get("stop", 99) >= 3:
         with contextlib.ExitStack() as st_:
            alloc_ws(st_, 3)
            xt = st_.enter_context(nc.sbuf_tensor(_u("xt"), [128, 4, D], F32))
            xn = st_.enter_context(nc.sbuf_tensor(_u("xn"), [128, 4, D], BF16))
            junk = st_.enter_context(nc.sbuf_tensor(_u("junk"), [128, D], F32))
            stt = st_.enter_context(nc.sbuf_tensor(_u("stt"), [128, 8], F32))
            hT = st_.enter_context(nc.sbuf_tensor(_u("hT"), [128, KD, 512], BF16))
            oT = st_.enter_context(nc.sbuf_tensor(_u("oT"), [128, H, 512], BF16))
            act = st_.enter_context(nc.sbuf_tensor(_u("act"), [128, NFC, 512], BF16))
            sg = [st_.enter_context(nc.sbuf_tensor(_u("sg%d" % i), [128, 512], BF16)) for i in range(GC)]
            cnt = 0
            for t in range(NT0):
                ld(xt[:], x_all[t * 512:(t + 1) * 512, :].rearrange("(b p) d -> p b d", p=128), "xt")
                ld(oT[:], oT0[t, :, :, :].rearrange("h d t -> d h t"), "oT", reads=[("dram", "oT0")])
                wo_tile(oT, "oT", "nao", xt, "xt")
                norm_tile(xt, "xt", xn, junk, stt, gffn[:, 0, :], hT, "hT", [6, 7])

                def epi(cg, b, bank):
                    S.op("vector", lambda e: e.tensor_tensor(
                        out=xt[:, b, cg * 512:(cg + 1) * 512], in0=PS[bank][:, :],
                        in1=xt[:, b, cg * 512:(cg + 1) * 512], op=ALU.add),
                        reads=[("ps", bank), "xt"], writes=["xt"])
                cnt = swiglu_tile(hT, "hT", act, sg, ("fg", "fu", "fd"), epi, cnt)
                S.dma("gpsimd", lambda e, t=t: e.dma_start(
                    out=x1[t * 512:(t + 1) * 512, :].rearrange("(b p) d -> p b d", p=128), in_=xt[:]),
                    "st_xt", reads=["xt"], writes=[("dram", "x1")])
                conv_some((n_moe_conv + 2 * NT0 - 1) // (2 * NT0))
            conv_some(100000)
            oi = st_.enter_context(nc.sbuf_tensor(_u("oi"), [128, NOWN * 4], I32))
            ld(oi[:], own_idx[:, :], "oi")
            for j in range(NOWN * 4):
                S.dma("gpsimd", lambda e, j=j: e.indirect_dma_start(
                    out=xt[:, j % 4, :], out_offset=None, in_=x1[0:NT0 * 512, :],
                    in_offset=bass.IndirectOffsetOnAxis(ap=oi[:, j:j + 1], axis=0)),
                    "xt", reads=[("dram", "x1"), "oi"], writes=["xt"])
                if j % 4 == 3:
                    tt_ = NT0 + j // 4
                    S.dma("gpsimd", lambda e, tt_=tt_: e.dma_start(
                        out=x1[tt_ * 512:(tt_ + 1) * 512, :].rearrange("(b p) d -> p b d", p=128), in_=xt[:]),
                        "st_xt", reads=["xt"], writes=[("dram", "x1")])
            S.barrier()
            S.run()

        if cfg.get("stop", 99) >= 4:
         with contextlib.ExitStack() as st_:
            alloc_ws(st_, 3)
            xb = [st_.enter_context(nc.sbuf_tensor(_u("xb%d" % i), [128, D], F32)) for i in range(2)]
            xn = st_.enter_context(nc.sbuf_tensor(_u("xn"), [128, 4, D], BF16))
            junk = st_.enter_context(nc.sbuf_tensor(_u("junk"), [128, D], F32))
            stt = st_.enter_context(nc.sbuf_tensor(_u("stt"), [128, 8], F32))
            hT = st_.enter_context(nc.sbuf_tensor(_u("hT"), [128, KD, 512], BF16))
            cT = st_.enter_context(nc.sbuf_tensor(_u("cT"), [128, 8, 512], BF16))
            cn = st_.enter_context(nc.sbuf_tensor(_u("cn"), [128, 1024], BF16))
            s2 = st_.enter_context(nc.sbuf_tensor(_u("s2"), [128, 8], F32))
            latf = st_.enter_context(nc.sbuf_tensor(_u("latf"), [128, 1088], F32))
            kvf = [st_.enter_context(nc.sbuf_tensor(_u("kvf%d" % i), [128, 512], F32)) for i in range(2)]
            rpt = st_.enter_context(nc.sbuf_tensor(_u("rpt"), [128, 4, 64], F32))
            kr = st_.enter_context(nc.sbuf_tensor(_u("kr"), [128, 64], F32))
            kr2 = st_.enter_context(nc.sbuf_tensor(_u("kr2"), [128, 64], F32))
            krb = st_.enter_context(nc.sbuf_tensor(_u("krb"), [128, 64], BF16))
            krT = st_.enter_context(nc.sbuf_tensor(_u("krT"), [64, 512], BF16))
            qf = st_.enter_context(nc.sbuf_tensor(_u("qf"), [128, H, 192], F32))
            qsq = st_.enter_context(nc.sbuf_tensor(_u("qsq"), [128, H, 192], F32))
            qs = st_.enter_context(nc.sbuf_tensor(_u("qs"), [128, 4 * H], F32))
            qb = st_.enter_context(nc.sbuf_tensor(_u("qb"), [128, H, 192], BF16))
            qr1 = st_.enter_context(nc.sbuf_tensor(_u("qr1"), [128, H, 64], F32))
            qr2 = st_.enter_context(nc.sbuf_tensor(_u("qr2"), [128, H, 64], F32))
            tq = st_.enter_context(nc.sbuf_tensor(_u("tq"), [128, H, 32], F32))
            kf = st_.enter_context(nc.sbuf_tensor(_u("kf"), [128, H, 128], F32))
            kb = st_.enter_context(nc.sbuf_tensor(_u("kb"), [128, H, 128], BF16))
            vb = st_.enter_context(nc.sbuf_tensor(_u("vb"), [128, H, 128], BF16))
            qnT = [st_.enter_context(nc.sbuf_tensor(_u("qnT%d" % i), [128, H, 128], BF16)) for i in range(2)]
            qrT = st_.enter_context(nc.sbuf_tensor(_u("qrT"), [64, H, 128], BF16))
            knT = [st_.enter_context(nc.sbuf_tensor(_u("knT%d" % i), [128, H, 128], BF16)) for i in range(2)]
            P4S = cfg.get("p4s", 9)
            NQG = (H * 192) // 384
            NKG = (H * 256) // 512
            for t in range(NT1):
                def xsrc4(b, t=t):
                    r0 = t * 512 + b * 128
                    ld(xb[b % 2][:, :], x1[r0:r0 + 128, :], ("xb", b % 2), reads=[("dram", "x1")])
                    return xb[b % 2][:, :], ("xb", b % 2)
                ld(rpt[:], rope_in[t * 512:(t + 1) * 512, :].rearrange("(b p) c -> p b c", p=128), "rpt")
                norm_tile(xsrc4, None, xn, junk, stt, gmix[:, 1, :], hT, "hT", [6, 7])
                wk0, w0v = wload("dqkv", 0, KD, 0, 512)
                wk1, w1v = wload("dqkv", 0, KD, 512, 512)
                wk2, w2v = wload("dqkv", 0, KD, 1024, 64)
                for b in range(4):
                    for (bank, wk, wv, ncol) in ((0, wk0, w0v, 512), (1, wk1, w1v, 512), (2, wk2, w2v, 64)):
                        for k in range(KD):
                            S.op("tensor", lambda e, k=k, b=b, bank=bank, wv=wv, ncol=ncol: e.matmul(
                                PS[bank][:, 0:ncol], lhsT=hT[:, k, b * 128:(b + 1) * 128], rhs=wv[:, k, :],
                                start=(k == 0), stop=(k == KD - 1)),
                                reads=[wk, ("hT", k)], writes=[("ps", bank)])
                    for li, ncol in ((0, 512), (1, 512), (2, 64)):
                        S.op("scalar", lambda e, li=li, ncol=ncol: e.copy(out=latf[:, li * 512:li * 512 + ncol], in_=PS[li][:, 0:ncol]),
                             reads=[("ps", li)], writes=[("latf", li)])
                    for li, ncol in ((0, 512), (1, 512), (2, 64)):
                        S.op("vector", lambda e, li=li, ncol=ncol: e.tensor_tensor(
                            out=junk[:, 0:ncol], in0=latf[:, li * 512:li * 512 + ncol], in1=latf[:, li * 512:li * 512 + ncol],
                            op=ALU.mult), reads=[("latf", li)], writes=["junk"])
                        S.op("vector", lambda e, li=li, ncol=ncol: e.tensor_reduce(
                            out=s2[:, li:li + 1], in_=junk[:, 0:ncol], axis=AX.X, op=ALU.add),
                            reads=["junk"], writes=[("s2", li)])
                    S.op("scalar", lambda e: e.activation(out=s2[:, 4:6], in_=s2[:, 0:2], func=AF.Sqrt, scale=1.0 / 512, bias=EPS),
                         reads=[("s2", 0), ("s2", 1)], writes=["s2b"])
                    S.op("scalar", lambda e: e.activation(out=s2[:, 6:7], in_=s2[:, 2:3], func=AF.Sqrt, scale=1.0 / 64, bias=EPS),
                         reads=[("s2", 2), "s2b"], writes=["s2b"])
                    S.op("vector", lambda e: e.reciprocal(out=s2[:, 4:7], in_=s2[:, 4:7]), reads=["s2b"], writes=["s2b"])
                    for li in range(2):
                        S.op("scalar", lambda e, li=li: e.activation(
                            out=cn[:, li * 512:(li + 1) * 512], in_=latf[:, li * 512:(li + 1) * 512], func=AF.Copy,
                            scale=s2[:, 4 + li:5 + li]),
                            reads=[("latf", li), "s2b"], writes=[("cn", li)])
                    S.op("vector", lambda e: e.scalar_tensor_tensor(
                        out=kr[:], in0=latf[:, 1024:1088], scalar=s2[:, 6:7], in1=gkr[:], op0=ALU.mult, op1=ALU.mult),
                        reads=[("latf", 2), "s2b", "gkr"], writes=["kr"])
                    S.op("vector", lambda e, b=b: e.tensor_tensor(out=kr2[:, 0:32], in0=kr[:, 0:32], in1=rpt[:, b, 0:32], op=ALU.mult),
                         reads=["kr", "rpt"], writes=["kr2a"])
                    S.op("vector", lambda e, b=b: e.tensor_tensor(out=kr2[:, 32:64], in0=kr[:, 32:64], in1=rpt[:, b, 32:64], op=ALU.mult),
                         reads=["kr", "rpt"], writes=["kr2b"])
                    S.op("vector", lambda e: e.tensor_tensor(out=krb[:, 0:32], in0=kr2[:, 0:32], in1=kr2[:, 32:64], op=ALU.subtract),
                         reads=["kr2a", "kr2b"], writes=["krb0"])
                    S.op("vector", lambda e, b=b: e.tensor_tensor(out=kr2[:, 0:32], in0=kr[:, 0:32], in1=rpt[:, b, 32:64], op=ALU.mult),
                         reads=["kr", "rpt", "krb0"], writes=["kr2a"])
                    S.op("vector", lambda e, b=b: e.tensor_tensor(out=kr2[:, 32:64], in0=kr[:, 32:64], in1=rpt[:, b, 0:32], op=ALU.mult),
                         reads=["kr", "rpt", "krb0"], writes=["kr2b"])
                    S.op("vector", lambda e: e.tensor_tensor(out=krb[:, 32:64], in0=kr2[:, 0:32], in1=kr2[:, 32:64], op=ALU.add),
                         reads=["kr2a", "kr2b"], writes=["krb1"])
                    S.op("tensor", lambda e, b=b: e.transpose(out=psb(3)[0:64, b * 128:(b + 1) * 128], in_=krb[:, :],
                                                             identity=ident_b[:]),
                         reads=["krb0", "krb1", "ident_b"], writes=[("ps", 3)])
                    for li in range(2):
                        for k in range(4):
                            S.op("tensor", lambda e, li=li, k=k: e.transpose(
                                out=psb(4 + li)[:, k * 128:(k + 1) * 128], in_=cn[:, li * 512 + k * 128:li * 512 + (k + 1) * 128],
                                identity=ident_b[:]),
                                reads=[("cn", li), "ident_b"], writes=[("ps", 4 + li)])
                        gofs = 4 + 4 * li
                        S.op("vector", lambda e, li=li, gofs=gofs, b=b: e.tensor_tensor(
                            out=cT[:, 4 * li:4 * li + 4, b * 128:(b + 1) * 128],
                            in0=psb(4 + li)[:, 0:512].rearrange("p (k t) -> p k t", k=4),
                            in1=gsm[:, gofs:gofs + 4].unsqueeze(2).to_broadcast([128, 4, 128]), op=ALU.mult),
                            reads=[("ps", 4 + li), "gsm"], writes=[("cT", li, b)])
                S.op("scalar", lambda e: e.copy(out=krT[:, :], in_=psb(3)[0:64, 0:512]), reads=[("ps", 3)], writes=["krT"])
                S.dma("gpsimd", lambda e, t=t: e.dma_start(out=k1r[t, :, :], in_=krT[:, :]), "st_krT",
                      reads=["krT"], writes=[("dram", "k1r")])
                is_prompt_tile = (NS * TS <= t < NT0)
                is_own_tile = (t >= NT0)
                for b in range(4 if P4S >= 2 else 0):
                    if not is_prompt_tile:
                        for g in range(NQG):
                            wk, wv = wload("uq", 0, 4, g * 384, 384)
                            bank = g % 3
                            for k in range(4):
                                S.op("tensor", lambda e, k=k, b=b, bank=bank, wv=wv: e.matmul(
                                    PS[bank][:, 0:384], lhsT=cT[:, k, b * 128:(b + 1) * 128], rhs=wv[:, k, :],
                                    start=(k == 0), stop=(k == 3)),
                                    reads=[wk, ("cT", 0, b)], writes=[("ps", bank)])
                            S.op("scalar", lambda e, g=g, bank=bank: e.copy(
                                out=qf[:, 2 * g:2 * g + 2, :].rearrange("p a c -> p (a c)"), in_=PS[bank][:, 0:384]),
                                reads=[("ps", bank)], writes=[("qf", g)])
                        if P4S < 2.2:
                            continue
                        qfk = [("qf", g) for g in range(NQG)]
                        S.op("vector", lambda e: e.tensor_tensor(out=qsq[:], in0=qf[:], in1=qf[:], op=ALU.mult),
                             reads=qfk, writes=["qsq"])
                        S.op("vector", lambda e: e.tensor_reduce(out=qs[:, 0:H], in_=qsq[:, :, 0:128], axis=AX.X, op=ALU.add),
                             reads=["qsq"], writes=["qs0"])
                        S.op("vector", lambda e: e.tensor_reduce(out=qs[:, H:2 * H], in_=qsq[:, :, 128:192], axis=AX.X, op=ALU.add),
                             reads=["qsq"], writes=["qs1"])
                        S.op("scalar", lambda e: e.activation(out=qs[:, 2 * H:3 * H], in_=qs[:, 0:H], func=AF.Sqrt, scale=1.0 / 128, bias=EPS),
                             reads=["qs0"], writes=["qs2"])
                        S.op("scalar", lambda e: e.activation(out=qs[:, 3 * H:4 * H], in_=qs[:, H:2 * H], func=AF.Sqrt, scale=1.0 / 64, bias=EPS),
                             reads=["qs1", "qs2"], writes=["qs2"])
                        S.op("vector", lambda e: e.reciprocal(out=qs[:, 2 * H:4 * H], in_=qs[:, 2 * H:4 * H]), reads=["qs2"], writes=["qs2"])
                        if P4S < 2.4:
                            continue
                        S.op("vector", lambda e: e.tensor_tensor(
                            out=qb[:, :, 0:128], in0=qf[:, :, 0:128],
                            in1=qs[:, 2 * H:3 * H].unsqueeze(2).to_broadcast([128, H, 128]), op=ALU.mult),
                            reads=qfk + ["qs2"], writes=["qbn"])
                        S.op("vector", lambda e: e.tensor_tensor(
                            out=qr1[:], in0=qf[:, :, 128:192],
                            in1=qs[:, 3 * H:4 * H].unsqueeze(2).to_broadcast([128, H, 64]), op=ALU.mult),
                            reads=qfk + ["qs2"], writes=["qr1"])
                        S.op("vector", lambda e: e.tensor_tensor(
                            out=qr1[:], in0=qr1[:], in1=gqr[:, :].unsqueeze(1).to_broadcast([128, H, 64]), op=ALU.mult),
                            reads=["qr1", "gqr"], writes=["qr1"])
                        cosb = rpt[:, b, 0:32].unsqueeze(1).to_broadcast([128, H, 32])
                        sinb = rpt[:, b, 32:64].unsqueeze(1).to_broadcast([128, H, 32])
                        S.op("vector", lambda e, cosb=cosb: e.tensor_tensor(out=qr2[:, :, 0:32], in0=qr1[:, :, 0:32], in1=cosb, op=ALU.mult),
                             reads=["qr1", "rpt"], writes=["qr2a"])
                        S.op("vector", lambda e, sinb=sinb: e.tensor_tensor(out=tq[:], in0=qr1[:, :, 32:64], in1=sinb, op=ALU.mult),
                             reads=["qr1", "rpt"], writes=["tq"])
                        S.op("vector", lambda e: e.tensor_tensor(out=qb[:, :, 128:160], in0=qr2[:, :, 0:32], in1=tq[:], op=ALU.subtract),
                             reads=["qr2a", "tq"], writes=["qbr0"])
                        S.op("vector", lambda e, sinb=sinb: e.tensor_tensor(out=qr2[:, :, 32:64], in0=qr1[:, :, 0:32], in1=sinb, op=ALU.mult),
                             reads=["qr1", "rpt"], writes=["qr2b"])
                        S.op("vector", lambda e, cosb=cosb: e.tensor_tensor(out=tq[:], in0=qr1[:, :, 32:64], in1=cosb, op=ALU.mult),
                             reads=["qr1", "rpt", "qbr0"], writes=["tq"])
                        S.op("vector", lambda e: e.tensor_tensor(out=qb[:, :, 160:192], in0=qr2[:, :, 32:64], in1=tq[:], op=ALU.add),
                             reads=["qr2b", "tq"], writes=["qbr1"])
                        if P4S < 2.6:
                            continue
                        for h4 in range(H // 4):
                            bank = 4 + h4 % 2
                            for hh in range(4):
                                h = h4 * 4 + hh
                                S.op("tensor", lambda e, h=h, hh=hh, bank=bank: e.transpose(
                                    out=psb(bank)[:, hh * 128:(hh + 1) * 128], in_=qb[:, h, 0:128], identity=ident_b[:]),
                                    reads=["qbn", "ident_b"], writes=[("ps", bank)])
                                if P4S >= 2.8: S.op("tensor", lambda e, h=h, hh=hh, bank=bank: e.transpose(
                                    out=psb(bank)[0:64, 512 + hh * 128:512 + (hh + 1) * 128], in_=qb[:, h, 128:192],
                                    identity=ident_b[:]),
                                    reads=["qbr0", "qbr1", "ident_b"], writes=[("ps", bank)])
                            S.op("scalar", lambda e, h4=h4, bank=bank, b=b: e.activation(
                                out=qnT[b % 2][:, h4 * 4:h4 * 4 + 4, :],
                                in_=psb(bank)[:, 0:512].rearrange("p (a t) -> p a t", a=4), func=AF.Copy, scale=gsm[:, 2:3]),
                                reads=[("ps", bank), "gsm"], writes=[("qnT", b % 2)])
                            if P4S >= 2.9: S.op("scalar", lambda e, h4=h4, bank=bank, b=b: e.copy(
                                out=qrT[:, h4 * 4:h4 * 4 + 4, :],
                                in_=psb(bank)[0:64, 512:1024].rearrange("p (a t) -> p a t", a=4)),
                                reads=[("ps", bank)], writes=["qrT"])
                        if P4S >= 4:
                            S.dma("gpsimd", lambda e, t=t, b=b: e.dma_start(
                                out=q1n[t, :, :, b * 128:(b + 1) * 128].rearrange("h d t -> d h t"), in_=qnT[b % 2][:]),
                                ("st_qnT", b % 2), reads=[("qnT", b % 2)], writes=[("dram", "q1n")])
                            S.dma("gpsimd", lambda e, t=t, b=b: e.dma_start(
                                out=q1r[t, :, :, b * 128:(b + 1) * 128].rearrange("h d t -> d h t"), in_=qrT[:]),
                                "st_qrT", reads=["qrT"], writes=[("dram", "q1r")])
                    if P4S < 3:
                        continue
                    if not is_own_tile:
                        for g in range(NKG):
                            wk, wv = wload("ukv", 0, 4, g * 512, 512)
                            bank = g % 3
                            for k in range(4):
                                S.op("tensor", lambda e, k=k, b=b, bank=bank, wv=wv: e.matmul(
                                    PS[bank][:, :], lhsT=cT[:, 4 + k, b * 128:(b + 1) * 128], rhs=wv[:, k, :],
                                    start=(k == 0), stop=(k == 3)),
                                    reads=[wk, ("cT", 1, b)], writes=[("ps", bank)])
                            pv = lambda bank=bank: PS[bank][:, :].rearrange("p (a c) -> p a c", a=2)
                            g2 = g % 2
                            S.op("scalar", lambda e, g2=g2, bank=bank: e.copy(out=kvf[g2][:], in_=PS[bank][:, :]),
                                 reads=[("ps", bank)], writes=[("kvf", g2)])
                            kvv = kvf[g2][:].rearrange("p (a c) -> p a c", a=2)
                            S.op("vector", lambda e, g=g, kvv=kvv: e.tensor_copy(out=kf[:, 2 * g:2 * g + 2, :], in_=kvv[:, :, 0:128]),
                                 reads=[("kvf", g2)], writes=[("kf", g)])
                            S.op("vector", lambda e, g=g, kvv=kvv: e.tensor_copy(out=vb[:, 2 * g:2 * g + 2, :], in_=kvv[:, :, 128:256]),
                                 reads=[("kvf", g2)], writes=[("vb", g)])
                        kfk = [("kf", g) for g in range(NKG)]
                        S.op("vector", lambda e: e.tensor_tensor(out=qsq[:, :, 0:128], in0=kf[:], in1=kf[:], op=ALU.mult),
                             reads=kfk, writes=["qsq"])
                        S.op("vector", lambda e: e.tensor_reduce(out=qs[:, 0:H], in_=qsq[:, :, 0:128], axis=AX.X, op=ALU.add),
                             reads=["qsq"], writes=["qs0"])
                        S.op("scalar", lambda e: e.activation(out=qs[:, 2 * H:3 * H], in_=qs[:, 0:H], func=AF.Sqrt, scale=1.0 / 128, bias=EPS),
                             reads=["qs0"], writes=["qs2"])
                        S.op("vector", lambda e: e.reciprocal(out=qs[:, 2 * H:3 * H], in_=qs[:, 2 * H:3 * H]), reads=["qs2"], writes=["qs2"])
                        S.op("vector", lambda e: e.tensor_tensor(
                            out=kb[:], in0=kf[:], in1=qs[:, 2 * H:3 * H].unsqueeze(2).to_broadcast([128, H, 128]), op=ALU.mult),
                            reads=kfk + ["qs2"], writes=["kb"])
                        for h4 in range(H // 4):
                            bank = 6 + h4 % 2
                            for hh in range(4):
                                h = h4 * 4 + hh
                                S.op("tensor", lambda e, h=h, hh=hh, bank=bank: e.transpose(
                                    out=psb(bank)[:, hh * 128:(hh + 1) * 128], in_=kb[:, h, :], identity=ident_b[:]),
                                    reads=["kb", "ident_b"], writes=[("ps", bank)])
                            S.op("scalar", lambda e, h4=h4, bank=bank, b=b: e.activation(
                                out=knT[b % 2][:, h4 * 4:h4 * 4 + 4, :],
                                in_=psb(bank)[:, 0:512].rearrange("p (a t) -> p a t", a=4), func=AF.Copy, scale=gsm[:, 3:4]),
                                reads=[("ps", bank), "gsm"], writes=[("knT", b % 2)])
                        if P4S >= 4:
                            S.dma("gpsimd", lambda e, t=t, b=b: e.dma_start(
                                out=k1n[t, :, :, b * 128:(b + 1) * 128].rearrange("h d t -> d h t"), in_=knT[b % 2][:]),
                                ("st_knT", b % 2), reads=[("knT", b % 2)], writes=[("dram", "k1n")])
                        r0 = t * 512 + b * 128
                        S.dma("gpsimd", lambda e, r0=r0: e.dma_start(
                            out=v1[r0:r0 + 128, :].rearrange("p (h c) -> p h c", h=H), in_=vb[:]),
                            "st_vb", reads=[("vb", g) for g in range(NKG)], writes=[("dram", "v1")])
            S.barrier()
            S.run()

        if cfg.get("stop", 99) >= 5:
         with contextlib.ExitStack() as st_:
            TK = max(RS * 64, RP * 64)
            krS = st_.enter_context(nc.sbuf_tensor(_u("krS"), [64, TK], BF16))
            knS = [st_.enter_context(nc.sbuf_tensor(_u("knS%d" % i), [128, TK], BF16)) for i in range(2)]
            vS = [st_.enter_context(nc.sbuf_tensor(_u("vS%d" % i), [128, TK // 128, 128], BF16)) for i in range(2)]
            qnS = [st_.enter_context(nc.sbuf_tensor(_u("qnS%d" % i), [128, 512], BF16)) for i in range(2)]
            qrS = [st_.enter_context(nc.sbuf_tensor(_u("qrS%d" % i), [64, 512], BF16)) for i in range(2)]
            pT = [st_.enter_context(nc.sbuf_tensor(_u("pT%d" % i), [128, 512], BF16)) for i in range(3)]
            rcp = [st_.enter_context(nc.sbuf_tensor(_u("rcp%d" % i), [128, 512], F32)) for i in range(2)]
            oo = [st_.enter_context(nc.sbuf_tensor(_u("oo%d" % i), [128, 512], BF16)) for i in range(2)]
            scale1 = 192 ** -0.5
            seqs1 = []
            for s in range(NS):
                seqs1.append(([s * TS + i for i in range(TS)], [s * TS + i for i in range(TS)],
                              [s * TS + i for i in range(TS)]))
            seqs1.append(([NT0 + i for i in range(NOWN)], [NS * TS + i for i in range(TP)],
                          [NS * TS + i for i in range(NOWN)]))
            hc = 0
            qc = 0
            cc = 0
            for (qtiles, kvtiles, otiles) in seqs1:
                T = len(kvtiles) * 512
                NC = T // 128
                for i, kt in enumerate(kvtiles):
                    S.dma("sync", lambda e, i=i, kt=kt: e.dma_start(out=krS[:, i * 512:(i + 1) * 512], in_=k1r[kt, :, :]),
                          "krS", reads=[("dram", "k1r")], writes=["krS"])
                for h in range(H):
                    hs = hc % 2
                    hc += 1
                    for i, kt in enumerate(kvtiles):
                        S.dma("sync", lambda e, i=i, kt=kt, h=h, hs=hs: e.dma_start(
                            out=knS[hs][:, i * 512:(i + 1) * 512], in_=k1n[kt, h, :, :]),
                            ("knS", hs), reads=[("dram", "k1n")], writes=[("knS", hs)])
                    tok0 = kvtiles[0] * 512
                    S.dma("sync", lambda e, h=h, hs=hs, tok0=tok0, T=T, NC=NC: e.dma_start(
                        out=vS[hs][:, 0:NC, :],
                        in_=v1[tok0:tok0 + T, h * 128:(h + 1) * 128].rearrange("(b p) c -> p b c", p=128)),
                        ("vS", hs), reads=[("dram", "v1")], writes=[("vS", hs)])
                    for qi, qt in enumerate(qtiles):
                        q2 = qc % 2
                        qc += 1
                        S.dma("sync", lambda e, qt=qt, h=h, q2=q2: e.dma_start(out=qnS[q2][:], in_=q1n[qt, h, :, :]),
                              ("qnS", q2), reads=[("dram", "q1n")], writes=[("qnS", q2)])
                        S.dma("sync", lambda e, qt=qt, h=h, q2=q2: e.dma_start(out=qrS[q2][:], in_=q1r[qt, h, :, :]),
                              ("qrS", q2), reads=[("dram", "q1r")], writes=[("qrS", q2)])
                        obk = 4 + q2
                        dbk = 6 + q2

                        def pv_step(c, p3, obk=obk, dbk=dbk, hs=hs, NC=NC):
                            S.op("tensor", lambda e: e.matmul(PS[obk][:, :], lhsT=vS[hs][:, c, :], rhs=pT[p3][:],
                                                              start=(c == 0), stop=(c == NC - 1)),
                                 reads=[("vS", hs), ("pT", p3)], writes=[("ps", obk)])
                            S.op("tensor", lambda e: e.matmul(PS[dbk][:, :], lhsT=ones_b[:], rhs=pT[p3][:],
                                                              start=(c == 0), stop=(c == NC - 1)),
                                 reads=["ones_b", ("pT", p3)], writes=[("ps", dbk)])
                        prev = None
                        for c in range(NC):
                            sbk = cc % 4
                            p3 = cc % 3
                            cc += 1
                            S.op("tensor", lambda e, c=c, sbk=sbk, hs=hs, q2=q2: e.matmul(
                                PS[sbk][:, :], lhsT=knS[hs][:, c * 128:(c + 1) * 128], rhs=qnS[q2][:], start=True, stop=False),
                                reads=[("knS", hs), ("qnS", q2)], writes=[("ps", sbk)])
                            S.op("tensor", lambda e, c=c, sbk=sbk, q2=q2: e.matmul(
                                PS[sbk][:, :], lhsT=krS[:, c * 128:(c + 1) * 128], rhs=qrS[q2][:], start=False, stop=True),
                                reads=["krS", ("qrS", q2)], writes=[("ps", sbk)])
                            S.op("scalar", lambda e, sbk=sbk, p3=p3: e.activation(out=pT[p3][:], in_=PS[sbk][:, :],
                                                                                  func=AF.Exp, scale=scale1),
                                 reads=[("ps", sbk)], writes=[("pT", p3)])
                            if prev is not None:
                                pv_step(*prev)
                            prev = (c, p3)
                        pv_step(*prev)
                        S.op("vector", lambda e, q2=q2, dbk=dbk: e.reciprocal(out=rcp[q2][:], in_=PS[dbk][:, :]),
                             reads=[("ps", dbk)], writes=[("rcp", q2)])
                        S.op("vector", lambda e, q2=q2, obk=obk: e.tensor_tensor(out=oo[q2][:], in0=PS[obk][:, :],
                                                                                 in1=rcp[q2][:], op=ALU.mult),
                             reads=[("ps", obk), ("rcp", q2)], writes=[("oo", q2)])
                        ot = otiles[qi]
                        S.dma("gpsimd", lambda e, ot=ot, h=h, q2=q2: e.dma_start(out=oT1[ot, h, :, :], in_=oo[q2][:]),
                              ("st_oo", q2), reads=[("oo", q2)], writes=[("dram", "oT1")])
            S.barrier()
            S.run()

        NB = 4 * NL1
        NST = 2 * NL1 + E - 1
        x2 = dscr("x2", [NL1 * 512, D], F32)
        hn = dscr("hn", [NL1 * 512, D], BF16)
        hs = dscr("hs", [NST * 512, D], BF16)
        ysl = dscr("ysl", [NST * 512, D], F32)
        MK = gst.enter_context(nc.sbuf_tensor(_u("MK"), [128, 2, NB, E], F32))
        GG = gst.enter_context(nc.sbuf_tensor(_u("GG"), [128, 2, NB], F32))
        SLI = gst.enter_context(nc.sbuf_tensor(_u("SLI"), [128, 2, NB], I32))
        IXG = gst.enter_context(nc.sbuf_tensor(_u("IXG"), [128, NST, NCH], I32))
        IXD = gst.enter_context(nc.sbuf_tensor(_u("IXD"), [128, NST, NCG * NDC], I32))
        if cfg.get("stop", 99) >= 6:
         with contextlib.ExitStack() as st_:
            alloc_ws(st_, 3)
            xt = st_.enter_context(nc.sbuf_tensor(_u("xt"), [128, 4, D], F32))
            xn = st_.enter_context(nc.sbuf_tensor(_u("xn"), [128, 4, D], BF16))
            junk = st_.enter_context(nc.sbuf_tensor(_u("junk"), [128, D], F32))
            stt = st_.enter_context(nc.sbuf_tensor(_u("stt"), [128, 8], F32))
            oT = st_.enter_context(nc.sbuf_tensor(_u("oT"), [128, H, 512], BF16))
            wr = st_.enter_context(nc.sbuf_tensor(_u("wr"), [128, KD, E], F32))
            h32 = st_.enter_context(nc.sbuf_tensor(_u("h32"), [128, KD, 128], F32))
            lg = st_.enter_context(nc.sbuf_tensor(_u("lg"), [128, 4, E], F32))
            m1 = st_.enter_context(nc.sbuf_tensor(_u("m1"), [128, 8], F32))
            lg2 = st_.enter_context(nc.sbuf_tensor(_u("lg2"), [128, E], F32))
            ld(wr[:], w_router[:, :, :], "wr")
            for ti, t in enumerate(l1_tiles):
                ld(xt[:], x1[t * 512:(t + 1) * 512, :].rearrange("(b p) d -> p b d", p=128), "xt", reads=[("dram", "x1")])
                ld(oT[:], oT1[ti, :, :, :].rearrange("h d t -> d h t"), "oT", reads=[("dram", "oT1")])
                wo_tile(oT, "oT", "mo", xt, "xt")
                S.dma("gpsimd", lambda e, ti=ti: e.dma_start(
                    out=x2[ti * 512:(ti + 1) * 512, :].rearrange("(b p) d -> p b d", p=128), in_=xt[:]),
                    "st_xt", reads=["xt"], writes=[("dram", "x2")])
                norm_tile(xt, "xt", xn, junk, stt, gffn[:, 1, :], None, "hT", [6, 7])
                S.dma("gpsimd", lambda e, ti=ti: e.dma_start(
                    out=hn[ti * 512:(ti + 1) * 512, :].rearrange("(b p) d -> p b d", p=128), in_=xn[:]),
                    "st_xn", reads=[("xn", b) for b in range(4)], writes=[("dram", "hn")])
                for b in range(4):
                    gb = ti * 4 + b
                    S.op("scalar", lambda e, b=b: e.activation(out=junk[:], in_=xt[:, b, :], func=AF.Copy,
                                                               scale=stt[:, 4 + b:5 + b]),
                         reads=["xt", "st2"], writes=["junk"])
                    for k4 in range(KD // 4):
                        bank = k4 % 2
                        for kk in range(4):
                            k = k4 * 4 + kk
                            S.op("tensor", lambda e, k=k, kk=kk, bank=bank: e.transpose(
                                out=PS[bank][:, kk * 128:(kk + 1) * 128], in_=junk[:, k * 128:(k + 1) * 128],
                                identity=ident_f[:]),
                                reads=["junk", "ident_f"], writes=[("ps", bank)])
                        S.op("vector", lambda e, k4=k4, bank=bank: e.tensor_tensor(
                            out=h32[:, k4 * 4:k4 * 4 + 4, :], in0=PS[bank][:, :].rearrange("p (a t) -> p a t", a=4),
                            in1=gffn[:, 1, k4 * 4:k4 * 4 + 4].unsqueeze(2).to_broadcast([128, 4, 128]), op=ALU.mult),
                            reads=[("ps", bank), "gffn"], writes=[("h32", k4)])
                    for k in range(KD):
                        S.op("tensor", lambda e, k=k: e.matmul(PS[2][:, 0:E], lhsT=h32[:, k, :], rhs=wr[:, k, :],
                                                               start=(k == 0), stop=(k == KD - 1)),
                             reads=[("h32", k // 4), "wr"], writes=[("ps", 2)])
                    S.op("vector", lambda e, b=b: e.tensor_scalar(out=lg[:, b, :], in0=PS[2][:, 0:E], scalar1=1.0, scalar2=None,
                                                                  op0=ALU.mult), reads=[("ps", 2)], writes=[("lg", b)])
                    S.op("vector", lambda e, b=b: e.tensor_reduce(out=m1[:, 0:1], in_=lg[:, b, :], axis=AX.X, op=ALU.max),
                         reads=[("lg", b)], writes=["m1a"])
                    S.op("vector", lambda e, b=b, gb=gb: e.tensor_scalar(out=MK[:, 0, gb, :], in0=lg[:, b, :], scalar1=m1[:, 0:1],
                                                                         scalar2=None, op0=ALU.is_equal),
                         reads=[("lg", b), "m1a"], writes=["MK"])
                    S.op("vector", lambda e, b=b, gb=gb: e.scalar_tensor_tensor(
                        out=lg2[:], in0=MK[:, 0, gb, :], scalar=-1e30, in1=lg[:, b, :], op0=ALU.mult, op1=ALU.add),
                        reads=["MK", ("lg", b)], writes=["lg2"])
                    S.op("vector", lambda e: e.tensor_reduce(out=m1[:, 1:2], in_=lg2[:], axis=AX.X, op=ALU.max),
                         reads=["lg2"], writes=["m1b"])
                    S.op("vector", lambda e, gb=gb: e.tensor_scalar(out=MK[:, 1, gb, :], in0=lg2[:], scalar1=m1[:, 1:2], scalar2=None,
                                                                    op0=ALU.is_equal), reads=["lg2", "m1b"], writes=["MK"])
                    S.op("vector", lambda e: e.tensor_tensor(out=m1[:, 2:3], in0=m1[:, 1:2], in1=m1[:, 0:1], op=ALU.subtract),
                         reads=["m1a", "m1b"], writes=["m1c"])
                    S.op("scalar", lambda e: e.activation(out=m1[:, 3:4], in_=m1[:, 2:3], func=AF.Exp),
                         reads=["m1c"], writes=["m1d"])
                    S.op("vector", lambda e: e.tensor_scalar(out=m1[:, 4:5], in0=m1[:, 3:4], scalar1=1.0, scalar2=None,
                                                             op0=ALU.add), reads=["m1d"], writes=["m1e"])
                    S.op("vector", lambda e, gb=gb: e.reciprocal(out=GG[:, 0, gb:gb + 1], in_=m1[:, 4:5]),
                         reads=["m1e"], writes=["GG"])
                    S.op("vector", lambda e, gb=gb: e.tensor_tensor(out=GG[:, 1, gb:gb + 1], in0=m1[:, 3:4], in1=GG[:, 0, gb:gb + 1],
                                                                    op=ALU.mult), reads=["m1d", "GG"], writes=["GG"])
            S.barrier()
            S.run()

        if cfg.get("stop", 99) >= 6:
         with contextlib.ExitStack() as st_:
            tri_f = st_.enter_context(nc.sbuf_tensor(_u("tri_f"), [128, 128], F32))
            tri_b = st_.enter_context(nc.sbuf_tensor(_u("tri_b"), [128, 128], BF16))
            cst = st_.enter_context(nc.sbuf_tensor(_u("cst"), [128, 64], F32))
            Mb = st_.enter_context(nc.sbuf_tensor(_u("Mb"), [128, NB, E], BF16))
            wi = st_.enter_context(nc.sbuf_tensor(_u("wi"), [128, NB, E], F32))
            tot = st_.enter_context(nc.sbuf_tensor(_u("tot"), [128, NB, E], F32))
            off = st_.enter_context(nc.sbuf_tensor(_u("off"), [128, NB, E], F32))
            sm = st_.enter_context(nc.sbuf_tensor(_u("sm"), [128, 8, E], F32))
            cmpA = st_.enter_context(nc.sbuf_tensor(_u("cmpA"), [128, E, NL1], F32))
            cmpB = st_.enter_context(nc.sbuf_tensor(_u("cmpB"), [128, NST, E], F32))
            prod = st_.enter_context(nc.sbuf_tensor(_u("prod"), [128, NB, E], F32))
            slf = st_.enter_context(nc.sbuf_tensor(_u("slf"), [128, 2, NB], F32))
            ej = st_.enter_context(nc.sbuf_tensor(_u("ej"), [128, NST], F32))
            ixf = st_.enter_context(nc.sbuf_tensor(_u("ixf"), [128, NST, max(NCH, NCG * NDC)], F32))
            ld(tri_f[:], tri_in[:, :], "tri_f")
            ld(cst[:], cst_in[:, :], "cst")
            S.op("vector", lambda e: e.tensor_copy(out=tri_b[:], in_=tri_f[:]), reads=["tri_f"], writes=["tri_b"])
            S.op("vector", lambda e: e.tensor_tensor(out=prod[:], in0=MK[:, 0, :, :], in1=MK[:, 1, :, :], op=ALU.add),
                 reads=["MK"], writes=["prod"])
            S.op("vector", lambda e: e.tensor_copy(out=Mb[:], in_=prod[:]), reads=["prod"], writes=["Mb"])
            mbf = Mb[:].rearrange("p a b -> p (a b)")
            S.op("tensor", lambda e: e.matmul(PS[0][:, 0:NB * E], lhsT=tri_b[:], rhs=mbf, start=True, stop=True),
                 reads=["tri_b", "Mb"], writes=[("ps", 0)])
            S.op("tensor", lambda e: e.matmul(PS[1][:, 0:NB * E], lhsT=ones_b[:], rhs=mbf, start=True, stop=True),
                 reads=["ones_b", "Mb"], writes=[("ps", 1)])
            S.op("scalar", lambda e: e.copy(out=wi[:].rearrange("p a b -> p (a b)"), in_=PS[0][:, 0:NB * E]),
                 reads=[("ps", 0)], writes=["wi"])
            S.op("scalar", lambda e: e.copy(out=tot[:].rearrange("p a b -> p (a b)"), in_=PS[1][:, 0:NB * E]),
                 reads=[("ps", 1)], writes=["tot"])
            S.op("vector", lambda e: e.memset(off[:, 0, :], 0.0), writes=["off"])
            for blk in range(1, NB):
                S.op("vector", lambda e, blk=blk: e.tensor_tensor(out=off[:, blk, :], in0=off[:, blk - 1, :], in1=tot[:, blk - 1, :],
                                                                  op=ALU.add), reads=["off", "tot"], writes=["off"])
            S.op("vector", lambda e: e.tensor_tensor(out=sm[:, 0, :], in0=off[:, NB - 1, :], in1=tot[:, NB - 1, :], op=ALU.add),
                 reads=["off", "tot"], writes=["sm0"])
            S.op("vector", lambda e: e.tensor_tensor(
                out=cmpA[:], in0=sm[:, 0, :].unsqueeze(2).to_broadcast([128, E, NL1]),
                in1=cst[:, 1:1 + NL1].unsqueeze(1).to_broadcast([128, E, NL1]), op=ALU.is_gt),
                reads=["sm0", "cst"], writes=["cmpA"])
            S.op("vector", lambda e: e.tensor_reduce(out=sm[:, 1, :], in_=cmpA[:], axis=AX.X, op=ALU.add),
                 reads=["cmpA"], writes=["sm1"])
            S.op("vector", lambda e: e.memset(sm[:, 2, 0:1], 0.0), reads=["sm1"], writes=["sm2"])
            for e_ in range(1, E):
                S.op("vector", lambda e, e_=e_: e.tensor_tensor(out=sm[:, 2, e_:e_ + 1], in0=sm[:, 2, e_ - 1:e_],
                                                                in1=sm[:, 1, e_ - 1:e_], op=ALU.add),
                     reads=["sm1", "sm2"], writes=["sm2"])
            S.op("vector", lambda e: e.tensor_tensor(out=sm[:, 3, :], in0=sm[:, 2, :], in1=sm[:, 1, :], op=ALU.add),
                 reads=["sm1", "sm2"], writes=["sm3"])
            S.op("vector", lambda e: e.tensor_scalar(out=sm[:, 4, :], in0=sm[:, 2, :], scalar1=512.0, scalar2=None, op0=ALU.mult),
                 reads=["sm2"], writes=["sm4"])
            S.op("vector", lambda e: e.tensor_tensor(out=wi[:], in0=wi[:], in1=off[:], op=ALU.add),
                 reads=["wi", "off"], writes=["wi"])
            S.op("vector", lambda e: e.tensor_tensor(out=wi[:], in0=wi[:], in1=sm[:, 4, :].unsqueeze(1).to_broadcast([128, NB, E]),
                                                     op=ALU.add), reads=["wi", "sm4"], writes=["wi"])
            for kk in range(2):
                S.op("vector", lambda e, kk=kk: e.tensor_tensor(out=prod[:], in0=MK[:, kk, :, :], in1=wi[:], op=ALU.mult),
                     reads=["MK", "wi"], writes=["prod"])
                S.op("vector", lambda e, kk=kk: e.tensor_reduce(out=slf[:, kk, :], in_=prod[:], axis=AX.X, op=ALU.add),
                     reads=["prod"], writes=["slf"])
            S.op("vector", lambda e: e.tensor_copy(out=SLI[:], in_=slf[:]), reads=["slf"], writes=["SLI"])
            S.op("vector", lambda e: e.tensor_tensor(
                out=cmpB[:], in0=sm[:, 3, :].unsqueeze(1).to_broadcast([128, NST, E]),
                in1=cst[:, 16:16 + NST].unsqueeze(2).to_broadcast([128, NST, E]), op=ALU.is_le),
                reads=["sm3", "cst"], writes=["cmpB"])
            S.op("vector", lambda e: e.tensor_reduce(out=ej[:], in_=cmpB[:], axis=AX.X, op=ALU.add),
                 reads=["cmpB"], writes=["ej"])
            S.op("vector", lambda e: e.tensor_scalar(out=ej[:], in0=ej[:], scalar1=float(E - 1), scalar2=None, op0=ALU.min),
                 reads=["ej"], writes=["ej"])
            for (IX, nper) in ((IXG, NCH), (IXD, NCG * NDC)):
                S.op("vector", lambda e, nper=nper: e.tensor_scalar(
                    out=ixf[:, :, 0:nper], in0=ej[:].unsqueeze(2).to_broadcast([128, NST, nper]),
                    scalar1=float(nper * 128), scalar2=cst[:, 0:1], op0=ALU.mult, op1=ALU.add),
                    reads=["ej", "cst"], writes=["ixf"])
                S.op("vector", lambda e, nper=nper: e.tensor_tensor(
                    out=ixf[:, :, 0:nper], in0=ixf[:, :, 0:nper],
                    in1=cst[:, 48:48 + nper].unsqueeze(1).to_broadcast([128, NST, nper]), op=ALU.add),
                    reads=["ixf", "cst"], writes=["ixf"])
                S.op("vector", lambda e, IX=IX, nper=nper: e.tensor_copy(out=IX[:], in_=ixf[:, :, 0:nper]),
                     reads=["ixf"], writes=["IX%d" % nper])
            S.barrier()
            S.run()

        if cfg.get("stop", 99) >= 6:
         with contextlib.ExitStack() as st_:
            zt = st_.enter_context(nc.sbuf_tensor(_u("zt"), [128, 4, D], BF16))
            hb = [st_.enter_context(nc.sbuf_tensor(_u("hb%d" % i), [128, D], BF16)) for i in range(2)]
            S.op("vector", lambda e: e.memset(zt[:], 0.0), writes=["zt"])
            for j in range(NST):
                S.dma("sync", lambda e, j=j: e.dma_start(
                    out=hs[j * 512:(j + 1) * 512, :].rearrange("(b p) d -> p b d", p=128), in_=zt[:]),
                    "st_zt", reads=["zt"], writes=[("dram", "hs0")])
            for blk in range(NB):
                i2 = blk % 2
                ld(hb[i2][:], hn[blk * 128:(blk + 1) * 128, :], ("hb", i2), reads=[("dram", "hn")])
                for kk in range(2):
                    S.dma("gpsimd", lambda e, i2=i2, kk=kk, blk=blk: e.indirect_dma_start(
                        out=hs[:, :], out_offset=bass.IndirectOffsetOnAxis(ap=SLI[:, kk, blk:blk + 1], axis=0),
                        in_=hb[i2][:, :], in_offset=None),
                        ("sc", i2), reads=[("hb", i2), "SLI", ("dram", "hs0")], writes=[("dram", "hs")])
            S.barrier()
            S.run()

        if cfg.get("stop", 99) >= 6:
         with contextlib.ExitStack() as st_:
            alloc_ws(st_, 3)
            xn = st_.enter_context(nc.sbuf_tensor(_u("xn"), [128, 4, D], BF16))
            hT = st_.enter_context(nc.sbuf_tensor(_u("hT"), [128, KD, 512], BF16))
            act = st_.enter_context(nc.sbuf_tensor(_u("act"), [128, NFC, 512], BF16))
            sg = [st_.enter_context(nc.sbuf_tensor(_u("sg%d" % i), [128, 512], BF16)) for i in range(GC)]
            yo = st_.enter_context(nc.sbuf_tensor(_u("yo"), [128, 4, D], F32))
            cnt = 0
            for j in range(NST):
                S.dma("sync", lambda e, j=j: e.dma_start(
                    out=xn[:], in_=hs[j * 512:(j + 1) * 512, :].rearrange("(b p) d -> p b d", p=128)),
                    "xn", reads=[("dram", "hs"), ("dram", "hs0")], writes=[("xn", b) for b in range(4)])
                transpose_tile(xn, gffn[:, 1, :], hT, "hT", [6, 7])

                def loader(kind, *a, j=j):
                    s_ = ws_i[0] % len(WS)
                    ws_i[0] += 1
                    if kind in ("g", "u"):
                        src_t, nmw = (war_g, "war_g") if kind == "g" else (war_u, "war_u")
                        ix = IXG[:, j, a[0]:a[0] + 1]
                        n = KD * 512
                        ikey = "IX%d" % NCH
                    else:
                        i_ = a[0] * NDC + a[1]
                        src_t, nmw, ix = war_d, "war_d", IXD[:, j, i_:i_ + 1]
                        n = DCm * 512
                        ikey = "IX%d" % (NCG * NDC)
                    view = WS[s_][:, 0:n].rearrange("p (k c) -> p k c", c=512)
                    S.dma("gpsimd", lambda e, s_=s_, n=n, src_t=src_t, ix=ix: e.indirect_dma_start(
                        out=WS[s_][:, 0:n], out_offset=None, in_=src_t[:, :],
                        in_offset=bass.IndirectOffsetOnAxis(ap=ix, axis=0)),
                        ("w", s_), reads=[("dram", nmw), ikey], writes=[("w", s_)])
                    return ("w", s_), view

                def epi(cg, b, bank):
                    if (cg + b) % 2 == 0:
                        S.op("scalar", lambda e: e.copy(out=yo[:, b, cg * 512:(cg + 1) * 512], in_=PS[bank][:, :]),
                             reads=[("ps", bank)], writes=[("yo", b, cg)])
                    else:
                        S.op("vector", lambda e: e.tensor_scalar(out=yo[:, b, cg * 512:(cg + 1) * 512], in0=PS[bank][:, :],
                                                                 scalar1=1.0, scalar2=None, op0=ALU.mult),
                             reads=[("ps", bank)], writes=[("yo", b, cg)])
                cnt = swiglu_tile(hT, "hT", act, sg, loader, epi, cnt)
                S.dma("sync", lambda e, j=j: e.dma_start(
                    out=ysl[j * 512:(j + 1) * 512, :].rearrange("(b p) d -> p b d", p=128), in_=yo[:]),
                    "st_yo", reads=[("yo", b, cg) for b in range(4) for cg in range(NCG)], writes=[("dram", "ysl")])
            S.barrier()
            S.run()

        if cfg.get("stop", 99) >= 6:
         with contextlib.ExitStack() as st_:
            xb2 = [st_.enter_context(nc.sbuf_tensor(_u("xb2%d" % i), [128, D], F32)) for i in range(2)]
            ya = [st_.enter_context(nc.sbuf_tensor(_u("ya%d" % i), [128, D], F32)) for i in range(2)]
            yb = [st_.enter_context(nc.sbuf_tensor(_u("yb%d" % i), [128, D], F32)) for i in range(2)]
            for blk in range(NB):
                i2 = blk % 2
                ld(xb2[i2][:], x2[blk * 128:(blk + 1) * 128, :], ("xb2", i2), reads=[("dram", "x2")])
                for kk, yy, nm in ((0, ya, "ya"), (1, yb, "yb")):
                    S.dma("gpsimd", lambda e, i2=i2, kk=kk, blk=blk, yy=yy: e.indirect_dma_start(
                        out=yy[i2][:, :], out_offset=None, in_=ysl[:, :],
                        in_offset=bass.IndirectOffsetOnAxis(ap=SLI[:, kk, blk:blk + 1], axis=0)),
                        (nm, i2), reads=[("dram", "ysl"), "SLI"], writes=[(nm, i2)])
                S.op("vector", lambda e, i2=i2, blk=blk: e.scalar_tensor_tensor(
                    out=xb2[i2][:], in0=ya[i2][:], scalar=GG[:, 0, blk:blk + 1], in1=xb2[i2][:], op0=ALU.mult, op1=ALU.add),
                    reads=[("ya", i2), "GG", ("xb2", i2)], writes=[("xb2", i2)])
                S.op("vector", lambda e, i2=i2, blk=blk: e.scalar_tensor_tensor(
                    out=xb2[i2][:], in0=yb[i2][:], scalar=GG[:, 1, blk:blk + 1], in1=xb2[i2][:], op0=ALU.mult, op1=ALU.add),
                    reads=[("yb", i2), "GG", ("xb2", i2)], writes=[("xb2", i2)])
                ti, b = blk // 4, blk % 4
                if ti < NS * TS:
                    dst = ys[ti * 512 + b * 128:ti * 512 + (b + 1) * 128, :]
                else:
                    r0 = (ti - NS * TS) * 512 + b * 128
                    dst = yp[r0:r0 + 128, :]
                S.dma("sync", lambda e, dst=dst, i2=i2: e.dma_start(out=dst, in_=xb2[i2][:]),
                      ("st_xb2", i2), reads=[("xb2", i2)], writes=[("dram", "y")])
            S.barrier()
            S.run()
    return nc


def _rope_table(pos_tokens, grid_w=64, theta=10000.0):
    t = np.asarray(pos_tokens)
    row = (t // grid_w).astype(np.float32)
    col = (t % grid_w).astype(np.float32)
    n_pairs = 16
    inv = (np.float32(theta) ** (-np.arange(n_pairs, dtype=np.float32) / np.float32(n_pairs))).astype(np.float32)
    ang = np.concatenate([row[:, None] * inv, col[:, None] * inv], axis=-1).astype(np.float32)
    return np.concatenate([np.cos(ang), np.sin(ang)], axis=-1).astype(np.float32)


def _cst_table():
    c = np.zeros((128, 64), np.float32)
    c[:, 0] = np.arange(128)
    c[:, 1:16] = 512.0 * np.arange(15)[None, :]
    c[:, 16:48] = np.arange(32)[None, :]
    c[:, 48:64] = 128.0 * np.arange(16)[None, :]
    return c


def make_core_inputs(cfg, inp, core):
    D, H, FF, E = cfg["D"], cfg["H"], cfg["FF"], cfg["E"]
    RS, RP, NS = cfg["RS"], cfg["RP"], cfg["NS"]
    KD = D // 128
    f = lambda a: np.ascontiguousarray(np.asarray(a, dtype=np.float32))
    xs = inp["x_sample"]
    xp = inp["x_prompt"]
    x_all = np.concatenate([f(xs[core * NS + s]) for s in range(NS)] + [f(xp[0])], axis=0)
    n_own_rows = RP // 8 * 64
    NOWN = RP // 64
    base = NS * RS * 64
    own = base + core * n_own_rows + np.arange(n_own_rows)
    own_idx = np.ascontiguousarray(own.reshape(NOWN * 4, 128).T.astype(np.int32))
    pos = np.concatenate([np.arange(RS * 64)] * NS + [np.arange(RP * 64)] + [core * n_own_rows + np.arange(n_own_rows)])
    rope = _rope_table(pos)
    pk = lambda g: np.ascontiguousarray(f(g).reshape(-1, 128).T)
    g_mix = np.ascontiguousarray(np.stack([pk(inp["mix_norm"][l]) for l in range(2)], axis=1))
    g_ffn = np.ascontiguousarray(np.stack([pk(inp["ffn_norm"][l]) for l in range(2)], axis=1))
    rpb = f(inp["na_rpb"][0])
    kc = np.arange(64)[:, None]
    qc = np.arange(64)[None, :]
    dc = np.clip(kc - qc + 15, 0, 30)
    ws = np.clip(qc - 8, 0, 48)
    valid = ((kc >= ws) & (kc < ws + 16)).astype(np.float32)
    rpbg = np.zeros((128, H, 14, 64), np.float32)
    for a in range(2):
        for m in range(14):
            rpbg[a * 64:(a + 1) * 64, :, m, :] = np.transpose(rpb[:, m + a][:, dc], (1, 0, 2))
    namask = np.concatenate([valid, valid], axis=0)
    d = {
        "x_all": x_all, "own_idx": own_idx, "rope": rope, "ident": np.eye(128, dtype=np.float32),
        "g_mix": g_mix, "g_ffn": g_ffn,
        "g_naq": pk(inp["na_q_gain"][0]), "g_nak": pk(inp["na_k_gain"][0]),
        "rpbg": rpbg, "namask": namask,
        "g_ql": pk(inp["mla_q_lora_gain"][0]), "g_kvl": pk(inp["mla_kv_lora_gain"][0]),
        "g_qn": pk(inp["mla_qn_gain"][0]), "g_kn": pk(inp["mla_kn_gain"][0]),
        "g_qr": np.ascontiguousarray(np.broadcast_to(f(inp["mla_qr_gain"][0])[None, :], (128, 64))),
        "g_kr": np.ascontiguousarray(np.broadcast_to(f(inp["mla_kr_gain"][0])[None, :], (128, 64))),
        "w_router": np.ascontiguousarray(f(inp["moe_w_router"][0]).reshape(KD, 128, E).transpose(1, 0, 2)),
        "tri": np.triu(np.ones((128, 128), np.float32), 1), "cst": _cst_table(),
        "na_w_qkv": f(inp["na_w_qkv"][0]), "na_w_o": f(inp["na_w_o"][0]),
        "ffn_w_gate": f(inp["ffn_w_gate"][0]), "ffn_w_up": f(inp["ffn_w_up"][0]), "ffn_w_down": f(inp["ffn_w_down"][0]),
        "mla_w_dqkv": f(inp["mla_w_dqkv"][0]), "mla_w_uq": f(inp["mla_w_uq"][0]), "mla_w_ukv": f(inp["mla_w_ukv"][0]),
        "mla_w_o": f(inp["mla_w_o"][0]),
    }
    for e_ in range(E):
        d["moe_w_gate%d" % e_] = f(inp["moe_w_gate"][0, e_])
        d["moe_w_up%d" % e_] = f(inp["moe_w_up"][0, e_])
        d["moe_w_down%d" % e_] = f(inp["moe_w_down"][0, e_])
    return d


def run_cfg(cfg, inp, trace=False):
    nc = build_program(cfg)
    shared = None
    in_maps = []
    for c in range(N_CORES):
        d = make_core_inputs(cfg, inp, c)
        if shared is None:
            shared = d
        else:
            for k in d:
                if k not in ("x_all", "own_idx", "rope"):
                    d[k] = shared[k]
        in_maps.append(d)
    res = run_bass_kernel_spmd(nc, in_maps, core_ids=list(range(N_CORES)), trace=trace)
    RS, RP, NS, D = cfg["RS"], cfg["RP"], cfg["NS"], cfg["D"]
    ys = np.stack([np.asarray(res.results[c]["ys"]).reshape(NS, RS * 64, D) for c in range(N_CORES)], axis=0)
    y_sample = ys.reshape(N_CORES * NS, RS * 64, D).astype(np.float32)
    y_prompt = np.concatenate([np.asarray(res.results[c]["yp"]) for c in range(N_CORES)], axis=0)[None].astype(np.float32)
    return (y_prompt, y_sample), res


def kernel(**inputs):
    out, _ = run_cfg(CFG_FULL, inputs)
    return out
```

```python
import contextlib
import numpy as np
import concourse.bass as bass
import concourse.mybir as mybir
from concourse.bass_utils import run_bass_kernel_spmd

F32 = mybir.dt.float32
BF16 = mybir.dt.bfloat16
I32 = mybir.dt.int32
AF = mybir.ActivationFunctionType
ALU = mybir.AluOpType
AX = mybir.AxisListType
ENGS = ("sync", "gpsimd", "scalar", "vector", "tensor")
EPS = 1e-6
N_CORES = 8

CFG_FULL = dict(D=2048, H=16, FF=5632, E=8, RS=32, RP=128, NS=2)


class Sched:
    def __init__(self, nc, stack, n_dma_sems=96):
        self.nc = nc
        self.ops = {e: [] for e in ENGS}
        self.esem = {e: stack.enter_context(nc.semaphore("es_" + e)) for e in ENGS}
        self.ecnt = {e: 0 for e in ENGS}
        self.seen = {e: {} for e in ENGS}
        self.free_sems = {"sync": [[stack.enter_context(nc.semaphore("dh%d" % i)), 0] for i in range(28)],
                          "gpsimd": [[stack.enter_context(nc.semaphore("dg%d" % i)), 0] for i in range(n_dma_sems - 28)]}
        self.dsem = {}
        self.lastw = {}
        self.reads = {}

    def _dma_sem(self, key, eng):
        key = (eng, key)
        if key not in self.dsem:
            self.dsem[key] = self.free_sems[eng].pop()
        return self.dsem[key]

    @staticmethod
    def _isdram(k):
        return isinstance(k, tuple) and len(k) > 0 and k[0] == "dram"

    def _deps(self, reads, writes):
        deps = {}

        def add(d):
            for sid, ev in d.items():
                if sid not in deps or deps[sid][1] < ev[1]:
                    deps[sid] = ev
        for r in reads:
            add(self.lastw.get(r, {}))
        for w in writes:
            if self._isdram(w):
                continue
            add(self.lastw.get(w, {}))
            add(self.reads.get(w, {}))
        return deps

    def _commit(self, reads, writes, ev):
        sid = id(ev[0])
        for r in reads:
            self.reads.setdefault(r, {})[sid] = ev
        for w in writes:
            if self._isdram(w):
                self.lastw.setdefault(w, {})[sid] = ev
            else:
                self.lastw[w] = {sid: ev}
                self.reads[w] = {}

    def _waits(self, eng, deps, skip_own):
        waits = []
        seen = self.seen[eng]
        own = id(self.esem[eng])
        for sid, (sem, val) in deps.items():
            if skip_own and sid == own:
                continue
            if seen.get(sid, 0) >= val:
                continue
            seen[sid] = val
            waits.append((sem, val))
        return waits

    def op(self, eng, fn, reads=(), writes=()):
        waits = self._waits(eng, self._deps(reads, writes), eng == "tensor")
        self.ecnt[eng] += 1
        ev = (self.esem[eng], self.ecnt[eng])

        def emit(e, fn=fn, waits=waits, sem=ev[0]):
            for s, v in waits:
                e.wait_ge(s, v)
            fn(e).then_inc(sem, 1)
        self.ops[eng].append(emit)
        self._commit(reads, writes, ev)

    def dma(self, eng, fn, semkey, reads=(), writes=()):
        waits = self._waits(eng, self._deps(reads, writes), False)
        ds = self._dma_sem(semkey, eng)
        ds[1] += 16
        ev = (ds[0], ds[1])

        def emit(e, fn=fn, waits=waits, sem=ev[0]):
            for s, v in waits:
                e.wait_ge(s, v)
            fn(e).then_inc(sem, 16)
        self.ops[eng].append(emit)
        self._commit(reads, writes, ev)

    def barrier(self):
        finals = [(ds[0], ds[1]) for ds in self.dsem.values() if ds[1] > 0]
        finals += [(self.esem[e], self.ecnt[e]) for e in ENGS if self.ecnt[e] > 0]
        for eng in ENGS:
            seen = self.seen[eng]
            w = []
            for s, v in finals:
                if seen.get(id(s), 0) >= v:
                    continue
                if id(s) == id(self.esem[eng]) and eng == "tensor":
                    continue
                seen[id(s)] = v
                w.append((s, v))

            def emit(e, w=w):
                for s, v in w:
                    e.wait_ge(s, v)
            self.ops[eng].append(emit)
        for (eng, _k), pair in self.dsem.items():
            self.free_sems[eng].append(pair)
        self.dsem = {}

    def run(self):
        ops = self.ops
        with self.nc.Block() as block:
            @block.sync
            def _(e):
                for f in ops["sync"]:
                    f(e)

            @block.gpsimd
            def _(e):
                for f in ops["gpsimd"]:
                    f(e)

            @block.scalar
            def _(e):
                for f in ops["scalar"]:
                    f(e)

            @block.vector
            def _(e):
                for f in ops["vector"]:
                    f(e)

            @block.tensor
            def _(e):
                for f in ops["tensor"]:
                    f(e)
        self.ops = {e: [] for e in ENGS}


_UCNT = [0]


def _u(name):
    _UCNT[0] += 1
    return "%s_%d" % (name, _UCNT[0])


def _chunk_div(n, cap):
    for c in range(min(n, cap), 0, -1):
        if n % c == 0:
            return c
    return 1


def build_program(cfg):
    D, H, FF, E = cfg["D"], cfg["H"], cfg["FF"], cfg["E"]
    RS, RP, NS = cfg["RS"], cfg["RP"], cfg["NS"]
    KD = D // 128
    NFC = FF // 128
    HD = H * 128
    KH = HD // 128
    assert D % 512 == 0 and FF % 512 == 0 and HD == D
    TS = RS // 8
    TP = RP // 8
    NOWN = RP // 64
    NT0 = NS * TS + TP
    NT1 = NT0 + NOWN
    NL1 = NS * TS + NOWN
    seqs0 = [(s * TS, TS, RS) for s in range(NS)] + [(NS * TS, TP, RP)]
    l1_tiles = list(range(NS * TS)) + [NT0 + i for i in range(NOWN)]
    QL = KVL = 512
    LAT = QL + KVL + 64

    nc = bass.Bass("TRN2", target_bir_lowering=False)

    def din(name, shape, dt=F32):
        return nc.dram_tensor(name, list(shape), dt, kind="ExternalInput").ap()

    def dscr(name, shape, dt):
        kind = "ExternalOutput" if name in cfg.get("dbg", ()) else "Internal"
        return nc.dram_tensor(name, list(shape), dt, kind=kind).ap()

    x_all = din("x_all", [NT0 * 512, D])
    own_idx = din("own_idx", [128, NOWN * 4], I32)
    rope_in = din("rope", [NT1 * 512, 64])
    ident_in = din("ident", [128, 128])
    g_mix = din("g_mix", [128, 2, KD])
    g_ffn = din("g_ffn", [128, 2, KD])
    g_naq = din("g_naq", [128, 1])
    g_nak = din("g_nak", [128, 1])
    rpbg = din("rpbg", [128, H, 14, 64])
    namask = din("namask", [128, 64])
    g_ql = din("g_ql", [128, 4])
    g_kvl = din("g_kvl", [128, 4])
    g_qn = din("g_qn", [128, 1])
    g_kn = din("g_kn", [128, 1])
    g_qr = din("g_qr", [128, 64])
    g_kr = din("g_kr", [128, 64])
    w_router = din("w_router", [128, KD, E])
    tri_in = din("tri", [128, 128])
    cst_in = din("cst", [128, 64])
    Wf = {
        "qkv": din("na_w_qkv", [D, 3 * HD]), "nao": din("na_w_o", [HD, D]),
        "fg": din("ffn_w_gate", [D, FF]), "fu": din("ffn_w_up", [D, FF]), "fd": din("ffn_w_down", [FF, D]),
        "dqkv": din("mla_w_dqkv", [D, LAT]), "uq": din("mla_w_uq", [QL, H * 192]),
        "ukv": din("mla_w_ukv", [KVL, H * 256]), "mo": din("mla_w_o", [HD, D]),
    }
    for e_ in range(E):
        Wf["mg%d" % e_] = din("moe_w_gate%d" % e_, [D, FF])
        Wf["mu%d" % e_] = din("moe_w_up%d" % e_, [D, FF])
        Wf["md%d" % e_] = din("moe_w_down%d" % e_, [FF, D])
    Wb = {k: dscr("wb_" + k, v.shape, BF16) for k, v in Wf.items() if not k.startswith("m") or k == "mo"}

    ys = nc.dram_tensor("ys", [NS * TS * 512, D], F32, kind="ExternalOutput").ap()
    yp = nc.dram_tensor("yp", [NOWN * 512, D], F32, kind="ExternalOutput").ap()

    qk0 = dscr("qk0", [NT0, 2 * H, 128, 512], BF16)
    v0 = dscr("v0", [NT0 * 512, HD], BF16)
    oT0 = dscr("oT0", [NT0, H, 128, 512], BF16)
    x1 = dscr("x1", [NT1 * 512, D], F32)
    q1n = dscr("q1n", [NT1, H, 128, 512], BF16)
    q1r = dscr("q1r", [NT1, H, 64, 512], BF16)
    k1n = dscr("k1n", [NT1, H, 128, 512], BF16)
    k1r = dscr("k1r", [NT1, 64, 512], BF16)
    v1 = dscr("v1", [NT1 * 512, HD], BF16)
    oT1 = dscr("oT1", [NL1, H, 128, 512], BF16)

    with contextlib.ExitStack() as gst:
        S = Sched(nc, gst)
        PS = [gst.enter_context(nc.psum_tensor("ps%d" % i, [128, 512], F32)) for i in range(8)]
        ident_f = gst.enter_context(nc.sbuf_tensor(_u("ident_f"), [128, 128], F32))
        ident_b = gst.enter_context(nc.sbuf_tensor(_u("ident_b"), [128, 128], BF16))
        ones_b = gst.enter_context(nc.sbuf_tensor(_u("ones_b"), [128, 128], BF16))
        gmix = gst.enter_context(nc.sbuf_tensor(_u("gmix"), [128, 2, KD], F32))
        gffn = gst.enter_context(nc.sbuf_tensor(_u("gffn"), [128, 2, KD], F32))
        gsm = gst.enter_context(nc.sbuf_tensor(_u("gsm"), [128, 12], F32))
        gqr = gst.enter_context(nc.sbuf_tensor(_u("gqr"), [128, 64], F32))
        gkr = gst.enter_context(nc.sbuf_tensor(_u("gkr"), [128, 64], F32))
        WS = []
        ws_i = [0]

        def alloc_ws(stk, n):
            WS[:] = [stk.enter_context(nc.sbuf_tensor(_u("ws%d" % i), [128, 8192], BF16)) for i in range(n)]

        def ld(dst, src, key, eng="sync", reads=()):
            S.dma(eng, lambda e: e.dma_start(out=dst, in_=src), key, reads=reads, writes=[key])

        ld(ident_f[:], ident_in[:, :], "ident_f")
        ld(gmix[:], g_mix[:, :, :], "gmix")
        ld(gffn[:], g_ffn[:, :, :], "gffn")
        ld(gsm[:, 0:1], g_naq[:, :], "gsm")
        ld(gsm[:, 1:2], g_nak[:, :], "gsm")
        ld(gsm[:, 2:3], g_qn[:, :], "gsm")
        ld(gsm[:, 3:4], g_kn[:, :], "gsm")
        ld(gsm[:, 4:8], g_ql[:, :], "gsm")
        ld(gsm[:, 8:12], g_kvl[:, :], "gsm")
        ld(gqr[:], g_qr[:, :], "gqr")
        ld(gkr[:], g_kr[:, :], "gkr")
        S.op("vector", lambda e: e.tensor_copy(out=ident_b[:], in_=ident_f[:]), reads=["ident_f"], writes=["ident_b"])
        S.op("vector", lambda e: e.memset(ones_b[:], 1.0), writes=["ones_b"])

        def conv(name):
            src, dst = Wf[name], Wb[name]
            rows, cols = src.shape
            step = max(128, (4 * 1024 * 1024 // cols) // 128 * 128)
            for r0 in range(0, rows, step):
                r1 = min(rows, r0 + step)
                S.dma("gpsimd", lambda e, r0=r0, r1=r1: e.dma_start(out=dst[r0:r1, :], in_=src[r0:r1, :]),
                      ("cv", name), writes=[("dram", "wb_" + name)])

        for nm in ("qkv", "nao", "fg", "fu", "fd", "dqkv", "uq", "ukv", "mo"):
            conv(nm)
        NCH = FF // 512
        NCG = D // 512
        NDC = NFC // _chunk_div(NFC, 16)
        DCm = _chunk_div(NFC, 16)
        war_g = dscr("war_g", [E * NCH * 128, KD * 512], BF16)
        war_u = dscr("war_u", [E * NCH * 128, KD * 512], BF16)
        war_d = dscr("war_d", [E * NCG * NDC * 128, DCm * 512], BF16)
        moe_conv_todo = []

        def _mk_gu(dst_t, src, e_, c, nm):
            r0 = (e_ * NCH + c) * 128
            return lambda e: e.dma_start(
                out=dst_t[r0:r0 + 128, :].rearrange("p (k f) -> p k f", f=512),
                in_=src[:, c * 512:(c + 1) * 512].rearrange("(k p) f -> p k f", p=128))

        def _mk_d(src, e_, cg, dc):
            r0 = ((e_ * NCG + cg) * NDC + dc) * 128
            return lambda e: e.dma_start(
                out=war_d[r0:r0 + 128, :].rearrange("p (f c) -> p f c", c=512),
                in_=src[dc * DCm * 128:(dc + 1) * DCm * 128, cg * 512:(cg + 1) * 512].rearrange("(f p) c -> p f c", p=128))

        for e_ in range(E):
            for c in range(NCH):
                moe_conv_todo.append((_mk_gu(war_g, Wf["mg%d" % e_], e_, c, "g"), "war_g"))
                moe_conv_todo.append((_mk_gu(war_u, Wf["mu%d" % e_], e_, c, "u"), "war_u"))
            for cg in range(NCG):
                for dc in range(NDC):
                    moe_conv_todo.append((_mk_d(Wf["md%d" % e_], e_, cg, dc), "war_d"))
        n_moe_conv = len(moe_conv_todo)
        cv_rr = [0]

        def conv_some(n):
            for _ in range(n):
                if moe_conv_todo:
                    fn, nm = moe_conv_todo.pop(0)
                    cv_rr[0] += 1
                    S.dma("gpsimd", fn, ("cvm", cv_rr[0] % 8), writes=[("dram", nm)])

        def wload(name, k0, nk, c0, ncols):
            s = ws_i[0] % len(WS)
            ws_i[0] += 1
            src = Wb[name][k0 * 128:(k0 + nk) * 128, c0:c0 + ncols].rearrange("(k p) f -> p k f", p=128)
            view = WS[s][:, 0:nk * ncols].rearrange("p (k c) -> p k c", c=ncols)
            S.dma("sync", lambda e: e.dma_start(out=view, in_=src), ("w", s),
                  reads=[("dram", "wb_" + name)], writes=[("w", s)])
            return ("w", s), view

        def psb(i):
            return PS[i][:, :].bitcast(BF16)

        def norm_tile(xt, xkey, xn, junk, st, gain, hT, hkey, tp_banks, hT32=None):
            if callable(xt):
                xsrc = xt
            else:
                xsrc = lambda b: (xt[:, b, :], xkey)
            for b in range(4):
                xa, xk = xsrc(b)
                S.op("vector", lambda e, xa=xa, b=b: e.scalar_tensor_tensor(
                    out=junk[:, :], in0=xa, scalar=1.0, in1=xa, op0=ALU.mult, op1=ALU.mult, accum_out=st[:, b:b + 1]),
                    reads=[xk], writes=["junk", ("st", b)])
                S.op("scalar", lambda e, b=b: e.activation(out=st[:, 4 + b:5 + b], in_=st[:, b:b + 1], func=AF.Sqrt,
                                                           scale=1.0 / D, bias=EPS),
                     reads=[("st", b)], writes=["st2"])
                S.op("vector", lambda e, b=b: e.reciprocal(out=st[:, 4 + b:5 + b], in_=st[:, 4 + b:5 + b]),
                     reads=["st2"], writes=["st2"])
                S.op("scalar", lambda e, b=b, xa=xa: e.activation(out=xn[:, b, :], in_=xa, func=AF.Copy,
                                                                  scale=st[:, 4 + b:5 + b]),
                     reads=[xk, "st2"], writes=[("xn", b)])
            if hT is None:
                return
            transpose_tile(xn, gain, hT, hkey, tp_banks)

        def transpose_tile(xn, gain, hT, hkey, tp_banks):
            for k in range(KD):
                bank = tp_banks[(k // 2) % len(tp_banks)]
                half = k % 2
                tpv = psb(bank)[:, half * 512:(half + 1) * 512]
                for b in range(4):
                    S.op("tensor", lambda e, b=b, k=k, tpv=tpv: e.transpose(
                        out=tpv[:, b * 128:(b + 1) * 128], in_=xn[:, b, k * 128:(k + 1) * 128], identity=ident_b[:]),
                        reads=[("xn", b), "ident_b"], writes=[("ps", bank)])
                if k % 2 == 0:
                    S.op("vector", lambda e, k=k, tpv=tpv: e.tensor_scalar(
                        out=hT[:, k, :], in0=tpv, scalar1=gain[:, k:k + 1], scalar2=None, op0=ALU.mult),
                        reads=[("ps", bank), "gmix", "gffn"], writes=[(hkey, k)])
                else:
                    S.op("scalar", lambda e, k=k, tpv=tpv: e.activation(
                        out=hT[:, k, :], in_=tpv, func=AF.Copy, scale=gain[:, k:k + 1]),
                        reads=[("ps", bank), "gmix", "gffn"], writes=[(hkey, k)])

        if cfg.get("stop", 99) >= 1:
         with contextlib.ExitStack() as st_:
            alloc_ws(st_, 3)
            xt = st_.enter_context(nc.sbuf_tensor(_u("xt"), [128, 4, D], F32))
            xn = st_.enter_context(nc.sbuf_tensor(_u("xn"), [128, 4, D], BF16))
            junk = st_.enter_context(nc.sbuf_tensor(_u("junk"), [128, D], F32))
            stt = st_.enter_context(nc.sbuf_tensor(_u("stt"), [128, 8], F32))
            hT = st_.enter_context(nc.sbuf_tensor(_u("hT"), [128, KD, 512], BF16))
            sq = [st_.enter_context(nc.sbuf_tensor(_u("sq%d" % i), [128, 512], BF16)) for i in range(2)]
            rs_ = [st_.enter_context(nc.sbuf_tensor(_u("rs%d" % i), [128, 512], F32)) for i in range(2)]
            qo = [st_.enter_context(nc.sbuf_tensor(_u("qo%d" % i), [128, 512], BF16)) for i in range(3)]
            vo = [st_.enter_context(nc.sbuf_tensor(_u("vo%d" % i), [128, 512], BF16)) for i in range(3)]
            cnt = 0
            vcnt = 0
            pend1 = [None]
            for t in range(NT0):
                ld(xt[:], x_all[t * 512:(t + 1) * 512, :].rearrange("(b p) d -> p b d", p=128), "xt")
                norm_tile(xt, "xt", xn, junk, stt, gmix[:, 0, :], hT, "hT", [6, 7])
                for ch in range(2 * H // 4):
                    wkey, wv = wload("qkv", 0, KD, ch * 512, 512)
                    for j in range(4):
                        hj = ch * 4 + j
                        pb = cnt % 4
                        sb = 4 + cnt % 2
                        i2 = cnt % 2
                        i3 = cnt % 3
                        cnt += 1
                        for k in range(KD):
                            S.op("tensor", lambda e, k=k, j=j, pb=pb, wv=wv: e.matmul(
                                PS[pb][:, :], lhsT=wv[:, k, j * 128:(j + 1) * 128], rhs=hT[:, k, :],
                                start=(k == 0), stop=(k == KD - 1)),
                                reads=[wkey, ("hT", k)], writes=[("ps", pb)])
                        S.op("scalar", lambda e, pb=pb, i2=i2: e.activation(out=sq[i2][:], in_=PS[pb][:, :], func=AF.Square),
                             reads=[("ps", pb)], writes=[("sq", i2)])
                        def tail1(pb=pb, sb=sb, i2=i2, i3=i3, hj=hj, t=t):
                            S.op("tensor", lambda e, sb=sb, i2=i2: e.matmul(PS[sb][:, :], lhsT=ones_b[:], rhs=sq[i2][:],
                                                                             start=True, stop=True),
                                 reads=[("sq", i2), "ones_b"], writes=[("ps", sb)])
                            S.op("scalar", lambda e, sb=sb, i2=i2: e.activation(
                                out=rs_[i2][:], in_=PS[sb][:, :], func=AF.Sqrt, scale=1.0 / 128, bias=EPS),
                                reads=[("ps", sb)], writes=[("rs", i2)])
                            S.op("vector", lambda e, i2=i2: e.reciprocal(out=rs_[i2][:], in_=rs_[i2][:]),
                                 reads=[("rs", i2)], writes=[("rs", i2)])
                            gcol = 0 if hj < H else 1
                            S.op("vector", lambda e, pb=pb, i2=i2, i3=i3, gcol=gcol: e.scalar_tensor_tensor(
                                out=qo[i3][:], in0=PS[pb][:, :], scalar=gsm[:, gcol:gcol + 1], in1=rs_[i2][:],
                                op0=ALU.mult, op1=ALU.mult),
                                reads=[("ps", pb), ("rs", i2), "gsm"], writes=[("qo", i3)])
                            S.dma("gpsimd", lambda e, i3=i3, t=t, hj=hj: e.dma_start(out=qk0[t, hj, :, :], in_=qo[i3][:]),
                                  ("st_qo", i3), reads=[("qo", i3)], writes=[("dram", "qk0")])
                        if pend1[0] is not None:
                            pend1[0]()
                        pend1[0] = tail1
                if pend1[0] is not None:
                    pend1[0]()
                    pend1[0] = None
                for cg in range(HD // 512):
                    wkey, wv = wload("qkv", 0, KD, 2 * HD + cg * 512, 512)
                    for b in range(4):
                        pb = cnt % 4
                        cnt += 1
                        i3 = vcnt % 3
                        vcnt += 1
                        for k in range(KD):
                            S.op("tensor", lambda e, k=k, b=b, pb=pb, wv=wv: e.matmul(
                                PS[pb][:, :], lhsT=hT[:, k, b * 128:(b + 1) * 128], rhs=wv[:, k, :],
                                start=(k == 0), stop=(k == KD - 1)),
                                reads=[wkey, ("hT", k)], writes=[("ps", pb)])
                        S.op("scalar", lambda e, pb=pb, i3=i3: e.copy(out=vo[i3][:], in_=PS[pb][:, :]),
                             reads=[("ps", pb)], writes=[("vo", i3)])
                        r0 = t * 512 + b * 128
                        S.dma("gpsimd", lambda e, i3=i3, r0=r0, cg=cg: e.dma_start(
                            out=v0[r0:r0 + 128, cg * 512:(cg + 1) * 512], in_=vo[i3][:]),
                            ("st_vo", i3), reads=[("vo", i3)], writes=[("dram", "v0")])
            S.barrier()
            S.run()

        if cfg.get("stop", 99) >= 2:
         with contextlib.ExitStack() as st_:
            TT = st_.enter_context(nc.sbuf_tensor(_u("TT"), [128, H, 14, 64], BF16))
            msk = st_.enter_context(nc.sbuf_tensor(_u("msk"), [128, 64], F32))
            rp = [st_.enter_context(nc.sbuf_tensor(_u("rp%d" % i), [128, 14, 64], F32)) for i in range(2)]
            WRmax = 16
            kw = st_.enter_context(nc.sbuf_tensor(_u("kw"), [128, H, WRmax * 64], BF16))
            vE = st_.enter_context(nc.sbuf_tensor(_u("vE"), [128, WRmax // 2, HD], BF16))
            vO = st_.enter_context(nc.sbuf_tensor(_u("vO"), [128, WRmax // 2 - 1, HD], BF16))
            qT = st_.enter_context(nc.sbuf_tensor(_u("qT"), [128, H, 512], BF16))
            oS = st_.enter_context(nc.sbuf_tensor(_u("oS"), [128, H, 512], BF16))
            Eb = [st_.enter_context(nc.sbuf_tensor(_u("Eb%d" % i), [128, 2, 4, 64], BF16)) for i in range(2)]
            Pb = [st_.enter_context(nc.sbuf_tensor(_u("Pb%d" % i), [128, 2, 4, 64], BF16)) for i in range(2)]
            rc = [st_.enter_context(nc.sbuf_tensor(_u("rc%d" % i), [128, 128], F32)) for i in range(2)]
            ld(msk[:], namask[:, :], "msk")
            for h in range(H):
                i2 = h % 2
                ld(rp[i2][:], rpbg[:, h, :, :], ("rp", i2))
                S.op("scalar", lambda e, i2=i2: e.activation(out=rp[i2][:], in_=rp[i2][:], func=AF.Exp),
                     reads=[("rp", i2)], writes=[("rp", i2)])
                S.op("vector", lambda e, i2=i2, h=h: e.tensor_tensor(
                    out=TT[:, h, :, :], in0=rp[i2][:], in1=msk[:, :].unsqueeze(1).to_broadcast([128, 14, 64]), op=ALU.mult),
                    reads=[("rp", i2), "msk"], writes=["TT"])
            scale = 128 ** -0.5
            gc = 0
            pend = [None]
            for (tile0, ntl, rows) in seqs0:
                WR = min(16, rows)
                for tl in range(ntl):
                    t = tile0 + tl
                    r0 = tl * 8
                    w0 = min(max(r0 - 4, 0), rows - WR)
                    rr = w0
                    while rr < w0 + WR:
                        st_tile = rr // 8
                        re = min(w0 + WR, (st_tile + 1) * 8)
                        n = (re - rr) * 64
                        off = (rr - st_tile * 8) * 64
                        dst = (rr - w0) * 64
                        S.dma("sync", lambda e, st_tile=st_tile, off=off, n=n, dst=dst, tile0=tile0: e.dma_start(
                            out=kw[:, :, dst:dst + n],
                            in_=qk0[tile0 + st_tile, H:2 * H, :, off:off + n].rearrange("h d t -> d h t")),
                            "kw", reads=[("dram", "qk0")], writes=["kw"])
                        rr = re
                    tok0 = tile0 * 512 + w0 * 64
                    S.dma("sync", lambda e, tok0=tok0, WR=WR: e.dma_start(
                        out=vE[:, 0:WR // 2, :], in_=v0[tok0:tok0 + WR * 64, :].rearrange("(b p) e -> p b e", p=128)),
                        "vE", reads=[("dram", "v0")], writes=["vE"])
                    if WR > 8:
                        S.dma("sync", lambda e, tok0=tok0, WR=WR: e.dma_start(
                            out=vO[:, 0:WR // 2 - 1, :],
                            in_=v0[tok0 + 64:tok0 + 64 + (WR - 2) * 64, :].rearrange("(b p) e -> p b e", p=128)),
                            "vO", reads=[("dram", "v0")], writes=["vO"])
                    S.dma("sync", lambda e, t=t: e.dma_start(
                        out=qT[:, :, :], in_=qk0[t, 0:H, :, :].rearrange("h d t -> d h t")),
                        "qT", reads=[("dram", "qk0")], writes=["qT"])
                    for rl in range(8):
                        r = r0 + rl
                        rs = min(max(r - 4, 0), rows - 8)
                        o = r - rs
                        rel = rs - w0
                        m0 = 7 - o
                        for hp in range(H // 2):
                            sbk = gc % 4
                            obk = 4 + gc % 2
                            dbk = 6 + gc % 2
                            i2 = gc % 2
                            gc += 1
                            for hh in range(2):
                                h = hp * 2 + hh
                                for p in range(4):
                                    kt0 = (rel + 2 * p) * 64
                                    S.op("tensor", lambda e, h=h, hh=hh, p=p, kt0=kt0, sbk=sbk, rl=rl: e.matmul(
                                        PS[sbk][:, (hh * 4 + p) * 64:(hh * 4 + p + 1) * 64],
                                        lhsT=kw[:, h, kt0:kt0 + 128], rhs=qT[:, h, rl * 64:(rl + 1) * 64],
                                        start=True, stop=True),
                                        reads=["kw", "qT"], writes=[("ps", sbk)])
                            S.op("scalar", lambda e, sbk=sbk, i2=i2: e.activation(
                                out=Eb[i2][:].rearrange("p a b c -> p (a b c)"), in_=PS[sbk][:, :], func=AF.Exp, scale=scale),
                                reads=[("ps", sbk)], writes=[("Eb", i2)])
                            S.op("vector", lambda e, i2=i2, hp=hp, m0=m0: e.tensor_tensor(
                                out=Pb[i2][:], in0=Eb[i2][:], in1=TT[:, 2 * hp:2 * hp + 2, m0:m0 + 7:2, :], op=ALU.mult),
                                reads=[("Eb", i2), "TT"], writes=[("Pb", i2)])
                            def tail(hp=hp, rl=rl, rel=rel, i2=i2, obk=obk, dbk=dbk):
                                for hh in range(2):
                                    h = hp * 2 + hh
                                    for p in range(4):
                                        if rel % 2 == 0:
                                            vv = vE[:, rel // 2 + p, h * 128:(h + 1) * 128]
                                            vk = "vE"
                                        else:
                                            vv = vO[:, (rel - 1) // 2 + p, h * 128:(h + 1) * 128]
                                            vk = "vO"
                                        S.op("tensor", lambda e, hh=hh, p=p, vv=vv, obk=obk, i2=i2: e.matmul(
                                            PS[obk][:, hh * 64:(hh + 1) * 64], lhsT=vv, rhs=Pb[i2][:, hh, p, :],
                                            start=(p == 0), stop=(p == 3)),
                                            reads=[vk, ("Pb", i2)], writes=[("ps", obk)])
                                for p in range(4):
                                    S.op("tensor", lambda e, p=p, dbk=dbk, i2=i2: e.matmul(
                                        PS[dbk][:, 0:128].rearrange("q (a b) -> q a b", a=2), lhsT=ones_b[:],
                                        rhs=Pb[i2][:, :, p, :], start=(p == 0), stop=(p == 3)),
                                        reads=[("Pb", i2), "ones_b"], writes=[("ps", dbk)])
                                S.op("vector", lambda e, dbk=dbk, i2=i2: e.reciprocal(out=rc[i2][:], in_=PS[dbk][:, 0:128]),
                                     reads=[("ps", dbk)], writes=[("rc", i2)])
                                S.op("vector", lambda e, obk=obk, i2=i2, hp=hp, rl=rl: e.tensor_tensor(
                                    out=oS[:, 2 * hp:2 * hp + 2, rl * 64:(rl + 1) * 64],
                                    in0=PS[obk][:, 0:128].rearrange("q (a b) -> q a b", a=2),
                                    in1=rc[i2][:].rearrange("q (a b) -> q a b", a=2), op=ALU.mult),
                                    reads=[("ps", obk), ("rc", i2)], writes=["oS"])
                            if pend[0] is not None:
                                pend[0]()
                            pend[0] = tail
                    if pend[0] is not None:
                        pend[0]()
                        pend[0] = None
                    S.dma("gpsimd", lambda e, t=t: e.dma_start(
                        out=oT0[t, :, :, :].rearrange("h d t -> d h t"), in_=oS[:, :, :]),
                        "st_oS", reads=["oS"], writes=[("dram", "oT0")])
                    conv_some((n_moe_conv + 2 * NT0 - 1) // (2 * NT0))
            S.barrier()
            S.run()

        GC = 4
        DC = _chunk_div(NFC, 16)

        def swiglu_tile(hT, hkey, act, sg, names, epilogue, cnt0):
            cnt = cnt0
            if callable(names):
                loader = names
            else:
                ng, nu, nd = names

                def loader(kind, *a):
                    if kind == "g":
                        return wload(ng, 0, KD, a[0] * GC * 128, GC * 128)
                    if kind == "u":
                        return wload(nu, 0, KD, a[0] * GC * 128, GC * 128)
                    return wload(nd, a[1] * DC, DC, a[0] * 512, 512)
            for c in range(NFC // GC):
                wkg, wg = loader("g", c)
                gb = []
                for j in range(GC):
                    pb = cnt % 4
                    cnt += 1
                    gb.append(pb)
                    for k in range(KD):
                        S.op("tensor", lambda e, k=k, j=j, pb=pb, wg=wg: e.matmul(
                            PS[pb][:, :], lhsT=wg[:, k, j * 128:(j + 1) * 128], rhs=hT[:, k, :],
                            start=(k == 0), stop=(k == KD - 1)),
                            reads=[wkg, (hkey, k)], writes=[("ps", pb)])
                    S.op("scalar", lambda e, j=j, pb=pb: e.activation(out=sg[j][:], in_=PS[pb][:, :], func=AF.Silu),
                         reads=[("ps", pb)], writes=[("sg", j)])
                wku, wu = loader("u", c)
                for j in range(GC):
                    pb = 4 + cnt % 4
                    cnt += 1
                    fc = c * GC + j
                    for k in range(KD):
                        S.op("tensor", lambda e, k=k, j=j, pb=pb, wu=wu: e.matmul(
                            PS[pb][:, :], lhsT=wu[:, k, j * 128:(j + 1) * 128], rhs=hT[:, k, :],
                            start=(k == 0), stop=(k == KD - 1)),
                            reads=[wku, (hkey, k)], writes=[("ps", pb)])
                    S.op("vector", lambda e, j=j, pb=pb, fc=fc: e.tensor_tensor(
                        out=act[:, fc, :], in0=PS[pb][:, :], in1=sg[j][:], op=ALU.mult),
                        reads=[("ps", pb), ("sg", j)], writes=[("act", fc)])
            for cg in range(D // 512):
                base = (cg % 2) * 4
                for dc in range(NFC // DC):
                    wkd, wd = loader("d", cg, dc)
                    for f in range(DC):
                        fc = dc * DC + f
                        for b in range(4):
                            S.op("tensor", lambda e, f=f, fc=fc, b=b, base=base, wd=wd: e.matmul(
                                PS[base + b][:, :], lhsT=act[:, fc, b * 128:(b + 1) * 128], rhs=wd[:, f, :],
                                start=(fc == 0), stop=(fc == NFC - 1)),
                                reads=[wkd, ("act", fc)], writes=[("ps", base + b)])
                for b in range(4):
                    epilogue(cg, b, base + b)
            return cnt

        def wo_tile(oT, okey, wname, xt, xkey):
            for cg in range(D // 512):
                base = (cg % 2) * 4
                wk, wv = wload(wname, 0, KH, cg * 512, 512)
                for b in range(4):
                    for k in range(KH):
                        S.op("tensor", lambda e, k=k, b=b, base=base, wv=wv: e.matmul(
                            PS[base + b][:, :], lhsT=oT[:, k, b * 128:(b + 1) * 128], rhs=wv[:, k, :],
                            start=(k == 0), stop=(k == KH - 1)),
                            reads=[wk, okey], writes=[("ps", base + b)])
                    S.op("vector", lambda e, b=b, base=base, cg=cg: e.tensor_tensor(
                        out=xt[:, b, cg * 512:(cg + 1) * 512], in0=PS[base + b][:, :],
                        in1=xt[:, b, cg * 512:(cg + 1) * 512], op=ALU.add),
                        reads=[("ps", base + b), xkey], writes=[xkey])

        if cfg.get("stop", 99) >= 3:
         with contextlib.ExitStack() as st_:
            alloc_ws(st_, 3)
            xt = st_.enter_context(nc.sbuf_tensor(_u("xt"), [128, 4, D], F32))
            xn = st_.enter_context(nc.sbuf_tensor(_u("xn"), [128, 4, D], BF16))
            junk = st_.enter_context(nc.sbuf_tensor(_u("junk"), [128, D], F32))
            stt = st_.enter_context(nc.sbuf_tensor(_u("stt"), [128, 8], F32))
            hT = st_.enter_context(nc.sbuf_tensor(_u("hT"), [128, KD, 512], BF16))
            oT = st_.enter_context(nc.sbuf_tensor(_u("oT"), [128, H, 512], BF16))
            act = st_.enter_context(nc.sbuf_tensor(_u("act"), [128, NFC, 512], BF16))
            sg = [st_.enter_context(nc.sbuf_tensor(_u("sg%d" % i), [128, 512], BF16)) for i in range(GC)]
            cnt = 0
            for t in range(NT0):
                ld(xt[:], x_all[t * 512:(t + 1) * 512, :].rearrange("(b p) d -> p b d", p=128), "xt")
                ld(oT[:], oT0[t, :, :, :].rearrange("h d t -> d h t"), "oT", reads=[("dram", "oT0")])
                wo_tile(oT, "oT", "nao", xt, "xt")
                norm_tile(xt, "xt", xn, junk, stt, gffn[:, 0, :], hT, "hT", [6, 7])

                def epi(cg, b, bank):
                    S.op("vector", lambda e: e.tensor_tensor(
                        out=xt[:, b, cg * 512:(cg + 1) * 512], in0=PS[bank][:, :],
                        in1=xt[:, b, cg * 512:(cg + 1) * 512], op=ALU.add),
                        reads=[("ps", bank), "xt"], writes=["xt"])
                cnt = swiglu_tile(hT, "hT", act, sg, ("fg", "fu", "fd"), epi, cnt)
                S.dma("gpsimd", lambda e, t=t: e.dma_start(
                    out=x1[t * 512:(t + 1) * 512, :].rearrange("(b p) d -> p b d", p=128), in_=xt[:]),
                    "st_xt", reads=["xt"], writes=[("dram", "x1")])
                conv_some((n_moe_conv + 2 * NT0 - 1) // (2 * NT0))
            conv_some(100000)
            oi = st_.enter_context(nc.sbuf_tensor(_u("oi"), [128, NOWN * 4], I32))
            ld(oi[:], own_idx[:, :], "oi")
            for j in range(NOWN * 4):
                S.dma("gpsimd", lambda e, j=j: e.indirect_dma_start(
                    out=xt[:, j % 4, :], out_offset=None, in_=x1[0:NT0 * 512, :],
                    in_offset=bass.IndirectOffsetOnAxis(ap=oi[:, j:j + 1], axis=0)),
                    "xt", reads=[("dram", "x1"), "oi"], writes=["xt"])
                if j % 4 == 3:
                    tt_ = NT0 + j // 4
                    S.dma("gpsimd", lambda e, tt_=tt_: e.dma_start(
                        out=x1[tt_ * 512:(tt_ + 1) * 512, :].rearrange("(b p) d -> p b d", p=128), in_=xt[:]),
                        "st_xt", reads=["xt"], writes=[("dram", "x1")])
            S.barrier()
            S.run()

        if cfg.get("stop", 99) >= 4:
         with contextlib.ExitStack() as st_:
            alloc_ws(st_, 3)
            xb = [st_.enter_context(nc.sbuf_tensor(_u("xb%d" % i), [128, D], F32)) for i in range(2)]
            xn = st_.enter_context(nc.sbuf_tensor(_u("xn"), [128, 4, D], BF16))
            junk = st_.enter_context(nc.sbuf_tensor(_u("junk"), [128, D], F32))
            stt = st_.enter_context(nc.sbuf_tensor(_u("stt"), [128, 8], F32))
            hT = st_.enter_context(nc.sbuf_tensor(_u("hT"), [128, KD, 512], BF16))
            cT = st_.enter_context(nc.sbuf_tensor(_u("cT"), [128, 8, 512], BF16))
            cn = st_.enter_context(nc.sbuf_tensor(_u("cn"), [128, 1024], BF16))
            s2 = st_.enter_context(nc.sbuf_tensor(_u("s2"), [128, 8], F32))
            latf = st_.enter_context(nc.sbuf_tensor(_u("latf"), [128, 1088], F32))
            kvf = [st_.enter_context(nc.sbuf_tensor(_u("kvf%d" % i), [128, 512], F32)) for i in range(2)]
            rpt = st_.enter_context(nc.sbuf_tensor(_u("rpt"), [128, 4, 64], F32))
            kr = st_.enter_context(nc.sbuf_tensor(_u("kr"), [128, 64], F32))
            kr2 = st_.enter_context(nc.sbuf_tensor(_u("kr2"), [128, 64], F32))
            krb = st_.enter_context(nc.sbuf_tensor(_u("krb"), [128, 64], BF16))
            krT = st_.enter_context(nc.sbuf_tensor(_u("krT"), [64, 512], BF16))
            qf = st_.enter_context(nc.sbuf_tensor(_u("qf"), [128, H, 192], F32))
            qsq = st_.enter_context(nc.sbuf_tensor(_u("qsq"), [128, H, 192], F32))
            qs = st_.enter_context(nc.sbuf_tensor(_u("qs"), [128, 4 * H], F32))
            qb = st_.enter_context(nc.sbuf_tensor(_u("qb"), [128, H, 192], BF16))
            qr1 = st_.enter_context(nc.sbuf_tensor(_u("qr1"), [128, H, 64], F32))
            qr2 = st_.enter_context(nc.sbuf_tensor(_u("qr2"), [128, H, 64], F32))
            tq = st_.enter_context(nc.sbuf_tensor(_u("tq"), [128, H, 32], F32))
            kf = st_.enter_context(nc.sbuf_tensor(_u("kf"), [128, H, 128], F32))
            kb = st_.enter_context(nc.sbuf_tensor(_u("kb"), [128, H, 128], BF16))
            vb = st_.enter_context(nc.sbuf_tensor(_u("vb"), [128, H, 128], BF16))
            qnT = [st_.enter_context(nc.sbuf_tensor(_u("qnT%d" % i), [128, H, 128], BF16)) for i in range(2)]
            qrT = st_.enter_context(nc.sbuf_tensor(_u("qrT"), [64, H, 128], BF16))
            knT = [st_.enter_context(nc.sbuf_tensor(_u("knT%d" % i), [128, H, 128], BF16)) for i in range(2)]
            P4S = cfg.get("p4s", 9)
            NQG = (H * 192) // 384
            NKG = (H * 256) // 512
            for t in range(NT1):
                def xsrc4(b, t=t):
                    r0 = t * 512 + b * 128
                    ld(xb[b % 2][:, :], x1[r0:r0 + 128, :], ("xb", b % 2), reads=[("dram", "x1")])
                    return xb[b % 2][:, :], ("xb", b % 2)
                ld(rpt[:], rope_in[t * 512:(t + 1) * 512, :].rearrange("(b p) c -> p b c", p=128), "rpt")
                norm_tile(xsrc4, None, xn, junk, stt, gmix[:, 1, :], hT, "hT", [6, 7])
                wk0, w0v = wload("dqkv", 0, KD, 0, 512)
                wk1, w1v = wload("dqkv", 0, KD, 512, 512)
                wk2, w2v = wload("dqkv", 0, KD, 1024, 64)
                for b in range(4):
                    for (bank, wk, wv, ncol) in ((0, wk0, w0v, 512), (1, wk1, w1v, 512), (2, wk2, w2v, 64)):
                        for k in range(KD):
                            S.op("tensor", lambda e, k=k, b=b, bank=bank, wv=wv, ncol=ncol: e.matmul(
                                PS[bank][:, 0:ncol], lhsT=hT[:, k, b * 128:(b + 1) * 128], rhs=wv[:, k, :],
                                start=(k == 0), stop=(k == KD - 1)),
                                reads=[wk, ("hT", k)], writes=[("ps", bank)])
                    for li, ncol in ((0, 512), (1, 512), (2, 64)):
                        S.op("scalar", lambda e, li=li, ncol=ncol: e.copy(out=latf[:, li * 512:li * 512 + ncol], in_=PS[li][:, 0:ncol]),
                             reads=[("ps", li)], writes=[("latf", li)])
                    for li, ncol in ((0, 512), (1, 512), (2, 64)):
                        S.op("vector", lambda e, li=li, ncol=ncol: e.tensor_tensor(
                            out=junk[:, 0:ncol], in0=latf[:, li * 512:li * 512 + ncol], in1=latf[:, li * 512:li * 512 + ncol],
                            op=ALU.mult), reads=[("latf", li)], writes=["junk"])
                        S.op("vector", lambda e, li=li, ncol=ncol: e.tensor_reduce(
                            out=s2[:, li:li + 1], in_=junk[:, 0:ncol], axis=AX.X, op=ALU.add),
                            reads=["junk"], writes=[("s2", li)])
                    S.op("scalar", lambda e: e.activation(out=s2[:, 4:6], in_=s2[:, 0:2], func=AF.Sqrt, scale=1.0 / 512, bias=EPS),
                         reads=[("s2", 0), ("s2", 1)], writes=["s2b"])
                    S.op("scalar", lambda e: e.activation(out=s2[:, 6:7], in_=s2[:, 2:3], func=AF.Sqrt, scale=1.0 / 64, bias=EPS),
                         reads=[("s2", 2), "s2b"], writes=["s2b"])
                    S.op("vector", lambda e: e.reciprocal(out=s2[:, 4:7], in_=s2[:, 4:7]), reads=["s2b"], writes=["s2b"])
                    for li in range(2):
                        S.op("scalar", lambda e, li=li: e.activation(
                            out=cn[:, li * 512:(li + 1) * 512], in_=latf[:, li * 512:(li + 1) * 512], func=AF.Copy,
                            scale=s2[:, 4 + li:5 + li]),
                            reads=[("latf", li), "s2b"], writes=[("cn", li)])
                    S.op("vector", lambda e: e.scalar_tensor_tensor(
                        out=kr[:], in0=latf[:, 1024:1088], scalar=s2[:, 6:7], in1=gkr[:], op0=ALU.mult, op1=ALU.mult),
                        reads=[("latf", 2), "s2b", "gkr"], writes=["kr"])
                    S.op("vector", lambda e, b=b: e.tensor_tensor(out=kr2[:, 0:32], in0=kr[:, 0:32], in1=rpt[:, b, 0:32], op=ALU.mult),
                         reads=["kr", "rpt"], writes=["kr2a"])
                    S.op("vector", lambda e, b=b: e.tensor_tensor(out=kr2[:, 32:64], in0=kr[:, 32:64], in1=rpt[:, b, 32:64], op=ALU.mult),
                         reads=["kr", "rpt"], writes=["kr2b"])
                    S.op("vector", lambda e: e.tensor_tensor(out=krb[:, 0:32], in0=kr2[:, 0:32], in1=kr2[:, 32:64], op=ALU.subtract),
                         reads=["kr2a", "kr2b"], writes=["krb0"])
                    S.op("vector", lambda e, b=b: e.tensor_tensor(out=kr2[:, 0:32], in0=kr[:, 0:32], in1=rpt[:, b, 32:64], op=ALU.mult),
                         reads=["kr", "rpt", "krb0"], writes=["kr2a"])
                    S.op("vector", lambda e, b=b: e.tensor_tensor(out=kr2[:, 32:64], in0=kr[:, 32:64], in1=rpt[:, b, 0:32], op=ALU.mult),
                         reads=["kr", "rpt", "krb0"], writes=["kr2b"])
                    S.op("vector", lambda e: e.tensor_tensor(out=krb[:, 32:64], in0=kr2[:, 0:32], in1=kr2[:, 32:64], op=ALU.add),
                         reads=["kr2a", "kr2b"], writes=["krb1"])
                    S.op("tensor", lambda e, b=b: e.transpose(out=psb(3)[0:64, b * 128:(b + 1) * 128], in_=krb[:, :],
                                                             identity=ident_b[:]),
                         reads=["krb0", "krb1", "ident_b"], writes=[("ps", 3)])
                    for li in range(2):
                        for k in range(4):
                            S.op("tensor", lambda e, li=li, k=k: e.transpose(
                                out=psb(4 + li)[:, k * 128:(k + 1) * 128], in_=cn[:, li * 512 + k * 128:li * 512 + (k + 1) * 128],
                                identity=ident_b[:]),
                                reads=[("cn", li), "ident_b"], writes=[("ps", 4 + li)])
                        gofs = 4 + 4 * li
                        S.op("vector", lambda e, li=li, gofs=gofs, b=b: e.tensor_tensor(
                            out=cT[:, 4 * li:4 * li + 4, b * 128:(b + 1) * 128],
                            in0=psb(4 + li)[:, 0:512].rearrange("p (k t) -> p k t", k=4),
                            in1=gsm[:, gofs:gofs + 4].unsqueeze(2).to_broadcast([128, 4, 128]), op=ALU.mult),
                            reads=[("ps", 4 + li), "gsm"], writes=[("cT", li, b)])
                S.op("scalar", lambda e: e.copy(out=krT[:, :], in_=psb(3)[0:64, 0:512]), reads=[("ps", 3)], writes=["krT"])
                S.dma("gpsimd", lambda e, t=t: e.dma_start(out=k1r[t, :, :], in_=krT[:, :]), "st_krT",
                      reads=["krT"], writes=[("dram", "k1r")])
                is_prompt_tile = (NS * TS <= t < NT0)
                is_own_tile = (t >= NT0)
                for b in range(4 if P4S >= 2 else 0):
                    if not is_prompt_tile:
                        for g in range(NQG):
                            wk, wv = wload("uq", 0, 4, g * 384, 384)
                            bank = g % 3
                            for k in range(4):
                                S.op("tensor", lambda e, k=k, b=b, bank=bank, wv=wv: e.matmul(
                                    PS[bank][:, 0:384], lhsT=cT[:, k, b * 128:(b + 1) * 128], rhs=wv[:, k, :],
                                    start=(k == 0), stop=(k == 3)),
                                    reads=[wk, ("cT", 0, b)], writes=[("ps", bank)])
                            S.op("scalar", lambda e, g=g, bank=bank: e.copy(
                                out=qf[:, 2 * g:2 * g + 2, :].rearrange("p a c -> p (a c)"), in_=PS[bank][:, 0:384]),
                                reads=[("ps", bank)], writes=[("qf", g)])
                        if P4S < 2.2:
                            continue
                        qfk = [("qf", g) for g in range(NQG)]
                        S.op("vector", lambda e: e.tensor_tensor(out=qsq[:], in0=qf[:], in1=qf[:], op=ALU.mult),
                             reads=qfk, writes=["qsq"])
                        S.op("vector", lambda e: e.tensor_reduce(out=qs[:, 0:H], in_=qsq[:, :, 0:128], axis=AX.X, op=ALU.add),
                             reads=["qsq"], writes=["qs0"])
                        S.op("vector", lambda e: e.tensor_reduce(out=qs[:, H:2 * H], in_=qsq[:, :, 128:192], axis=AX.X, op=ALU.add),
                             reads=["qsq"], writes=["qs1"])
                        S.op("scalar", lambda e: e.activation(out=qs[:, 2 * H:3 * H], in_=qs[:, 0:H], func=AF.Sqrt, scale=1.0 / 128, bias=EPS),
                             reads=["qs0"], writes=["qs2"])
                        S.op("scalar", lambda e: e.activation(out=qs[:, 3 * H:4 * H], in_=qs[:, H:2 * H], func=AF.Sqrt, scale=1.0 / 64, bias=EPS),
                             reads=["qs1", "qs2"], writes=["qs2"])
                        S.op("vector", lambda e: e.reciprocal(out=qs[:, 2 * H:4 * H], in_=qs[:, 2 * H:4 * H]), reads=["qs2"], writes=["qs2"])
                        if P4S < 2.4:
                            continue
                        S.op("vector", lambda e: e.tensor_tensor(
                            out=qb[:, :, 0:128], in0=qf[:, :, 0:128],
                            in1=qs[:, 2 * H:3 * H].unsqueeze(2).to_broadcast([128, H, 128]), op=ALU.mult),
                            reads=qfk + ["qs2"], writes=["qbn"])
                        S.op("vector", lambda e: e.tensor_tensor(
                            out=qr1[:], in0=qf[:, :, 128:192],
                            in1=qs[:, 3 * H:4 * H].unsqueeze(2).to_broadcast([128, H, 64]), op=ALU.mult),
                            reads=qfk + ["qs2"], writes=["qr1"])
                        S.op("vector", lambda e: e.tensor_tensor(
                            out=qr1[:], in0=qr1[:], in1=gqr[:, :].unsqueeze(1).to_broadcast([128, H, 64]), op=ALU.mult),
                            reads=["qr1", "gqr"], writes=["qr1"])
                        cosb = rpt[:, b, 0:32].unsqueeze(1).to_broadcast([128, H, 32])
                        sinb = rpt[:, b, 32:64].unsqueeze(1).to_broadcast([128, H, 32])
                        S.op("vector", lambda e, cosb=cosb: e.tensor_tensor(out=qr2[:, :, 0:32], in0=qr1[:, :, 0:32], in1=cosb, op=ALU.mult),
                             reads=["qr1", "rpt"], writes=["qr2a"])
                        S.op("vector", lambda e, sinb=sinb: e.tensor_tensor(out=tq[:], in0=qr1[:, :, 32:64], in1=sinb, op=ALU.mult),
                             reads=["qr1", "rpt"], writes=["tq"])
                        S.op("vector", lambda e: e.tensor_tensor(out=qb[:, :, 128:160], in0=qr2[:, :, 0:32], in1=tq[:], op=ALU.subtract),
                             reads=["qr2a", "tq"], writes=["qbr0"])
                        S.op("vector", lambda e, sinb=sinb: e.tensor_tensor(out=qr2[:, :, 32:64], in0=qr1[:, :, 0:32], in1=sinb, op=ALU.mult),
                             reads=["qr1", "rpt"], writes=["qr2b"])
                        S.op("vector", lambda e, cosb=cosb: e.tensor_tensor(out=tq[:], in0=qr1[:, :, 32:64], in1=cosb, op=ALU.mult),
                             reads=["qr1", "rpt", "qbr0"], writes=["tq"])
                        S.op("vector", lambda e: e.tensor_tensor(out=qb[:, :, 160:192], in0=qr2[:, :, 32:64], in1=tq[:], op=ALU.add),
                             reads=["qr2b", "tq"], writes=["qbr1"])
                        if P4S < 2.6:
                            continue
                        for h4 in range(H // 4):
                            bank = 4 + h4 % 2
                            for hh in range(4):
                                h = h4 * 4 + hh
                                S.op("tensor", lambda e, h=h, hh=hh, bank=bank: e.transpose(
                                    out=psb(bank)[:, hh * 128:(hh + 1) * 128], in_=qb[:, h, 0:128], identity=ident_b[:]),
                                    reads=["qbn", "ident_b"], writes=[("ps", bank)])
                                if P4S >= 2.8: S.op("tensor", lambda e, h=h, hh=hh, bank=bank: e.transpose(
                                    out=psb(bank)[0:64, 512 + hh * 128:512 + (hh + 1) * 128], in_=qb[:, h, 128:192],
                                    identity=ident_b[:]),
                                    reads=["qbr0", "qbr1", "ident_b"], writes=[("ps", bank)])
                            S.op("scalar", lambda e, h4=h4, bank=bank, b=b: e.activation(
                                out=qnT[b % 2][:, h4 * 4:h4 * 4 + 4, :],
                                in_=psb(bank)[:, 0:512].rearrange("p (a t) -> p a t", a=4), func=AF.Copy, scale=gsm[:, 2:3]),
                                reads=[("ps", bank), "gsm"], writes=[("qnT", b % 2)])
                            if P4S >= 2.9: S.op("scalar", lambda e, h4=h4, bank=bank, b=b: e.copy(
                                out=qrT[:, h4 * 4:h4 * 4 + 4, :],
                                in_=psb(bank)[0:64, 512:1024].rearrange("p (a t) -> p a t", a=4)),
                                reads=[("ps", bank)], writes=["qrT"])
                        if P4S >= 4:
                            S.dma("gpsimd", lambda e, t=t, b=b: e.dma_start(
                                out=q1n[t, :, :, b * 128:(b + 1) * 128].rearrange("h d t -> d h t"), in_=qnT[b % 2][:]),
                                ("st_qnT", b % 2), reads=[("qnT", b % 2)], writes=[("dram", "q1n")])
                            S.dma("gpsimd", lambda e, t=t, b=b: e.dma_start(
                                out=q1r[t, :, :, b * 128:(b + 1) * 128].rearrange("h d t -> d h t"), in_=qrT[:]),
                                "st_qrT", reads=["qrT"], writes=[("dram", "q1r")])
                    if P4S < 3:
                        continue
                    if not is_own_tile:
                        for g in range(NKG):
                            wk, wv = wload("ukv", 0, 4, g * 512, 512)
                            bank = g % 3
                            for k in range(4):
                                S.op("tensor", lambda e, k=k, b=b, bank=bank, wv=wv: e.matmul(
                                    PS[bank][:, :], lhsT=cT[:, 4 + k, b * 128:(b + 1) * 128], rhs=wv[:, k, :],
                                    start=(k == 0), stop=(k == 3)),
                                    reads=[wk, ("cT", 1, b)], writes=[("ps", bank)])
                            pv = lambda bank=bank: PS[bank][:, :].rearrange("p (a c) -> p a c", a=2)
                            g2 = g % 2
                            S.op("scalar", lambda e, g2=g2, bank=bank: e.copy(out=kvf[g2][:], in_=PS[bank][:, :]),
                                 reads=[("ps", bank)], writes=[("kvf", g2)])
                            kvv = kvf[g2][:].rearrange("p (a c) -> p a c", a=2)
                            S.op("vector", lambda e, g=g, kvv=kvv: e.tensor_copy(out=kf[:, 2 * g:2 * g + 2, :], in_=kvv[:, :, 0:128]),
                                 reads=[("kvf", g2)], writes=[("kf", g)])
                            S.op("vector", lambda e, g=g, kvv=kvv: e.tensor_copy(out=vb[:, 2 * g:2 * g + 2, :], in_=kvv[:, :, 128:256]),
                                 reads=[("kvf", g2)], writes=[("vb", g)])
                        kfk = [("kf", g) for g in range(NKG)]
                        S.op("vector", lambda e: e.tensor_tensor(out=qsq[:, :, 0:128], in0=kf[:], in1=kf[:], op=ALU.mult),
                             reads=kfk, writes=["qsq"])
                        S.op("vector", lambda e: e.tensor_reduce(out=qs[:, 0:H], in_=qsq[:, :, 0:128], axis=AX.X, op=ALU.add),
                             reads=["qsq"], writes=["qs0"])
                        S.op("scalar", lambda e: e.activation(out=qs[:, 2 * H:3 * H], in_=qs[:, 0:H], func=AF.Sqrt, scale=1.0 / 128, bias=EPS),
                             reads=["qs0"], writes=["qs2"])
                        S.op("vector", lambda e: e.reciprocal(out=qs[:, 2 * H:3 * H], in_=qs[:, 2 * H:3 * H]), reads=["qs2"], writes=["qs2"])
                        S.op("vector", lambda e: e.tensor_tensor(
                            out=kb[:], in0=kf[:], in1=qs[:, 2 * H:3 * H].unsqueeze(2).to_broadcast([128, H, 128]), op=ALU.mult),
                            reads=kfk + ["qs2"], writes=["kb"])
                        for h4 in range(H // 4):
                            bank = 6 + h4 % 2
                            for hh in range(4):
                                h = h4 * 4 + hh
                                S.op("tensor", lambda e, h=h, hh=hh, bank=bank: e.transpose(
                                    out=psb(bank)[:, hh * 128:(hh + 1) * 128], in_=kb[:, h, :], identity=ident_b[:]),
                                    reads=["kb", "ident_b"], writes=[("ps", bank)])
                            S.op("scalar", lambda e, h4=h4, bank=bank, b=b: e.activation(
                                out=knT[b % 2][:, h4 * 4:h4 * 4 + 4, :],
                                in_=psb(bank)[:, 0:512].rearrange("p (a t) -> p a t", a=4), func=AF.Copy, scale=gsm[:, 3:4]),
                                reads=[("ps", bank), "gsm"], writes=[("knT", b % 2)])
                        if P4S >= 4:
                            S.dma("gpsimd", lambda e, t=t, b=b: e.dma_start(
                                out=k1n[t, :, :, b * 128:(b + 1) * 128].rearrange("h d t -> d h t"), in_=knT[b % 2][:]),
                                ("st_knT", b % 2), reads=[("knT", b % 2)], writes=[("dram", "k1n")])
                        r0 = t * 512 + b * 128
                        S.dma("gpsimd", lambda e, r0=r0: e.dma_start(
                            out=v1[r0:r0 + 128, :].rearrange("p (h c) -> p h c", h=H), in_=vb[:]),
                            "st_vb", reads=[("vb", g) for g in range(NKG)], writes=[("dram", "v1")])
            S.barrier()
            S.run()

        if cfg.get("stop", 99) >= 5:
         with contextlib.ExitStack() as st_:
            TK = max(RS * 64, RP * 64)
            krS = st_.enter_context(nc.sbuf_tensor(_u("krS"), [64, TK], BF16))
            knS = [st_.enter_context(nc.sbuf_tensor(_u("knS%d" % i), [128, TK], BF16)) for i in range(2)]
            vS = [st_.enter_context(nc.sbuf_tensor(_u("vS%d" % i), [128, TK // 128, 128], BF16)) for i in range(2)]
            qnS = [st_.enter_context(nc.sbuf_tensor(_u("qnS%d" % i), [128, 512], BF16)) for i in range(2)]
            qrS = [st_.enter_context(nc.sbuf_tensor(_u("qrS%d" % i), [64, 512], BF16)) for i in range(2)]
            pT = [st_.enter_context(nc.sbuf_tensor(_u("pT%d" % i), [128, 512], BF16)) for i in range(3)]
            rcp = [st_.enter_context(nc.sbuf_tensor(_u("rcp%d" % i), [128, 512], F32)) for i in range(2)]
            oo = [st_.enter_context(nc.sbuf_tensor(_u("oo%d" % i), [128, 512], BF16)) for i in range(2)]
            scale1 = 192 ** -0.5
            seqs1 = []
            for s in range(NS):
                seqs1.append(([s * TS + i for i in range(TS)], [s * TS + i for i in range(TS)],
                              [s * TS + i for i in range(TS)]))
            seqs1.append(([NT0 + i for i in range(NOWN)], [NS * TS + i for i in range(TP)],
                          [NS * TS + i for i in range(NOWN)]))
            hc = 0
            qc = 0
            cc = 0
            for (qtiles, kvtiles, otiles) in seqs1:
                T = len(kvtiles) * 512
                NC = T // 128
                for i, kt in enumerate(kvtiles):
                    S.dma("sync", lambda e, i=i, kt=kt: e.dma_start(out=krS[:, i * 512:(i + 1) * 512], in_=k1r[kt, :, :]),
                          "krS", reads=[("dram", "k1r")], writes=["krS"])
                for h in range(H):
                    hs = hc % 2
                    hc += 1
                    for i, kt in enumerate(kvtiles):
                        S.dma("sync", lambda e, i=i, kt=kt, h=h, hs=hs: e.dma_start(
                            out=knS[hs][:, i * 512:(i + 1) * 512], in_=k1n[kt, h, :, :]),
                            ("knS", hs), reads=[("dram", "k1n")], writes=[("knS", hs)])
                    tok0 = kvtiles[0] * 512
                    S.dma("sync", lambda e, h=h, hs=hs, tok0=tok0, T=T, NC=NC: e.dma_start(
                        out=vS[hs][:, 0:NC, :],
                        in_=v1[tok0:tok0 + T, h * 128:(h + 1) * 128].rearrange("(b p) c -> p b c", p=128)),
                        ("vS", hs), reads=[("dram", "v1")], writes=[("vS", hs)])
                    for qi, qt in enumerate(qtiles):
                        q2 = qc % 2
                        qc += 1
                        S.dma("sync", lambda e, qt=qt, h=h, q2=q2: e.dma_start(out=qnS[q2][:], in_=q1n[qt, h, :, :]),
                              ("qnS", q2), reads=[("dram", "q1n")], writes=[("qnS", q2)])
                        S.dma("sync", lambda e, qt=qt, h=h, q2=q2: e.dma_start(out=qrS[q2][:], in_=q1r[qt, h, :, :]),
                              ("qrS", q2), reads=[("dram", "q1r")], writes=[("qrS", q2)])
                        obk = 4 + q2
                        dbk = 6 + q2

                        def pv_step(c, p3, obk=obk, dbk=dbk, hs=hs, NC=NC):
                            S.op("tensor", lambda e: e.matmul(PS[obk][:, :], lhsT=vS[hs][:, c, :], rhs=pT[p3][:],
                                                              start=(c == 0), stop=(c == NC - 1)),
                                 reads=[("vS", hs), ("pT", p3)], writes=[("ps", obk)])
                            S.op("tensor", lambda e: e.matmul(PS[dbk][:, :], lhsT=ones_b[:], rhs=pT[p3][:],
                                                              start=(c == 0), stop=(c == NC - 1)),
                                 reads=["ones_b", ("pT", p3)], writes=[("ps", dbk)])
                        prev = None
                        for c in range(NC):
                            sbk = cc % 4
                            p3 = cc % 3
                            cc += 1
                            S.op("tensor", lambda e, c=c, sbk=sbk, hs=hs, q2=q2: e.matmul(
                                PS[sbk][:, :], lhsT=knS[hs][:, c * 128:(c + 1) * 128], rhs=qnS[q2][:], start=True, stop=False),
                                reads=[("knS", hs), ("qnS", q2)], writes=[("ps", sbk)])
                            S.op("tensor", lambda e, c=c, sbk=sbk, q2=q2: e.matmul(
                                PS[sbk][:, :], lhsT=krS[:, c * 128:(c + 1) * 128], rhs=qrS[q2][:], start=False, stop=True),
                                reads=["krS", ("qrS", q2)], writes=[("ps", sbk)])
                            S.op("scalar", lambda e, sbk=sbk, p3=p3: e.activation(out=pT[p3][:], in_=PS[sbk][:, :],
                                                                                  func=AF.Exp, scale=scale1),
                                 reads=[("ps", sbk)], writes=[("pT", p3)])
                            if prev is not None:
                                pv_step(*prev)
                            prev = (c, p3)
                        pv_step(*prev)
                        S.op("vector", lambda e, q2=q2, dbk=dbk: e.reciprocal(out=rcp[q2][:], in_=PS[dbk][:, :]),
                             reads=[("ps", dbk)], writes=[("rcp", q2)])
                        S.op("vector", lambda e, q2=q2, obk=obk: e.tensor_tensor(out=oo[q2][:], in0=PS[obk][:, :],
                                                                                 in1=rcp[q2][:], op=ALU.mult),
                             reads=[("ps", obk), ("rcp", q2)], writes=[("oo", q2)])
                        ot = otiles[qi]
                        S.dma("gpsimd", lambda e, ot=ot, h=h, q2=q2: e.dma_start(out=oT1[ot, h, :, :], in_=oo[q2][:]),
                              ("st_oo", q2), reads=[("oo", q2)], writes=[("dram", "oT1")])
            S.barrier()
            S.run()

        NB = 4 * NL1
        NST = 2 * NL1 + E - 1
        x2 = dscr("x2", [NL1 * 512, D], F32)
        hn = dscr("hn", [NL1 * 512, D], BF16)
        hs = dscr("hs", [NST * 512, D], BF16)
        ysl = dscr("ysl", [NST * 512, D], F32)
        MK = gst.enter_context(nc.sbuf_tensor(_u("MK"), [128, 2, NB, E], F32))
        GG = gst.enter_context(nc.sbuf_tensor(_u("GG"), [128, 2, NB], F32))
        SLI = gst.enter_context(nc.sbuf_tensor(_u("SLI"), [128, 2, NB], I32))
        IXG = gst.enter_context(nc.sbuf_tensor(_u("IXG"), [128, NST, NCH], I32))
        IXD = gst.enter_context(nc.sbuf_tensor(_u("IXD"), [128, NST, NCG * NDC], I32))
        if cfg.get("stop", 99) >= 6:
         with contextlib.ExitStack() as st_:
            alloc_ws(st_, 3)
            xt = st_.enter_context(nc.sbuf_tensor(_u("xt"), [128, 4, D], F32))
            xn = st_.enter_context(nc.sbuf_tensor(_u("xn"), [128, 4, D], BF16))
            junk = st_.enter_context(nc.sbuf_tensor(_u("junk"), [128, D], F32))
            stt = st_.enter_context(nc.sbuf_tensor(_u("stt"), [128, 8], F32))
            oT = st_.enter_context(nc.sbuf_tensor(_u("oT"), [128, H, 512], BF16))
            wr = st_.enter_context(nc.sbuf_tensor(_u("wr"), [128, KD, E], F32))
            h32 = st_.enter_context(nc.sbuf_tensor(_u("h32"), [128, KD, 128], F32))
            lg = st_.enter_context(nc.sbuf_tensor(_u("lg"), [128, 4, E], F32))
            m1 = st_.enter_context(nc.sbuf_tensor(_u("m1"), [128, 8], F32))
            lg2 = st_.enter_context(nc.sbuf_tensor(_u("lg2"), [128, E], F32))
            ld(wr[:], w_router[:, :, :], "wr")
            for ti, t in enumerate(l1_tiles):
                ld(xt[:], x1[t * 512:(t + 1) * 512, :].rearrange("(b p) d -> p b d", p=128), "xt", reads=[("dram", "x1")])
                ld(oT[:], oT1[ti, :, :, :].rearrange("h d t -> d h t"), "oT", reads=[("dram", "oT1")])
                wo_tile(oT, "oT", "mo", xt, "xt")
                S.dma("gpsimd", lambda e, ti=ti: e.dma_start(
                    out=x2[ti * 512:(ti + 1) * 512, :].rearrange("(b p) d -> p b d", p=128), in_=xt[:]),
                    "st_xt", reads=["xt"], writes=[("dram", "x2")])
                norm_tile(xt, "xt", xn, junk, stt, gffn[:, 1, :], None, "hT", [6, 7])
                S.dma("gpsimd", lambda e, ti=ti: e.dma_start(
                    out=hn[ti * 512:(ti + 1) * 512, :].rearrange("(b p) d -> p b d", p=128), in_=xn[:]),
                    "st_xn", reads=[("xn", b) for b in range(4)], writes=[("dram", "hn")])
                for b in range(4):
                    gb = ti * 4 + b
                    S.op("scalar", lambda e, b=b: e.activation(out=junk[:], in_=xt[:, b, :], func=AF.Copy,
                                                               scale=stt[:, 4 + b:5 + b]),
                         reads=["xt", "st2"], writes=["junk"])
                    for k4 in range(KD // 4):
                        bank = k4 % 2
                        for kk in range(4):
                            k = k4 * 4 + kk
                            S.op("tensor", lambda e, k=k, kk=kk, bank=bank: e.transpose(
                                out=PS[bank][:, kk * 128:(kk + 1) * 128], in_=junk[:, k * 128:(k + 1) * 128],
                                identity=ident_f[:]),
                                reads=["junk", "ident_f"], writes=[("ps", bank)])
                        S.op("vector", lambda e, k4=k4, bank=bank: e.tensor_tensor(
                            out=h32[:, k4 * 4:k4 * 4 + 4, :], in0=PS[bank][:, :].rearrange("p (a t) -> p a t", a=4),
                            in1=gffn[:, 1, k4 * 4:k4 * 4 + 4].unsqueeze(2).to_broadcast([128, 4, 128]), op=ALU.mult),
                            reads=[("ps", bank), "gffn"], writes=[("h32", k4)])
                    for k in range(KD):
                        S.op("tensor", lambda e, k=k: e.matmul(PS[2][:, 0:E], lhsT=h32[:, k, :], rhs=wr[:, k, :],
                                                               start=(k == 0), stop=(k == KD - 1)),
                             reads=[("h32", k // 4), "wr"], writes=[("ps", 2)])
                    S.op("vector", lambda e, b=b: e.tensor_scalar(out=lg[:, b, :], in0=PS[2][:, 0:E], scalar1=1.0, scalar2=None,
                                                                  op0=ALU.mult), reads=[("ps", 2)], writes=[("lg", b)])
                    S.op("vector", lambda e, b=b: e.tensor_reduce(out=m1[:, 0:1], in_=lg[:, b, :], axis=AX.X, op=ALU.max),
                         reads=[("lg", b)], writes=["m1a"])
                    S.op("vector", lambda e, b=b, gb=gb: e.tensor_scalar(out=MK[:, 0, gb, :], in0=lg[:, b, :], scalar1=m1[:, 0:1],
                                                                         scalar2=None, op0=ALU.is_equal),
                         reads=[("lg", b), "m1a"], writes=["MK"])
                    S.op("vector", lambda e, b=b, gb=gb: e.scalar_tensor_tensor(
                        out=lg2[:], in0=MK[:, 0, gb, :], scalar=-1e30, in1=lg[:, b, :], op0=ALU.mult, op1=ALU.add),
                        reads=["MK", ("lg", b)], writes=["lg2"])
                    S.op("vector", lambda e: e.tensor_reduce(out=m1[:, 1:2], in_=lg2[:], axis=AX.X, op=ALU.max),
                         reads=["lg2"], writes=["m1b"])
                    S.op("vector", lambda e, gb=gb: e.tensor_scalar(out=MK[:, 1, gb, :], in0=lg2[:], scalar1=m1[:, 1:2], scalar2=None,
                                                                    op0=ALU.is_equal), reads=["lg2", "m1b"], writes=["MK"])
                    S.op("vector", lambda e: e.tensor_tensor(out=m1[:, 2:3], in0=m1[:, 1:2], in1=m1[:, 0:1], op=ALU.subtract),
                         reads=["m1a", "m1b"], writes=["m1c"])
                    S.op("scalar", lambda e: e.activation(out=m1[:, 3:4], in_=m1[:, 2:3], func=AF.Exp),
                         reads=["m1c"], writes=["m1d"])
                    S.op("vector", lambda e: e.tensor_scalar(out=m1[:, 4:5], in0=m1[:, 3:4], scalar1=1.0, scalar2=None,
                                                             op0=ALU.add), reads=["m1d"], writes=["m1e"])
                    S.op("vector", lambda e, gb=gb: e.reciprocal(out=GG[:, 0, gb:gb + 1], in_=m1[:, 4:5]),
                         reads=["m1e"], writes=["GG"])
                    S.op("vector", lambda e, gb=gb: e.tensor_tensor(out=GG[:, 1, gb:gb + 1], in0=m1[:, 3:4], in1=GG[:, 0, gb:gb + 1],
                                                                    op=ALU.mult), reads=["m1d", "GG"], writes=["GG"])
            S.barrier()
            S.run()

        if cfg.get("stop", 99) >= 6:
         with contextlib.ExitStack() as st_:
            tri_f = st_.enter_context(nc.sbuf_tensor(_u("tri_f"), [128, 128], F32))
            tri_b = st_.enter_context(nc.sbuf_tensor(_u("tri_b"), [128, 128], BF16))
            cst = st_.enter_context(nc.sbuf_tensor(_u("cst"), [128, 64], F32))
            Mb = st_.enter_context(nc.sbuf_tensor(_u("Mb"), [128, NB, E], BF16))
            wi = st_.enter_context(nc.sbuf_tensor(_u("wi"), [128, NB, E], F32))
            tot = st_.enter_context(nc.sbuf_tensor(_u("tot"), [128, NB, E], F32))
            off = st_.enter_context(nc.sbuf_tensor(_u("off"), [128, NB, E], F32))
            sm = st_.enter_context(nc.sbuf_tensor(_u("sm"), [128, 8, E], F32))
            cmpA = st_.enter_context(nc.sbuf_tensor(_u("cmpA"), [128, E, NL1], F32))
            cmpB = st_.enter_context(nc.sbuf_tensor(_u("cmpB"), [128, NST, E], F32))
            prod = st_.enter_context(nc.sbuf_tensor(_u("prod"), [128, NB, E], F32))
            slf = st_.enter_context(nc.sbuf_tensor(_u("slf"), [128, 2, NB], F32))
            ej = st_.enter_context(nc.sbuf_tensor(_u("ej"), [128, NST], F32))
            ixf = st_.enter_context(nc.sbuf_tensor(_u("ixf"), [128, NST, max(NCH, NCG * NDC)], F32))
            ld(tri_f[:], tri_in[:, :], "tri_f")
            ld(cst[:], cst_in[:, :], "cst")
            S.op("vector", lambda e: e.tensor_copy(out=tri_b[:], in_=tri_f[:]), reads=["tri_f"], writes=["tri_b"])
            S.op("vector", lambda e: e.tensor_tensor(out=prod[:], in0=MK[:, 0, :, :], in1=MK[:, 1, :, :], op=ALU.add),
                 reads=["MK"], writes=["prod"])
            S.op("vector", lambda e: e.tensor_copy(out=Mb[:], in_=prod[:]), reads=["prod"], writes=["Mb"])
            mbf = Mb[:].rearrange("p a b -> p (a b)")
            S.op("tensor", lambda e: e.matmul(PS[0][:, 0:NB * E], lhsT=tri_b[:], rhs=mbf, start=True, stop=True),
                 reads=["tri_b", "Mb"], writes=[("ps", 0)])
            S.op("tensor", lambda e: e.matmul(PS[1][:, 0:NB * E], lhsT=ones_b[:], rhs=mbf, start=True, stop=True),
                 reads=["ones_b", "Mb"], writes=[("ps", 1)])
            S.op("scalar", lambda e: e.copy(out=wi[:].rearrange("p a b -> p (a b)"), in_=PS[0][:, 0:NB * E]),
                 reads=[("ps", 0)], writes=["wi"])
            S.op("scalar", lambda e: e.copy(out=tot[:].rearrange("p a b -> p (a b)"), in_=PS[1][:, 0:NB * E]),
                 reads=[("ps", 1)], writes=["tot"])
            S.op("vector", lambda e: e.memset(off[:, 0, :], 0.0), writes=["off"])
            for blk in range(1, NB):
                S.op("vector", lambda e, blk=blk: e.tensor_tensor(out=off[:, blk, :], in0=off[:, blk - 1, :], in1=tot[:, blk - 1, :],
                                                                  op=ALU.add), reads=["off", "tot"], writes=["off"])
            S.op("vector", lambda e: e.tensor_tensor(out=sm[:, 0, :], in0=off[:, NB - 1, :], in1=tot[:, NB - 1, :], op=ALU.add),
                 reads=["off", "tot"], writes=["sm0"])
            S.op("vector", lambda e: e.tensor_tensor(
                out=cmpA[:], in0=sm[:, 0, :].unsqueeze(2).to_broadcast([128, E, NL1]),
                in1=cst[:, 1:1 + NL1].unsqueeze(1).to_broadcast([128, E, NL1]), op=ALU.is_gt),
                reads=["sm0", "cst"], writes=["cmpA"])
            S.op("vector", lambda e: e.tensor_reduce(out=sm[:, 1, :], in_=cmpA[:], axis=AX.X, op=ALU.add),
                 reads=["cmpA"], writes=["sm1"])
            S.op("vector", lambda e: e.memset(sm[:, 2, 0:1], 0.0), reads=["sm1"], writes=["sm2"])
            for e_ in range(1, E):
                S.op("vector", lambda e, e_=e_: e.tensor_tensor(out=sm[:, 2, e_:e_ + 1], in0=sm[:, 2, e_ - 1:e_],
                                                                in1=sm[:, 1, e_ - 1:e_], op=ALU.add),
                     reads=["sm1", "sm2"], writes=["sm2"])
            S.op("vector", lambda e: e.tensor_tensor(out=sm[:, 3, :], in0=sm[:, 2, :], in1=sm[:, 1, :], op=ALU.add),
                 reads=["sm1", "sm2"], writes=["sm3"])
            S.op("vector", lambda e: e.tensor_scalar(out=sm[:, 4, :], in0=sm[:, 2, :], scalar1=512.0, scalar2=None, op0=ALU.mult),
                 reads=["sm2"], writes=["sm4"])
            S.op("vector", lambda e: e.tensor_tensor(out=wi[:], in0=wi[:], in1=off[:], op=ALU.add),
                 reads=["wi", "off"], writes=["wi"])
            S.op("vector", lambda e: e.tensor_tensor(out=wi[:], in0=wi[:], in1=sm[:, 4, :].unsqueeze(1).to_broadcast([128, NB, E]),
                                                     op=ALU.add), reads=["wi", "sm4"], writes=["wi"])
            for kk in range(2):
                S.op("vector", lambda e, kk=kk: e.tensor_tensor(out=prod[:], in0=MK[:, kk, :, :], in1=wi[:], op=ALU.mult),
                     reads=["MK", "wi"], writes=["prod"])
                S.op("vector", lambda e, kk=kk: e.tensor_reduce(out=slf[:, kk, :], in_=prod[:], axis=AX.X, op=ALU.add),
                     reads=["prod"], writes=["slf"])
            S.op("vector", lambda e: e.tensor_copy(out=SLI[:], in_=slf[:]), reads=["slf"], writes=["SLI"])
            S.op("vector", lambda e: e.tensor_tensor(
                out=cmpB[:], in0=sm[:, 3, :].unsqueeze(1).to_broadcast([128, NST, E]),
                in1=cst[:, 16:16 + NST].unsqueeze(2).to_broadcast([128, NST, E]), op=ALU.is_le),
                reads=["sm3", "cst"], writes=["cmpB"])
            S.op("vector", lambda e: e.tensor_reduce(out=ej[:], in_=cmpB[:], axis=AX.X, op=ALU.add),
                 reads=["cmpB"], writes=["ej"])
            S.op("vector", lambda e: e.tensor_scalar(out=ej[:], in0=ej[:], scalar1=float(E - 1), scalar2=None, op0=ALU.min),
                 reads=["ej"], writes=["ej"])
            for (IX, nper) in ((IXG, NCH), (IXD, NCG * NDC)):
                S.op("vector", lambda e, nper=nper: e.tensor_scalar(
                    out=ixf[:, :, 0:nper], in0=ej[:].unsqueeze(2).to_broadcast([128, NST, nper]),
                    scalar1=float(nper * 128), scalar2=cst[:, 0:1], op0=ALU.mult, op1=ALU.add),
                    reads=["ej", "cst"], writes=["ixf"])
                S.op("vector", lambda e, nper=nper: e.tensor_tensor(
                    out=ixf[:, :, 0:nper], in0=ixf[:, :, 0:nper],
                    in1=cst[:, 48:48 + nper].unsqueeze(1).to_broadcast([128, NST, nper]), op=ALU.add),
                    reads=["ixf", "cst"], writes=["ixf"])
                S.op("vector", lambda e, IX=IX, nper=nper: e.tensor_copy(out=IX[:], in_=ixf[:, :, 0:nper]),
                     reads=["ixf"], writes=["IX%d" % nper])
            S.barrier()
            S.run()

        if cfg.get("stop", 99) >= 6:
         with contextlib.ExitStack() as st_:
            zt = st_.enter_context(nc.sbuf_tensor(_u("zt"), [128, 4, D], BF16))
            hb = [st_.enter_context(nc.sbuf_tensor(_u("hb%d" % i), [128, D], BF16)) for i in range(2)]
            S.op("vector", lambda e: e.memset(zt[:], 0.0), writes=["zt"])
            for j in range(NST):
                S.dma("sync", lambda e, j=j: e.dma_start(
                    out=hs[j * 512:(j + 1) * 512, :].rearrange("(b p) d -> p b d", p=128), in_=zt[:]),
                    "st_zt", reads=["zt"], writes=[("dram", "hs0")])
            for blk in range(NB):
                i2 = blk % 2
                ld(hb[i2][:], hn[blk * 128:(blk + 1) * 128, :], ("hb", i2), reads=[("dram", "hn")])
                for kk in range(2):
                    S.dma("gpsimd", lambda e, i2=i2, kk=kk, blk=blk: e.indirect_dma_start(
                        out=hs[:, :], out_offset=bass.IndirectOffsetOnAxis(ap=SLI[:, kk, blk:blk + 1], axis=0),
                        in_=hb[i2][:, :], in_offset=None),
                        ("sc", i2), reads=[("hb", i2), "SLI", ("dram", "hs0")], writes=[("dram", "hs")])
            S.barrier()
            S.run()

        if cfg.get("stop", 99) >= 6:
         with contextlib.ExitStack() as st_:
            alloc_ws(st_, 3)
            xn = st_.enter_context(nc.sbuf_tensor(_u("xn"), [128, 4, D], BF16))
            hT = st_.enter_context(nc.sbuf_tensor(_u("hT"), [128, KD, 512], BF16))
            act = st_.enter_context(nc.sbuf_tensor(_u("act"), [128, NFC, 512], BF16))
            sg = [st_.enter_context(nc.sbuf_tensor(_u("sg%d" % i), [128, 512], BF16)) for i in range(GC)]
            yo = st_.enter_context(nc.sbuf_tensor(_u("yo"), [128, 4, D], F32))
            cnt = 0
            for j in range(NST):
                S.dma("sync", lambda e, j=j: e.dma_start(
                    out=xn[:], in_=hs[j * 512:(j + 1) * 512, :].rearrange("(b p) d -> p b d", p=128)),
                    "xn", reads=[("dram", "hs"), ("dram", "hs0")], writes=[("xn", b) for b in range(4)])
                transpose_tile(xn, gffn[:, 1, :], hT, "hT", [6, 7])

                def loader(kind, *a, j=j):
                    s_ = ws_i[0] % len(WS)
                    ws_i[0] += 1
                    if kind in ("g", "u"):
                        src_t, nmw = (war_g, "war_g") if kind == "g" else (war_u, "war_u")
                        ix = IXG[:, j, a[0]:a[0] + 1]
                        n = KD * 512
                        ikey = "IX%d" % NCH
                    else:
                        i_ = a[0] * NDC + a[1]
                        src_t, nmw, ix = war_d, "war_d", IXD[:, j, i_:i_ + 1]
                        n = DCm * 512
                        ikey = "IX%d" % (NCG * NDC)
                    view = WS[s_][:, 0:n].rearrange("p (k c) -> p k c", c=512)
                    S.dma("gpsimd", lambda e, s_=s_, n=n, src_t=src_t, ix=ix: e.indirect_dma_start(
                        out=WS[s_][:, 0:n], out_offset=None, in_=src_t[:, :],
                        in_offset=bass.IndirectOffsetOnAxis(ap=ix, axis=0)),
                        ("w", s_), reads=[("dram", nmw), ikey], writes=[("w", s_)])
                    return ("w", s_), view

                def epi(cg, b, bank):
                    if (cg + b) % 2 == 0:
                        S.op("scalar", lambda e: e.copy(out=yo[:, b, cg * 512:(cg + 1) * 512], in_=PS[bank][:, :]),
                             reads=[("ps", bank)], writes=[("yo", b, cg)])
                    else:
                        S.op("vector", lambda e: e.tensor_scalar(out=yo[:, b, cg * 512:(cg + 1) * 512], in0=PS[bank][:, :],
                                                                 scalar1=1.0, scalar2=None, op0=ALU.mult),
                             reads=[("ps", bank)], writes=[("yo", b, cg)])
                cnt = swiglu_tile(hT, "hT", act, sg, loader, epi, cnt)
                S.dma("sync", lambda e, j=j: e.dma_start(
                    out=ysl[j * 512:(j + 1) * 512, :].rearrange("(b p) d -> p b d", p=128), in_=yo[:]),
                    "st_yo", reads=[("yo", b, cg) for b in range(4) for cg in range(NCG)], writes=[("dram", "ysl")])
            S.barrier()
            S.run()

        if cfg.get("stop", 99) >= 6:
         with contextlib.ExitStack() as st_:
            xb2 = [st_.enter_context(nc.sbuf_tensor(_u("xb2%d" % i), [128, D], F32)) for i in range(2)]
            ya = [st_.enter_context(nc.sbuf_tensor(_u("ya%d" % i), [128, D], F32)) for i in range(2)]
            yb = [st_.enter_context(nc.sbuf_tensor(_u("yb%d" % i), [128, D], F32)) for i in range(2)]
            for blk in range(NB):
                i2 = blk % 2
                ld(xb2[i2][:], x2[blk * 128:(blk + 1) * 128, :], ("xb2", i2), reads=[("dram", "x2")])
                for kk, yy, nm in ((0, ya, "ya"), (1, yb, "yb")):
                    S.dma("gpsimd", lambda e, i2=i2, kk=kk, blk=blk, yy=yy: e.indirect_dma_start(
                        out=yy[i2][:, :], out_offset=None, in_=ysl[:, :],
                        in_offset=bass.IndirectOffsetOnAxis(ap=SLI[:, kk, blk:blk + 1], axis=0)),
                        (nm, i2), reads=[("dram", "ysl"), "SLI"], writes=[(nm, i2)])
                S.op("vector", lambda e, i2=i2, blk=blk: e.scalar_tensor_tensor(
                    out=xb2[i2][:], in0=ya[i2][:], scalar=GG[:, 0, blk:blk + 1], in1=xb2[i2][:], op0=ALU.mult, op1=ALU.add),
                    reads=[("ya", i2), "GG", ("xb2", i2)], writes=[("xb2", i2)])
                S.op("vector", lambda e, i2=i2, blk=blk: e.scalar_tensor_tensor(
                    out=xb2[i2][:], in0=yb[i2][:], scalar=GG[:, 1, blk:blk + 1], in1=xb2[i2][:], op0=ALU.mult, op1=ALU.add),
                    reads=[("yb", i2), "GG", ("xb2", i2)], writes=[("xb2", i2)])
                ti, b = blk // 4, blk % 4
                if ti < NS * TS:
                    dst = ys[ti * 512 + b * 128:ti * 512 + (b + 1) * 128, :]
                else:
                    r0 = (ti - NS * TS) * 512 + b * 128
                    dst = yp[r0:r0 + 128, :]
                S.dma("sync", lambda e, dst=dst, i2=i2: e.dma_start(out=dst, in_=xb2[i2][:]),
                      ("st_xb2", i2), reads=[("xb2", i2)], writes=[("dram", "y")])
            S.barrier()
            S.run()
    return nc


def _rope_table(pos_tokens, grid_w=64, theta=10000.0):
    t = np.asarray(pos_tokens)
    row = (t // grid_w).astype(np.float32)
    col = (t % grid_w).astype(np.float32)
    n_pairs = 16
    inv = (np.float32(theta) ** (-np.arange(n_pairs, dtype=np.float32) / np.float32(n_pairs))).astype(np.float32)
    ang = np.concatenate([row[:, None] * inv, col[:, None] * inv], axis=-1).astype(np.float32)
    return np.concatenate([np.cos(ang), np.sin(ang)], axis=-1).astype(np.float32)


def _cst_table():
    c = np.zeros((128, 64), np.float32)
    c[:, 0] = np.arange(128)
    c[:, 1:16] = 512.0 * np.arange(15)[None, :]
    c[:, 16:48] = np.arange(32)[None, :]
    c[:, 48:64] = 128.0 * np.arange(16)[None, :]
    return c


def make_core_inputs(cfg, inp, core):
    D, H, FF, E = cfg["D"], cfg["H"], cfg["FF"], cfg["E"]
    RS, RP, NS = cfg["RS"], cfg["RP"], cfg["NS"]
    KD = D // 128
    f = lambda a: np.ascontiguousarray(np.asarray(a, dtype=np.float32))
    xs = inp["x_sample"]
    xp = inp["x_prompt"]
    x_all = np.concatenate([f(xs[core * NS + s]) for s in range(NS)] + [f(xp[0])], axis=0)
    n_own_rows = RP // 8 * 64
    NOWN = RP // 64
    base = NS * RS * 64
    own = base + core * n_own_rows + np.arange(n_own_rows)
    own_idx = np.ascontiguousarray(own.reshape(NOWN * 4, 128).T.astype(np.int32))
    pos = np.concatenate([np.arange(RS * 64)] * NS + [np.arange(RP * 64)] + [core * n_own_rows + np.arange(n_own_rows)])
    rope = _rope_table(pos)
    pk = lambda g: np.ascontiguousarray(f(g).reshape(-1, 128).T)
    g_mix = np.ascontiguousarray(np.stack([pk(inp["mix_norm"][l]) for l in range(2)], axis=1))
    g_ffn = np.ascontiguousarray(np.stack([pk(inp["ffn_norm"][l]) for l in range(2)], axis=1))
    rpb = f(inp["na_rpb"][0])
    kc = np.arange(64)[:, None]
    qc = np.arange(64)[None, :]
    dc = np.clip(kc - qc + 15, 0, 30)
    ws = np.clip(qc - 8, 0, 48)
    valid = ((kc >= ws) & (kc < ws + 16)).astype(np.float32)
    rpbg = np.zeros((128, H, 14, 64), np.float32)
    for a in range(2):
        for m in range(14):
            rpbg[a * 64:(a + 1) * 64, :, m, :] = np.transpose(rpb[:, m + a][:, dc], (1, 0, 2))
    namask = np.concatenate([valid, valid], axis=0)
    d = {
        "x_all": x_all, "own_idx": own_idx, "rope": rope, "ident": np.eye(128, dtype=np.float32),
        "g_mix": g_mix, "g_ffn": g_ffn,
        "g_naq": pk(inp["na_q_gain"][0]), "g_nak": pk(inp["na_k_gain"][0]),
        "rpbg": rpbg, "namask": namask,
        "g_ql": pk(inp["mla_q_lora_gain"][0]), "g_kvl": pk(inp["mla_kv_lora_gain"][0]),
        "g_qn": pk(inp["mla_qn_gain"][0]), "g_kn": pk(inp["mla_kn_gain"][0]),
        "g_qr": np.ascontiguousarray(np.broadcast_to(f(inp["mla_qr_gain"][0])[None, :], (128, 64))),
        "g_kr": np.ascontiguousarray(np.broadcast_to(f(inp["mla_kr_gain"][0])[None, :], (128, 64))),
        "w_router": np.ascontiguousarray(f(inp["moe_w_router"][0]).reshape(KD, 128, E).transpose(1, 0, 2)),
        "tri": np.triu(np.ones((128, 128), np.float32), 1), "cst": _cst_table(),
        "na_w_qkv": f(inp["na_w_qkv"][0]), "na_w_o": f(inp["na_w_o"][0]),
        "ffn_w_gate": f(inp["ffn_w_gate"][0]), "ffn_w_up": f(inp["ffn_w_up"][0]), "ffn_w_down": f(inp["ffn_w_down"][0]),
        "mla_w_dqkv": f(inp["mla_w_dqkv"][0]), "mla_w_uq": f(inp["mla_w_uq"][0]), "mla_w_ukv": f(inp["mla_w_ukv"][0]),
        "mla_w_o": f(inp["mla_w_o"][0]),
    }
    for e_ in range(E):
        d["moe_w_gate%d" % e_] = f(inp["moe_w_gate"][0, e_])
        d["moe_w_up%d" % e_] = f(inp["moe_w_up"][0, e_])
        d["moe_w_down%d" % e_] = f(inp["moe_w_down"][0, e_])
    return d


def run_cfg(cfg, inp, trace=False):
    nc = build_program(cfg)
    shared = None
    in_maps = []
    for c in range(N_CORES):
        d = make_core_inputs(cfg, inp, c)
        if shared is None:
            shared = d
        else:
            for k in d:
                if k not in ("x_all", "own_idx", "rope"):
                    d[k] = shared[k]
        in_maps.append(d)
    res = run_bass_kernel_spmd(nc, in_maps, core_ids=list(range(N_CORES)), trace=trace)
    RS, RP, NS, D = cfg["RS"], cfg["RP"], cfg["NS"], cfg["D"]
    ys = np.stack([np.asarray(res.results[c]["ys"]).reshape(NS, RS * 64, D) for c in range(N_CORES)], axis=0)
    y_sample = ys.reshape(N_CORES * NS, RS * 64, D).astype(np.float32)
    y_prompt = np.concatenate([np.asarray(res.results[c]["yp"]) for c in range(N_CORES)], axis=0)[None].astype(np.float32)
    return (y_prompt, y_sample), res


def kernel(**inputs):
    out, _ = run_cfg(CFG_FULL, inputs)
    return out
```

```python
import contextlib
import numpy as np
import concourse.bass as bass
import concourse.mybir as mybir
from concourse.bass_utils import run_bass_kernel_spmd

F32 = mybir.dt.float32
BF16 = mybir.dt.bfloat16
I32 = mybir.dt.int32
AF = mybir.ActivationFunctionType
ALU = mybir.AluOpType
AX = mybir.AxisListType
ENGS = ("sync", "gpsimd", "scalar", "vector", "tensor")
EPS = 1e-6
N_CORES = 8

CFG_FULL = dict(D=2048, H=16, FF=5632, E=8, RS=32, RP=128, NS=2)


class Sched:
    def __init__(self, nc, stack, n_dma_sems=96):
        self.nc = nc
        self.ops = {e: [] for e in ENGS}
        self.esem = {e: stack.enter_context(nc.semaphore("es_" + e)) for e in ENGS}
        self.ecnt = {e: 0 for e in ENGS}
        self.seen = {e: {} for e in ENGS}
        self.free_sems = {"sync": [[stack.enter_context(nc.semaphore("dh%d" % i)), 0] for i in range(28)],
                          "gpsimd": [[stack.enter_context(nc.semaphore("dg%d" % i)), 0] for i in range(n_dma_sems - 28)]}
        self.dsem = {}
        self.lastw = {}
        self.reads = {}

    def _dma_sem(self, key, eng):
        key = (eng, key)
        if key not in self.dsem:
            self.dsem[key] = self.free_sems[eng].pop()
        return self.dsem[key]

    @staticmethod
    def _isdram(k):
        return isinstance(k, tuple) and len(k) > 0 and k[0] == "dram"

    def _deps(self, reads, writes):
        deps = {}

        def add(d):
            for sid, ev in d.items():
                if sid not in deps or deps[sid][1] < ev[1]:
                    deps[sid] = ev
        for r in reads:
            add(self.lastw.get(r, {}))
        for w in writes:
            if self._isdram(w):
                continue
            add(self.lastw.get(w, {}))
            add(self.reads.get(w, {}))
        return deps

    def _commit(self, reads, writes, ev):
        sid = id(ev[0])
        for r in reads:
            self.reads.setdefault(r, {})[sid] = ev
        for w in writes:
            if self._isdram(w):
                self.lastw.setdefault(w, {})[sid] = ev
            else:
                self.lastw[w] = {sid: ev}
                self.reads[w] = {}

    def _waits(self, eng, deps, skip_own):
        waits = []
        seen = self.seen[eng]
        own = id(self.esem[eng])
        for sid, (sem, val) in deps.items():
            if skip_own and sid == own:
                continue
            if seen.get(sid, 0) >= val:
                continue
            seen[sid] = val
            waits.append((sem, val))
        return waits

    def op(self, eng, fn, reads=(), writes=()):
        waits = self._waits(eng, self._deps(reads, writes), eng == "tensor")
        self.ecnt[eng] += 1
        ev = (self.esem[eng], self.ecnt[eng])

        def emit(e, fn=fn, waits=waits, sem=ev[0]):
            for s, v in waits:
                e.wait_ge(s, v)
            fn(e).then_inc(sem, 1)
        self.ops[eng].append(emit)
        self._commit(reads, writes, ev)

    def dma(self, eng, fn, semkey, reads=(), writes=()):
        waits = self._waits(eng, self._deps(reads, writes), False)
        ds = self._dma_sem(semkey, eng)
        ds[1] += 16
        ev = (ds[0], ds[1])

        def emit(e, fn=fn, waits=waits, sem=ev[0]):
            for s, v in waits:
                e.wait_ge(s, v)
            fn(e).then_inc(sem, 16)
        self.ops[eng].append(emit)
        self._commit(reads, writes, ev)

    def barrier(self):
        finals = [(ds[0], ds[1]) for ds in self.dsem.values() if ds[1] > 0]
        finals += [(self.esem[e], self.ecnt[e]) for e in ENGS if self.ecnt[e] > 0]
        for eng in ENGS:
            seen = self.seen[eng]
            w = []
            for s, v in finals:
                if seen.get(id(s), 0) >= v:
                    continue
                if id(s) == id(self.esem[eng]) and eng == "tensor":
                    continue
                seen[id(s)] = v
                w.append((s, v))

            def emit(e, w=w):
                for s, v in w:
                    e.wait_ge(s, v)
            self.ops[eng].append(emit)
        for (eng, _k), pair in self.dsem.items():
            self.free_sems[eng].append(pair)
        self.dsem = {}

    def run(self):
        ops = self.ops
        with self.nc.Block() as block:
            @block.sync
            def _(e):
                for f in ops["sync"]:
                    f(e)

            @block.gpsimd
            def _(e):
                for f in ops["gpsimd"]:
                    f(e)

            @block.scalar
            def _(e):
                for f in ops["scalar"]:
                    f(e)

            @block.vector
            def _(e):
                for f in ops["vector"]:
                    f(e)

            @block.tensor
            def _(e):
                for f in ops["tensor"]:
                    f(e)
        self.ops = {e: [] for e in ENGS}


_UCNT = [0]


def _u(name):
    _UCNT[0] += 1
    return "%s_%d" % (name, _UCNT[0])


def _chunk_div(n, cap):
    for c in range(min(n, cap), 0, -1):
        if n % c == 0:
            return c
    return 1


def build_program(cfg):
    D, H, FF, E = cfg["D"], cfg["H"], cfg["FF"], cfg["E"]
    RS, RP, NS = cfg["RS"], cfg["RP"], cfg["NS"]
    KD = D // 128
    NFC = FF // 128
    HD = H * 128
    KH = HD // 128
    assert D % 512 == 0 and FF % 512 == 0 and HD == D
    TS = RS // 8
    TP = RP // 8
    NOWN = RP // 64
    NT0 = NS * TS + TP
    NT1 = NT0 + NOWN
    NL1 = NS * TS + NOWN
    seqs0 = [(s * TS, TS, RS) for s in range(NS)] + [(NS * TS, TP, RP)]
    l1_tiles = list(range(NS * TS)) + [NT0 + i for i in range(NOWN)]
    QL = KVL = 512
    LAT = QL + KVL + 64

    nc = bass.Bass("TRN2", target_bir_lowering=False)

    def din(name, shape, dt=F32):
        return nc.dram_tensor(name, list(shape), dt, kind="ExternalInput").ap()

    def dscr(name, shape, dt):
        kind = "ExternalOutput" if name in cfg.get("dbg", ()) else "Internal"
        return nc.dram_tensor(name, list(shape), dt, kind=kind).ap()

    x_all = din("x_all", [NT0 * 512, D])
    own_idx = din("own_idx", [128, NOWN * 4], I32)
    rope_in = din("rope", [NT1 * 512, 64])
    ident_in = din("ident", [128, 128])
    g_mix = din("g_mix", [128, 2, KD])
    g_ffn = din("g_ffn", [128, 2, KD])
    g_naq = din("g_naq", [128, 1])
    g_nak = din("g_nak", [128, 1])
    rpbg = din("rpbg", [128, H, 14, 64])
    namask = din("namask", [128, 64])
    g_ql = din("g_ql", [128, 4])
    g_kvl = din("g_kvl", [128, 4])
    g_qn = din("g_qn", [128, 1])
    g_kn = din("g_kn", [128, 1])
    g_qr = din("g_qr", [128, 64])
    g_kr = din("g_kr", [128, 64])
    w_router = din("w_router", [128, KD, E])
    tri_in = din("tri", [128, 128])
    cst_in = din("cst", [128, 64])
    Wf = {
        "qkv": din("na_w_qkv", [D, 3 * HD]), "nao": din("na_w_o", [HD, D]),
        "fg": din("ffn_w_gate", [D, FF]), "fu": din("ffn_w_up", [D, FF]), "fd": din("ffn_w_down", [FF, D]),
        "dqkv": din("mla_w_dqkv", [D, LAT]), "uq": din("mla_w_uq", [QL, H * 192]),
        "ukv": din("mla_w_ukv", [KVL, H * 256]), "mo": din("mla_w_o", [HD, D]),
    }
    for e_ in range(E):
        Wf["mg%d" % e_] = din("moe_w_gate%d" % e_, [D, FF])
        Wf["mu%d" % e_] = din("moe_w_up%d" % e_, [D, FF])
        Wf["md%d" % e_] = din("moe_w_down%d" % e_, [FF, D])
    Wb = {k: dscr("wb_" + k, v.shape, BF16) for k, v in Wf.items() if not k.startswith("m") or k == "mo"}

    ys = nc.dram_tensor("ys", [NS * TS * 512, D], F32, kind="ExternalOutput").ap()
    yp = nc.dram_tensor("yp", [NOWN * 512, D], F32, kind="ExternalOutput").ap()

    qk0 = dscr("qk0", [NT0, 2 * H, 128, 512], BF16)
    v0 = dscr("v0", [NT0 * 512, HD], BF16)
    oT0 = dscr("oT0", [NT0, H, 128, 512], BF16)
    x1 = dscr("x1", [NT1 * 512, D], F32)
    q1n = dscr("q1n", [NT1, H, 128, 512], BF16)
    q1r = dscr("q1r", [NT1, H, 64, 512], BF16)
    k1n = dscr("k1n", [NT1, H, 128, 512], BF16)
    k1r = dscr("k1r", [NT1, 64, 512], BF16)
    v1 = dscr("v1", [NT1 * 512, HD], BF16)
    oT1 = dscr("oT1", [NL1, H, 128, 512], BF16)

    with contextlib.ExitStack() as gst:
        S = Sched(nc, gst)
        PS = [gst.enter_context(nc.psum_tensor("ps%d" % i, [128, 512], F32)) for i in range(8)]
        ident_f = gst.enter_context(nc.sbuf_tensor(_u("ident_f"), [128, 128], F32))
        ident_b = gst.enter_context(nc.sbuf_tensor(_u("ident_b"), [128, 128], BF16))
        ones_b = gst.enter_context(nc.sbuf_tensor(_u("ones_b"), [128, 128], BF16))
        gmix = gst.enter_context(nc.sbuf_tensor(_u("gmix"), [128, 2, KD], F32))
        gffn = gst.enter_context(nc.sbuf_tensor(_u("gffn"), [128, 2, KD], F32))
        gsm = gst.enter_context(nc.sbuf_tensor(_u("gsm"), [128, 12], F32))
        gqr = gst.enter_context(nc.sbuf_tensor(_u("gqr"), [128, 64], F32))
        gkr = gst.enter_context(nc.sbuf_tensor(_u("gkr"), [128, 64], F32))
        WS = []
        ws_i = [0]

        def alloc_ws(stk, n):
            WS[:] = [stk.enter_context(nc.sbuf_tensor(_u("ws%d" % i), [128, 8192], BF16)) for i in range(n)]

        def ld(dst, src, key, eng="sync", reads=()):
            S.dma(eng, lambda e: e.dma_start(out=dst, in_=src), key, reads=reads, writes=[key])

        ld(ident_f[:], ident_in[:, :], "ident_f")
        ld(gmix[:], g_mix[:, :, :], "gmix")
        ld(gffn[:], g_ffn[:, :, :], "gffn")
        ld(gsm[:, 0:1], g_naq[:, :], "gsm")
        ld(gsm[:, 1:2], g_nak[:, :], "gsm")
        ld(gsm[:, 2:3], g_qn[:, :], "gsm")
        ld(gsm[:, 3:4], g_kn[:, :], "gsm")
        ld(gsm[:, 4:8], g_ql[:, :], "gsm")
        ld(gsm[:, 8:12], g_kvl[:, :], "gsm")
        ld(gqr[:], g_qr[:, :], "gqr")
        ld(gkr[:], g_kr[:, :], "gkr")
        S.op("vector", lambda e: e.tensor_copy(out=ident_b[:], in_=ident_f[:]), reads=["ident_f"], writes=["ident_b"])
        S.op("vector", lambda e: e.memset(ones_b[:], 1.0), writes=["ones_b"])

        def conv(name):
            src, dst = Wf[name], Wb[name]
            rows, cols = src.shape
            step = max(128, (4 * 1024 * 1024 // cols) // 128 * 128)
            for r0 in range(0, rows, step):
                r1 = min(rows, r0 + step)
                S.dma("gpsimd", lambda e, r0=r0, r1=r1: e.dma_start(out=dst[r0:r1, :], in_=src[r0:r1, :]),
                      ("cv", name), writes=[("dram", "wb_" + name)])

        for nm in ("qkv", "nao", "fg", "fu", "fd", "dqkv", "uq", "ukv", "mo"):
            conv(nm)
        NCH = FF // 512
        NCG = D // 512
        NDC = NFC // _chunk_div(NFC, 16)
        DCm = _chunk_div(NFC, 16)
        war_g = dscr("war_g", [E * NCH * 128, KD * 512], BF16)
        war_u = dscr("war_u", [E * NCH * 128, KD * 512], BF16)
        war_d = dscr("war_d", [E * NCG * NDC * 128, DCm * 512], BF16)
        moe_conv_todo = []

        def _mk_gu(dst_t, src, e_, c, nm):
            r0 = (e_ * NCH + c) * 128
            return lambda e: e.dma_start(
                out=dst_t[r0:r0 + 128, :].rearrange("p (k f) -> p k f", f=512),
                in_=src[:, c * 512:(c + 1) * 512].rearrange("(k p) f -> p k f", p=128))

        def _mk_d(src, e_, cg, dc):
            r0 = ((e_ * NCG + cg) * NDC + dc) * 128
            return lambda e: e.dma_start(
                out=war_d[r0:r0 + 128, :].rearrange("p (f c) -> p f c", c=512),
                in_=src[dc * DCm * 128:(dc + 1) * DCm * 128, cg * 512:(cg + 1) * 512].rearrange("(f p) c -> p f c", p=128))

        for e_ in range(E):
            for c in range(NCH):
                moe_conv_todo.append((_mk_gu(war_g, Wf["mg%d" % e_], e_, c, "g"), "war_g"))
                moe_conv_todo.append((_mk_gu(war_u, Wf["mu%d" % e_], e_, c, "u"), "war_u"))
            for cg in range(NCG):
                for dc in range(NDC):
                    moe_conv_todo.append((_mk_d(Wf["md%d" % e_], e_, cg, dc), "war_d"))
        n_moe_conv = len(moe_conv_todo)
        cv_rr = [0]

        def conv_some(n):
            for _ in range(n):
                if moe_conv_todo:
                    fn, nm = moe_conv_todo.pop(0)
                    cv_rr[0] += 1
                    S.dma("gpsimd", fn, ("cvm", cv_rr[0] % 8), writes=[("dram", nm)])

        def wload(name, k0, nk, c0, ncols):
            s = ws_i[0] % len(WS)
            ws_i[0] += 1
            src = Wb[name][k0 * 128:(k0 + nk) * 128, c0:c0 + ncols].rearrange("(k p) f -> p k f", p=128)
            view = WS[s][:, 0:nk * ncols].rearrange("p (k c) -> p k c", c=ncols)
            S.dma("sync", lambda e: e.dma_start(out=view, in_=src), ("w", s),
                  reads=[("dram", "wb_" + name)], writes=[("w", s)])
            return ("w", s), view

        def psb(i):
            return PS[i][:, :].bitcast(BF16)

        def norm_tile(xt, xkey, xn, junk, st, gain, hT, hkey, tp_banks, hT32=None):
            if callable(xt):
                xsrc = xt
            else:
                xsrc = lambda b: (xt[:, b, :], xkey)
            for b in range(4):
                xa, xk = xsrc(b)
                S.op("vector", lambda e, xa=xa, b=b: e.scalar_tensor_tensor(
                    out=junk[:, :], in0=xa, scalar=1.0, in1=xa, op0=ALU.mult, op1=ALU.mult, accum_out=st[:, b:b + 1]),
                    reads=[xk], writes=["junk", ("st", b)])
                S.op("scalar", lambda e, b=b: e.activation(out=st[:, 4 + b:5 + b], in_=st[:, b:b + 1], func=AF.Sqrt,
                                                           scale=1.0 / D, bias=EPS),
                     reads=[("st", b)], writes=["st2"])
                S.op("vector", lambda e, b=b: e.reciprocal(out=st[:, 4 + b:5 + b], in_=st[:, 4 + b:5 + b]),
                     reads=["st2"], writes=["st2"])
                S.op("scalar", lambda e, b=b, xa=xa: e.activation(out=xn[:, b, :], in_=xa, func=AF.Copy,
                                                                  scale=st[:, 4 + b:5 + b]),
                     reads=[xk, "st2"], writes=[("xn", b)])
            if hT is None:
                return
            transpose_tile(xn, gain, hT, hkey, tp_banks)

        def transpose_tile(xn, gain, hT, hkey, tp_banks):
            for k in range(KD):
                bank = tp_banks[(k // 2) % len(tp_banks)]
                half = k % 2
                tpv = psb(bank)[:, half * 512:(half + 1) * 512]
                for b in range(4):
                    S.op("tensor", lambda e, b=b, k=k, tpv=tpv: e.transpose(
                        out=tpv[:, b * 128:(b + 1) * 128], in_=xn[:, b, k * 128:(k + 1) * 128], identity=ident_b[:]),
                        reads=[("xn", b), "ident_b"], writes=[("ps", bank)])
                if k % 2 == 0:
                    S.op("vector", lambda e, k=k, tpv=tpv: e.tensor_scalar(
                        out=hT[:, k, :], in0=tpv, scalar1=gain[:, k:k + 1], scalar2=None, op0=ALU.mult),
                        reads=[("ps", bank), "gmix", "gffn"], writes=[(hkey, k)])
                else:
                    S.op("scalar", lambda e, k=k, tpv=tpv: e.activation(
                        out=hT[:, k, :], in_=tpv, func=AF.Copy, scale=gain[:, k:k + 1]),
                        reads=[("ps", bank), "gmix", "gffn"], writes=[(hkey, k)])

        if cfg.get("stop", 99) >= 1:
         with contextlib.ExitStack() as st_:
            alloc_ws(st_, 3)
            xt = st_.enter_context(nc.sbuf_tensor(_u("xt"), [128, 4, D], F32))
            xn = st_.enter_context(nc.sbuf_tensor(_u("xn"), [128, 4, D], BF16))
            junk = st_.enter_context(nc.sbuf_tensor(_u("junk"), [128, D], F32))
            stt = st_.enter_context(nc.sbuf_tensor(_u("stt"), [128, 8], F32))
            hT = st_.enter_context(nc.sbuf_tensor(_u("hT"), [128, KD, 512], BF16))
            sq = [st_.enter_context(nc.sbuf_tensor(_u("sq%d" % i), [128, 512], BF16)) for i in range(2)]
            rs_ = [st_.enter_context(nc.sbuf_tensor(_u("rs%d" % i), [128, 512], F32)) for i in range(2)]
            qo = [st_.enter_context(nc.sbuf_tensor(_u("qo%d" % i), [128, 512], BF16)) for i in range(3)]
            vo = [st_.enter_context(nc.sbuf_tensor(_u("vo%d" % i), [128, 512], BF16)) for i in range(3)]
            cnt = 0
            vcnt = 0
            pend1 = [None]
            for t in range(NT0):
                ld(xt[:], x_all[t * 512:(t + 1) * 512, :].rearrange("(b p) d -> p b d", p=128), "xt")
                norm_tile(xt, "xt", xn, junk, stt, gmix[:, 0, :], hT, "hT", [6, 7])
                for ch in range(2 * H // 4):
                    wkey, wv = wload("qkv", 0, KD, ch * 512, 512)
                    for j in range(4):
                        hj = ch * 4 + j
                        pb = cnt % 4
                        sb = 4 + cnt % 2
                        i2 = cnt % 2
                        i3 = cnt % 3
                        cnt += 1
                        for k in range(KD):
                            S.op("tensor", lambda e, k=k, j=j, pb=pb, wv=wv: e.matmul(
                                PS[pb][:, :], lhsT=wv[:, k, j * 128:(j + 1) * 128], rhs=hT[:, k, :],
                                start=(k == 0), stop=(k == KD - 1)),
                                reads=[wkey, ("hT", k)], writes=[("ps", pb)])
                        S.op("scalar", lambda e, pb=pb, i2=i2: e.activation(out=sq[i2][:], in_=PS[pb][:, :], func=AF.Square),
                             reads=[("ps", pb)], writes=[("sq", i2)])
                        def tail1(pb=pb, sb=sb, i2=i2, i3=i3, hj=hj, t=t):
                            S.op("tensor", lambda e, sb=sb, i2=i2: e.matmul(PS[sb][:, :], lhsT=ones_b[:], rhs=sq[i2][:],
                                                                             start=True, stop=True),
                                 reads=[("sq", i2), "ones_b"], writes=[("ps", sb)])
                            S.op("scalar", lambda e, sb=sb, i2=i2: e.activation(
                                out=rs_[i2][:], in_=PS[sb][:, :], func=AF.Sqrt, scale=1.0 / 128, bias=EPS),
                                reads=[("ps", sb)], writes=[("rs", i2)])
                            S.op("vector", lambda e, i2=i2: e.reciprocal(out=rs_[i2][:], in_=rs_[i2][:]),
                                 reads=[("rs", i2)], writes=[("rs", i2)])
                            gcol = 0 if hj < H else 1
                            S.op("vector", lambda e, pb=pb, i2=i2, i3=i3, gcol=gcol: e.scalar_tensor_tensor(
                                out=qo[i3][:], in0=PS[pb][:, :], scalar=gsm[:, gcol:gcol + 1], in1=rs_[i2][:],
                                op0=ALU.mult, op1=ALU.mult),
                                reads=[("ps", pb), ("rs", i2), "gsm"], writes=[("qo", i3)])
                            S.dma("gpsimd", lambda e, i3=i3, t=t, hj=hj: e.dma_start(out=qk0[t, hj, :, :], in_=qo[i3][:]),
                                  ("st_qo", i3), reads=[("qo", i3)], writes=[("dram", "qk0")])
                        if pend1[0] is not None:
                            pend1[0]()
                        pend1[0] = tail1
                if pend1[0] is not None:
                    pend1[0]()
                    pend1[0] = None
                for cg in range(HD // 512):
                    wkey, wv = wload("qkv", 0, KD, 2 * HD + cg * 512, 512)
                    for b in range(4):
                        pb = cnt % 4
                        cnt += 1
                        i3 = vcnt % 3
                        vcnt += 1
                        for k in range(KD):
                            S.op("tensor", lambda e, k=k, b=b, pb=pb, wv=wv: e.matmul(
                                PS[pb][:, :], lhsT=hT[:, k, b * 128:(b + 1) * 128], rhs=wv[:, k, :],
                                start=(k == 0), stop=(k == KD - 1)),
                                reads=[wkey, ("hT", k)], writes=[("ps", pb)])
                        S.op("scalar", lambda e, pb=pb, i3=i3: e.copy(out=vo[i3][:], in_=PS[pb][:, :]),
                             reads=[("ps", pb)], writes=[("vo", i3)])
                        r0 = t * 512 + b * 128
                        S.dma("gpsimd", lambda e, i3=i3, r0=r0, cg=cg: e.dma_start(
                            out=v0[r0:r0 + 128, cg * 512:(cg + 1) * 512], in_=vo[i3][:]),
                            ("st_vo", i3), reads=[("vo", i3)], writes=[("dram", "v0")])
            S.barrier()
            S.run()

        if cfg.get("stop", 99) >= 2:
         with contextlib.ExitStack() as st_:
            TT = st_.enter_context(nc.sbuf_tensor(_u("TT"), [128, H, 14, 64], BF16))
            msk = st_.enter_context(nc.sbuf_tensor(_u("msk"), [128, 64], F32))
            rp = [st_.enter_context(nc.sbuf_tensor(_u("rp%d" % i), [128, 14, 64], F32)) for i in range(2)]
            WRmax = 16
            kw = st_.enter_context(nc.sbuf_tensor(_u("kw"), [128, H, WRmax * 64], BF16))
            vE = st_.enter_context(nc.sbuf_tensor(_u("vE"), [128, WRmax // 2, HD], BF16))
            vO = st_.enter_context(nc.sbuf_tensor(_u("vO"), [128, WRmax // 2 - 1, HD], BF16))
            qT = st_.enter_context(nc.sbuf_tensor(_u("qT"), [128, H, 512], BF16))
            oS = st_.enter_context(nc.sbuf_tensor(_u("oS"), [128, H, 512], BF16))
            GH = 4 if H % 4 == 0 else 2
            Eb = [st_.enter_context(nc.sbuf_tensor(_u("Eb%d" % i), [128, GH, 4, 64], BF16)) for i in range(2)]
            Pb = [st_.enter_context(nc.sbuf_tensor(_u("Pb%d" % i), [128, GH, 4, 64], BF16)) for i in range(2)]
            rc = [st_.enter_context(nc.sbuf_tensor(_u("rc%d" % i), [128, GH * 64], F32)) for i in range(2)]
            ld(msk[:], namask[:, :], "msk")
            for h in range(H):
                i2 = h % 2
                ld(rp[i2][:], rpbg[:, h, :, :], ("rp", i2))
                S.op("scalar", lambda e, i2=i2: e.activation(out=rp[i2][:], in_=rp[i2][:], func=AF.Exp),
                     reads=[("rp", i2)], writes=[("rp", i2)])
                S.op("vector", lambda e, i2=i2, h=h: e.tensor_tensor(
                    out=TT[:, h, :, :], in0=rp[i2][:], in1=msk[:, :].unsqueeze(1).to_broadcast([128, 14, 64]), op=ALU.mult),
                    reads=[("rp", i2), "msk"], writes=["TT"])
            scale = 128 ** -0.5
            gc = 0
            pend = [None]
            for (tile0, ntl, rows) in seqs0:
                WR = min(16, rows)
                for tl in range(ntl):
                    t = tile0 + tl
                    r0 = tl * 8
                    w0 = min(max(r0 - 4, 0), rows - WR)
                    rr = w0
                    while rr < w0 + WR:
                        st_tile = rr // 8
                        re = min(w0 + WR, (st_tile + 1) * 8)
                        n = (re - rr) * 64
                        off = (rr - st_tile * 8) * 64
                        dst = (rr - w0) * 64
                        S.dma("sync", lambda e, st_tile=st_tile, off=off, n=n, dst=dst, tile0=tile0: e.dma_start(
                            out=kw[:, :, dst:dst + n],
                            in_=qk0[tile0 + st_tile, H:2 * H, :, off:off + n].rearrange("h d t -> d h t")),
                            "kw", reads=[("dram", "qk0")], writes=["kw"])
                        rr = re
                    tok0 = tile0 * 512 + w0 * 64
                    S.dma("sync", lambda e, tok0=tok0, WR=WR: e.dma_start(
                        out=vE[:, 0:WR // 2, :], in_=v0[tok0:tok0 + WR * 64, :].rearrange("(b p) e -> p b e", p=128)),
                        "vE", reads=[("dram", "v0")], writes=["vE"])
                    if WR > 8:
                        S.dma("sync", lambda e, tok0=tok0, WR=WR: e.dma_start(
                            out=vO[:, 0:WR // 2 - 1, :],
                            in_=v0[tok0 + 64:tok0 + 64 + (WR - 2) * 64, :].rearrange("(b p) e -> p b e", p=128)),
                            "vO", reads=[("dram", "v0")], writes=["vO"])
                    S.dma("sync", lambda e, t=t: e.dma_start(
                        out=qT[:, :, :], in_=qk0[t, 0:H, :, :].rearrange("h d t -> d h t")),
                        "qT", reads=[("dram", "qk0")], writes=["qT"])
                    for rl in range(8):
                        r = r0 + rl
                        rs = min(max(r - 4, 0), rows - 8)
                        o = r - rs
                        rel = rs - w0
                        m0 = 7 - o
                        for hp in range(H // GH):
                            sb0 = (gc % 2) * (GH // 2)
                            obk = 4 + gc % 2
                            dbk = 6 + gc % 2
                            i2 = gc % 2
                            gc += 1
                            for hh in range(GH):
                                h = hp * GH + hh
                                sbk = sb0 + hh // 2
                                for p in range(4):
                                    kt0 = (rel + 2 * p) * 64
                                    S.op("tensor", lambda e, h=h, hh=hh, p=p, kt0=kt0, sbk=sbk, rl=rl: e.matmul(
                                        PS[sbk][:, ((hh % 2) * 4 + p) * 64:((hh % 2) * 4 + p + 1) * 64],
                                        lhsT=kw[:, h, kt0:kt0 + 128], rhs=qT[:, h, rl * 64:(rl + 1) * 64],
                                        start=True, stop=True),
                                        reads=["kw", "qT"], writes=[("ps", sbk)])
                            for bb in range(GH // 2):
                                sbk = sb0 + bb
                                S.op("scalar", lambda e, sbk=sbk, i2=i2, bb=bb: e.activation(
                                    out=Eb[i2][:, 2 * bb:2 * bb + 2, :, :].rearrange("p a b c -> p (a b c)"),
                                    in_=PS[sbk][:, :], func=AF.Exp, scale=scale),
                                    reads=[("ps", sbk)], writes=[("Eb", i2, bb)])
                            S.op("vector", lambda e, i2=i2, hp=hp, m0=m0: e.tensor_tensor(
                                out=Pb[i2][:], in0=Eb[i2][:], in1=TT[:, GH * hp:GH * hp + GH, m0:m0 + 7:2, :], op=ALU.mult),
                                reads=[("Eb", i2, bb) for bb in range(GH // 2)] + ["TT"], writes=[("Pb", i2)])

                            def tail(hp=hp, rl=rl, rel=rel, i2=i2, obk=obk, dbk=dbk):
                                for hh in range(GH):
                                    h = hp * GH + hh
                                    for p in range(4):
                                        if rel % 2 == 0:
                                            vv = vE[:, rel // 2 + p, h * 128:(h + 1) * 128]
                                            vk = "vE"
                                        else:
                                            vv = vO[:, (rel - 1) // 2 + p, h * 128:(h + 1) * 128]
                                            vk = "vO"
                                        S.op("tensor", lambda e, hh=hh, p=p, vv=vv, obk=obk, i2=i2: e.matmul(
                                            PS[obk][:, hh * 64:(hh + 1) * 64], lhsT=vv, rhs=Pb[i2][:, hh, p, :],
                                            start=(p == 0), stop=(p == 3)),
                                            reads=[vk, ("Pb", i2)], writes=[("ps", obk)])
                                for p in range(4):
                                    S.op("tensor", lambda e, p=p, dbk=dbk, i2=i2: e.matmul(
                                        PS[dbk][:, 0:GH * 64].rearrange("q (a b) -> q a b", a=GH), lhsT=ones_b[:],
                                        rhs=Pb[i2][:, :, p, :], start=(p == 0), stop=(p == 3)),
                                        reads=[("Pb", i2), "ones_b"], writes=[("ps", dbk)])
                                S.op("vector", lambda e, dbk=dbk, i2=i2: e.reciprocal(out=rc[i2][:], in_=PS[dbk][:, 0:GH * 64]),
                                     reads=[("ps", dbk)], writes=[("rc", i2)])
                                S.op("vector", lambda e, obk=obk, i2=i2, hp=hp, rl=rl: e.tensor_tensor(
                                    out=oS[:, GH * hp:GH * hp + GH, rl * 64:(rl + 1) * 64],
                                    in0=PS[obk][:, 0:GH * 64].rearrange("q (a b) -> q a b", a=GH),
                                    in1=rc[i2][:].rearrange("q (a b) -> q a b", a=GH), op=ALU.mult),
                                    reads=[("ps", obk), ("rc", i2)], writes=["oS"])
                            if pend[0] is not None:
                                pend[0]()
                            pend[0] = tail
                    if pend[0] is not None:
                        pend[0]()
                        pend[0] = None
                    S.dma("gpsimd", lambda e, t=t: e.dma_start(
                        out=oT0[t, :, :, :].rearrange("h d t -> d h t"), in_=oS[:, :, :]),
                        "st_oS", reads=["oS"], writes=[("dram", "oT0")])
                    conv_some((n_moe_conv + 2 * NT0 - 1) // (2 * NT0))
            S.barrier()
            S.run()

        GC = 4
        DC = _chunk_div(NFC, 16)

        def swiglu_tile(hT, hkey, act, sg, names, epilogue, cnt0):
            cnt = cnt0
            if callable(names):
                loader = names
            else:
                ng, nu, nd = names

                def loader(kind, *a):
                    if kind == "g":
                        return wload(ng, 0, KD, a[0] * GC * 128, GC * 128)
                    if kind == "u":
                        return wload(nu, 0, KD, a[0] * GC * 128, GC * 128)
                    return wload(nd, a[1] * DC, DC, a[0] * 512, 512)
            for c in range(NFC // GC):
                wkg, wg = loader("g", c)
                gb = []
                for j in range(GC):
                    pb = cnt % 4
                    cnt += 1
                    gb.append(pb)
                    for k in range(KD):
                        S.op("tensor", lambda e, k=k, j=j, pb=pb, wg=wg: e.matmul(
                            PS[pb][:, :], lhsT=wg[:, k, j * 128:(j + 1) * 128], rhs=hT[:, k, :],
                            start=(k == 0), stop=(k == KD - 1)),
                            reads=[wkg, (hkey, k)], writes=[("ps", pb)])
                    S.op("scalar", lambda e, j=j, pb=pb: e.activation(out=sg[j][:], in_=PS[pb][:, :], func=AF.Silu),
                         reads=[("ps", pb)], writes=[("sg", j)])
                wku, wu = loader("u", c)
                for j in range(GC):
                    pb = 4 + cnt % 4
                    cnt += 1
                    fc = c * GC + j
                    for k in range(KD):
                        S.op("tensor", lambda e, k=k, j=j, pb=pb, wu=wu: e.matmul(
                            PS[pb][:, :], lhsT=wu[:, k, j * 128:(j + 1) * 128], rhs=hT[:, k, :],
                            start=(k == 0), stop=(k == KD - 1)),
                            reads=[wku, (hkey, k)], writes=[("ps", pb)])
                    S.op("vector", lambda e, j=j, pb=pb, fc=fc: e.tensor_tensor(
                        out=act[:, fc, :], in0=PS[pb][:, :], in1=sg[j][:], op=ALU.mult),
                        reads=[("ps", pb), ("sg", j)], writes=[("act", fc)])
            for cg in range(D // 512):
                base = (cg % 2) * 4
                for dc in range(NFC // DC):
                    wkd, wd = loader("d", cg, dc)
                    for f in range(DC):
                        fc = dc * DC + f
                        for b in range(4):
                            S.op("tensor", lambda e, f=f, fc=fc, b=b, base=base, wd=wd: e.matmul(
                                PS[base + b][:, :], lhsT=act[:, fc, b * 128:(b + 1) * 128], rhs=wd[:, f, :],
                                start=(fc == 0), stop=(fc == NFC - 1)),
                                reads=[wkd, ("act", fc)], writes=[("ps", base + b)])
                for b in range(4):
                    epilogue(cg, b, base + b)
            return cnt

        def wo_tile(oT, okey, wname, xt, xkey):
            for cg in range(D // 512):
                base = (cg % 2) * 4
                wk, wv = wload(wname, 0, KH, cg * 512, 512)
                for b in range(4):
                    for k in range(KH):
                        S.op("tensor", lambda e, k=k, b=b, base=base, wv=wv: e.matmul(
                            PS[base + b][:, :], lhsT=oT[:, k, b * 128:(b + 1) * 128], rhs=wv[:, k, :],
                            start=(k == 0), stop=(k == KH - 1)),
                            reads=[wk, okey], writes=[("ps", base + b)])
                    S.op("vector", lambda e, b=b, base=base, cg=cg: e.tensor_tensor(
                        out=xt[:, b, cg * 512:(cg + 1) * 512], in0=PS[base + b][:, :],
                        in1=xt[:, b, cg * 512:(cg + 1) * 512], op=ALU.add),
                        reads=[("ps", base + b), xkey], writes=[xkey])

        if cfg.get("stop", 99) >= 3:
         with contextlib.ExitStack() as st_:
            alloc_ws(st_, 3)
            xt = st_.enter_context(nc.sbuf_tensor(_u("xt"), [128, 4, D], F32))
            xn = st_.enter_context(nc.sbuf_tensor(_u("xn"), [128, 4, D], BF16))
            junk = st_.enter_context(nc.sbuf_tensor(_u("junk"), [128, D], F32))
            stt = st_.enter_context(nc.sbuf_tensor(_u("stt"), [128, 8], F32))
            hT = st_.enter_context(nc.sbuf_tensor(_u("hT"), [128, KD, 512], BF16))
            oT = st_.enter_context(nc.sbuf_tensor(_u("oT"), [128, H, 512], BF16))
            act = st_.enter_context(nc.sbuf_tensor(_u("act"), [128, NFC, 512], BF16))
            sg = [st_.enter_context(nc.sbuf_tensor(_u("sg%d" % i), [128, 512], BF16)) for i in range(GC)]
            cnt = 0
            for t in range(NT0):
                ld(xt[:], x_all[t * 512:(t + 1) * 512, :].rearrange("(b p) d -> p b d", p=128), "xt")
                ld(oT[:], oT0[t, :, :, :].rearrange("h d t -> d h t"), "oT", reads=[("dram", "oT0")])
                wo_tile(oT, "oT", "nao", xt, "xt")
                norm_tile(xt, "xt", xn, junk, stt, gffn[:, 0, :], hT, "hT", [6, 7])

                def epi(cg, b, bank):
                    S.op("vector", lambda e: e.tensor_tensor(
                        out=xt[:, b, cg * 512:(cg + 1) * 512], in0=PS[bank][:, :],
                        in1=xt[:, b, cg * 512:(cg + 1) * 512], op=ALU.add),
                        reads=[("ps", bank), "xt"], writes=["xt"])
                cnt = swiglu_tile(hT, "hT", act, sg, ("fg", "fu", "fd"), epi, cnt)
                S.dma("gpsimd", lambda e, t=t: e.dma_start(
                    out=x1[t * 512:(t + 1) * 512, :].rearrange("(b p) d -> p b d", p=128), in_=xt[:]),
                    "st_xt", reads=["xt"], writes=[("dram", "x1")])
                conv_some((n_moe_conv + 2 * NT0 - 1) // (2 * NT0))
            conv_some(100000)
            oi = st_.enter_context(nc.sbuf_tensor(_u("oi"), [128, NOWN * 4], I32))
            ld(oi[:], own_idx[:, :], "oi")
            for j in range(NOWN * 4):
                S.dma("gpsimd", lambda e, j=j: e.indirect_dma_start(
                    out=xt[:, j % 4, :], out_offset=None, in_=x1[0:NT0 * 512, :],
                    in_offset=bass.IndirectOffsetOnAxis(ap=oi[:, j:j + 1], axis=0)),
                    "xt", reads=[("dram", "x1"), "oi"], writes=["xt"])
                if j % 4 == 3:
                    tt_ = NT0 + j // 4
                    S.dma("gpsimd", lambda e, tt_=tt_: e.dma_start(
                        out=x1[tt_ * 512:(tt_ + 1) * 512, :].rearrange("(b p) d -> p b d", p=128), in_=xt[:]),
                        "st_xt", reads=["xt"], writes=[("dram", "x1")])
            S.barrier()
            S.run()

        if cfg.get("stop", 99) >= 4:
         with contextlib.ExitStack() as st_:
            alloc_ws(st_, 3)
            xb = [st_.enter_context(nc.sbuf_tensor(_u("xb%d" % i), [128, D], F32)) for i in range(2)]
            xn = st_.enter_context(nc.sbuf_tensor(_u("xn"), [128, 4, D], BF16))
            junk = st_.enter_context(nc.sbuf_tensor(_u("junk"), [128, D], F32))
            stt = st_.enter_context(nc.sbuf_tensor(_u("stt"), [128, 8], F32))
            hT = st_.enter_context(nc.sbuf_tensor(_u("hT"), [128, KD, 512], BF16))
            cT = st_.enter_context(nc.sbuf_tensor(_u("cT"), [128, 8, 512], BF16))
            cn = st_.enter_context(nc.sbuf_tensor(_u("cn"), [128, 1024], BF16))
            s2 = st_.enter_context(nc.sbuf_tensor(_u("s2"), [128, 8], F32))
            latf = st_.enter_context(nc.sbuf_tensor(_u("latf"), [128, 1088], F32))
            kvf = [st_.enter_context(nc.sbuf_tensor(_u("kvf%d" % i), [128, 512], F32)) for i in range(2)]
            rpt = st_.enter_context(nc.sbuf_tensor(_u("rpt"), [128, 4, 64], F32))
            kr = st_.enter_context(nc.sbuf_tensor(_u("kr"), [128, 64], F32))
            kr2 = st_.enter_context(nc.sbuf_tensor(_u("kr2"), [128, 64], F32))
            krb = st_.enter_context(nc.sbuf_tensor(_u("krb"), [128, 64], BF16))
            krT = st_.enter_context(nc.sbuf_tensor(_u("krT"), [64, 512], BF16))
            qf = st_.enter_context(nc.sbuf_tensor(_u("qf"), [128, H, 192], F32))
            qsq = st_.enter_context(nc.sbuf_tensor(_u("qsq"), [128, H, 192], F32))
            qs = st_.enter_context(nc.sbuf_tensor(_u("qs"), [128, 4 * H], F32))
            qb = st_.enter_context(nc.sbuf_tensor(_u("qb"), [128, H, 192], BF16))
            qr1 = st_.enter_context(nc.sbuf_tensor(_u("qr1"), [128, H, 64], F32))
            qr2 = st_.enter_context(nc.sbuf_tensor(_u("qr2"), [128, H, 64], F32))
            tq = st_.enter_context(nc.sbuf_tensor(_u("tq"), [128, H, 32], F32))
            kf = st_.enter_context(nc.sbuf_tensor(_u("kf"), [128, H, 128], F32))
            kb = st_.enter_context(nc.sbuf_tensor(_u("kb"), [128, H, 128], BF16))
            vb = st_.enter_context(nc.sbuf_tensor(_u("vb"), [128, H, 128], BF16))
            qnT = [st_.enter_context(nc.sbuf_tensor(_u("qnT%d" % i), [128, H, 128], BF16)) for i in range(2)]
            qrT = st_.enter_context(nc.sbuf_tensor(_u("qrT"), [64, H, 128], BF16))
            knT = [st_.enter_context(nc.sbuf_tensor(_u("knT%d" % i), [128, H, 128], BF16)) for i in range(2)]
            P4S = cfg.get("p4s", 9)
            NQG = (H * 192) // 384
            NKG = (H * 256) // 512
            for t in range(NT1):
                def xsrc4(b, t=t):
                    r0 = t * 512 + b * 128
                    ld(xb[b % 2][:, :], x1[r0:r0 + 128, :], ("xb", b % 2), reads=[("dram", "x1")])
                    return xb[b % 2][:, :], ("xb", b % 2)
                ld(rpt[:], rope_in[t * 512:(t + 1) * 512, :].rearrange("(b p) c -> p b c", p=128), "rpt")
                norm_tile(xsrc4, None, xn, junk, stt, gmix[:, 1, :], hT, "hT", [6, 7])
                wk0, w0v = wload("dqkv", 0, KD, 0, 512)
                wk1, w1v = wload("dqkv", 0, KD, 512, 512)
                wk2, w2v = wload("dqkv", 0, KD, 1024, 64)
                for b in range(4):
                    for (bank, wk, wv, ncol) in ((0, wk0, w0v, 512), (1, wk1, w1v, 512), (2, wk2, w2v, 64)):
                        for k in range(KD):
                            S.op("tensor", lambda e, k=k, b=b, bank=bank, wv=wv, ncol=ncol: e.matmul(
                                PS[bank][:, 0:ncol], lhsT=hT[:, k, b * 128:(b + 1) * 128], rhs=wv[:, k, :],
                                start=(k == 0), stop=(k == KD - 1)),
                                reads=[wk, ("hT", k)], writes=[("ps", bank)])
                    for li, ncol in ((0, 512), (1, 512), (2, 64)):
                        S.op("scalar", lambda e, li=li, ncol=ncol: e.copy(out=latf[:, li * 512:li * 512 + ncol], in_=PS[li][:, 0:ncol]),
                             reads=[("ps", li)], writes=[("latf", li)])
                    for li, ncol in ((0, 512), (1, 512), (2, 64)):
                        S.op("vector", lambda e, li=li, ncol=ncol: e.tensor_tensor(
                            out=junk[:, 0:ncol], in0=latf[:, li * 512:li * 512 + ncol], in1=latf[:, li * 512:li * 512 + ncol],
                            op=ALU.mult), reads=[("latf", li)], writes=["junk"])
                        S.op("vector", lambda e, li=li, ncol=ncol: e.tensor_reduce(
                            out=s2[:, li:li + 1], in_=junk[:, 0:ncol], axis=AX.X, op=ALU.add),
                            reads=["junk"], writes=[("s2", li)])
                    S.op("scalar", lambda e: e.activation(out=s2[:, 4:6], in_=s2[:, 0:2], func=AF.Sqrt, scale=1.0 / 512, bias=EPS),
                         reads=[("s2", 0), ("s2", 1)], writes=["s2b"])
                    S.op("scalar", lambda e: e.activation(out=s2[:, 6:7], in_=s2[:, 2:3], func=AF.Sqrt, scale=1.0 / 64, bias=EPS),
                         reads=[("s2", 2), "s2b"], writes=["s2b"])
                    S.op("vector", lambda e: e.reciprocal(out=s2[:, 4:7], in_=s2[:, 4:7]), reads=["s2b"], writes=["s2b"])
                    for li in range(2):
                        S.op("scalar", lambda e, li=li: e.activation(
                            out=cn[:, li * 512:(li + 1) * 512], in_=latf[:, li * 512:(li + 1) * 512], func=AF.Copy,
                            scale=s2[:, 4 + li:5 + li]),
                            reads=[("latf", li), "s2b"], writes=[("cn", li)])
                    S.op("vector", lambda e: e.scalar_tensor_tensor(
                        out=kr[:], in0=latf[:, 1024:1088], scalar=s2[:, 6:7], in1=gkr[:], op0=ALU.mult, op1=ALU.mult),
                        reads=[("latf", 2), "s2b", "gkr"], writes=["kr"])
                    S.op("vector", lambda e, b=b: e.tensor_tensor(out=kr2[:, 0:32], in0=kr[:, 0:32], in1=rpt[:, b, 0:32], op=ALU.mult),
                         reads=["kr", "rpt"], writes=["kr2a"])
                    S.op("vector", lambda e, b=b: e.tensor_tensor(out=kr2[:, 32:64], in0=kr[:, 32:64], in1=rpt[:, b, 32:64], op=ALU.mult),
                         reads=["kr", "rpt"], writes=["kr2b"])
                    S.op("vector", lambda e: e.tensor_tensor(out=krb[:, 0:32], in0=kr2[:, 0:32], in1=kr2[:, 32:64], op=ALU.subtract),
                         reads=["kr2a", "kr2b"], writes=["krb0"])
                    S.op("vector", lambda e, b=b: e.tensor_tensor(out=kr2[:, 0:32], in0=kr[:, 0:32], in1=rpt[:, b, 32:64], op=ALU.mult),
                         reads=["kr", "rpt", "krb0"], writes=["kr2a"])
                    S.op("vector", lambda e, b=b: e.tensor_tensor(out=kr2[:, 32:64], in0=kr[:, 32:64], in1=rpt[:, b, 0:32], op=ALU.mult),
                         reads=["kr", "rpt", "krb0"], writes=["kr2b"])
                    S.op("vector", lambda e: e.tensor_tensor(out=krb[:, 32:64], in0=kr2[:, 0:32], in1=kr2[:, 32:64], op=ALU.add),
                         reads=["kr2a", "kr2b"], writes=["krb1"])
                    S.op("tensor", lambda e, b=b: e.transpose(out=psb(3)[0:64, b * 128:(b + 1) * 128], in_=krb[:, :],
                                                             identity=ident_b[:]),
                         reads=["krb0", "krb1", "ident_b"], writes=[("ps", 3)])
                    for li in range(2):
                        for k in range(4):
                            S.op("tensor", lambda e, li=li, k=k: e.transpose(
                                out=psb(4 + li)[:, k * 128:(k + 1) * 128], in_=cn[:, li * 512 + k * 128:li * 512 + (k + 1) * 128],
                                identity=ident_b[:]),
                                reads=[("cn", li), "ident_b"], writes=[("ps", 4 + li)])
                        gofs = 4 + 4 * li
                        S.op("vector", lambda e, li=li, gofs=gofs, b=b: e.tensor_tensor(
                            out=cT[:, 4 * li:4 * li + 4, b * 128:(b + 1) * 128],
                            in0=psb(4 + li)[:, 0:512].rearrange("p (k t) -> p k t", k=4),
                            in1=gsm[:, gofs:gofs + 4].unsqueeze(2).to_broadcast([128, 4, 128]), op=ALU.mult),
                            reads=[("ps", 4 + li), "gsm"], writes=[("cT", li, b)])
                S.op("scalar", lambda e: e.copy(out=krT[:, :], in_=psb(3)[0:64, 0:512]), reads=[("ps", 3)], writes=["krT"])
                S.dma("gpsimd", lambda e, t=t: e.dma_start(out=k1r[t, :, :], in_=krT[:, :]), "st_krT",
                      reads=["krT"], writes=[("dram", "k1r")])
                is_prompt_tile = (NS * TS <= t < NT0)
                is_own_tile = (t >= NT0)
                for b in range(4 if P4S >= 2 else 0):
                    if not is_prompt_tile:
                        for g in range(NQG):
                            wk, wv = wload("uq", 0, 4, g * 384, 384)
                            bank = g % 3
                            for k in range(4):
                                S.op("tensor", lambda e, k=k, b=b, bank=bank, wv=wv: e.matmul(
                                    PS[bank][:, 0:384], lhsT=cT[:, k, b * 128:(b + 1) * 128], rhs=wv[:, k, :],
                                    start=(k == 0), stop=(k == 3)),
                                    reads=[wk, ("cT", 0, b)], writes=[("ps", bank)])
                            S.op("scalar", lambda e, g=g, bank=bank: e.copy(
                                out=qf[:, 2 * g:2 * g + 2, :].rearrange("p a c -> p (a c)"), in_=PS[bank][:, 0:384]),
                                reads=[("ps", bank)], writes=[("qf", g)])
                        if P4S < 2.2:
                            continue
                        qfk = [("qf", g) for g in range(NQG)]
                        S.op("vector", lambda e: e.tensor_tensor(out=qsq[:], in0=qf[:], in1=qf[:], op=ALU.mult),
                             reads=qfk, writes=["qsq"])
                        S.op("vector", lambda e: e.tensor_reduce(out=qs[:, 0:H], in_=qsq[:, :, 0:128], axis=AX.X, op=ALU.add),
                             reads=["qsq"], writes=["qs0"])
                        S.op("vector", lambda e: e.tensor_reduce(out=qs[:, H:2 * H], in_=qsq[:, :, 128:192], axis=AX.X, op=ALU.add),
                             reads=["qsq"], writes=["qs1"])
                        S.op("scalar", lambda e: e.activation(out=qs[:, 2 * H:3 * H], in_=qs[:, 0:H], func=AF.Sqrt, scale=1.0 / 128, bias=EPS),
                             reads=["qs0"], writes=["qs2"])
                        S.op("scalar", lambda e: e.activation(out=qs[:, 3 * H:4 * H], in_=qs[:, H:2 * H], func=AF.Sqrt, scale=1.0 / 64, bias=EPS),
                             reads=["qs1", "qs2"], writes=["qs2"])
                        S.op("vector", lambda e: e.reciprocal(out=qs[:, 2 * H:4 * H], in_=qs[:, 2 * H:4 * H]), reads=["qs2"], writes=["qs2"])
                        if P4S < 2.4:
                            continue
                        S.op("vector", lambda e: e.tensor_tensor(
                            out=qb[:, :, 0:128], in0=qf[:, :, 0:128],
                            in1=qs[:, 2 * H:3 * H].unsqueeze(2).to_broadcast([128, H, 128]), op=ALU.mult),
                            reads=qfk + ["qs2"], writes=["qbn"])
                        S.op("vector", lambda e: e.tensor_tensor(
                            out=qr1[:], in0=qf[:, :, 128:192],
                            in1=qs[:, 3 * H:4 * H].unsqueeze(2).to_broadcast([128, H, 64]), op=ALU.mult),
                            reads=qfk + ["qs2"], writes=["qr1"])
                        S.op("vector", lambda e: e.tensor_tensor(
                            out=qr1[:], in0=qr1[:], in1=gqr[:, :].unsqueeze(1).to_broadcast([128, H, 64]), op=ALU.mult),
                            reads=["qr1", "gqr"], writes=["qr1"])
                        cosb = rpt[:, b, 0:32].unsqueeze(1).to_broadcast([128, H, 32])
                        sinb = rpt[:, b, 32:64].unsqueeze(1).to_broadcast([128, H, 32])
                        S.op("vector", lambda e, cosb=cosb: e.tensor_tensor(out=qr2[:, :, 0:32], in0=qr1[:, :, 0:32], in1=cosb, op=ALU.mult),
                             reads=["qr1", "rpt"], writes=["qr2a"])
                        S.op("vector", lambda e, sinb=sinb: e.tensor_tensor(out=tq[:], in0=qr1[:, :, 32:64], in1=sinb, op=ALU.mult),
                             reads=["qr1", "rpt"], writes=["tq"])
                        S.op("vector", lambda e: e.tensor_tensor(out=qb[:, :, 128:160], in0=qr2[:, :, 0:32], in1=tq[:], op=ALU.subtract),
                             reads=["qr2a", "tq"], writes=["qbr0"])
                        S.op("vector", lambda e, sinb=sinb: e.tensor_tensor(out=qr2[:, :, 32:64], in0=qr1[:, :, 0:32], in1=sinb, op=ALU.mult),
                             reads=["qr1", "rpt"], writes=["qr2b"])
                        S.op("vector", lambda e, cosb=cosb: e.tensor_tensor(out=tq[:], in0=qr1[:, :, 32:64], in1=cosb, op=ALU.mult),
                             reads=["qr1", "rpt", "qbr0"], writes=["tq"])
                        S.op("vector", lambda e: e.tensor_tensor(out=qb[:, :, 160:192], in0=qr2[:, :, 32:64], in1=tq[:], op=ALU.add),
                             reads=["qr2b", "tq"], writes=["qbr1"])
                        if P4S < 2.6:
                            continue
                        for h4 in range(H // 4):
                            bank = 4 + h4 % 2
                            for hh in range(4):
                                h = h4 * 4 + hh
                                S.op("tensor", lambda e, h=h, hh=hh, bank=bank: e.transpose(
                                    out=psb(bank)[:, hh * 128:(hh + 1) * 128], in_=qb[:, h, 0:128], identity=ident_b[:]),
                                    reads=["qbn", "ident_b"], writes=[("ps", bank)])
                                if P4S >= 2.8: S.op("tensor", lambda e, h=h, hh=hh, bank=bank: e.transpose(
                                    out=psb(bank)[0:64, 512 + hh * 128:512 + (hh + 1) * 128], in_=qb[:, h, 128:192],
                                    identity=ident_b[:]),
                                    reads=["qbr0", "qbr1", "ident_b"], writes=[("ps", bank)])
                            S.op("scalar", lambda e, h4=h4, bank=bank, b=b: e.activation(
                                out=qnT[b % 2][:, h4 * 4:h4 * 4 + 4, :],
                                in_=psb(bank)[:, 0:512].rearrange("p (a t) -> p a t", a=4), func=AF.Copy, scale=gsm[:, 2:3]),
                                reads=[("ps", bank), "gsm"], writes=[("qnT", b % 2)])
                            if P4S >= 2.9: S.op("scalar", lambda e, h4=h4, bank=bank, b=b: e.copy(
                                out=qrT[:, h4 * 4:h4 * 4 + 4, :],
                                in_=psb(bank)[0:64, 512:1024].rearrange("p (a t) -> p a t", a=4)),
                                reads=[("ps", bank)], writes=["qrT"])
                        if P4S >= 4:
                            S.dma("gpsimd", lambda e, t=t, b=b: e.dma_start(
                                out=q1n[t, :, :, b * 128:(b + 1) * 128].rearrange("h d t -> d h t"), in_=qnT[b % 2][:]),
                                ("st_qnT", b % 2), reads=[("qnT", b % 2)], writes=[("dram", "q1n")])
                            S.dma("gpsimd", lambda e, t=t, b=b: e.dma_start(
                                out=q1r[t, :, :, b * 128:(b + 1) * 128].rearrange("h d t -> d h t"), in_=qrT[:]),
                                "st_qrT", reads=["qrT"], writes=[("dram", "q1r")])
                    if P4S < 3:
                        continue
                    if not is_own_tile:
                        for g in range(NKG):
                            wk, wv = wload("ukv", 0, 4, g * 512, 512)
                            bank = g % 3
                            for k in range(4):
                                S.op("tensor", lambda e, k=k, b=b, bank=bank, wv=wv: e.matmul(
                                    PS[bank][:, :], lhsT=cT[:, 4 + k, b * 128:(b + 1) * 128], rhs=wv[:, k, :],
                                    start=(k == 0), stop=(k == 3)),
                                    reads=[wk, ("cT", 1, b)], writes=[("ps", bank)])
                            pv = lambda bank=bank: PS[bank][:, :].rearrange("p (a c) -> p a c", a=2)
                            g2 = g % 2
                            S.op("scalar", lambda e, g2=g2, bank=bank: e.copy(out=kvf[g2][:], in_=PS[bank][:, :]),
                                 reads=[("ps", bank)], writes=[("kvf", g2)])
                            kvv = kvf[g2][:].rearrange("p (a c) -> p a c", a=2)
                            S.op("vector", lambda e, g=g, kvv=kvv: e.tensor_copy(out=kf[:, 2 * g:2 * g + 2, :], in_=kvv[:, :, 0:128]),
                                 reads=[("kvf", g2)], writes=[("kf", g)])
                            S.op("vector", lambda e, g=g, kvv=kvv: e.tensor_copy(out=vb[:, 2 * g:2 * g + 2, :], in_=kvv[:, :, 128:256]),
                                 reads=[("kvf", g2)], writes=[("vb", g)])
                        kfk = [("kf", g) for g in range(NKG)]
                        S.op("vector", lambda e: e.tensor_tensor(out=qsq[:, :, 0:128], in0=kf[:], in1=kf[:], op=ALU.mult),
                             reads=kfk, writes=["qsq"])
                        S.op("vector", lambda e: e.tensor_reduce(out=qs[:, 0:H], in_=qsq[:, :, 0:128], axis=AX.X, op=ALU.add),
                             reads=["qsq"], writes=["qs0"])
                        S.op("scalar", lambda e: e.activation(out=qs[:, 2 * H:3 * H], in_=qs[:, 0:H], func=AF.Sqrt, scale=1.0 / 128, bias=EPS),
                             reads=["qs0"], writes=["qs2"])
                        S.op("vector", lambda e: e.reciprocal(out=qs[:, 2 * H:3 * H], in_=qs[:, 2 * H:3 * H]), reads=["qs2"], writes=["qs2"])
                        S.op("vector", lambda e: e.tensor_tensor(
                            out=kb[:], in0=kf[:], in1=qs[:, 2 * H:3 * H].unsqueeze(2).to_broadcast([128, H, 128]), op=ALU.mult),
                            reads=kfk + ["qs2"], writes=["kb"])
                        for h4 in range(H // 4):
                            bank = 6 + h4 % 2
                            for hh in range(4):
                                h = h4 * 4 + hh
                                S.op("tensor", lambda e, h=h, hh=hh, bank=bank: e.transpose(
                                    out=psb(bank)[:, hh * 128:(hh + 1) * 128], in_=kb[:, h, :], identity=ident_b[:]),
                                    reads=["kb", "ident_b"], writes=[("ps", bank)])
                            S.op("scalar", lambda e, h4=h4, bank=bank, b=b: e.activation(
                                out=knT[b % 2][:, h4 * 4:h4 * 4 + 4, :],
                                in_=psb(bank)[:, 0:512].rearrange("p (a t) -> p a t", a=4), func=AF.Copy, scale=gsm[:, 3:4]),
                                reads=[("ps", bank), "gsm"], writes=[("knT", b % 2)])
                        if P4S >= 4:
                            S.dma("gpsimd", lambda e, t=t, b=b: e.dma_start(
                                out=k1n[t, :, :, b * 128:(b + 1) * 128].rearrange("h d t -> d h t"), in_=knT[b % 2][:]),
                                ("st_knT", b % 2), reads=[("knT", b % 2)], writes=[("dram", "k1n")])
                        r0 = t * 512 + b * 128
                        S.dma("gpsimd", lambda e, r0=r0: e.dma_start(
                            out=v1[r0:r0 + 128, :].rearrange("p (h c) -> p h c", h=H), in_=vb[:]),
                            "st_vb", reads=[("vb", g) for g in range(NKG)], writes=[("dram", "v1")])
            S.barrier()
            S.run()

        if cfg.get("stop", 99) >= 5:
         with contextlib.ExitStack() as st_:
            TK = max(RS * 64, RP * 64)
            krS = st_.enter_context(nc.sbuf_tensor(_u("krS"), [64, TK], BF16))
            knS = [st_.enter_context(nc.sbuf_tensor(_u("knS%d" % i), [128, TK], BF16)) for i in range(2)]
            vS = [st_.enter_context(nc.sbuf_tensor(_u("vS%d" % i), [128, TK // 128, 128], BF16)) for i in range(2)]
            qnS = [st_.enter_context(nc.sbuf_tensor(_u("qnS%d" % i), [128, 512], BF16)) for i in range(2)]
            qrS = [st_.enter_context(nc.sbuf_tensor(_u("qrS%d" % i), [64, 512], BF16)) for i in range(2)]
            pT = [st_.enter_context(nc.sbuf_tensor(_u("pT%d" % i), [128, 512], BF16)) for i in range(3)]
            rcp = [st_.enter_context(nc.sbuf_tensor(_u("rcp%d" % i), [128, 512], F32)) for i in range(2)]
            oo = [st_.enter_context(nc.sbuf_tensor(_u("oo%d" % i), [128, 512], BF16)) for i in range(2)]
            scale1 = 192 ** -0.5
            seqs1 = []
            for s in range(NS):
                seqs1.append(([s * TS + i for i in range(TS)], [s * TS + i for i in range(TS)],
                              [s * TS + i for i in range(TS)]))
            seqs1.append(([NT0 + i for i in range(NOWN)], [NS * TS + i for i in range(TP)],
                          [NS * TS + i for i in range(NOWN)]))
            hc = 0
            qc = 0
            cc = 0
            for (qtiles, kvtiles, otiles) in seqs1:
                T = len(kvtiles) * 512
                NC = T // 128
                for i, kt in enumerate(kvtiles):
                    S.dma("sync", lambda e, i=i, kt=kt: e.dma_start(out=krS[:, i * 512:(i + 1) * 512], in_=k1r[kt, :, :]),
                          "krS", reads=[("dram", "k1r")], writes=["krS"])
                for h in range(H):
                    hs = hc % 2
                    hc += 1
                    for i, kt in enumerate(kvtiles):
                        S.dma("sync", lambda e, i=i, kt=kt, h=h, hs=hs: e.dma_start(
                            out=knS[hs][:, i * 512:(i + 1) * 512], in_=k1n[kt, h, :, :]),
                            ("knS", hs), reads=[("dram", "k1n")], writes=[("knS", hs)])
                    tok0 = kvtiles[0] * 512
                    S.dma("sync", lambda e, h=h, hs=hs, tok0=tok0, T=T, NC=NC: e.dma_start(
                        out=vS[hs][:, 0:NC, :],
                        in_=v1[tok0:tok0 + T, h * 128:(h + 1) * 128].rearrange("(b p) c -> p b c", p=128)),
                        ("vS", hs), reads=[("dram", "v1")], writes=[("vS", hs)])
                    for qi, qt in enumerate(qtiles):
                        q2 = qc % 2
                        qc += 1
                        S.dma("sync", lambda e, qt=qt, h=h, q2=q2: e.dma_start(out=qnS[q2][:], in_=q1n[qt, h, :, :]),
                              ("qnS", q2), reads=[("dram", "q1n")], writes=[("qnS", q2)])
                        S.dma("sync", lambda e, qt=qt, h=h, q2=q2: e.dma_start(out=qrS[q2][:], in_=q1r[qt, h, :, :]),
                              ("qrS", q2), reads=[("dram", "q1r")], writes=[("qrS", q2)])
                        obk = 4 + q2
                        dbk = 6 + q2

                        def pv_step(c, p3, obk=obk, dbk=dbk, hs=hs, NC=NC):
                            S.op("tensor", lambda e: e.matmul(PS[obk][:, :], lhsT=vS[hs][:, c, :], rhs=pT[p3][:],
                                                              start=(c == 0), stop=(c == NC - 1)),
                                 reads=[("vS", hs), ("pT", p3)], writes=[("ps", obk)])
                            S.op("tensor", lambda e: e.matmul(PS[dbk][:, :], lhsT=ones_b[:], rhs=pT[p3][:],
                                                              start=(c == 0), stop=(c == NC - 1)),
                                 reads=["ones_b", ("pT", p3)], writes=[("ps", dbk)])
                        prev = None
                        for c in range(NC):
                            sbk = cc % 4
                            p3 = cc % 3
                            cc += 1
                            S.op("tensor", lambda e, c=c, sbk=sbk, hs=hs, q2=q2: e.matmul(
                                PS[sbk][:, :], lhsT=knS[hs][:, c * 128:(c + 1) * 128], rhs=qnS[q2][:], start=True, stop=False),
                                reads=[("knS", hs), ("qnS", q2)], writes=[("ps", sbk)])
                            S.op("tensor", lambda e, c=c, sbk=sbk, q2=q2: e.matmul(
                                PS[sbk][:, :], lhsT=krS[:, c * 128:(c + 1) * 128], rhs=qrS[q2][:], start=False, stop=True),
                                reads=["krS", ("qrS", q2)], writes=[("ps", sbk)])
                            S.op("scalar", lambda e, sbk=sbk, p3=p3: e.activation(out=pT[p3][:], in_=PS[sbk][:, :],
                                                                                  func=AF.Exp, scale=scale1),
                                 reads=[("ps", sbk)], writes=[("pT", p3)])
                            if prev is not None:
                                pv_step(*prev)
                            prev = (c, p3)
                        pv_step(*prev)
                        S.op("vector", lambda e, q2=q2, dbk=dbk: e.reciprocal(out=rcp[q2][:], in_=PS[dbk][:, :]),
                             reads=[("ps", dbk)], writes=[("rcp", q2)])
                        S.op("vector", lambda e, q2=q2, obk=obk: e.tensor_tensor(out=oo[q2][:], in0=PS[obk][:, :],
                                                                                 in1=rcp[q2][:], op=ALU.mult),
                             reads=[("ps", obk), ("rcp", q2)], writes=[("oo", q2)])
                        ot = otiles[qi]
                        S.dma("gpsimd", lambda e, ot=ot, h=h, q2=q2: e.dma_start(out=oT1[ot, h, :, :], in_=oo[q2][:]),
                              ("st_oo", q2), reads=[("oo", q2)], writes=[("dram", "oT1")])
            S.barrier()
            S.run()

        NB = 4 * NL1
        NST = 2 * NL1 + E - 1
        x2 = dscr("x2", [NL1 * 512, D], F32)
        hn = dscr("hn", [NL1 * 512, D], BF16)
        hs = dscr("hs", [NST * 512, D], BF16)
        ysl = dscr("ysl", [NST * 512, D], F32)
        MK = gst.enter_context(nc.sbuf_tensor(_u("MK"), [128, 2, NB, E], F32))
        GG = gst.enter_context(nc.sbuf_tensor(_u("GG"), [128, 2, NB], F32))
        SLI = gst.enter_context(nc.sbuf_tensor(_u("SLI"), [128, 2, NB], I32))
        IXG = gst.enter_context(nc.sbuf_tensor(_u("IXG"), [128, NST, NCH], I32))
        IXD = gst.enter_context(nc.sbuf_tensor(_u("IXD"), [128, NST, NCG * NDC], I32))
        if cfg.get("stop", 99) >= 6:
         with contextlib.ExitStack() as st_:
            alloc_ws(st_, 3)
            xt = st_.enter_context(nc.sbuf_tensor(_u("xt"), [128, 4, D], F32))
            xn = st_.enter_context(nc.sbuf_tensor(_u("xn"), [128, 4, D], BF16))
            junk = st_.enter_context(nc.sbuf_tensor(_u("junk"), [128, D], F32))
            stt = st_.enter_context(nc.sbuf_tensor(_u("stt"), [128, 8], F32))
            oT = st_.enter_context(nc.sbuf_tensor(_u("oT"), [128, H, 512], BF16))
            wr = st_.enter_context(nc.sbuf_tensor(_u("wr"), [128, KD, E], F32))
            h32 = st_.enter_context(nc.sbuf_tensor(_u("h32"), [128, KD, 128], F32))
            lg = st_.enter_context(nc.sbuf_tensor(_u("lg"), [128, 4, E], F32))
            m1 = st_.enter_context(nc.sbuf_tensor(_u("m1"), [128, 8], F32))
            lg2 = st_.enter_context(nc.sbuf_tensor(_u("lg2"), [128, E], F32))
            ld(wr[:], w_router[:, :, :], "wr")
            for ti, t in enumerate(l1_tiles):
                ld(xt[:], x1[t * 512:(t + 1) * 512, :].rearrange("(b p) d -> p b d", p=128), "xt", reads=[("dram", "x1")])
                ld(oT[:], oT1[ti, :, :, :].rearrange("h d t -> d h t"), "oT", reads=[("dram", "oT1")])
                wo_tile(oT, "oT", "mo", xt, "xt")
                S.dma("gpsimd", lambda e, ti=ti: e.dma_start(
                    out=x2[ti * 512:(ti + 1) * 512, :].rearrange("(b p) d -> p b d", p=128), in_=xt[:]),
                    "st_xt", reads=["xt"], writes=[("dram", "x2")])
                norm_tile(xt, "xt", xn, junk, stt, gffn[:, 1, :], None, "hT", [6, 7])
                S.dma("gpsimd", lambda e, ti=ti: e.dma_start(
                    out=hn[ti * 512:(ti + 1) * 512, :].rearrange("(b p) d -> p b d", p=128), in_=xn[:]),
                    "st_xn", reads=[("xn", b) for b in range(4)], writes=[("dram", "hn")])
                for b in range(4):
                    gb = ti * 4 + b
                    S.op("scalar", lambda e, b=b: e.activation(out=junk[:], in_=xt[:, b, :], func=AF.Copy,
                                                               scale=stt[:, 4 + b:5 + b]),
                         reads=["xt", "st2"], writes=["junk"])
                    for k4 in range(KD // 4):
                        bank = k4 % 2
                        for kk in range(4):
                            k = k4 * 4 + kk
                            S.op("tensor", lambda e, k=k, kk=kk, bank=bank: e.transpose(
                                out=PS[bank][:, kk * 128:(kk + 1) * 128], in_=junk[:, k * 128:(k + 1) * 128],
                                identity=ident_f[:]),
                                reads=["junk", "ident_f"], writes=[("ps", bank)])
                        S.op("vector", lambda e, k4=k4, bank=bank: e.tensor_tensor(
                            out=h32[:, k4 * 4:k4 * 4 + 4, :], in0=PS[bank][:, :].rearrange("p (a t) -> p a t", a=4),
                            in1=gffn[:, 1, k4 * 4:k4 * 4 + 4].unsqueeze(2).to_broadcast([128, 4, 128]), op=ALU.mult),
                            reads=[("ps", bank), "gffn"], writes=[("h32", k4)])
                    for k in range(KD):
                        S.op("tensor", lambda e, k=k: e.matmul(PS[2][:, 0:E], lhsT=h32[:, k, :], rhs=wr[:, k, :],
                                                               start=(k == 0), stop=(k == KD - 1)),
                             reads=[("h32", k // 4), "wr"], writes=[("ps", 2)])
                    S.op("vector", lambda e, b=b: e.tensor_scalar(out=lg[:, b, :], in0=PS[2][:, 0:E], scalar1=1.0, scalar2=None,
                                                                  op0=ALU.mult), reads=[("ps", 2)], writes=[("lg", b)])
                    S.op("vector", lambda e, b=b: e.tensor_reduce(out=m1[:, 0:1], in_=lg[:, b, :], axis=AX.X, op=ALU.max),
                         reads=[("lg", b)], writes=["m1a"])
                    S.op("vector", lambda e, b=b, gb=gb: e.tensor_scalar(out=MK[:, 0, gb, :], in0=lg[:, b, :], scalar1=m1[:, 0:1],
                                                                         scalar2=None, op0=ALU.is_equal),
                         reads=[("lg", b), "m1a"], writes=["MK"])
                    S.op("vector", lambda e, b=b, gb=gb: e.scalar_tensor_tensor(
                        out=lg2[:], in0=MK[:, 0, gb, :], scalar=-1e30, in1=lg[:, b, :], op0=ALU.mult, op1=ALU.add),
                        reads=["MK", ("lg", b)], writes=["lg2"])
                    S.op("vector", lambda e: e.tensor_reduce(out=m1[:, 1:2], in_=lg2[:], axis=AX.X, op=ALU.max),
                         reads=["lg2"], writes=["m1b"])
                    S.op("vector", lambda e, gb=gb: e.tensor_scalar(out=MK[:, 1, gb, :], in0=lg2[:], scalar1=m1[:, 1:2], scalar2=None,
                                                                    op0=ALU.is_equal), reads=["lg2", "m1b"], writes=["MK"])
                    S.op("vector", lambda e: e.tensor_tensor(out=m1[:, 2:3], in0=m1[:, 1:2], in1=m1[:, 0:1], op=ALU.subtract),
                         reads=["m1a", "m1b"], writes=["m1c"])
                    S.op("scalar", lambda e: e.activation(out=m1[:, 3:4], in_=m1[:, 2:3], func=AF.Exp),
                         reads=["m1c"], writes=["m1d"])
                    S.op("vector", lambda e: e.tensor_scalar(out=m1[:, 4:5], in0=m1[:, 3:4], scalar1=1.0, scalar2=None,
                                                             op0=ALU.add), reads=["m1d"], writes=["m1e"])
                    S.op("vector", lambda e, gb=gb: e.reciprocal(out=GG[:, 0, gb:gb + 1], in_=m1[:, 4:5]),
                         reads=["m1e"], writes=["GG"])
                    S.op("vector", lambda e, gb=gb: e.tensor_tensor(out=GG[:, 1, gb:gb + 1], in0=m1[:, 3:4], in1=GG[:, 0, gb:gb + 1],
                                                                    op=ALU.mult), reads=["m1d", "GG"], writes=["GG"])
            S.barrier()
            S.run()

        if cfg.get("stop", 99) >= 6:
         with contextlib.ExitStack() as st_:
            tri_f = st_.enter_context(nc.sbuf_tensor(_u("tri_f"), [128, 128], F32))
            tri_b = st_.enter_context(nc.sbuf_tensor(_u("tri_b"), [128, 128], BF16))
            cst = st_.enter_context(nc.sbuf_tensor(_u("cst"), [128, 64], F32))
            Mb = st_.enter_context(nc.sbuf_tensor(_u("Mb"), [128, NB, E], BF16))
            wi = st_.enter_context(nc.sbuf_tensor(_u("wi"), [128, NB, E], F32))
            tot = st_.enter_context(nc.sbuf_tensor(_u("tot"), [128, NB, E], F32))
            off = st_.enter_context(nc.sbuf_tensor(_u("off"), [128, NB, E], F32))
            sm = st_.enter_context(nc.sbuf_tensor(_u("sm"), [128, 8, E], F32))
            cmpA = st_.enter_context(nc.sbuf_tensor(_u("cmpA"), [128, E, NL1], F32))
            cmpB = st_.enter_context(nc.sbuf_tensor(_u("cmpB"), [128, NST, E], F32))
            prod = st_.enter_context(nc.sbuf_tensor(_u("prod"), [128, NB, E], F32))
            slf = st_.enter_context(nc.sbuf_tensor(_u("slf"), [128, 2, NB], F32))
            ej = st_.enter_context(nc.sbuf_tensor(_u("ej"), [128, NST], F32))
            ixf = st_.enter_context(nc.sbuf_tensor(_u("ixf"), [128, NST, max(NCH, NCG * NDC)], F32))
            ld(tri_f[:], tri_in[:, :], "tri_f")
            ld(cst[:], cst_in[:, :], "cst")
            S.op("vector", lambda e: e.tensor_copy(out=tri_b[:], in_=tri_f[:]), reads=["tri_f"], writes=["tri_b"])
            S.op("vector", lambda e: e.tensor_tensor(out=prod[:], in0=MK[:, 0, :, :], in1=MK[:, 1, :, :], op=ALU.add),
                 reads=["MK"], writes=["prod"])
            S.op("vector", lambda e: e.tensor_copy(out=Mb[:], in_=prod[:]), reads=["prod"], writes=["Mb"])
            mbf = Mb[:].rearrange("p a b -> p (a b)")
            S.op("tensor", lambda e: e.matmul(PS[0][:, 0:NB * E], lhsT=tri_b[:], rhs=mbf, start=True, stop=True),
                 reads=["tri_b", "Mb"], writes=[("ps", 0)])
            S.op("tensor", lambda e: e.matmul(PS[1][:, 0:NB * E], lhsT=ones_b[:], rhs=mbf, start=True, stop=True),
                 reads=["ones_b", "Mb"], writes=[("ps", 1)])
            S.op("scalar", lambda e: e.copy(out=wi[:].rearrange("p a b -> p (a b)"), in_=PS[0][:, 0:NB * E]),
                 reads=[("ps", 0)], writes=["wi"])
            S.op("scalar", lambda e: e.copy(out=tot[:].rearrange("p a b -> p (a b)"), in_=PS[1][:, 0:NB * E]),
                 reads=[("ps", 1)], writes=["tot"])
            S.op("vector", lambda e: e.memset(off[:, 0, :], 0.0), writes=["off"])
            for blk in range(1, NB):
                S.op("vector", lambda e, blk=blk: e.tensor_tensor(out=off[:, blk, :], in0=off[:, blk - 1, :], in1=tot[:, blk - 1, :],
                                                                  op=ALU.add), reads=["off", "tot"], writes=["off"])
            S.op("vector", lambda e: e.tensor_tensor(out=sm[:, 0, :], in0=off[:, NB - 1, :], in1=tot[:, NB - 1, :], op=ALU.add),
                 reads=["off", "tot"], writes=["sm0"])
            S.op("vector", lambda e: e.tensor_tensor(
                out=cmpA[:], in0=sm[:, 0, :].unsqueeze(2).to_broadcast([128, E, NL1]),
                in1=cst[:, 1:1 + NL1].unsqueeze(1).to_broadcast([128, E, NL1]), op=ALU.is_gt),
                reads=["sm0", "cst"], writes=["cmpA"])
            S.op("vector", lambda e: e.tensor_reduce(out=sm[:, 1, :], in_=cmpA[:], axis=AX.X, op=ALU.add),
                 reads=["cmpA"], writes=["sm1"])
            S.op("vector", lambda e: e.memset(sm[:, 2, 0:1], 0.0), reads=["sm1"], writes=["sm2"])
            for e_ in range(1, E):
                S.op("vector", lambda e, e_=e_: e.tensor_tensor(out=sm[:, 2, e_:e_ + 1], in0=sm[:, 2, e_ - 1:e_],
                                                                in1=sm[:, 1, e_ - 1:e_], op=ALU.add),
                     reads=["sm1", "sm2"], writes=["sm2"])
            S.op("vector", lambda e: e.tensor_tensor(out=sm[:, 3, :], in0=sm[:, 2, :], in1=sm[:, 1, :], op=ALU.add),
                 reads=["sm1", "sm2"], writes=["sm3"])
            S.op("vector", lambda e: e.tensor_scalar(out=sm[:, 4, :], in0=sm[:, 2, :], scalar1=512.0, scalar2=None, op0=ALU.mult),
                 reads=["sm2"], writes=["sm4"])
            S.op("vector", lambda e: e.tensor_tensor(out=wi[:], in0=wi[:], in1=off[:], op=ALU.add),
                 reads=["wi", "off"], writes=["wi"])
            S.op("vector", lambda e: e.tensor_tensor(out=wi[:], in0=wi[:], in1=sm[:, 4, :].unsqueeze(1).to_broadcast([128, NB, E]),
                                                     op=ALU.add), reads=["wi", "sm4"], writes=["wi"])
            for kk in range(2):
                S.op("vector", lambda e, kk=kk: e.tensor_tensor(out=prod[:], in0=MK[:, kk, :, :], in1=wi[:], op=ALU.mult),
                     reads=["MK", "wi"], writes=["prod"])
                S.op("vector", lambda e, kk=kk: e.tensor_reduce(out=slf[:, kk, :], in_=prod[:], axis=AX.X, op=ALU.add),
                     reads=["prod"], writes=["slf"])
            S.op("vector", lambda e: e.tensor_copy(out=SLI[:], in_=slf[:]), reads=["slf"], writes=["SLI"])
            S.op("vector", lambda e: e.tensor_tensor(
                out=cmpB[:], in0=sm[:, 3, :].unsqueeze(1).to_broadcast([128, NST, E]),
                in1=cst[:, 16:16 + NST].unsqueeze(2).to_broadcast([128, NST, E]), op=ALU.is_le),
                reads=["sm3", "cst"], writes=["cmpB"])
            S.op("vector", lambda e: e.tensor_reduce(out=ej[:], in_=cmpB[:], axis=AX.X, op=ALU.add),
                 reads=["cmpB"], writes=["ej"])
            S.op("vector", lambda e: e.tensor_scalar(out=ej[:], in0=ej[:], scalar1=float(E - 1), scalar2=None, op0=ALU.min),
                 reads=["ej"], writes=["ej"])
            for (IX, nper) in ((IXG, NCH), (IXD, NCG * NDC)):
                S.op("vector", lambda e, nper=nper: e.tensor_scalar(
                    out=ixf[:, :, 0:nper], in0=ej[:].unsqueeze(2).to_broadcast([128, NST, nper]),
                    scalar1=float(nper * 128), scalar2=cst[:, 0:1], op0=ALU.mult, op1=ALU.add),
                    reads=["ej", "cst"], writes=["ixf"])
                S.op("vector", lambda e, nper=nper: e.tensor_tensor(
                    out=ixf[:, :, 0:nper], in0=ixf[:, :, 0:nper],
                    in1=cst[:, 48:48 + nper].unsqueeze(1).to_broadcast([128, NST, nper]), op=ALU.add),
                    reads=["ixf", "cst"], writes=["ixf"])
                S.op("vector", lambda e, IX=IX, nper=nper: e.tensor_copy(out=IX[:], in_=ixf[:, :, 0:nper]),
                     reads=["ixf"], writes=["IX%d" % nper])
            S.barrier()
            S.run()

        if cfg.get("stop", 99) >= 6:
         with contextlib.ExitStack() as st_:
            zt = st_.enter_context(nc.sbuf_tensor(_u("zt"), [128, 4, D], BF16))
            hb = [st_.enter_context(nc.sbuf_tensor(_u("hb%d" % i), [128, D], BF16)) for i in range(2)]
            S.op("vector", lambda e: e.memset(zt[:], 0.0), writes=["zt"])
            for j in range(NST):
                S.dma("sync", lambda e, j=j: e.dma_start(
                    out=hs[j * 512:(j + 1) * 512, :].rearrange("(b p) d -> p b d", p=128), in_=zt[:]),
                    "st_zt", reads=["zt"], writes=[("dram", "hs0")])
            for blk in range(NB):
                i2 = blk % 2
                ld(hb[i2][:], hn[blk * 128:(blk + 1) * 128, :], ("hb", i2), reads=[("dram", "hn")])
                for kk in range(2):
                    S.dma("gpsimd", lambda e, i2=i2, kk=kk, blk=blk: e.indirect_dma_start(
                        out=hs[:, :], out_offset=bass.IndirectOffsetOnAxis(ap=SLI[:, kk, blk:blk + 1], axis=0),
                        in_=hb[i2][:, :], in_offset=None),
                        ("sc", i2), reads=[("hb", i2), "SLI", ("dram", "hs0")], writes=[("dram", "hs")])
            S.barrier()
            S.run()

        if cfg.get("stop", 99) >= 6:
         with contextlib.ExitStack() as st_:
            alloc_ws(st_, 3)
            xn = st_.enter_context(nc.sbuf_tensor(_u("xn"), [128, 4, D], BF16))
            hT = st_.enter_context(nc.sbuf_tensor(_u("hT"), [128, KD, 512], BF16))
            act = st_.enter_context(nc.sbuf_tensor(_u("act"), [128, NFC, 512], BF16))
            sg = [st_.enter_context(nc.sbuf_tensor(_u("sg%d" % i), [128, 512], BF16)) for i in range(GC)]
            yo = st_.enter_context(nc.sbuf_tensor(_u("yo"), [128, 4, D], F32))
            cnt = 0
            for j in range(NST):
                S.dma("sync", lambda e, j=j: e.dma_start(
                    out=xn[:], in_=hs[j * 512:(j + 1) * 512, :].rearrange("(b p) d -> p b d", p=128)),
                    "xn", reads=[("dram", "hs"), ("dram", "hs0")], writes=[("xn", b) for b in range(4)])
                transpose_tile(xn, gffn[:, 1, :], hT, "hT", [6, 7])

                def loader(kind, *a, j=j):
                    s_ = ws_i[0] % len(WS)
                    ws_i[0] += 1
                    if kind in ("g", "u"):
                        src_t, nmw = (war_g, "war_g") if kind == "g" else (war_u, "war_u")
                        ix = IXG[:, j, a[0]:a[0] + 1]
                        n = KD * 512
                        ikey = "IX%d" % NCH
                    else:
                        i_ = a[0] * NDC + a[1]
                        src_t, nmw, ix = war_d, "war_d", IXD[:, j, i_:i_ + 1]
                        n = DCm * 512
                        ikey = "IX%d" % (NCG * NDC)
                    view = WS[s_][:, 0:n].rearrange("p (k c) -> p k c", c=512)
                    S.dma("gpsimd", lambda e, s_=s_, n=n, src_t=src_t, ix=ix: e.indirect_dma_start(
                        out=WS[s_][:, 0:n], out_offset=None, in_=src_t[:, :],
                        in_offset=bass.IndirectOffsetOnAxis(ap=ix, axis=0)),
                        ("w", s_), reads=[("dram", nmw), ikey], writes=[("w", s_)])
                    return ("w", s_), view

                def epi(cg, b, bank):
                    if (cg + b) % 2 == 0:
                        S.op("scalar", lambda e: e.copy(out=yo[:, b, cg * 512:(cg + 1) * 512], in_=PS[bank][:, :]),
                             reads=[("ps", bank)], writes=[("yo", b, cg)])
                    else:
                        S.op("vector", lambda e: e.tensor_scalar(out=yo[:, b, cg * 512:(cg + 1) * 512], in0=PS[bank][:, :],
                                                                 scalar1=1.0, scalar2=None, op0=ALU.mult),
                             reads=[("ps", bank)], writes=[("yo", b, cg)])
                cnt = swiglu_tile(hT, "hT", act, sg, loader, epi, cnt)
                S.dma("sync", lambda e, j=j: e.dma_start(
                    out=ysl[j * 512:(j + 1) * 512, :].rearrange("(b p) d -> p b d", p=128), in_=yo[:]),
                    "st_yo", reads=[("yo", b, cg) for b in range(4) for cg in range(NCG)], writes=[("dram", "ysl")])
            S.barrier()
            S.run()

        if cfg.get("stop", 99) >= 6:
         with contextlib.ExitStack() as st_:
            xb2 = [st_.enter_context(nc.sbuf_tensor(_u("xb2%d" % i), [128, D], F32)) for i in range(2)]
            ya = [st_.enter_context(nc.sbuf_tensor(_u("ya%d" % i), [128, D], F32)) for i in range(2)]
            yb = [st_.enter_context(nc.sbuf_tensor(_u("yb%d" % i), [128, D], F32)) for i in range(2)]
            for blk in range(NB):
                i2 = blk % 2
                ld(xb2[i2][:], x2[blk * 128:(blk + 1) * 128, :], ("xb2", i2), reads=[("dram", "x2")])
                for kk, yy, nm in ((0, ya, "ya"), (1, yb, "yb")):
                    S.dma("gpsimd", lambda e, i2=i2, kk=kk, blk=blk, yy=yy: e.indirect_dma_start(
                        out=yy[i2][:, :], out_offset=None, in_=ysl[:, :],
                        in_offset=bass.IndirectOffsetOnAxis(ap=SLI[:, kk, blk:blk + 1], axis=0)),
                        (nm, i2), reads=[("dram", "ysl"), "SLI"], writes=[(nm, i2)])
                S.op("vector", lambda e, i2=i2, blk=blk: e.scalar_tensor_tensor(
                    out=xb2[i2][:], in0=ya[i2][:], scalar=GG[:, 0, blk:blk + 1], in1=xb2[i2][:], op0=ALU.mult, op1=ALU.add),
                    reads=[("ya", i2), "GG", ("xb2", i2)], writes=[("xb2", i2)])
                S.op("vector", lambda e, i2=i2, blk=blk: e.scalar_tensor_tensor(
                    out=xb2[i2][:], in0=yb[i2][:], scalar=GG[:, 1, blk:blk + 1], in1=xb2[i2][:], op0=ALU.mult, op1=ALU.add),
                    reads=[("yb", i2), "GG", ("xb2", i2)], writes=[("xb2", i2)])
                ti, b = blk // 4, blk % 4
                if ti < NS * TS:
                    dst = ys[ti * 512 + b * 128:ti * 512 + (b + 1) * 128, :]
                else:
                    r0 = (ti - NS * TS) * 512 + b * 128
                    dst = yp[r0:r0 + 128, :]
                S.dma("sync", lambda e, dst=dst, i2=i2: e.dma_start(out=dst, in_=xb2[i2][:]),
                      ("st_xb2", i2), reads=[("xb2", i2)], writes=[("dram", "y")])
            S.barrier()
            S.run()
    return nc


def _rope_table(pos_tokens, grid_w=64, theta=10000.0):
    t = np.asarray(pos_tokens)
    row = (t // grid_w).astype(np.float32)
    col = (t % grid_w).astype(np.float32)
    n_pairs = 16
    inv = (np.float32(theta) ** (-np.arange(n_pairs, dtype=np.float32) / np.float32(n_pairs))).astype(np.float32)
    ang = np.concatenate([row[:, None] * inv, col[:, None] * inv], axis=-1).astype(np.float32)
    return np.concatenate([np.cos(ang), np.sin(ang)], axis=-1).astype(np.float32)


def _cst_table():
    c = np.zeros((128, 64), np.float32)
    c[:, 0] = np.arange(128)
    c[:, 1:16] = 512.0 * np.arange(15)[None, :]
    c[:, 16:48] = np.arange(32)[None, :]
    c[:, 48:64] = 128.0 * np.arange(16)[None, :]
    return c


def make_core_inputs(cfg, inp, core):
    D, H, FF, E = cfg["D"], cfg["H"], cfg["FF"], cfg["E"]
    RS, RP, NS = cfg["RS"], cfg["RP"], cfg["NS"]
    KD = D // 128
    f = lambda a: np.ascontiguousarray(np.asarray(a, dtype=np.float32))
    xs = inp["x_sample"]
    xp = inp["x_prompt"]
    x_all = np.concatenate([f(xs[core * NS + s]) for s in range(NS)] + [f(xp[0])], axis=0)
    n_own_rows = RP // 8 * 64
    NOWN = RP // 64
    base = NS * RS * 64
    own = base + core * n_own_rows + np.arange(n_own_rows)
    own_idx = np.ascontiguousarray(own.reshape(NOWN * 4, 128).T.astype(np.int32))
    pos = np.concatenate([np.arange(RS * 64)] * NS + [np.arange(RP * 64)] + [core * n_own_rows + np.arange(n_own_rows)])
    rope = _rope_table(pos)
    pk = lambda g: np.ascontiguousarray(f(g).reshape(-1, 128).T)
    g_mix = np.ascontiguousarray(np.stack([pk(inp["mix_norm"][l]) for l in range(2)], axis=1))
    g_ffn = np.ascontiguousarray(np.stack([pk(inp["ffn_norm"][l]) for l in range(2)], axis=1))
    rpb = f(inp["na_rpb"][0])
    kc = np.arange(64)[:, None]
    qc = np.arange(64)[None, :]
    dc = np.clip(kc - qc + 15, 0, 30)
    ws = np.clip(qc - 8, 0, 48)
    valid = ((kc >= ws) & (kc < ws + 16)).astype(np.float32)
    rpbg = np.zeros((128, H, 14, 64), np.float32)
    for a in range(2):
        for m in range(14):
            rpbg[a * 64:(a + 1) * 64, :, m, :] = np.transpose(rpb[:, m + a][:, dc], (1, 0, 2))
    namask = np.concatenate([valid, valid], axis=0)
    d = {
        "x_all": x_all, "own_idx": own_idx, "rope": rope, "ident": np.eye(128, dtype=np.float32),
        "g_mix": g_mix, "g_ffn": g_ffn,
        "g_naq": pk(inp["na_q_gain"][0]), "g_nak": pk(inp["na_k_gain"][0]),
        "rpbg": rpbg, "namask": namask,
        "g_ql": pk(inp["mla_q_lora_gain"][0]), "g_kvl": pk(inp["mla_kv_lora_gain"][0]),
        "g_qn": pk(inp["mla_qn_gain"][0]), "g_kn": pk(inp["mla_kn_gain"][0]),
        "g_qr": np.ascontiguousarray(np.broadcast_to(f(inp["mla_qr_gain"][0])[None, :], (128, 64))),
        "g_kr": np.ascontiguousarray(np.broadcast_to(f(inp["mla_kr_gain"][0])[None, :], (128, 64))),
        "w_router": np.ascontiguousarray(f(inp["moe_w_router"][0]).reshape(KD, 128, E).transpose(1, 0, 2)),
        "tri": np.triu(np.ones((128, 128), np.float32), 1), "cst": _cst_table(),
        "na_w_qkv": f(inp["na_w_qkv"][0]), "na_w_o": f(inp["na_w_o"][0]),
        "ffn_w_gate": f(inp["ffn_w_gate"][0]), "ffn_w_up": f(inp["ffn_w_up"][0]), "ffn_w_down": f(inp["ffn_w_down"][0]),
        "mla_w_dqkv": f(inp["mla_w_dqkv"][0]), "mla_w_uq": f(inp["mla_w_uq"][0]), "mla_w_ukv": f(inp["mla_w_ukv"][0]),
        "mla_w_o": f(inp["mla_w_o"][0]),
    }
    for e_ in range(E):
        d["moe_w_gate%d" % e_] = f(inp["moe_w_gate"][0, e_])
        d["moe_w_up%d" % e_] = f(inp["moe_w_up"][0, e_])
        d["moe_w_down%d" % e_] = f(inp["moe_w_down"][0, e_])
    return d


def run_cfg(cfg, inp, trace=False):
    nc = build_program(cfg)
    shared = None
    in_maps = []
    for c in range(N_CORES):
        d = make_core_inputs(cfg, inp, c)
        if shared is None:
            shared = d
        else:
            for k in d:
                if k not in ("x_all", "own_idx", "rope"):
                    d[k] = shared[k]
        in_maps.append(d)
    res = run_bass_kernel_spmd(nc, in_maps, core_ids=list(range(N_CORES)), trace=trace)
    RS, RP, NS, D = cfg["RS"], cfg["RP"], cfg["NS"], cfg["D"]
    ys = np.stack([np.asarray(res.results[c]["ys"]).reshape(NS, RS * 64, D) for c in range(N_CORES)], axis=0)
    y_sample = ys.reshape(N_CORES * NS, RS * 64, D).astype(np.float32)
    y_prompt = np.concatenate([np.asarray(res.results[c]["yp"]) for c in range(N_CORES)], axis=0)[None].astype(np.float32)
    return (y_prompt, y_sample), res


def kernel(**inputs):
    out, _ = run_cfg(CFG_FULL, inputs)
    return out
```
